# Optimizing a Trainium2 kernel written in Bass

```python
import math
import jax, jax.numpy as jnp
from jax import lax
import numpy as np

D_MODEL = 2048
BATCH = 4
SEQ = 4096
DEPTH = 2

CHUNK = 64
Q_BLOCK = 128
N_MIXERS = 2
NORM_EPS = 1e-6
ROPE_THETA = 500000.0

DA_HEADS = 8
DA_HEAD_DIM = D_MODEL // (2 * DA_HEADS)
DA_V_DIM = 2 * DA_HEAD_DIM
ROPE_DIM = DA_HEAD_DIM // 4
DA_IN_COLS = 3 * DA_HEADS * DA_V_DIM

ML_HEADS = 8
ML_V_DIM = D_MODEL // ML_HEADS
ML_QK_DIM = ML_V_DIM // 2
ML_CONV = 4
ML_QK_COLS = 2 * ML_HEADS * ML_QK_DIM
ML_V_COLS = ML_HEADS * ML_V_DIM
ML_IN_COLS = ML_QK_COLS + 2 * ML_V_COLS + 2 * ML_HEADS

MOE_GROUPS = 4
MOE_PER_GROUP = 8
MOE_EXPERTS = MOE_GROUPS * MOE_PER_GROUP
MOE_TOPK = 2
MOE_HIDDEN = D_MODEL // 2
MOE_BLOCK = 128

N_ATTN_LAYERS = (DEPTH + 1) // N_MIXERS
N_MLSTM_LAYERS = DEPTH // N_MIXERS

kernel_name = 'hybrid_diffattn_mlstm_hmoe'


def rms_norm(x, g):
    xf = x.astype(jnp.float32)
    y = xf * lax.rsqrt(jnp.mean(xf * xf, axis=-1, keepdims=True) + NORM_EPS)
    return (y * g.astype(jnp.float32)).astype(x.dtype)


def partial_rope(t, cos, sin):
    half = ROPE_DIM // 2
    t1 = t[..., :half]
    t2 = t[..., half:ROPE_DIM]
    rot = jnp.concatenate([t1 * cos - t2 * sin, t2 * cos + t1 * sin], axis=-1)
    return jnp.concatenate([rot, t[..., ROPE_DIM:]], axis=-1)


def diff_attention(h, positions, w_in, w_out, lq1, lk1, lq2, lk2, head_g, layer_idx):
    B, S, _ = h.shape
    q, k, v = jnp.split(h @ w_in, 3, axis=-1)
    q = q.reshape(B, S, DA_HEADS, 2, DA_HEAD_DIM)
    k = k.reshape(B, S, DA_HEADS, 2, DA_HEAD_DIM)
    v = v.reshape(B, S, DA_HEADS, DA_V_DIM)
    half = ROPE_DIM // 2
    inv_freq = ROPE_THETA ** (-jnp.arange(half, dtype=jnp.float32) * 2.0 / ROPE_DIM)
    ang = positions.astype(jnp.float32)[..., None] * inv_freq
    cos = jnp.cos(ang)[:, :, None, None, :].astype(h.dtype)
    sin = jnp.sin(ang)[:, :, None, None, :].astype(h.dtype)
    q = partial_rope(q, cos, sin) * (DA_HEAD_DIM ** -0.5)
    k = partial_rope(k, cos, sin)
    lam_init = 0.8 - 0.6 * math.exp(-0.3 * layer_idx)
    lam = (jnp.exp(jnp.sum(lq1.astype(jnp.float32) * lk1.astype(jnp.float32)))
           - jnp.exp(jnp.sum(lq2.astype(jnp.float32) * lk2.astype(jnp.float32))) + lam_init)
    outs = []
    for qb in range(S // Q_BLOCK):
        q0 = qb * Q_BLOCK
        kv_end = q0 + Q_BLOCK
        s = jnp.einsum('bqhmd,bkhmd->bhmqk', q[:, q0:kv_end], k[:, :kv_end]).astype(jnp.float32)
        q_chunk = (q0 + jnp.arange(Q_BLOCK)) // CHUNK
        k_chunk = jnp.arange(kv_end) // CHUNK
        mask = k_chunk[None, :] <= q_chunk[:, None]
        p = jax.nn.softmax(jnp.where(mask, s, -jnp.inf), axis=-1)
        a = p[:, :, 0] - lam * p[:, :, 1]
        outs.append(jnp.einsum('bhqk,bkhe->bqhe', a.astype(v.dtype), v[:, :kv_end]))
    o = jnp.concatenate(outs, axis=1)
    o = rms_norm(o, head_g) * (1.0 - lam_init)
    return o.reshape(B, S, DA_HEADS * DA_V_DIM) @ w_out


def mlstm_chunk_step(carry, xs):
    C, n, m = carry
    q, k, v, ig, lf = xs
    causal = jnp.tril(jnp.ones((CHUNK, CHUNK), dtype=bool))
    b = jnp.cumsum(lf, axis=-1)
    dmat = jnp.where(causal, b[..., :, None] - b[..., None, :] + ig[..., None, :], -jnp.inf)
    inter = b + m[..., None]
    m_t = jnp.maximum(inter, jnp.max(dmat, axis=-1))
    w = jnp.exp(dmat - m_t[..., None])
    s = jnp.einsum('bhtd,bhsd->bhts', q, k) * w
    decay = jnp.exp(inter - m_t)
    num = jnp.einsum('bhts,bhse->bhte', s, v) + decay[..., None] * jnp.einsum('bhed,bhtd->bhte', C, q)
    den = jnp.sum(s, axis=-1) + decay * jnp.einsum('bhd,bhtd->bht', n, q)
    h = num / jnp.maximum(jnp.abs(den), jnp.exp(-m_t))[..., None]
    b_last = b[..., -1]
    g = b_last[..., None] - b + ig
    m_new = jnp.maximum(b_last + m, jnp.max(g, axis=-1))
    carry_decay = jnp.exp(b_last + m - m_new)
    wg = jnp.exp(g - m_new[..., None])
    C_new = carry_decay[..., None, None] * C + jnp.einsum('bhs,bhse,bhsd->bhed', wg, v, k)
    n_new = carry_decay[..., None] * n + jnp.einsum('bhs,bhsd->bhd', wg, k)
    return (C_new, n_new, m_new), h


def mlstm_mixer(h, w_in, conv_w, conv_b, gate_b, head_g, w_out):
    B, S, _ = h.shape
    H = ML_HEADS
    proj = h @ w_in
    qk_pre = proj[..., :ML_QK_COLS]
    v = proj[..., ML_QK_COLS:ML_QK_COLS + ML_V_COLS]
    o_pre = proj[..., ML_QK_COLS + ML_V_COLS:ML_QK_COLS + 2 * ML_V_COLS]
    gates = proj[..., ML_QK_COLS + 2 * ML_V_COLS:].astype(jnp.float32) + gate_b.astype(jnp.float32)
    qk = lax.conv_general_dilated(qk_pre, conv_w[:, None, :], window_strides=(1,),
                                  padding=[(ML_CONV - 1, 0)], dimension_numbers=('NWC', 'WIO', 'NWC'),
                                  feature_group_count=ML_QK_COLS)
    qk = jax.nn.silu(qk + conv_b)
    q, k = jnp.split(qk, 2, axis=-1)
    q = q.reshape(B, S, H, ML_QK_DIM).astype(jnp.float32)
    k = k.reshape(B, S, H, ML_QK_DIM).astype(jnp.float32) * (ML_QK_DIM ** -0.5)
    vf = v.reshape(B, S, H, ML_V_DIM).astype(jnp.float32)
    ig = gates[..., :H]
    lf = jax.nn.log_sigmoid(gates[..., H:])
    nc = S // CHUNK

    def to_chunks(t):
        return t.reshape(B, nc, CHUNK, H, -1).transpose(1, 0, 3, 2, 4)

    def gate_chunks(t):
        return t.reshape(B, nc, CHUNK, H).transpose(1, 0, 3, 2)

    init = (jnp.zeros((B, H, ML_V_DIM, ML_QK_DIM), jnp.float32),
            jnp.zeros((B, H, ML_QK_DIM), jnp.float32),
            jnp.zeros((B, H), jnp.float32))
    _, hs = lax.scan(mlstm_chunk_step, init,
                     (to_chunks(q), to_chunks(k), to_chunks(vf), gate_chunks(ig), gate_chunks(lf)))
    hs = hs.transpose(1, 0, 3, 2, 4).reshape(B, S, H, ML_V_DIM)
    hn = rms_norm(hs, head_g.reshape(H, ML_V_DIM)).reshape(B, S, H * ML_V_DIM).astype(h.dtype)
    return (jax.nn.sigmoid(o_pre) * hn) @ w_out


def hier_moe(h, w_group, b_group, w_expert, b_expert, w_gu, w_down):
    B, S, D = h.shape
    xt = h.reshape(-1, D)
    N = xt.shape[0]
    gl = (xt @ w_group + b_group).astype(jnp.float32)
    grp = jnp.argmax(gl, axis=-1)
    p_group = jnp.take_along_axis(jax.nn.softmax(gl, axis=-1), grp[:, None], axis=-1)
    el = (xt @ w_expert + b_expert).astype(jnp.float32).reshape(N, MOE_GROUPS, MOE_PER_GROUP)
    el_g = jnp.take_along_axis(el, grp[:, None, None], axis=1)[:, 0]
    top_v, top_i = lax.top_k(el_g, MOE_TOPK)
    gate = jax.nn.softmax(top_v, axis=-1) * p_group
    eid = (grp[:, None] * MOE_PER_GROUP + top_i).reshape(-1).astype(jnp.int32)
    tok = jnp.repeat(jnp.arange(N, dtype=jnp.int32), MOE_TOPK)
    wts = gate.reshape(-1)
    A = N * MOE_TOPK
    order = jnp.argsort(eid)
    se, stok, sw = eid[order], tok[order], wts[order]
    counts = jnp.bincount(eid, length=MOE_EXPERTS)
    padded = ((counts + MOE_BLOCK - 1) // MOE_BLOCK) * MOE_BLOCK
    start = jnp.cumsum(counts) - counts
    pad_end = jnp.cumsum(padded)
    pad_start = pad_end - padded
    dest = pad_start[se] + jnp.arange(A, dtype=jnp.int32) - start[se]
    P = A + MOE_EXPERTS * MOE_BLOCK
    row_tok = jnp.zeros((P,), jnp.int32).at[dest].set(stok)
    row_w = jnp.zeros((P,), jnp.float32).at[dest].set(sw)
    nb = P // MOE_BLOCK
    block_e = jnp.minimum(jnp.searchsorted(pad_end, jnp.arange(nb) * MOE_BLOCK, side='right'),
                          MOE_EXPERTS - 1)
    xin = xt[row_tok].reshape(nb, MOE_BLOCK, D)

    def expert_block(args):
        xb, e = args
        gt, up = jnp.split(xb @ w_gu[e], 2, axis=-1)
        return (jax.nn.silu(gt) * up) @ w_down[e]

    y = lax.map(expert_block, (xin, block_e)).reshape(P, D)
    out = jnp.zeros_like(xt).at[row_tok].add(y * row_w[:, None].astype(y.dtype))
    return out.reshape(B, S, D)


def setup_inputs(seed: int = 0) -> dict:
    key = jax.random.key(seed)
    ks = jax.random.split(key, 32)
    f32 = jnp.float32
    D = D_MODEL

    def nrm(k, shape, scale):
        return jax.random.normal(k, shape, f32) * scale

    forget_b = jnp.broadcast_to(jnp.linspace(3.0, 6.0, ML_HEADS, dtype=f32), (N_MLSTM_LAYERS, ML_HEADS))
    gate_b = jnp.concatenate([nrm(ks[20], (N_MLSTM_LAYERS, ML_HEADS), 0.1),
                              forget_b + nrm(ks[21], (N_MLSTM_LAYERS, ML_HEADS), 0.1)], axis=-1)
    return {
        'x': nrm(ks[0], (BATCH, SEQ, D), 1.0),
        'c': nrm(ks[1], (BATCH, D), 1.0),
        'positions': (jnp.arange(SEQ, dtype=jnp.int32)[None, :]
                      + jax.random.randint(ks[2], (BATCH, 1), 0, 4096, dtype=jnp.int32)),
        'ada_w': nrm(ks[3], (DEPTH, D, 6 * D), 0.5 * D ** -0.5),
        'ada_b': nrm(ks[4], (DEPTH, 6 * D), 0.02),
        'norm_mix_g': 1.0 + nrm(ks[5], (DEPTH, D), 0.02),
        'norm_ffn_g': 1.0 + nrm(ks[6], (DEPTH, D), 0.02),
        'final_norm_g': 1.0 + nrm(ks[7], (D,), 0.02),
        'attn_w_in': nrm(ks[8], (N_ATTN_LAYERS, D, DA_IN_COLS), D ** -0.5),
        'attn_w_out': nrm(ks[9], (N_ATTN_LAYERS, DA_HEADS * DA_V_DIM, D), (DA_HEADS * DA_V_DIM) ** -0.5),
        'attn_lambda_q1': nrm(ks[10], (N_ATTN_LAYERS, DA_HEAD_DIM), 0.1),
        'attn_lambda_k1': nrm(ks[11], (N_ATTN_LAYERS, DA_HEAD_DIM), 0.1),
        'attn_lambda_q2': nrm(ks[12], (N_ATTN_LAYERS, DA_HEAD_DIM), 0.1),
        'attn_lambda_k2': nrm(ks[13], (N_ATTN_LAYERS, DA_HEAD_DIM), 0.1),
        'attn_head_norm_g': 1.0 + nrm(ks[14], (N_ATTN_LAYERS, DA_V_DIM), 0.02),
        'mlstm_w_in': nrm(ks[15], (N_MLSTM_LAYERS, D, ML_IN_COLS), D ** -0.5),
        'mlstm_conv_w': nrm(ks[16], (N_MLSTM_LAYERS, ML_CONV, ML_QK_COLS), ML_CONV ** -0.5),
        'mlstm_conv_b': nrm(ks[17], (N_MLSTM_LAYERS, ML_QK_COLS), 0.02),
        'mlstm_gate_b': gate_b,
        'mlstm_head_norm_g': 1.0 + nrm(ks[18], (N_MLSTM_LAYERS, ML_V_COLS), 0.02),
        'mlstm_w_out': nrm(ks[19], (N_MLSTM_LAYERS, ML_V_COLS, D), ML_V_COLS ** -0.5),
        'moe_w_group': nrm(ks[22], (DEPTH, D, MOE_GROUPS), D ** -0.5),
        'moe_b_group': nrm(ks[23], (DEPTH, MOE_GROUPS), 0.01),
        'moe_w_expert': nrm(ks[24], (DEPTH, D, MOE_EXPERTS), D ** -0.5),
        'moe_b_expert': nrm(ks[25], (DEPTH, MOE_EXPERTS), 0.01),
        'moe_w_gu': nrm(ks[26], (DEPTH, MOE_EXPERTS, D, 2 * MOE_HIDDEN), D ** -0.5),
        'moe_w_down': nrm(ks[27], (DEPTH, MOE_EXPERTS, MOE_HIDDEN, D), MOE_HIDDEN ** -0.5),
    }


def reference(x, c, positions, ada_w, ada_b, norm_mix_g, norm_ffn_g, final_norm_g,
              attn_w_in, attn_w_out, attn_lambda_q1, attn_lambda_k1, attn_lambda_q2, attn_lambda_k2,
              attn_head_norm_g, mlstm_w_in, mlstm_conv_w, mlstm_conv_b, mlstm_gate_b,
              mlstm_head_norm_g, mlstm_w_out, moe_w_group, moe_b_group, moe_w_expert, moe_b_expert,
              moe_w_gu, moe_w_down):
    cond = jax.nn.silu(c)
    for i in range(DEPTH):
        mod = (cond @ ada_w[i] + ada_b[i])[:, None, :]
        sh1, sc1, gt1, sh2, sc2, gt2 = jnp.split(mod, 6, axis=-1)
        hmix = rms_norm(x, norm_mix_g[i]) * (1.0 + sc1) + sh1
        j = i // N_MIXERS
        if i % N_MIXERS == 0:
            y = diff_attention(hmix, positions, attn_w_in[j], attn_w_out[j], attn_lambda_q1[j],
                               attn_lambda_k1[j], attn_lambda_q2[j], attn_lambda_k2[j],
                               attn_head_norm_g[j], i)
        else:
            y = mlstm_mixer(hmix, mlstm_w_in[j], mlstm_conv_w[j], mlstm_conv_b[j], mlstm_gate_b[j],
                            mlstm_head_norm_g[j], mlstm_w_out[j])
        x = x + gt1 * y
        hffn = rms_norm(x, norm_ffn_g[i]) * (1.0 + sc2) + sh2
        x = x + gt2 * hier_moe(hffn, moe_w_group[i], moe_b_group[i], moe_w_expert[i], moe_b_expert[i],
                               moe_w_gu[i], moe_w_down[i])
    return rms_norm(x, final_norm_g)
```

```python
import math
from contextlib import ExitStack
import numpy as np
import concourse.bass as bass
import concourse.mybir as mybir
from concourse.bass_utils import run_bass_kernel_spmd

F32 = mybir.dt.float32
BF16 = mybir.dt.bfloat16
I32 = mybir.dt.int32
AF = mybir.ActivationFunctionType
ALU = mybir.AluOpType
AX = mybir.AxisListType

D = 2048
TOK = 2048
NT = TOK // 128
NE = 32
CAP = 512
NBLK = CAP // 128
NSLOT = NE * CAP
HID = 1024
EPS = 1e-6

ENGS = ("pe", "act", "dve", "pool", "sp")
SAME_ENG_SYNC = True
N_DMA_SEMS = 40
REPLICA = [[0, 1], [2, 3], [4, 5], [6, 7]]
NCORES_DEBUG = 0
STAGES = None


class Prog:
    def __init__(self, nc, stack):
        self.nc = nc
        self.stack = stack
        self.cnt = {e: 0 for e in ENGS}
        self.esem = {e: stack.enter_context(nc.semaphore("s_" + e)) for e in ENGS}
        self.dsem = [stack.enter_context(nc.semaphore("d%d" % i)) for i in range(N_DMA_SEMS)]
        self.dval = [0] * N_DMA_SEMS
        self.drr = 0
        self.csem = stack.enter_context(nc.semaphore("csem"))
        self.cval = 0
        self.known = {e: {} for e in ENGS}
        self._reset()
        self.uid = 0

    def _reset(self):
        self.ops = {e: [] for e in ENGS}
        self.last_w = {}
        self.readers = {}

    def sb(self, st, name, shape, dt):
        self.uid += 1
        return st.enter_context(self.nc.sbuf_tensor("%s_%d" % (name, self.uid), list(shape), dt))

    def _deps(self, eng, reads, writes):
        deps = []
        for r in reads:
            t = self.last_w.get(r)
            if t is not None:
                deps.append(t)
        for w in writes:
            t = self.last_w.get(w)
            if t is not None:
                deps.append(t)
            deps.extend(self.readers.get(w, ()))
        waits = []
        kn = self.known[eng]
        for (sem, val, deng, sid) in deps:
            if deng == eng and (eng == "pe" or not SAME_ENG_SYNC):
                continue
            if kn.get(sid, 0) >= val:
                continue
            kn[sid] = val
            waits.append((sem, val))
        return waits

    def _commit(self, tok, reads, writes):
        for w in writes:
            self.last_w[w] = tok
            self.readers[w] = []
        for r in reads:
            if r in writes:
                continue
            lst = self.readers.setdefault(r, [])
            lst.append(tok)
            if len(lst) > 48:
                latest = {}
                keep = []
                for t in lst:
                    if t[2] == "dma":
                        keep.append(t)
                    else:
                        latest[t[2]] = t
                self.readers[r] = keep[-40:] + list(latest.values())

    def op(self, eng, fn, reads=(), writes=(), inc=True):
        waits = self._deps(eng, reads, writes)
        if inc:
            self.cnt[eng] += 1
            tok = (self.esem[eng], self.cnt[eng], eng, "e_" + eng)
            self.ops[eng].append((waits, fn, (self.esem[eng], 1)))
        else:
            tok = (self.esem[eng], self.cnt[eng] + 1, eng, "e_" + eng)
            self.ops[eng].append((waits, fn, None))
        self._commit(tok, reads, writes)

    def coll(self, fn, reads=(), writes=()):
        waits = self._deps("pool", reads, writes)
        self.cval += 1
        tok = (self.csem, self.cval, "dma", "csem")
        self.ops["pool"].append((waits, fn, (self.csem, 1)))
        self._commit(tok, reads, writes)

    def dma(self, q, fn, reads=(), writes=()):
        s = self.drr
        self.drr = (self.drr + 1) % N_DMA_SEMS
        waits = self._deps(q, reads, writes)
        prev = self.dval[s]
        sid = "d%d" % s
        if prev > 0 and self.known[q].get(sid, 0) < prev:
            self.known[q][sid] = prev
            waits.append((self.dsem[s], prev))
        self.dval[s] += 16
        tok = (self.dsem[s], self.dval[s], "dma", sid)
        self.ops[q].append((waits, fn, (self.dsem[s], 16)))
        self._commit(tok, reads, writes)

    def barrier(self):
        for e in ENGS:
            waits = []
            for e2 in ENGS:
                if e2 != e and self.cnt[e2] > 0 and self.known[e].get("e_" + e2, 0) < self.cnt[e2]:
                    self.known[e]["e_" + e2] = self.cnt[e2]
                    waits.append((self.esem[e2], self.cnt[e2]))
            for i in range(N_DMA_SEMS):
                sid = "d%d" % i
                if self.dval[i] > 0 and self.known[e].get(sid, 0) < self.dval[i]:
                    self.known[e][sid] = self.dval[i]
                    waits.append((self.dsem[i], self.dval[i]))
            if self.cval > 0 and self.known[e].get("csem", 0) < self.cval:
                self.known[e]["csem"] = self.cval
                waits.append((self.csem, self.cval))
            if waits:
                self.ops[e].append((waits, None, None))

    def emit(self):
        self.barrier()
        ops = self.ops
        with self.nc.Block() as block:
            def run(engname):
                def body(e):
                    for (waits, fn, inc) in ops[engname]:
                        for (sem, val) in waits:
                            e.wait_ge(sem, val)
                        if fn is not None:
                            ins = fn(e)
                            if inc is not None:
                                ins.then_inc(inc[0], inc[1])
                return body
            block.tensor(run("pe"))
            block.scalar(run("act"))
            block.vector(run("dve"))
            block.gpsimd(run("pool"))
            block.sync(run("sp"))
        self._reset()


C_ID, C_LS, C_ON, C_ECAP, C_TRI, C_CM, C_PERM, C_INVF, C_SGN, C_IOTA, C_W = 0, 128, 256, 384, 416, 544, 672, 704, 705, 706, 738


def make_consts():
    c = np.zeros((128, C_W), np.float32)
    i = np.arange(128)
    c[:, C_ID:C_ID + 128] = np.eye(128)
    c[:, C_LS:C_LS + 128] = (i[:, None] < i[None, :])
    c[:, C_ON:C_ON + 128] = 1.0
    c[:, C_ECAP:C_ECAP + 32] = (np.arange(32) * CAP)[None, :]
    c[:, C_TRI:C_TRI + 128] = (i[:, None] <= i[None, :])
    c[:, C_CM:C_CM + 128] = (i[:, None] <= i[None, :])
    for pp in range(32):
        c[(pp + 16) % 32, C_PERM + pp] = 1.0
    half = 16
    invf = 500000.0 ** (-np.arange(half, dtype=np.float32) * 2.0 / 32)
    c[:32, C_INVF] = np.tile(invf, 2)
    c[:16, C_SGN] = -1.0
    c[16:32, C_SGN] = 1.0
    c[:, C_IOTA:C_IOTA + 32] = np.arange(32)[None, :]
    return c


class Ctx:
    pass


def bc(ap1d, n=128):
    return ap1d.partition_broadcast(n)


def load_consts(p, cx, st):
    cx.cf = p.sb(st, "cf", [128, C_W], F32)
    cx.cb = p.sb(st, "cb", [128, C_W], BF16)
    p.dma("sp", lambda e: e.dma_start(out=cx.cf[:], in_=cx.CONSTS.ap()), writes=["cf"])
    p.op("dve", lambda e: e.tensor_copy(out=cx.cb[:], in_=cx.cf[:]), reads=["cf"], writes=["cb"])
    p.emit()


def rms_mod_tile(p, cx, xs, G, SH, hf, hb, ss, nm, rd=(), wr=()):
    junk = cx.junk
    p.op("act", lambda e: e.activation(out=junk[:], in_=xs[:], func=AF.Square, accum_out=ss[:, 0:1]),
         reads=[nm + "xs"], writes=["junk", nm + "ss"])
    p.op("dve", lambda e: e.tensor_scalar(out=ss[:, 1:2], in0=ss[:, 0:1], scalar1=1.0 / D, scalar2=EPS, op0=ALU.mult, op1=ALU.add),
         reads=[nm + "ss"], writes=[nm + "ss"])
    p.op("act", lambda e: e.activation(out=ss[:, 2:3], in_=ss[:, 1:2], func=AF.Sqrt), reads=[nm + "ss"], writes=[nm + "ss"])
    p.op("dve", lambda e: e.reciprocal(out=ss[:, 3:4], in_=ss[:, 2:3]), reads=[nm + "ss"], writes=[nm + "ss"])
    tmp = cx.tmpf
    p.op("dve", lambda e: e.scalar_tensor_tensor(out=tmp[:], in0=xs[:], scalar=ss[:, 3:4], in1=G[:], op0=ALU.mult, op1=ALU.mult),
         reads=[nm + "xs", nm + "ss"] + list(rd), writes=["tmpf"])
    if hf is not None:
        p.op("pool", lambda e: e.tensor_tensor(out=hf[:], in0=tmp[:], in1=SH[:], op=ALU.add), reads=["tmpf"] + list(rd), writes=[nm + "hf"])
        p.op("act", lambda e: e.activation(out=hb[:], in_=hf[:], func=AF.Copy), reads=[nm + "hf"], writes=[nm + "hb"])
    else:
        p.op("pool", lambda e: e.tensor_tensor(out=hb[:], in0=tmp[:], in1=SH[:], op=ALU.add), reads=["tmpf"] + list(rd), writes=[nm + "hb"])


def load_mod_tiles(p, cx, st, L, which, gname):
    base = 3 * D * which
    G = p.sb(st, "G", [128, D], F32)
    SH = p.sb(st, "SH", [128, D], F32)
    GT = p.sb(st, "GT", [128, D], F32)
    gn = p.sb(st, "gn", [128, D], F32)
    md = cx.MODD.ap()
    p.dma("sp", lambda e: e.dma_start(out=SH[:], in_=bc(md[L, base:base + D])), reads=["MODD"], writes=["SH"])
    p.dma("act", lambda e: e.dma_start(out=G[:], in_=bc(md[L, base + D:base + 2 * D])), reads=["MODD"], writes=["G"])
    p.dma("sp", lambda e: e.dma_start(out=GT[:], in_=bc(md[L, base + 2 * D:base + 3 * D])), reads=["MODD"], writes=["GT"])
    p.dma("act", lambda e: e.dma_start(out=gn[:], in_=bc(gname.ap()[L, :])), writes=["gn"])
    p.op("dve", lambda e: e.scalar_tensor_tensor(out=G[:], in0=G[:], scalar=1.0, in1=gn[:], op0=ALU.add, op1=ALU.mult),
         reads=["G", "gn"], writes=["G"])
    return G, SH, GT


def mod_stage(p, cx, L):
    with ExitStack() as st:
        cT = p.sb(st, "cT", [128, 16], F32)
        cTb = p.sb(st, "cTb", [128, 16], BF16)
        ring = [p.sb(st, "aw%d" % i, [128, 4096], BF16) for i in range(4)]
        row = p.sb(st, "mrow", [1, 4096], F32)
        brow = p.sb(st, "brow", [1, 4096], F32)
        p.dma("sp", lambda e: e.dma_start(out=cT[:], in_=cx.CVT.ap()), writes=["cT"])
        p.op("act", lambda e: e.activation(out=cTb[:], in_=cT[:], func=AF.Silu), reads=["cT"], writes=["cTb"])
        aw = cx.ADA_W.ap()
        k = 0
        for ps_ in range(3):
            p.dma("sp", lambda e, ps_=ps_: e.dma_start(out=brow[:], in_=cx.ADA_B.ap()[L:L + 1, ps_ * 4096:(ps_ + 1) * 4096]), writes=["brow"])
            for kc in range(16):
                buf = ring[k % 4]
                bn = "aw%d" % (k % 4)
                k += 1
                p.dma("pool", lambda e, buf=buf, kc=kc, ps_=ps_: e.dma_start(out=buf[:], in_=aw[L, kc * 128:(kc + 1) * 128, ps_ * 4096:(ps_ + 1) * 4096]),
                      writes=[bn])
                for n in range(8):
                    p.op("pe", lambda e, buf=buf, kc=kc, n=n: e.matmul(cx.bank[n][0:1, :], lhsT=cTb[:, kc:kc + 1], rhs=buf[:, n * 512:(n + 1) * 512],
                                                                      start=(kc == 0), stop=(kc == 15)),
                         reads=[bn, "cTb"], writes=["bank%d" % n], inc=(n == 7))
            for n in range(8):
                p.op("dve", lambda e, n=n: e.tensor_tensor(out=row[:, n * 512:(n + 1) * 512], in0=cx.bank[n][0:1, :], in1=brow[:, n * 512:(n + 1) * 512], op=ALU.add),
                     reads=["bank%d" % n, "brow"], writes=["mrow"])
            p.dma("sp", lambda e, ps_=ps_: e.dma_start(out=cx.MODD.ap()[L:L + 1, ps_ * 4096:(ps_ + 1) * 4096], in_=row[:]), reads=["mrow"], writes=["MODD"])
        p.emit()


def moe_stage(p, cx, L, XIN, XOUT, final_g=None):
    with ExitStack() as st0:
        d1i = p.sb(st0, "d1i", [128, NT], I32)
        d2i = p.sb(st0, "d2i", [128, NT], I32)
        gts = p.sb(st0, "gts", [128, 2 * NT], F32)
        xin = XIN.ap()
        with ExitStack() as st:
            G, SH, GT_ = load_mod_tiles(p, cx, st, L, 1, cx.NORM_FFN_G)
            cf, cb = cx.cf, cx.cb
            xs2 = [p.sb(st, "xs%d" % i, [128, D], F32) for i in range(2)]
            hf2 = [p.sb(st, "hf%d" % i, [128, D], F32) for i in range(2)]
            hb2 = [p.sb(st, "hb%d" % i, [128, D], BF16) for i in range(2)]
            cx.junk = p.sb(st, "junk", [128, D], BF16)
            cx.tmpf = p.sb(st, "tmpf", [128, D], F32)
            hT = p.sb(st, "hTf", [128, 16, 128], F32)
            wr = p.sb(st, "wr", [128, 16, 36], F32)
            br = p.sb(st, "br", [128, 36], F32)
            macc = p.sb(st, "macc", [128, 32], BF16)
            sm = [p.sb(st, "sm%d" % i, [128, 160], F32) for i in range(2)]
            mk = [p.sb(st, "mk%d" % i, [128, 32], BF16) for i in range(2)]
            p.dma("sp", lambda e: e.dma_start(out=wr[:], in_=cx.WR.ap()[L]), writes=["wr"])
            p.dma("sp", lambda e: e.dma_start(out=br[:], in_=bc(cx.BRT.ap()[L, :])), writes=["br"])
            p.op("dve", lambda e: e.memset(macc[:], 0.0), writes=["macc"])
            for t in range(NT):
                b = t % 2
                nm = "r%d" % b
                xs, hf, hb, s, m = xs2[b], hf2[b], hb2[b], sm[b], mk[b]
                p.dma("sp", lambda e, xs=xs, t=t: e.dma_start(out=xs[:], in_=xin[t * 128:(t + 1) * 128, :]), reads=["XIN"], writes=[nm + "xs"])
                rms_mod_tile(p, cx, xs, G, SH, hf, hb, s, nm, rd=["G", "SH"])
                for q4 in range(4):
                    for j in range(4):
                        kc = q4 * 4 + j
                        p.op("pe", lambda e, hf=hf, kc=kc, j=j, q4=q4: e.transpose(out=cx.bank[q4][:, j * 128:(j + 1) * 128], in_=hf[:, kc * 128:(kc + 1) * 128], identity=cf[:, C_ID:C_ID + 128]),
                             reads=[nm + "hf"], writes=["bank%d" % q4])
                    eng = "act" if q4 % 2 == 0 else "dve"
                    if eng == "act":
                        p.op("act", lambda e, q4=q4: e.activation(out=hT[:, q4 * 4:(q4 + 1) * 4, :], in_=cx.bank[q4][:].rearrange("p (a b) -> p a b", a=4), func=AF.Copy),
                             reads=["bank%d" % q4], writes=["hT%d" % q4])
                    else:
                        p.op("dve", lambda e, q4=q4: e.tensor_copy(out=hT[:, q4 * 4:(q4 + 1) * 4, :], in_=cx.bank[q4][:].rearrange("p (a b) -> p a b", a=4)),
                             reads=["bank%d" % q4], writes=["hT%d" % q4])
                lgp = cx.bank[4]
                for kc in range(16):
                    p.op("pe", lambda e, kc=kc: e.matmul(lgp[:, 0:36], lhsT=hT[:, kc, :], rhs=wr[:, kc, :], start=(kc == 0), stop=(kc == 15)),
                         reads=["hT%d" % (kc // 4), "wr"], writes=["bank4"], inc=(kc == 15))
                lg = s[:, 8:44]
                sn = nm + "s"
                p.op("dve", lambda e, lg=lg: e.tensor_tensor(out=lg, in0=lgp[:, 0:36], in1=br[:], op=ALU.add), reads=["bank4", "br"], writes=[sn])
                p.op("dve", lambda e, s=s: e.reduce_max(out=s[:, 44:45], in_=s[:, 8:12], axis=AX.X), reads=[sn], writes=[sn])
                p.op("dve", lambda e, s=s: e.tensor_scalar(out=s[:, 45:46], in0=s[:, 44:45], scalar1=-1.0, scalar2=None, op0=ALU.mult), reads=[sn], writes=[sn])
                p.op("dve", lambda e, s=s: e.tensor_scalar(out=s[:, 48:52], in0=s[:, 8:12], scalar1=s[:, 44:45], scalar2=None, op0=ALU.is_equal), reads=[sn], writes=[sn])
                p.op("act", lambda e, s=s: e.activation(out=s[:, 100:104], in_=s[:, 8:12], func=AF.Exp, bias=s[:, 45:46], accum_out=s[:, 46:47]), reads=[sn], writes=[sn])
                p.op("dve", lambda e, s=s: e.reciprocal(out=s[:, 47:48], in_=s[:, 46:47]), reads=[sn], writes=[sn])
                p.op("dve", lambda e, s=s: e.tensor_scalar(out=s[:, 52:56], in0=s[:, 48:52], scalar1=1e30, scalar2=-1e30, op0=ALU.mult, op1=ALU.add), reads=[sn], writes=[sn])
                p.op("dve", lambda e, s=s: e.tensor_tensor(out=s[:, 56:88].rearrange("p (g k) -> p g k", g=4), in0=s[:, 12:44].rearrange("p (g k) -> p g k", g=4),
                                                           in1=s[:, 52:56].unsqueeze(2).to_broadcast([128, 4, 8]), op=ALU.add), reads=[sn], writes=[sn])
                p.op("dve", lambda e, s=s: e.max(out=s[:, 88:96], in_=s[:, 56:88]), reads=[sn], writes=[sn])
                p.op("dve", lambda e, s=s: e.tensor_tensor(out=s[:, 96:97], in0=s[:, 89:90], in1=s[:, 88:89], op=ALU.subtract), reads=[sn], writes=[sn])
                p.op("act", lambda e, s=s: e.activation(out=s[:, 97:98], in_=s[:, 96:97], func=AF.Exp), reads=[sn], writes=[sn])
                p.op("dve", lambda e, s=s: e.tensor_scalar(out=s[:, 98:99], in0=s[:, 97:98], scalar1=1.0, scalar2=None, op0=ALU.add), reads=[sn], writes=[sn])
                p.op("dve", lambda e, s=s: e.reciprocal(out=s[:, 99:100], in_=s[:, 98:99]), reads=[sn], writes=[sn])
                p.op("dve", lambda e, s=s, t=t: e.tensor_tensor(out=gts[:, 2 * t:2 * t + 1], in0=s[:, 99:100], in1=s[:, 47:48], op=ALU.mult), reads=[sn], writes=["gts"])
                p.op("dve", lambda e, s=s, t=t: e.tensor_tensor(out=gts[:, 2 * t + 1:2 * t + 2], in0=gts[:, 2 * t:2 * t + 1], in1=s[:, 97:98], op=ALU.mult), reads=[sn, "gts"], writes=["gts"])
                p.op("dve", lambda e, s=s: e.tensor_scalar(out=s[:, 100:132], in0=s[:, 56:88], scalar1=s[:, 88:89], scalar2=None, op0=ALU.is_equal), reads=[sn], writes=[sn])
                p.op("dve", lambda e, s=s: e.tensor_scalar(out=s[:, 8:40], in0=s[:, 56:88], scalar1=s[:, 89:90], scalar2=None, op0=ALU.is_equal), reads=[sn], writes=[sn])
                p.op("dve", lambda e, s=s, m=m: e.tensor_tensor(out=m[:], in0=s[:, 100:132], in1=s[:, 8:40], op=ALU.add), reads=[sn], writes=[nm + "mk"])
                pp = cx.bank[5]
                p.op("pe", lambda e, m=m: e.matmul(pp[:, 0:32], lhsT=cb[:, C_LS:C_LS + 128], rhs=m[:], start=True, stop=False), reads=[nm + "mk"], writes=["bank5"])
                p.op("pe", lambda e: e.matmul(pp[:, 0:32], lhsT=cb[:, C_ON:C_ON + 128], rhs=macc[:], start=False, stop=True), reads=["macc"], writes=["bank5"])
                p.op("dve", lambda e, s=s: e.tensor_tensor(out=s[:, 56:88], in0=pp[:, 0:32], in1=cf[:, C_ECAP:C_ECAP + 32], op=ALU.add), reads=["bank5", sn], writes=[sn])
                p.op("dve", lambda e, m=m: e.tensor_tensor(out=macc[:], in0=macc[:], in1=m[:], op=ALU.add), reads=["macc", nm + "mk", "bank5"], writes=["macc"])
                p.op("dve", lambda e, s=s: e.tensor_tensor(out=s[:, 100:132], in0=s[:, 100:132], in1=s[:, 56:88], op=ALU.mult), reads=[sn], writes=[sn])
                p.op("dve", lambda e, s=s: e.reduce_sum(out=s[:, 132:133], in_=s[:, 100:132], axis=AX.X), reads=[sn], writes=[sn])
                p.op("dve", lambda e, s=s: e.tensor_tensor(out=s[:, 8:40], in0=s[:, 8:40], in1=s[:, 56:88], op=ALU.mult), reads=[sn], writes=[sn])
                p.op("dve", lambda e, s=s: e.reduce_sum(out=s[:, 133:134], in_=s[:, 8:40], axis=AX.X), reads=[sn], writes=[sn])
                p.op("dve", lambda e, s=s, t=t: e.tensor_copy(out=d1i[:, t:t + 1], in_=s[:, 132:133]), reads=[sn], writes=["d1i"])
                p.op("dve", lambda e, s=s, t=t: e.tensor_copy(out=d2i[:, t:t + 1], in_=s[:, 133:134]), reads=[sn], writes=["d2i"])
                p.dma("pool", lambda e, hb=hb, t=t: e.indirect_dma_start(out=cx.XS.ap(), out_offset=bass.IndirectOffsetOnAxis(ap=d1i[:, t:t + 1], axis=0), in_=hb[:], in_offset=None),
                      reads=[nm + "hb", "d1i"], writes=["XSa%d" % t])
                p.dma("pool", lambda e, hb=hb, t=t: e.indirect_dma_start(out=cx.XS.ap(), out_offset=bass.IndirectOffsetOnAxis(ap=d2i[:, t:t + 1], axis=0), in_=hb[:], in_offset=None),
                      reads=[nm + "hb", "d2i"], writes=["XSb%d" % t])
            p.emit()
        with ExitStack() as st:
            cb = cx.cb
            NWG = 24
            NWD = 12
            wg = [p.sb(st, "wg%d" % i, [128, 2, 512], BF16) for i in range(NWG)]
            wd = [p.sb(st, "wd%d" % i, [128, D], BF16) for i in range(NWD)]
            xg = p.sb(st, "xg", [128, NBLK, D], BF16)
            xT = [p.sb(st, "xT%d" % i, [128, 16, CAP], BF16) for i in range(2)]
            hTb = p.sb(st, "hTb", [128, 8, CAP], BF16)
            sg = [p.sb(st, "sg%d" % i, [128, 256], F32) for i in range(2)]
            yb = [p.sb(st, "yb%d" % i, [128, D], F32) for i in range(2)]
            xsv = cx.XS.ap()[0:NSLOT].rearrange("(e b p) d -> e p b d", b=NBLK, p=128)
            ysv = cx.YS.ap()[0:NSLOT].rearrange("(e b p) d -> e b p d", b=NBLK, p=128)
            wguv = cx.W_GU.ap()
            wdv = cx.W_DN.ap()
            kg = 0
            kd = 0
            ky = 0
            for ex in range(NE):
                xb_ = ex % 2
                xTb = xT[xb_]
                p.dma("sp", lambda e, ex=ex: e.dma_start(out=xg[:], in_=xsv[ex]), writes=["xg"])
                tcnt = 0
                for blk in range(NBLK):
                    for hh in range(2):
                        bk = cx.bank[4 + tcnt % 4]
                        bkn = "bank%d" % (4 + tcnt % 4)
                        tcnt += 1
                        bkv = bk[:].bitcast(BF16)
                        for j in range(8):
                            kc = hh * 8 + j
                            p.op("pe", lambda e, bkv=bkv, j=j, kc=kc, blk=blk: e.transpose(out=bkv[:, j * 128:(j + 1) * 128], in_=xg[:, blk, kc * 128:(kc + 1) * 128], identity=cb[:, C_ID:C_ID + 128]),
                                 reads=["xg"], writes=[bkn], inc=(j == 7))
                        if tcnt % 2 == 0:
                            p.op("act", lambda e, bkv=bkv, hh=hh, blk=blk, xTb=xTb: e.activation(out=xTb[:, hh * 8:(hh + 1) * 8, blk * 128:(blk + 1) * 128], in_=bkv.rearrange("p (a b) -> p a b", a=8), func=AF.Copy),
                                 reads=[bkn], writes=["xT%d" % xb_])
                        else:
                            p.op("dve", lambda e, bkv=bkv, hh=hh, blk=blk, xTb=xTb: e.tensor_copy(out=xTb[:, hh * 8:(hh + 1) * 8, blk * 128:(blk + 1) * 128], in_=bkv.rearrange("p (a b) -> p a b", a=8)),
                                 reads=[bkn], writes=["xT%d" % xb_])
                wdl = []
                for hc in range(8):
                    i = kd % NWD
                    kd += 1
                    wdl.append(i)
                    p.dma("pool", lambda e, i=i, ex=ex, hc=hc: e.dma_start(out=wd[i][:], in_=wdv[cx.WL(L), ex, hc * 128:(hc + 1) * 128, :]), writes=["wd%d" % i])
                for hp in range(2):
                    wgl = []
                    for kc in range(16):
                        i = kg % NWG
                        kg += 1
                        wgl.append(i)
                        p.dma("pool", lambda e, i=i, ex=ex, kc=kc, hp=hp: e.dma_start(out=wg[i][:], in_=wguv[cx.WL(L), ex, kc * 128:(kc + 1) * 128, :].rearrange("k (g c) -> k g c", g=2)[:, :, hp * 512:(hp + 1) * 512]),
                              writes=["wg%d" % i])
                    for sb_ in range(CAP // 256):
                        for kc in range(16):
                            i = wgl[kc]
                            for gi in range(2):
                                for j in range(4):
                                    bi = gi * 2 + j // 2
                                    p.op("pe", lambda e, i=i, gi=gi, j=j, bi=bi, kc=kc, xTb=xTb, sb_=sb_: e.matmul(cx.bank[bi][:, (j % 2) * 256:(j % 2 + 1) * 256], lhsT=wg[i][:, gi, j * 128:(j + 1) * 128], rhs=xTb[:, kc, sb_ * 256:(sb_ + 1) * 256],
                                                                                                                  start=(kc == 0 and j % 2 == 0), stop=(kc == 15), skip_group_check=True),
                                         reads=["wg%d" % i, "xT%d" % xb_], writes=["bank%d" % bi], inc=(kc == 15))
                        for j in range(4):
                            s_ = sg[j % 2]
                            p.op("act", lambda e, s_=s_, j=j: e.activation(out=s_[:], in_=cx.bank[j // 2][:, (j % 2) * 256:(j % 2 + 1) * 256], func=AF.Silu),
                                 reads=["bank%d" % (j // 2)], writes=["sg%d" % (j % 2)])
                            p.op("dve", lambda e, s_=s_, j=j, hp=hp, sb_=sb_: e.tensor_tensor(out=hTb[:, hp * 4 + j, sb_ * 256:(sb_ + 1) * 256], in0=s_[:], in1=cx.bank[2 + j // 2][:, (j % 2) * 256:(j % 2 + 1) * 256], op=ALU.mult),
                                 reads=["sg%d" % (j % 2), "bank%d" % (2 + j // 2)], writes=["hTb"])
                for blk in range(NBLK):
                    y = yb[ky % 2]
                    yn = "yb%d" % (ky % 2)
                    ky += 1
                    for cg in range(4):
                        for hc in range(8):
                            i = wdl[hc]
                            p.op("pe", lambda e, i=i, cg=cg, hc=hc, blk=blk: e.matmul(cx.bank[4 + cg][:], lhsT=hTb[:, hc, blk * 128:(blk + 1) * 128], rhs=wd[i][:, cg * 512:(cg + 1) * 512], start=(hc == 0), stop=(hc == 7)),
                                 reads=["hTb", "wd%d" % i], writes=["bank%d" % (4 + cg)], inc=(hc == 7))
                        if cg % 2 == 0:
                            p.op("act", lambda e, y=y, cg=cg: e.activation(out=y[:, cg * 512:(cg + 1) * 512], in_=cx.bank[4 + cg][:], func=AF.Copy), reads=["bank%d" % (4 + cg)], writes=[yn])
                        else:
                            p.op("dve", lambda e, y=y, cg=cg: e.tensor_copy(out=y[:, cg * 512:(cg + 1) * 512], in_=cx.bank[4 + cg][:]), reads=["bank%d" % (4 + cg)], writes=[yn])
                    p.dma("act", lambda e, y=y, ex=ex, blk=blk: e.dma_start(out=ysv[ex, blk], in_=y[:]), reads=[yn], writes=["YS%d_%d" % (ex, blk)])
            p.emit()
        with ExitStack() as st:
            xs2 = [p.sb(st, "cxs%d" % i, [128, D], F32) for i in range(2)]
            y1 = [p.sb(st, "y1_%d" % i, [128, D], F32) for i in range(2)]
            y2 = [p.sb(st, "y2_%d" % i, [128, D], F32) for i in range(2)]
            GT = p.sb(st, "GT", [128, D], F32)
            p.dma("sp", lambda e: e.dma_start(out=GT[:], in_=bc(cx.MODD.ap()[L, 5 * D:6 * D])), writes=["GT"])
            if final_g is not None:
                FG = p.sb(st, "FG", [128, D], F32)
                p.dma("sp", lambda e: e.dma_start(out=FG[:], in_=bc(final_g.ap()[:])), writes=["FG"])
                cx.junk = p.sb(st, "junk", [128, D], BF16)
                fs = [p.sb(st, "fs%d" % i, [128, 4], F32) for i in range(2)]
            xout = XOUT.ap()
            for t in range(NT):
                b = t % 2
                xs, a1, a2 = xs2[b], y1[b], y2[b]
                p.dma("sp", lambda e, xs=xs, t=t: e.dma_start(out=xs[:], in_=xin[t * 128:(t + 1) * 128, :]), reads=["XIN"], writes=["cxs%d" % b])
                p.dma("pool", lambda e, a1=a1, t=t: e.indirect_dma_start(out=a1[:], out_offset=None, in_=cx.YS.ap(), in_offset=bass.IndirectOffsetOnAxis(ap=d1i[:, t:t + 1], axis=0)),
                      reads=["d1i"], writes=["y1_%d" % b])
                p.dma("pool", lambda e, a2=a2, t=t: e.indirect_dma_start(out=a2[:], out_offset=None, in_=cx.YS.ap(), in_offset=bass.IndirectOffsetOnAxis(ap=d2i[:, t:t + 1], axis=0)),
                      reads=["d2i"], writes=["y2_%d" % b])
                p.op("act", lambda e, a1=a1, t=t: e.activation(out=a1[:], in_=a1[:], func=AF.Copy, scale=gts[:, 2 * t:2 * t + 1]), reads=["y1_%d" % b, "gts"], writes=["y1_%d" % b])
                p.op("dve", lambda e, a1=a1, a2=a2, t=t: e.scalar_tensor_tensor(out=a2[:], in0=a2[:], scalar=gts[:, 2 * t + 1:2 * t + 2], in1=a1[:], op0=ALU.mult, op1=ALU.add),
                     reads=["y1_%d" % b, "y2_%d" % b, "gts"], writes=["y2_%d" % b])
                p.op("pool", lambda e, a2=a2: e.tensor_tensor(out=a2[:], in0=a2[:], in1=GT[:], op=ALU.mult), reads=["y2_%d" % b, "GT"], writes=["y2_%d" % b])
                p.op("dve", lambda e, a2=a2, xs=xs: e.tensor_tensor(out=xs[:], in0=a2[:], in1=xs[:], op=ALU.add), reads=["y2_%d" % b, "cxs%d" % b], writes=["cxs%d" % b])
                if final_g is not None:
                    f = fs[b]
                    p.op("act", lambda e, xs=xs, f=f: e.activation(out=cx.junk[:], in_=xs[:], func=AF.Square, accum_out=f[:, 0:1]), reads=["cxs%d" % b], writes=["junk", "fs%d" % b])
                    p.op("dve", lambda e, f=f: e.tensor_scalar(out=f[:, 1:2], in0=f[:, 0:1], scalar1=1.0 / D, scalar2=EPS, op0=ALU.mult, op1=ALU.add), reads=["fs%d" % b], writes=["fs%d" % b])
                    p.op("act", lambda e, f=f: e.activation(out=f[:, 2:3], in_=f[:, 1:2], func=AF.Sqrt), reads=["fs%d" % b], writes=["fs%d" % b])
                    p.op("dve", lambda e, f=f: e.reciprocal(out=f[:, 3:4], in_=f[:, 2:3]), reads=["fs%d" % b], writes=["fs%d" % b])
                    p.op("dve", lambda e, xs=xs, f=f: e.scalar_tensor_tensor(out=xs[:], in0=xs[:], scalar=f[:, 3:4], in1=FG[:], op0=ALU.mult, op1=ALU.mult),
                         reads=["cxs%d" % b, "fs%d" % b, "FG"], writes=["cxs%d" % b])
                p.dma("sp", lambda e, xs=xs, t=t: e.dma_start(out=xout[t * 128:(t + 1) * 128, :], in_=xs[:]), reads=["cxs%d" % b], writes=["XOUT"])
            p.emit()


def norm_to_hT(p, cx, XSRC, G, SH, hT, tb0=6):
    with ExitStack() as st:
        xs2 = [p.sb(st, "nxs%d" % i, [128, D], F32) for i in range(2)]
        hb2 = [p.sb(st, "nhb%d" % i, [128, D], BF16) for i in range(2)]
        ss2 = [p.sb(st, "nss%d" % i, [128, 4], F32) for i in range(2)]
        cx.junk = p.sb(st, "junk", [128, D], BF16)
        cx.tmpf = p.sb(st, "tmpf", [128, D], F32)
        cb = cx.cb
        for t in range(NT):
            b = t % 2
            nm = "n%d" % b
            xs, hb, ss = xs2[b], hb2[b], ss2[b]
            p.dma("sp", lambda e, xs=xs, t=t: e.dma_start(out=xs[:], in_=XSRC[t * 128:(t + 1) * 128, :]), writes=[nm + "xs"])
            rms_mod_tile(p, cx, xs, G, SH, None, hb, ss, nm, rd=["G", "SH"])
            for hh in range(2):
                bk = cx.bank[tb0 + hh]
                bkn = "bank%d" % (tb0 + hh)
                bkv = bk[:].bitcast(BF16)
                for j in range(8):
                    kc = hh * 8 + j
                    p.op("pe", lambda e, bkv=bkv, j=j, kc=kc, hb=hb: e.transpose(out=bkv[:, j * 128:(j + 1) * 128], in_=hb[:, kc * 128:(kc + 1) * 128], identity=cb[:, C_ID:C_ID + 128]),
                         reads=[nm + "hb"], writes=[bkn], inc=(j == 7))
                if hh == 0:
                    p.op("act", lambda e, bkv=bkv, hh=hh, t=t: e.activation(out=hT[:, hh * 8:(hh + 1) * 8, t * 128:(t + 1) * 128], in_=bkv.rearrange("p (a b) -> p a b", a=8), func=AF.Copy),
                         reads=[bkn], writes=["hT_%d_%d" % (t, hh)])
                else:
                    p.op("dve", lambda e, bkv=bkv, hh=hh, t=t: e.tensor_copy(out=hT[:, hh * 8:(hh + 1) * 8, t * 128:(t + 1) * 128], in_=bkv.rearrange("p (a b) -> p a b", a=8)),
                         reads=[bkn], writes=["hT_%d_%d" % (t, hh)])
        p.emit()


def proj_out_stage(p, cx, SRC, Wap, GT, XIN, XOUT):
    with ExitStack() as st:
        cb = cx.cb
        W = p.sb(st, "Wout", [128, 16, D], BF16)
        wv = Wap.rearrange("(kc k) c -> k kc c", k=128)
        for i in range(4):
            p.dma("pool", lambda e, i=i: e.dma_start(out=W[:, :, i * 512:(i + 1) * 512], in_=wv[:, :, i * 512:(i + 1) * 512]), writes=["Wout%d" % i])
        sr2 = [p.sb(st, "osr%d" % i, [128, D], BF16) for i in range(2)]
        sT2 = [p.sb(st, "osT%d" % i, [128, 16, 128], BF16) for i in range(2)]
        xs2 = [p.sb(st, "oxs%d" % i, [128, D], F32) for i in range(2)]
        tm2 = [p.sb(st, "otm%d" % i, [128, D], F32) for i in range(2)]
        for t in range(NT):
            b = t % 2
            sr, sT, xs, tm = sr2[b], sT2[b], xs2[b], tm2[b]
            p.dma("sp", lambda e, sr=sr, t=t: e.dma_start(out=sr[:], in_=SRC[t * 128:(t + 1) * 128, :]), writes=["osr%d" % b])
            p.dma("act", lambda e, xs=xs, t=t: e.dma_start(out=xs[:], in_=XIN[t * 128:(t + 1) * 128, :]), writes=["oxs%d" % b])
            for hh in range(2):
                bk = cx.bank[4 + hh]
                bkn = "bank%d" % (4 + hh)
                bkv = bk[:].bitcast(BF16)
                for j in range(8):
                    kc = hh * 8 + j
                    p.op("pe", lambda e, bkv=bkv, j=j, kc=kc, sr=sr: e.transpose(out=bkv[:, j * 128:(j + 1) * 128], in_=sr[:, kc * 128:(kc + 1) * 128], identity=cb[:, C_ID:C_ID + 128]),
                         reads=["osr%d" % b], writes=[bkn], inc=(j == 7))
                if hh == 0:
                    p.op("act", lambda e, bkv=bkv, hh=hh, sT=sT: e.activation(out=sT[:, hh * 8:(hh + 1) * 8, :], in_=bkv.rearrange("p (a b) -> p a b", a=8), func=AF.Copy),
                         reads=[bkn], writes=["osT%d" % b])
                else:
                    p.op("dve", lambda e, bkv=bkv, hh=hh, sT=sT: e.tensor_copy(out=sT[:, hh * 8:(hh + 1) * 8, :], in_=bkv.rearrange("p (a b) -> p a b", a=8)),
                         reads=[bkn], writes=["osT%d" % b])
            for cg in range(4):
                for kc in range(16):
                    p.op("pe", lambda e, cg=cg, kc=kc, sT=sT: e.matmul(cx.bank[cg][:], lhsT=sT[:, kc, :], rhs=W[:, kc, cg * 512:(cg + 1) * 512], start=(kc == 0), stop=(kc == 15)),
                         reads=["osT%d" % b, "Wout%d" % cg], writes=["bank%d" % cg], inc=(kc == 15))
                p.op("dve", lambda e, cg=cg, tm=tm: e.tensor_tensor(out=tm[:, cg * 512:(cg + 1) * 512], in0=cx.bank[cg][:], in1=GT[:, cg * 512:(cg + 1) * 512], op=ALU.mult),
                     reads=["bank%d" % cg, "GT"], writes=["otm%d_%d" % (b, cg)])
                p.op("pool", lambda e, cg=cg, tm=tm, xs=xs: e.tensor_tensor(out=tm[:, cg * 512:(cg + 1) * 512], in0=tm[:, cg * 512:(cg + 1) * 512], in1=xs[:, cg * 512:(cg + 1) * 512], op=ALU.add),
                     reads=["otm%d_%d" % (b, cg), "oxs%d" % b], writes=["otm%d_%d" % (b, cg)])
            p.dma("sp", lambda e, tm=tm, t=t: e.dma_start(out=XOUT[t * 128:(t + 1) * 128, :], in_=tm[:]), reads=["otm%d_%d" % (b, cg) for cg in range(4)], writes=["XO%d" % t])
        p.emit()


TWO_PI = 2.0 * math.pi
C1 = 6.28125
C2 = TWO_PI - C1


def rope_tables(p, cx, POSap, COS, SINS, tag):
    with ExitStack() as st:
        cf = cx.cf
        a = p.sb(st, "ra", [32, TOK], F32)
        k = p.sb(st, "rk", [32, TOK], F32)
        ki = p.sb(st, "rki", [32, TOK], I32)
        m = p.sb(st, "rm", [32, TOK], F32)
        p.dma("pool", lambda e: e.dma_start(out=a[:], in_=bc(POSap, 32)), writes=["ra"])
        p.op("dve", lambda e: e.tensor_scalar(out=a[:], in0=a[:], scalar1=cf[0:32, C_INVF:C_INVF + 1], scalar2=None, op0=ALU.mult), reads=["ra"], writes=["ra"])

        def reduce_(src, dst, shift):
            p.op("dve", lambda e: e.tensor_scalar(out=k[:], in0=src[:], scalar1=shift, scalar2=1.0 / TWO_PI, op0=ALU.add, op1=ALU.mult), reads=["ra", "rd"], writes=["rk"])
            p.op("dve", lambda e: e.tensor_copy(out=ki[:], in_=k[:]), reads=["rk"], writes=["rki"])
            p.op("dve", lambda e: e.tensor_copy(out=k[:], in_=ki[:]), reads=["rki"], writes=["rk"])
            p.op("dve", lambda e: e.scalar_tensor_tensor(out=dst[:], in0=k[:], scalar=-C1, in1=src[:], op0=ALU.mult, op1=ALU.add), reads=["rk", "ra"], writes=["rd"])
            p.op("dve", lambda e: e.scalar_tensor_tensor(out=dst[:], in0=k[:], scalar=-C2, in1=dst[:], op0=ALU.mult, op1=ALU.add), reads=["rk", "rd"], writes=["rd"])
            if shift != 0.0:
                p.op("dve", lambda e: e.tensor_scalar(out=dst[:], in0=dst[:], scalar1=shift, scalar2=None, op0=ALU.add), reads=["rd"], writes=["rd"])
            p.op("dve", lambda e: e.tensor_scalar(out=m[:], in0=dst[:], scalar1=math.pi, scalar2=-TWO_PI, op0=ALU.is_gt, op1=ALU.mult), reads=["rd"], writes=["rm"])
            p.op("dve", lambda e: e.tensor_tensor(out=dst[:], in0=dst[:], in1=m[:], op=ALU.add), reads=["rd", "rm"], writes=["rd"])
            p.op("dve", lambda e: e.tensor_scalar(out=m[:], in0=dst[:], scalar1=-math.pi, scalar2=TWO_PI, op0=ALU.is_lt, op1=ALU.mult), reads=["rd"], writes=["rm"])
            p.op("dve", lambda e: e.tensor_tensor(out=dst[:], in0=dst[:], in1=m[:], op=ALU.add), reads=["rd", "rm"], writes=["rd"])
            p.op("dve", lambda e: e.tensor_scalar(out=dst[:], in0=dst[:], scalar1=math.pi, scalar2=-math.pi, op0=ALU.min, op1=ALU.max), reads=["rd"], writes=["rd"])

        reduce_(a, SINS, 0.0)
        p.op("act", lambda e: e.activation(out=SINS[:], in_=SINS[:], func=AF.Sin), reads=["rd"], writes=["rd"])
        p.op("dve", lambda e: e.tensor_scalar(out=SINS[:], in0=SINS[:], scalar1=cf[0:32, C_SGN:C_SGN + 1], scalar2=None, op0=ALU.mult), reads=["rd"], writes=["rd"])
        reduce_(a, COS, math.pi / 2)
        p.op("act", lambda e: e.activation(out=COS[:], in_=COS[:], func=AF.Sin), reads=["rd"], writes=["rd"])
        p.emit()


def attn_proj(p, cx, hT, own, COS, SINS, nmax):
    with ExitStack() as st:
        cb = cx.cb
        wp = [p.sb(st, "wp%d" % i, [128, 16, 512], BF16) for i in range(2)]
        qk = [p.sb(st, "qk%d" % i, [128, 512], BF16) for i in range(4)]
        sq = [p.sb(st, "sq%d" % i, [128, 512], BF16) for i in range(2)]
        t1 = [p.sb(st, "t1_%d" % i, [32, 512], F32) for i in range(2)]
        t2 = [p.sb(st, "t2_%d" % i, [32, 512], F32) for i in range(2)]
        nm1 = p.sb(st, "nm1", [1, 2], F32)
        vs = [p.sb(st, "vs%d" % i, [128, 2, 257], BF16) for i in range(4)]
        for i in range(4):
            p.op("pool", lambda e, i=i: e.memset(vs[i][:], 1.0), writes=["vs%d" % i])
        wv = cx.ATTN_W_IN.ap().rearrange("(kc k) c -> k kc c", k=128)
        toff = TOK if own else 0
        pieces = range(12) if own else range(4, 12)
        cnt = 0
        vcnt = 0
        for pi, j in enumerate(pieces):
            w = wp[pi % 2]
            wn = "wp%d" % (pi % 2)
            p.dma("pool", lambda e, w=w, j=j: e.dma_start(out=w[:], in_=wv[:, :, j * 512:(j + 1) * 512]), writes=[wn])
            if j < 8:
                isq = j < 4
                for cc in range(4):
                    gcc = (j % 4) * 4 + cc
                    for tg in range(4):
                        bi = cnt % 4
                        bk = cx.bank[bi]
                        q_ = qk[cnt % 4]
                        qn = "qk%d" % (cnt % 4)
                        s_ = sq[cnt % 2]
                        sn = "sq%d" % (cnt % 2)
                        a1, a2 = t1[cnt % 2], t2[cnt % 2]
                        an = "t12_%d" % (cnt % 2)
                        nb = cx.bank[4 + cnt % 2]
                        nbn = "bank%d" % (4 + cnt % 2)
                        sb_ = cx.bank[6 + cnt % 2]
                        sbn = "bank%d" % (6 + cnt % 2)
                        cnt += 1
                        for kc in range(16):
                            p.op("pe", lambda e, bk=bk, w=w, kc=kc, cc=cc, tg=tg: e.matmul(bk[:], lhsT=w[:, kc, cc * 128:(cc + 1) * 128], rhs=hT[:, kc, tg * 512:(tg + 1) * 512], start=(kc == 0), stop=(kc == 15)),
                                 reads=[wn], writes=["bank%d" % bi], inc=(kc == 15))
                        p.op("act", lambda e, bk=bk, q_=q_: e.activation(out=q_[:], in_=bk[:], func=AF.Copy), reads=["bank%d" % bi], writes=[qn])
                        p.op("act", lambda e, bk=bk, s_=s_: e.activation(out=s_[:], in_=bk[:], func=AF.Square), reads=["bank%d" % bi], writes=[sn])
                        p.op("pe", lambda e, nb=nb, s_=s_: e.matmul(nb[0:1, :], lhsT=cb[:, C_ON:C_ON + 1], rhs=s_[:], start=True, stop=True), reads=[sn], writes=[nbn])
                        p.op("dve", lambda e, nb=nb: e.reduce_max(out=nm1[:, 0:1], in_=nb[0:1, :], axis=AX.X), reads=[nbn], writes=["nm1"])
                        ix = gcc if isq else 16 + gcc
                        p.op("dve", lambda e, ix=ix: e.tensor_tensor(out=nmax[:, ix:ix + 1], in0=nmax[:, ix:ix + 1], in1=nm1[:, 0:1], op=ALU.max), reads=["nm1", "nmax"], writes=["nmax"])
                        p.op("pe", lambda e, sb_=sb_, q_=q_: e.matmul(sb_[0:32, :], lhsT=cb[0:32, C_PERM:C_PERM + 32], rhs=q_[0:32, :], start=True, stop=True), reads=[qn], writes=[sbn])
                        p.op("dve", lambda e, a1=a1, q_=q_, tg=tg: e.tensor_tensor(out=a1[:], in0=q_[0:32, :], in1=COS[:, tg * 512:(tg + 1) * 512], op=ALU.mult), reads=[qn], writes=[an + "a"])
                        p.op("dve", lambda e, a2=a2, sb_=sb_, tg=tg: e.tensor_tensor(out=a2[:], in0=sb_[0:32, :], in1=SINS[:, tg * 512:(tg + 1) * 512], op=ALU.mult), reads=[sbn], writes=[an + "b"])
                        p.op("dve", lambda e, a1=a1, a2=a2, q_=q_: e.tensor_tensor(out=q_[0:32, :], in0=a1[:], in1=a2[:], op=ALU.add), reads=[an + "a", an + "b", qn], writes=[qn])
                        if isq:
                            dst = cx.QTD.ap()[gcc, :, tg * 512:(tg + 1) * 512]
                        else:
                            dst = cx.KTD.ap()[gcc, :, toff + tg * 512:toff + (tg + 1) * 512]
                        p.dma("sp", lambda e, dst=dst, q_=q_: e.dma_start(out=dst, in_=q_[:]), reads=[qn], writes=["qkd%d" % cnt])
            else:
                vj = j - 8
                for t in range(NT):
                    bi = cnt % 4
                    bk = cx.bank[bi]
                    cnt += 1
                    v_ = vs[vcnt % 4]
                    vn = "vs%d" % (vcnt % 4)
                    vcnt += 1
                    for kc in range(16):
                        p.op("pe", lambda e, bk=bk, w=w, kc=kc, t=t: e.matmul(bk[:], lhsT=hT[:, kc, t * 128:(t + 1) * 128], rhs=w[:, kc, :], start=(kc == 0), stop=(kc == 15)),
                             reads=[wn], writes=["bank%d" % bi], inc=(kc == 15))
                    if t % 2 == 0:
                        p.op("act", lambda e, bk=bk, v_=v_: e.activation(out=v_[:, :, 0:256], in_=bk[:].rearrange("p (a b) -> p a b", a=2), func=AF.Copy), reads=["bank%d" % bi], writes=[vn])
                    else:
                        p.op("dve", lambda e, bk=bk, v_=v_: e.tensor_copy(out=v_[:, :, 0:256], in_=bk[:].rearrange("p (a b) -> p a b", a=2)), reads=["bank%d" % bi], writes=[vn])
                    r0 = toff + t * 128
                    p.dma("act", lambda e, v_=v_, r0=r0, vj=vj: e.dma_start(out=cx.VD.ap()[r0:r0 + 128, 2 * vj:2 * vj + 2, :], in_=v_[:]), reads=[vn], writes=["vd%d" % cnt])
        p.emit()


def attn_consts(p, cx, st0, nmax):
    negc = p.sb(st0, "negc", [128, 16], F32)
    negcp = p.sb(st0, "negcp", [128, 16], F32)
    nlam = p.sb(st0, "nlam", [128, 1], F32)
    HG = p.sb(st0, "HG", [128, 256], F32)
    with ExitStack() as st:
        cf = cx.cf
        r = p.sb(st, "acr", [1, 64], F32)
        lm = p.sb(st, "lm", [1, 4, 128], F32)
        pf = p.sb(st, "pf", [128, 1], F32)
        for i, nm_ in enumerate([cx.LQ1, cx.LK1, cx.LQ2, cx.LK2]):
            p.dma("sp", lambda e, i=i, nm_=nm_: e.dma_start(out=lm[:, i, :], in_=nm_.ap()), writes=["lm"])
        p.dma("sp", lambda e: e.dma_start(out=pf[:], in_=cx.PREVFLAG.ap()), writes=["pf"])
        p.dma("sp", lambda e: e.dma_start(out=HG[:], in_=bc(cx.ATTN_HG.ap()[0, :])), writes=["HG"])
        p.op("dve", lambda e: e.tensor_scalar(out=HG[:], in0=HG[:], scalar1=0.8, scalar2=None, op0=ALU.mult), reads=["HG"], writes=["HG"])
        p.op("dve", lambda e: e.tensor_tensor(out=r[:, 0:16], in0=nmax[:, 0:16], in1=nmax[:, 16:32], op=ALU.mult), reads=["nmax"], writes=["acr"])
        p.op("act", lambda e: e.activation(out=r[:, 0:16], in_=r[:, 0:16], func=AF.Sqrt), reads=["acr"], writes=["acr"])
        p.op("dve", lambda e: e.tensor_scalar(out=r[:, 0:16], in0=r[:, 0:16], scalar1=-(128.0 ** -0.5), scalar2=None, op0=ALU.mult), reads=["acr"], writes=["acr"])
        p.op("dve", lambda e: e.tensor_tensor(out=lm[:, 0, :], in0=lm[:, 0, :], in1=lm[:, 1, :], op=ALU.mult), reads=["lm"], writes=["lm"])
        p.op("dve", lambda e: e.tensor_tensor(out=lm[:, 2, :], in0=lm[:, 2, :], in1=lm[:, 3, :], op=ALU.mult), reads=["lm"], writes=["lm"])
        p.op("dve", lambda e: e.reduce_sum(out=r[:, 32:33], in_=lm[:, 0, :], axis=AX.X), reads=["lm"], writes=["acr"])
        p.op("dve", lambda e: e.reduce_sum(out=r[:, 33:34], in_=lm[:, 2, :], axis=AX.X), reads=["lm"], writes=["acr"])
        p.op("act", lambda e: e.activation(out=r[:, 32:34], in_=r[:, 32:34], func=AF.Exp), reads=["acr"], writes=["acr"])
        p.op("dve", lambda e: e.tensor_tensor(out=r[:, 16:17], in0=r[:, 33:34], in1=r[:, 32:33], op=ALU.subtract), reads=["acr"], writes=["acr"])
        p.op("dve", lambda e: e.tensor_scalar(out=r[:, 16:17], in0=r[:, 16:17], scalar1=-0.2, scalar2=None, op0=ALU.add), reads=["acr"], writes=["acr"])
        p.op("pe", lambda e: e.matmul(cx.bank[0][:, 0:18], lhsT=cf[0:1, C_ON:C_ON + 128], rhs=r[0:1, 0:18], start=True, stop=True), reads=["acr"], writes=["bank0"])
        p.op("dve", lambda e: e.tensor_copy(out=negc[:], in_=cx.bank[0][:, 0:16]), reads=["bank0"], writes=["negc"])
        p.op("dve", lambda e: e.tensor_copy(out=nlam[:], in_=cx.bank[0][:, 16:17]), reads=["bank0"], writes=["nlam"])
        p.op("dve", lambda e: e.tensor_scalar(out=negcp[:], in0=negc[:], scalar1=pf[:, 0:1], scalar2=None, op0=ALU.add), reads=["negc", "pf"], writes=["negcp"])
        p.emit()
    return negc, negcp, nlam, HG


def attn_core(p, cx, negc, negcp, nlam, HG):
    SCALE = 128.0 ** -0.5
    with ExitStack() as st:
        QT = [[p.sb(st, "QT%d_%d" % (b, m), [128, TOK], BF16) for m in range(2)] for b in range(2)]
        KT = [[p.sb(st, "KT%d_%d" % (b, m), [128, 2 * TOK], BF16) for m in range(2)] for b in range(2)]
        V = [p.sb(st, "V%d" % b, [128, 32, 257], BF16) for b in range(2)]
        PT = [p.sb(st, "PT%d" % m, [128, 36, 512], BF16) for m in range(2)]
        o0 = p.sb(st, "o0", [128, 4, 256], F32)
        oa = [p.sb(st, "oa%d" % i, [128, 4, 256], BF16) for i in range(2)]
        rd = [p.sb(st, "rd%d" % i, [128, 8], F32) for i in range(4)]
        junk = p.sb(st, "cjunk", [128, 256], BF16)
        vdv = cx.VD.ap().rearrange("(t k) h e -> h k t e", k=128)

        def loads(h):
            b = h % 2
            for m in range(2):
                p.dma("sp", lambda e, b=b, m=m, h=h: e.dma_start(out=QT[b][m][:], in_=cx.QTD.ap()[2 * h + m]), writes=["QT%d_%d" % (b, m)])
                p.dma("sp", lambda e, b=b, m=m, h=h: e.dma_start(out=KT[b][m][:], in_=cx.KTD.ap()[2 * h + m]), writes=["KT%d_%d" % (b, m)])
            p.dma("act", lambda e, b=b, h=h: e.dma_start(out=V[b][:], in_=vdv[h]), writes=["V%d" % b])

        stb = [0]
        accb = [0]
        oac = [0]
        rdc = [0]

        def stageA(h, g, m):
            b = h % 2
            hm = 2 * h + m
            steps = []
            nk = 16 + 4 * g + 4
            for j in range(nk):
                def step(j=j):
                    qoff = max(0, j - 16 - 4 * g)
                    bi = stb[0] % 3
                    stb[0] += 1
                    bk = cx.bank[bi]
                    p.op("pe", lambda e: e.matmul(bk[:, qoff * 128:512], lhsT=KT[b][m][:, j * 128:(j + 1) * 128], rhs=QT[b][m][:, g * 512 + qoff * 128:(g + 1) * 512], start=True, stop=True),
                         reads=["KT%d_%d" % (b, m), "QT%d_%d" % (b, m)], writes=["bank%d" % bi])
                    bias = negcp[:, hm:hm + 1] if j < 16 else negc[:, hm:hm + 1]
                    p.op("act", lambda e: e.activation(out=PT[m][:, j, qoff * 128:512], in_=bk[:, qoff * 128:512], func=AF.Exp, bias=bias, scale=SCALE),
                         reads=["bank%d" % bi], writes=["PT%d_%d" % (m, j)])
                    if j >= 16 + 4 * g:
                        ii = j - 16 - 4 * g
                        p.op("pool", lambda e: e.memset(PT[m][64:128, j, ii * 128:ii * 128 + 64], 0.0), reads=[], writes=["PT%d_%d" % (m, j)])
                steps.append(step)
            return steps

        def stageB(h, g, m):
            b = h % 2
            steps = []
            for ii in range(4):
                ai = 3 + accb[0] % 5
                accb[0] += 1
                acc = cx.bank[ai]
                an = "bank%d" % ai
                nkk = 16 + 4 * g + ii + 1
                for j in range(nkk):
                    def step(j=j, ii=ii, acc=acc, an=an, nkk=nkk):
                        p.op("pe", lambda e: e.matmul(acc[:, 0:257], lhsT=PT[m][:, j, ii * 128:(ii + 1) * 128], rhs=V[b][:, j, :], start=(j == 0), stop=(j == nkk - 1)),
                             reads=["PT%d_%d" % (m, j), "V%d" % b], writes=[an], inc=(j == nkk - 1))
                    steps.append(step)

                def fin(ii=ii, acc=acc, an=an):
                    r = rd[rdc[0] % 4]
                    rn = "rd%d" % (rdc[0] % 4)
                    rdc[0] += 1
                    p.op("dve", lambda e: e.reciprocal(out=r[:, 0:1], in_=acc[:, 256:257]), reads=[an], writes=[rn])
                    if m == 0:
                        p.op("act", lambda e: e.activation(out=o0[:, ii, :], in_=acc[:, 0:256], func=AF.Copy, scale=r[:, 0:1]), reads=[an, rn], writes=["o0_%d" % ii])
                    else:
                        o = oa[oac[0] % 2]
                        on = "oa%d" % (oac[0] % 2)
                        p.op("dve", lambda e: e.tensor_tensor(out=r[:, 1:2], in0=r[:, 0:1], in1=nlam[:, 0:1], op=ALU.mult), reads=[rn], writes=[rn])
                        p.op("dve", lambda e: e.scalar_tensor_tensor(out=o0[:, ii, :], in0=acc[:, 0:256], scalar=r[:, 1:2], in1=o0[:, ii, :], op0=ALU.mult, op1=ALU.add),
                             reads=[an, rn, "o0_%d" % ii], writes=["o0_%d" % ii])
                        p.op("act", lambda e: e.activation(out=junk[:], in_=o0[:, ii, :], func=AF.Square, accum_out=r[:, 2:3]), reads=["o0_%d" % ii], writes=["cjunk", rn])
                        p.op("dve", lambda e: e.tensor_scalar(out=r[:, 3:4], in0=r[:, 2:3], scalar1=1.0 / 256, scalar2=EPS, op0=ALU.mult, op1=ALU.add), reads=[rn], writes=[rn])
                        p.op("act", lambda e: e.activation(out=r[:, 4:5], in_=r[:, 3:4], func=AF.Sqrt), reads=[rn], writes=[rn])
                        p.op("dve", lambda e: e.reciprocal(out=r[:, 5:6], in_=r[:, 4:5]), reads=[rn], writes=[rn])
                        p.op("dve", lambda e: e.scalar_tensor_tensor(out=o[:, ii, :], in0=o0[:, ii, :], scalar=r[:, 5:6], in1=HG[:], op0=ALU.mult, op1=ALU.mult),
                             reads=["o0_%d" % ii, rn], writes=[on + "_%d" % ii])
                        if ii == 3:
                            oac[0] += 1
                            dst = cx.OAD.ap()[g * 512:(g + 1) * 512, h * 256:(h + 1) * 256].rearrange("(i q) c -> q i c", q=128)
                            p.dma("sp", lambda e: e.dma_start(out=dst, in_=o[:]), reads=[on + "_%d" % i_ for i_ in range(4)], writes=["oad_%d_%d" % (h, g)])
                steps.append(fin)
            return steps

        def interleave(A, B):
            na, nb = len(A), len(B)
            ia = ib = 0
            while ia < na or ib < nb:
                if ib >= nb or (ia < na and ia * max(nb, 1) <= ib * max(na, 1)):
                    A[ia]()
                    ia += 1
                else:
                    B[ib]()
                    ib += 1

        loads(0)
        pend = []
        for h in range(8):
            for g in range(4):
                for m in range(2):
                    A = stageA(h, g, m)
                    interleave(A, pend)
                    pend = stageB(h, g, m)
                    if g == 0 and m == 0 and h + 1 < 8:
                        loads(h + 1)
        interleave([], pend)
        p.emit()


def attn_layer(p, cx, XIN, XPREV, XOUT):
    with ExitStack() as st0:
        nmax = p.sb(st0, "nmax", [1, 32], F32)
        p.op("dve", lambda e: e.memset(nmax[:], 0.0), writes=["nmax"])
        with ExitStack() as st1:
            G, SH, GT = load_mod_tiles(p, cx, st1, 0, 0, cx.NORM_MIX_G)
            COS = p.sb(st1, "COS", [32, TOK], F32)
            SINS = p.sb(st1, "SINS", [32, TOK], F32)
            hT = p.sb(st1, "hT", [128, 16, TOK], BF16)
            rope_tables(p, cx, cx.POS_PREV.ap()[0, :], COS, SINS, "p")
            norm_to_hT(p, cx, XPREV.ap(), G, SH, hT)
            attn_proj(p, cx, hT, False, COS, SINS, nmax)
            rope_tables(p, cx, cx.POS_OWN.ap()[0, :], COS, SINS, "o")
            norm_to_hT(p, cx, XIN.ap(), G, SH, hT)
            attn_proj(p, cx, hT, True, COS, SINS, nmax)
        negc, negcp, nlam, HG = attn_consts(p, cx, st0, nmax)
        attn_core(p, cx, negc, negcp, nlam, HG)
    with ExitStack() as st2:
        GT = p.sb(st2, "GT", [128, D], F32)
        p.dma("sp", lambda e: e.dma_start(out=GT[:], in_=bc(cx.MODD.ap()[0, 2 * D:3 * D])), writes=["GT"])
        proj_out_stage(p, cx, cx.OAD.ap(), cx.ATTN_W_OUT.ap(), GT, XIN.ap(), XOUT.ap())


def mlstm_proj(p, cx, hT, full=True):
    with ExitStack() as st:
        wp = [p.sb(st, "wp%d" % i, [128, 16, 512], BF16) for i in range(2)]
        wgt = p.sb(st, "wgt", [128, 16, 16], BF16)
        qk = [p.sb(st, "qk%d" % i, [128, 512], BF16) for i in range(4)]
        vs = [p.sb(st, "vs%d" % i, [128, 2, 257], BF16) for i in range(4)]
        ob = [p.sb(st, "ob%d" % i, [128, 512], BF16) for i in range(4)]
        gsb = [p.sb(st, "gsb%d" % i, [128, 16], F32) for i in range(2)]
        for i in range(4):
            p.op("pool", lambda e, i=i: e.memset(vs[i][:], 1.0), writes=["vs%d" % i])
        wv = cx.ML_W_IN.ap().rearrange("(kc k) c -> k kc c", k=128)
        p.dma("pool", lambda e: e.dma_start(out=wgt[:], in_=wv[:, :, 3 * D:3 * D + 16]), writes=["wgt"])
        cnt = 0
        for j in range(12 if full else 8):
            w = wp[j % 2]
            wn = "wp%d" % (j % 2)
            p.dma("pool", lambda e, w=w, j=j: e.dma_start(out=w[:], in_=wv[:, :, j * 512:(j + 1) * 512]), writes=[wn])
            if j < 4:
                for cc in range(4):
                    gcc = j * 4 + cc
                    for tg in range(4):
                        bi = cnt % 4
                        bk = cx.bank[bi]
                        q_ = qk[cnt % 4]
                        qn = "qk%d" % (cnt % 4)
                        cnt += 1
                        for kc in range(16):
                            p.op("pe", lambda e, bk=bk, w=w, kc=kc, cc=cc, tg=tg: e.matmul(bk[:], lhsT=w[:, kc, cc * 128:(cc + 1) * 128], rhs=hT[:, kc, tg * 512:(tg + 1) * 512], start=(kc == 0), stop=(kc == 15)),
                                 reads=[wn], writes=["bank%d" % bi], inc=(kc == 15))
                        if cnt % 2 == 0:
                            p.op("act", lambda e, bk=bk, q_=q_: e.activation(out=q_[:], in_=bk[:], func=AF.Copy), reads=["bank%d" % bi], writes=[qn])
                        else:
                            p.op("dve", lambda e, bk=bk, q_=q_: e.tensor_copy(out=q_[:], in_=bk[:]), reads=["bank%d" % bi], writes=[qn])
                        dst = cx.QKP.ap()[gcc, :, 3 + tg * 512:3 + (tg + 1) * 512]
                        p.dma("sp", lambda e, dst=dst, q_=q_: e.dma_start(out=dst, in_=q_[:]), reads=[qn], writes=["qkd%d" % cnt])
            else:
                for t in range(NT):
                    bi = cnt % 4
                    bk = cx.bank[bi]
                    cnt += 1
                    for kc in range(16):
                        p.op("pe", lambda e, bk=bk, w=w, kc=kc, t=t: e.matmul(bk[:], lhsT=hT[:, kc, t * 128:(t + 1) * 128], rhs=w[:, kc, :], start=(kc == 0), stop=(kc == 15)),
                             reads=[wn], writes=["bank%d" % bi], inc=(kc == 15))
                    if j < 8:
                        vj = j - 4
                        v_ = vs[cnt % 4]
                        vn = "vs%d" % (cnt % 4)
                        if t % 2 == 0:
                            p.op("act", lambda e, bk=bk, v_=v_: e.activation(out=v_[:, :, 0:256], in_=bk[:].rearrange("p (a b) -> p a b", a=2), func=AF.Copy), reads=["bank%d" % bi], writes=[vn])
                        else:
                            p.op("dve", lambda e, bk=bk, v_=v_: e.tensor_copy(out=v_[:, :, 0:256], in_=bk[:].rearrange("p (a b) -> p a b", a=2)), reads=["bank%d" % bi], writes=[vn])
                        p.dma("act", lambda e, v_=v_, t=t, vj=vj: e.dma_start(out=cx.VM.ap()[t * 128:(t + 1) * 128, 2 * vj:2 * vj + 2, :], in_=v_[:]), reads=[vn], writes=["vd%d" % cnt])
                    else:
                        oj = j - 8
                        o_ = ob[cnt % 4]
                        on = "ob%d" % (cnt % 4)
                        if t % 2 == 0:
                            p.op("act", lambda e, bk=bk, o_=o_: e.activation(out=o_[:], in_=bk[:], func=AF.Copy), reads=["bank%d" % bi], writes=[on])
                        else:
                            p.op("dve", lambda e, bk=bk, o_=o_: e.tensor_copy(out=o_[:], in_=bk[:]), reads=["bank%d" % bi], writes=[on])
                        p.dma("act", lambda e, o_=o_, t=t, oj=oj: e.dma_start(out=cx.OPRE.ap()[t * 128:(t + 1) * 128, oj * 512:(oj + 1) * 512], in_=o_[:]), reads=[on], writes=["od%d" % cnt])
        for t in range(NT):
            bk = cx.bank[4 + t % 2]
            g_ = gsb[t % 2]
            for kc in range(16):
                p.op("pe", lambda e, bk=bk, kc=kc, t=t: e.matmul(bk[:, 0:16], lhsT=hT[:, kc, t * 128:(t + 1) * 128], rhs=wgt[:, kc, :], start=(kc == 0), stop=(kc == 15)),
                     reads=["wgt"], writes=["bank%d" % (4 + t % 2)], inc=(kc == 15))
            p.op("dve", lambda e, bk=bk, g_=g_: e.tensor_copy(out=g_[:], in_=bk[:, 0:16]), reads=["bank%d" % (4 + t % 2)], writes=["gsb%d" % (t % 2)])
            p.dma("sp", lambda e, g_=g_, t=t: e.dma_start(out=cx.GATES.ap()[t * 128:(t + 1) * 128, :], in_=g_[:]), reads=["gsb%d" % (t % 2)], writes=["gd%d" % t])
        p.emit()


def mlstm_rec(p, cx, full=True, st_in="dram", st_out="dram"):
    RS = 128.0 ** -0.5
    with ExitStack() as st:
        cf, cb = cx.cf, cx.cb
        C = p.sb(st, "Cst", [128, 8, 257], F32)
        Cb = p.sb(st, "Cstb", [128, 8, 257], BF16)
        CW = p.sb(st, "CW", [128, 16, 4], F32)
        CB = p.sb(st, "CB", [128, 16], F32)
        GB = p.sb(st, "GB", [128, 16], F32)
        HGm = p.sb(st, "HGm", [128, D], F32)
        hl = p.sb(st, "hl", [128, 16, 3], F32)
        hlb = p.sb(st, "hlb", [128, 16, 3], BF16)
        qkp = [p.sb(st, "qkp%d" % i, [128, 16, 131], BF16) for i in range(2)]
        vt = [p.sb(st, "vt%d" % i, [128, 8, 257], BF16) for i in range(2)]
        gt_ = [p.sb(st, "gtl%d" % i, [128, 16], F32) for i in range(2)]
        op_ = [p.sb(st, "opl%d" % i, [128, D], BF16) for i in range(2)]
        sig = p.sb(st, "sig", [128, D], F32)
        cacc = [p.sb(st, "cacc%d" % i, [128, 128], F32) for i in range(4)]
        qs = p.sb(st, "qs", [128, 16, 128], BF16)
        gs = p.sb(st, "gs", [128, 64], F32)
        PTm = [p.sb(st, "PTm%d" % i, [128, 128], BF16) for i in range(2)]
        Kp = [p.sb(st, "Kp%d" % i, [128, 128], BF16) for i in range(2)]
        tmpC = [p.sb(st, "tmpC%d" % i, [128, 257], F32) for i in range(2)]
        ho = [p.sb(st, "ho%d" % i, [128, 256], F32) for i in range(2)]
        hn = [p.sb(st, "hn%d" % i, [128, D], BF16) for i in range(2)]
        rr = [p.sb(st, "rr%d" % i, [128, 8], F32) for i in range(4)]
        junk = p.sb(st, "mjunk", [128, 256], BF16)
        if st_in == "dram":
            p.dma("sp", lambda e: e.dma_start(out=C[:], in_=cx.STATE_IN.ap()), writes=["Cst"])
        elif st_in == "zero":
            p.op("dve", lambda e: e.memset(C[:], 0.0), writes=["Cst"])
        else:
            of = p.sb(st, "oddf", [128, 1], F32)
            p.dma("sp", lambda e: e.dma_start(out=of[:], in_=cx.ODDFLAG.ap()), writes=["oddf"])
            p.dma("sp", lambda e: e.dma_start(out=C[:].rearrange("p h e -> p (h e)"), in_=cx.ST_ALL.ap()[0:128, 0:8 * 257]), writes=["Cst"])
            p.op("dve", lambda e: e.tensor_scalar(out=C[:], in0=C[:], scalar1=of[:, 0:1], scalar2=None, op0=ALU.mult), reads=["Cst", "oddf"], writes=["Cst"])
        p.op("act", lambda e: e.activation(out=Cb[:], in_=C[:], func=AF.Copy), reads=["Cst"], writes=["Cstb"])
        p.dma("sp", lambda e: e.dma_start(out=CW[:], in_=cx.CONV_W.ap()), writes=["CW"])
        p.dma("sp", lambda e: e.dma_start(out=CB[:], in_=cx.CONV_B.ap()), writes=["CB"])
        p.dma("sp", lambda e: e.dma_start(out=GB[:], in_=bc(cx.GATE_B.ap()[0, :])), writes=["GB"])
        p.dma("sp", lambda e: e.dma_start(out=HGm[:], in_=bc(cx.ML_HG.ap()[0, :])), writes=["HGm"])
        if st_in == "dram":
            p.dma("sp", lambda e: e.dma_start(out=hl[:], in_=cx.HALO_IN.ap()), writes=["hl"])
        elif st_in == "zero":
            p.op("dve", lambda e: e.memset(hl[:], 0.0), writes=["hl"])
        else:
            p.dma("sp", lambda e: e.dma_start(out=hl[:].rearrange("p c t -> p (c t)"), in_=cx.ST_ALL.ap()[0:128, 8 * 257:8 * 257 + 48]), writes=["hl"])
            p.op("dve", lambda e: e.tensor_scalar(out=hl[:], in0=hl[:], scalar1=of[:, 0:1], scalar2=None, op0=ALU.mult), reads=["hl", "oddf"], writes=["hl"])
        p.op("dve", lambda e: e.tensor_copy(out=hlb[:], in_=hl[:]), reads=["hl"], writes=["hlb"])
        qkv = cx.QKP.ap().rearrange("c k t -> k c t")
        p.dma("sp", lambda e: e.dma_start(out=qkv[:, :, 0:3], in_=hlb[:], allow_slow_non_contiguous=True), reads=["hlb"], writes=["halo"])
        for c in range(NT):
            b = c % 2
            q_, v_, g_, o_ = qkp[b], vt[b], gt_[b], op_[b]
            rdh = ["halo"] if c == 0 else []
            p.dma("sp", lambda e, q_=q_, c=c: e.dma_start(out=q_[:], in_=qkv[:, :, c * 128:c * 128 + 131]), reads=rdh, writes=["qkp%d" % b])
            p.dma("act", lambda e, v_=v_, c=c: e.dma_start(out=v_[:], in_=cx.VM.ap()[c * 128:(c + 1) * 128]), writes=["vt%d" % b])
            p.dma("sp", lambda e, g_=g_, c=c: e.dma_start(out=g_[:], in_=cx.GATES.ap()[c * 128:(c + 1) * 128, :]), writes=["gtl%d" % b])
            if full:
                p.dma("act", lambda e, o_=o_, c=c: e.dma_start(out=o_[:], in_=cx.OPRE.ap()[c * 128:(c + 1) * 128, :]), writes=["opl%d" % b])
                p.op("act", lambda e, o_=o_: e.activation(out=sig[:], in_=o_[:], func=AF.Sigmoid), reads=["opl%d" % b], writes=["sig"])
            p.op("dve", lambda e, g_=g_: e.tensor_tensor(out=gs[:, 0:16], in0=g_[:], in1=GB[:], op=ALU.add), reads=["gtl%d" % b, "GB"], writes=["gs"])
            p.op("act", lambda e: e.activation(out=gs[:, 16:24], in_=gs[:, 8:16], func=AF.Exp, scale=-1.0), reads=["gs"], writes=["gs"])
            p.op("dve", lambda e: e.tensor_scalar(out=gs[:, 16:24], in0=gs[:, 16:24], scalar1=1.0, scalar2=None, op0=ALU.add), reads=["gs"], writes=["gs"])
            p.op("act", lambda e: e.activation(out=gs[:, 16:24], in_=gs[:, 16:24], func=AF.Ln), reads=["gs"], writes=["gs"])
            gbk = cx.bank[0]
            p.op("pe", lambda e: e.matmul(gbk[:, 0:8], lhsT=cf[:, C_TRI:C_TRI + 128], rhs=gs[:, 16:24], start=True, stop=True), reads=["gs"], writes=["bank0"])
            p.op("pe", lambda e: e.matmul(gbk[:, 8:16], lhsT=cf[:, C_ON:C_ON + 128], rhs=gs[:, 16:24], start=False, stop=True, skip_group_check=True), reads=["gs"], writes=["bank0"])
            p.op("dve", lambda e: e.tensor_tensor(out=gs[:, 32:40], in0=gs[:, 0:8], in1=gbk[:, 0:8], op=ALU.add), reads=["gs", "bank0"], writes=["gs"])
            p.op("act", lambda e: e.activation(out=gs[:, 32:40], in_=gs[:, 32:40], func=AF.Exp), reads=["gs"], writes=["gs"])
            p.op("dve", lambda e: e.tensor_scalar(out=gs[:, 32:40], in0=gs[:, 32:40], scalar1=RS, scalar2=None, op0=ALU.mult), reads=["gs"], writes=["gs"])
            p.op("act", lambda e: e.activation(out=gs[:, 40:56], in_=gbk[:, 0:16], func=AF.Exp, scale=-1.0), reads=["bank0"], writes=["gs"])
            for cc in range(16):
                if not full and cc < 8:
                    continue
                a_ = cacc[cc % 4]
                an = "cacc%d" % (cc % 4)
                p.op("act", lambda e, a_=a_, q_=q_, cc=cc: e.activation(out=a_[:], in_=q_[:, cc, 0:128], func=AF.Identity, scale=CW[:, cc, 0:1], bias=CB[:, cc:cc + 1]),
                     reads=["qkp%d" % b, "CW", "CB"], writes=[an])
                for j in range(1, 4):
                    p.op("dve", lambda e, a_=a_, q_=q_, cc=cc, j=j: e.scalar_tensor_tensor(out=a_[:], in0=q_[:, cc, j:j + 128], scalar=CW[:, cc, j:j + 1], in1=a_[:], op0=ALU.mult, op1=ALU.add),
                         reads=["qkp%d" % b, an], writes=[an])
                p.op("act", lambda e, a_=a_, cc=cc: e.activation(out=qs[:, cc, :], in_=a_[:], func=AF.Silu), reads=[an], writes=["qs%d" % cc])
            for h in range(8):
                s4 = (h % 2) * 4
                bS, bT, bA, bU = cx.bank[s4], cx.bank[s4 + 1], cx.bank[s4 + 2], cx.bank[s4 + 3]
                nS, nT, nA, nU = ["bank%d" % (s4 + i) for i in range(4)]
                kT = qs[:, 8 + h, :]
                qT = qs[:, h, :]
                pt = PTm[h % 2]
                kp = Kp[h % 2]
                r = rr[h % 4]
                rn = "rr%d" % (h % 4)
                if full:
                    p.op("pe", lambda e, bS=bS, kT=kT, qT=qT: e.matmul(bS[:, 0:128], lhsT=kT, rhs=qT, start=True, stop=True), reads=["qs%d" % (8 + h), "qs%d" % h], writes=[nS])
                    p.op("dve", lambda e, bS=bS, pt=pt, h=h: e.scalar_tensor_tensor(out=pt[:], in0=bS[:, 0:128], scalar=gs[:, 32 + h:33 + h], in1=cf[:, C_CM:C_CM + 128], op0=ALU.mult, op1=ALU.mult),
                         reads=[nS, "gs"], writes=["PTm%d" % (h % 2)])
                bTv = bT[:].bitcast(BF16)
                p.op("pe", lambda e, bTv=bTv, kT=kT: e.transpose(out=bTv[:, 0:128], in_=kT, identity=cb[:, C_ID:C_ID + 128]), reads=["qs%d" % (8 + h)], writes=[nT])
                p.op("act", lambda e, bTv=bTv, kp=kp, h=h: e.activation(out=kp[:], in_=bTv[:, 0:128], func=AF.Copy, scale=gs[:, 32 + h:33 + h]), reads=[nT, "gs"], writes=["Kp%d" % (h % 2)])
                if full:
                    p.op("pe", lambda e, bA=bA, pt=pt, v_=v_, h=h: e.matmul(bA[:, 0:257], lhsT=pt[:], rhs=v_[:, h, :], start=True, stop=False), reads=["PTm%d" % (h % 2), "vt%d" % b], writes=[nA])
                    p.op("pe", lambda e, bA=bA, qT=qT, h=h: e.matmul(bA[:, 0:257], lhsT=qT, rhs=Cb[:, h, :], start=False, stop=True), reads=["qs%d" % h, "Cstb%d" % h], writes=[nA])
                p.op("pe", lambda e, bU=bU, kp=kp, v_=v_, h=h: e.matmul(bU[:, 0:257], lhsT=kp[:], rhs=v_[:, h, :], start=True, stop=True), reads=["Kp%d" % (h % 2), "vt%d" % b], writes=[nU])
                tc_ = tmpC[h % 2]
                p.op("dve", lambda e, bU=bU, tc_=tc_, h=h: e.tensor_tensor(out=tc_[:], in0=bU[:, 0:257], in1=C[:, h, :], op=ALU.add), reads=[nU, "Cst%d" % h, nA], writes=["tmpC%d" % (h % 2)])
                p.op("dve", lambda e, tc_=tc_, h=h: e.tensor_scalar(out=C[:, h, :], in0=tc_[:], scalar1=gs[:, 48 + h:49 + h], scalar2=None, op0=ALU.mult), reads=["tmpC%d" % (h % 2), "gs"], writes=["Cst%d" % h])
                p.op("act", lambda e, h=h: e.activation(out=Cb[:, h, :], in_=C[:, h, :], func=AF.Copy), reads=["Cst%d" % h], writes=["Cstb%d" % h])
                if full:
                    o_h = ho[h % 2]
                    on = "ho%d" % (h % 2)
                    hn_ = hn[b]
                    p.op("dve", lambda e, bA=bA, r=r, h=h: e.tensor_tensor(out=r[:, 0:1], in0=bA[:, 256:257], in1=gs[:, 40 + h:41 + h], op=ALU.mult), reads=[nA, "gs"], writes=[rn])
                    p.op("dve", lambda e, r=r: e.tensor_scalar(out=r[:, 2:3], in0=r[:, 0:1], scalar1=-1.0, scalar2=None, op0=ALU.mult), reads=[rn], writes=[rn])
                    p.op("dve", lambda e, r=r: e.tensor_scalar(out=r[:, 1:2], in0=r[:, 0:1], scalar1=r[:, 2:3], scalar2=1.0, op0=ALU.max, op1=ALU.max), reads=[rn], writes=[rn])
                    p.op("dve", lambda e, r=r: e.reciprocal(out=r[:, 2:3], in_=r[:, 1:2]), reads=[rn], writes=[rn])
                    p.op("dve", lambda e, r=r, h=h: e.tensor_tensor(out=r[:, 3:4], in0=r[:, 2:3], in1=gs[:, 40 + h:41 + h], op=ALU.mult), reads=[rn, "gs"], writes=[rn])
                    p.op("act", lambda e, bA=bA, o_h=o_h, r=r: e.activation(out=o_h[:], in_=bA[:, 0:256], func=AF.Copy, scale=r[:, 3:4]), reads=[nA, rn], writes=[on])
                    p.op("act", lambda e, o_h=o_h, r=r: e.activation(out=junk[:], in_=o_h[:], func=AF.Square, accum_out=r[:, 4:5]), reads=[on], writes=["mjunk", rn])
                    p.op("dve", lambda e, r=r: e.tensor_scalar(out=r[:, 5:6], in0=r[:, 4:5], scalar1=1.0 / 256, scalar2=EPS, op0=ALU.mult, op1=ALU.add), reads=[rn], writes=[rn])
                    p.op("act", lambda e, r=r: e.activation(out=r[:, 6:7], in_=r[:, 5:6], func=AF.Sqrt), reads=[rn], writes=[rn])
                    p.op("dve", lambda e, r=r: e.reciprocal(out=r[:, 7:8], in_=r[:, 6:7]), reads=[rn], writes=[rn])
                    p.op("dve", lambda e, o_h=o_h, r=r, h=h: e.scalar_tensor_tensor(out=o_h[:], in0=o_h[:], scalar=r[:, 7:8], in1=HGm[:, h * 256:(h + 1) * 256], op0=ALU.mult, op1=ALU.mult),
                         reads=[on, rn, "HGm"], writes=[on])
                    p.op("pool", lambda e, o_h=o_h, hn_=hn_, h=h: e.tensor_tensor(out=hn_[:, h * 256:(h + 1) * 256], in0=o_h[:], in1=sig[:, h * 256:(h + 1) * 256], op=ALU.mult),
                         reads=[on, "sig"], writes=["hn%d_%d" % (b, h)])
            if full:
                p.dma("sp", lambda e, b=b, c=c: e.dma_start(out=cx.HN.ap()[c * 128:(c + 1) * 128, :], in_=hn[b][:]), reads=["hn%d_%d" % (b, h) for h in range(8)], writes=["hnd%d" % c])
        if st_out is not None:
            so = cx.STATE_OUT.ap() if st_out == "dram" else cx.ST_LOC.ap()[:, 0:8 * 257].rearrange("p (h e) -> p h e", h=8)
            ho_ = cx.HALO_OUT.ap() if st_out == "dram" else cx.ST_LOC.ap()[:, 8 * 257:8 * 257 + 48].rearrange("p (c t) -> p c t", c=16)
            p.dma("sp", lambda e: e.dma_start(out=so, in_=C[:]), reads=["Cst%d" % h for h in range(8)], writes=["so"])
            p.dma("sp", lambda e: e.dma_start(out=hlb[:], in_=qkv[:, :, TOK:TOK + 3], allow_slow_non_contiguous=True), reads=["halo"], writes=["hlb"])
            p.op("dve", lambda e: e.tensor_copy(out=hl[:], in_=hlb[:]), reads=["hlb"], writes=["hl"])
            p.dma("sp", lambda e: e.dma_start(out=ho_, in_=hl[:]), reads=["hl"], writes=["ho_"])
        p.emit()


def mlstm_layer(p, cx, XIN, XOUT, full=True, fused=False):
    with ExitStack() as st1:
        G, SH, GT = load_mod_tiles(p, cx, st1, 1, 0, cx.NORM_MIX_G)
        hT = p.sb(st1, "hT", [128, 16, TOK], BF16)
        norm_to_hT(p, cx, XIN.ap(), G, SH, hT)
        mlstm_proj(p, cx, hT, full)
    if fused:
        mlstm_rec(p, cx, False, st_in="zero", st_out="loc")
        p.coll(lambda e: e.collective_compute("AllGather", ALU.bypass, replica_groups=REPLICA,
                                              ins=[cx.ST_LOC.ap()], outs=[cx.ST_ALL.ap()]), writes=["ST_ALL"])
        p.emit()
        mlstm_rec(p, cx, True, st_in="gather", st_out=None)
    else:
        mlstm_rec(p, cx, full)
    if not full:
        return
    with ExitStack() as st2:
        GT = p.sb(st2, "GT", [128, D], F32)
        p.dma("sp", lambda e: e.dma_start(out=GT[:], in_=bc(cx.MODD.ap()[1, 2 * D:3 * D])), writes=["GT"])
        proj_out_stage(p, cx, cx.HN.ap(), cx.ML_W_OUT.ap(), GT, XIN.ap(), XOUT.ap())


def _common(nc, cx, st):
    cx.nc = nc
    cx.CONSTS = nc.dram_tensor("CONSTS", [128, C_W], F32, kind="ExternalInput")
    cx.NORM_MIX_G = nc.dram_tensor("NORM_MIX_G", [2, D], F32, kind="ExternalInput")
    cx.NORM_FFN_G = nc.dram_tensor("NORM_FFN_G", [2, D], F32, kind="ExternalInput")
    cx.WR = nc.dram_tensor("WR", [2, 128, 16, 36], F32, kind="ExternalInput")
    cx.BRT = nc.dram_tensor("BRT", [2, 36], F32, kind="ExternalInput")
    cx.XS = nc.dram_tensor("XS", [NSLOT + TOK, D], BF16, kind="Internal")
    cx.YS = nc.dram_tensor("YS", [NSLOT + TOK, D], F32, kind="Internal")
    p = Prog(nc, st)
    cx.bank = [st.enter_context(nc.psum_tensor("bank%d" % i, [128, 512], F32)) for i in range(8)]
    load_consts(p, cx, st)
    return p


def build_A():
    nc = bass.Bass("TRN2", target_bir_lowering=False)
    cx = Ctx()
    with ExitStack() as st:
        p = _common(nc, cx, st)
        cx.WL = lambda L: 0
        cx.MODD = nc.dram_tensor("MODD", [2, 6 * D], F32, kind="ExternalOutput")
        cx.CVT = nc.dram_tensor("CVT", [128, 16], F32, kind="ExternalInput")
        cx.ADA_W = nc.dram_tensor("ADA_W", [2, D, 6 * D], F32, kind="ExternalInput")
        cx.ADA_B = nc.dram_tensor("ADA_B", [2, 6 * D], F32, kind="ExternalInput")
        cx.ATTN_W_IN = nc.dram_tensor("ATTN_W_IN", [D, 3 * D], F32, kind="ExternalInput")
        cx.ATTN_W_OUT = nc.dram_tensor("ATTN_W_OUT", [D, D], F32, kind="ExternalInput")
        cx.LQ1 = nc.dram_tensor("LQ1", [1, 128], F32, kind="ExternalInput")
        cx.LK1 = nc.dram_tensor("LK1", [1, 128], F32, kind="ExternalInput")
        cx.LQ2 = nc.dram_tensor("LQ2", [1, 128], F32, kind="ExternalInput")
        cx.LK2 = nc.dram_tensor("LK2", [1, 128], F32, kind="ExternalInput")
        cx.ATTN_HG = nc.dram_tensor("ATTN_HG", [1, 256], F32, kind="ExternalInput")
        cx.PREVFLAG = nc.dram_tensor("PREVFLAG", [128, 1], F32, kind="ExternalInput")
        cx.POS_OWN = nc.dram_tensor("POS_OWN", [1, TOK], I32, kind="ExternalInput")
        cx.POS_PREV = nc.dram_tensor("POS_PREV", [1, TOK], I32, kind="ExternalInput")
        cx.W_GU = nc.dram_tensor("W_GU", [1, NE, D, 2 * HID], F32, kind="ExternalInput")
        cx.W_DN = nc.dram_tensor("W_DN", [1, NE, HID, D], F32, kind="ExternalInput")
        XIN = nc.dram_tensor("XIN", [TOK, D], F32, kind="ExternalInput")
        XPREV = nc.dram_tensor("XPREV", [TOK, D], F32, kind="ExternalInput")
        X1 = nc.dram_tensor("X1", [TOK, D], F32, kind="ExternalOutput")
        XMID = nc.dram_tensor("XMID", [TOK, D], F32, kind="Internal")
        cx.QTD = nc.dram_tensor("QTD", [16, 128, TOK], BF16, kind="Internal")
        cx.KTD = nc.dram_tensor("KTD", [16, 128, 2 * TOK], BF16, kind="Internal")
        cx.VD = nc.dram_tensor("VD", [2 * TOK, 8, 257], BF16, kind="Internal")
        cx.OAD = nc.dram_tensor("OAD", [TOK, D], BF16, kind="Internal")
        mod_stage(p, cx, 0)
        mod_stage(p, cx, 1)
        attn_layer(p, cx, XIN, XPREV, XMID)
        moe_stage(p, cx, 0, XMID, X1)
    return nc


def build_B(full=True):
    nc = bass.Bass("TRN2", target_bir_lowering=False)
    cx = Ctx()
    with ExitStack() as st:
        p = _common(nc, cx, st)
        cx.WL = lambda L: 0
        cx.MODD = nc.dram_tensor("MODD", [2, 6 * D], F32, kind="ExternalInput")
        cx.ML_W_IN = nc.dram_tensor("ML_W_IN", [D, 3 * D + 16], F32, kind="ExternalInput")
        cx.CONV_W = nc.dram_tensor("CONV_W", [128, 16, 4], F32, kind="ExternalInput")
        cx.CONV_B = nc.dram_tensor("CONV_B", [128, 16], F32, kind="ExternalInput")
        cx.GATE_B = nc.dram_tensor("GATE_B", [1, 16], F32, kind="ExternalInput")
        cx.ML_HG = nc.dram_tensor("ML_HG", [1, D], F32, kind="ExternalInput")
        if full:
            cx.ML_W_OUT = nc.dram_tensor("ML_W_OUT", [D, D], F32, kind="ExternalInput")
            cx.FINAL_G = nc.dram_tensor("FINAL_G", [D], F32, kind="ExternalInput")
            cx.W_GU = nc.dram_tensor("W_GU", [1, NE, D, 2 * HID], F32, kind="ExternalInput")
            cx.W_DN = nc.dram_tensor("W_DN", [1, NE, HID, D], F32, kind="ExternalInput")
            OUT = nc.dram_tensor("OUT", [TOK, D], F32, kind="ExternalOutput")
        cx.STATE_IN = nc.dram_tensor("STATE_IN", [128, 8, 257], F32, kind="ExternalInput")
        cx.HALO_IN = nc.dram_tensor("HALO_IN", [128, 16, 3], F32, kind="ExternalInput")
        cx.STATE_OUT = nc.dram_tensor("STATE_OUT", [128, 8, 257], F32, kind="ExternalOutput")
        cx.HALO_OUT = nc.dram_tensor("HALO_OUT", [128, 16, 3], F32, kind="ExternalOutput")
        XIN = nc.dram_tensor("XIN", [TOK, D], F32, kind="ExternalInput")
        XMID = nc.dram_tensor("XMID", [TOK, D], F32, kind="Internal")
        cx.QKP = nc.dram_tensor("QKP", [16, 128, 3 + TOK], BF16, kind="Internal")
        cx.VM = nc.dram_tensor("VM", [TOK, 8, 257], BF16, kind="Internal")
        cx.OPRE = nc.dram_tensor("OPRE", [TOK, D], BF16, kind="Internal")
        cx.GATES = nc.dram_tensor("GATES", [TOK, 16], F32, kind="Internal")
        cx.HN = nc.dram_tensor("HN", [TOK, D], BF16, kind="Internal")
        mlstm_layer(p, cx, XIN, XMID, full)
        if full:
            moe_stage(p, cx, 1, XMID, OUT, final_g=cx.FINAL_G)
    return nc


def build_fused():
    nc = bass.Bass("TRN2", target_bir_lowering=False)
    cx = Ctx()
    with ExitStack() as st:
        p = _common(nc, cx, st)
        cx.WL = lambda L: L
        ei = lambda name, shape, dt=F32: nc.dram_tensor(name, list(shape), dt, kind="ExternalInput")
        it = lambda name, shape, dt=F32: nc.dram_tensor(name, list(shape), dt, kind="Internal")
        cx.MODD = it("MODD", [2, 6 * D])
        cx.CVT = ei("CVT", [128, 16])
        cx.ADA_W = ei("ADA_W", [2, D, 6 * D])
        cx.ADA_B = ei("ADA_B", [2, 6 * D])
        cx.ATTN_W_IN = ei("ATTN_W_IN", [D, 3 * D])
        cx.ATTN_W_OUT = ei("ATTN_W_OUT", [D, D])
        cx.LQ1 = ei("LQ1", [1, 128]); cx.LK1 = ei("LK1", [1, 128]); cx.LQ2 = ei("LQ2", [1, 128]); cx.LK2 = ei("LK2", [1, 128])
        cx.ATTN_HG = ei("ATTN_HG", [1, 256])
        cx.PREVFLAG = ei("PREVFLAG", [128, 1])
        cx.ODDFLAG = ei("ODDFLAG", [128, 1])
        cx.POS_OWN = ei("POS_OWN", [1, TOK], I32)
        cx.POS_PREV = ei("POS_PREV", [1, TOK], I32)
        cx.W_GU = ei("W_GU", [2, NE, D, 2 * HID])
        cx.W_DN = ei("W_DN", [2, NE, HID, D])
        cx.ML_W_IN = ei("ML_W_IN", [D, 3 * D + 16])
        cx.ML_W_OUT = ei("ML_W_OUT", [D, D])
        cx.CONV_W = ei("CONV_W", [128, 16, 4]); cx.CONV_B = ei("CONV_B", [128, 16]); cx.GATE_B = ei("GATE_B", [1, 16])
        cx.ML_HG = ei("ML_HG", [1, D]); cx.FINAL_G = ei("FINAL_G", [D])
        XIN = ei("XIN", [TOK, D]); XPREV = ei("XPREV", [TOK, D])
        OUT = nc.dram_tensor("OUT", [TOK, D], F32, kind="ExternalOutput")
        XMID0 = it("XMID0", [TOK, D]); X1 = it("X1", [TOK, D]); XMID1 = it("XMID1", [TOK, D])
        cx.QTD = it("QTD", [16, 128, TOK], BF16); cx.KTD = it("KTD", [16, 128, 2 * TOK], BF16)
        cx.VD = it("VD", [2 * TOK, 8, 257], BF16); cx.OAD = it("OAD", [TOK, D], BF16)
        cx.QKP = it("QKP", [16, 128, 3 + TOK], BF16); cx.VM = it("VM", [TOK, 8, 257], BF16)
        cx.OPRE = it("OPRE", [TOK, D], BF16); cx.GATES = it("GATES", [TOK, 16]); cx.HN = it("HN", [TOK, D], BF16)
        cx.ST_LOC = it("ST_LOC", [128, 8 * 257 + 48]); cx.ST_ALL = it("ST_ALL", [256, 8 * 257 + 48])
        S = STAGES or ("mod", "attn", "moe0", "ml", "moe1")
        if "mod" in S:
            mod_stage(p, cx, 0)
            mod_stage(p, cx, 1)
        if "attn" in S:
            attn_layer(p, cx, XIN, XPREV, XMID0)
        if "moe0" in S:
            moe_stage(p, cx, 0, XMID0, X1)
        if "ml" in S:
            mlstm_layer(p, cx, X1, XMID1, True, fused=True)
        if "moe1" in S:
            moe_stage(p, cx, 1, XMID1, OUT, final_g=cx.FINAL_G)
    return nc


def kernel(**inp):
    f32 = np.float32
    x = np.asarray(inp["x"], f32)
    c = np.asarray(inp["c"], f32)
    pos = np.asarray(inp["positions"], np.int32)
    wr = np.concatenate([inp["moe_w_group"], inp["moe_w_expert"]], axis=-1).reshape(2, 16, 128, 36).transpose(0, 2, 1, 3)
    wr = np.ascontiguousarray(wr, f32)
    brt = np.ascontiguousarray(np.concatenate([inp["moe_b_group"], inp["moe_b_expert"]], axis=-1), f32)
    cw = np.ascontiguousarray(np.asarray(inp["mlstm_conv_w"][0], f32).reshape(4, 16, 128).transpose(2, 1, 0))
    cbias = np.ascontiguousarray(np.asarray(inp["mlstm_conv_b"][0], f32).reshape(16, 128).T)
    n = 8
    common = {"CONSTS": make_consts(), "NORM_MIX_G": np.ascontiguousarray(inp["norm_mix_g"], f32), "NORM_FFN_G": np.ascontiguousarray(inp["norm_ffn_g"], f32),
              "WR": wr, "BRT": brt, "ADA_W": inp["ada_w"], "ADA_B": inp["ada_b"],
              "ATTN_W_IN": inp["attn_w_in"][0], "ATTN_W_OUT": inp["attn_w_out"][0],
              "LQ1": inp["attn_lambda_q1"], "LK1": inp["attn_lambda_k1"], "LQ2": inp["attn_lambda_q2"], "LK2": inp["attn_lambda_k2"],
              "ATTN_HG": inp["attn_head_norm_g"], "W_GU": inp["moe_w_gu"], "W_DN": inp["moe_w_down"],
              "ML_W_IN": inp["mlstm_w_in"][0], "ML_W_OUT": inp["mlstm_w_out"][0], "CONV_W": cw, "CONV_B": cbias,
              "GATE_B": inp["mlstm_gate_b"], "ML_HG": inp["mlstm_head_norm_g"], "FINAL_G": inp["final_norm_g"]}
    zx = np.zeros((TOK, D), f32)
    zp = np.zeros((1, TOK), np.int32)
    maps = []
    for core in range(n):
        b, hf = core // 2, core % 2
        sl = slice(hf * TOK, (hf + 1) * TOK)
        m = dict(common)
        m.update({
            "CVT": np.ascontiguousarray(c[b].reshape(16, 128).T),
            "PREVFLAG": np.full((128, 1), 0.0 if hf == 1 else -30000.0, f32),
            "ODDFLAG": np.full((128, 1), float(hf), f32),
            "POS_OWN": np.ascontiguousarray(pos[b, sl].reshape(1, TOK)),
            "POS_PREV": np.ascontiguousarray(pos[b, :TOK].reshape(1, TOK)) if hf == 1 else zp,
            "XIN": np.ascontiguousarray(x[b, sl]),
            "XPREV": np.ascontiguousarray(x[b, :TOK]) if hf == 1 else zx,
        })
        maps.append(m)
    nc = build_fused()
    if NCORES_DEBUG:
        res = run_bass_kernel_spmd(nc, maps[:NCORES_DEBUG], core_ids=list(range(NCORES_DEBUG))).results
        return res
    res = run_bass_kernel_spmd(nc, maps, core_ids=list(range(n))).results
    out = np.empty((4, 2 * TOK, D), f32)
    for core in range(n):
        b, hf = core // 2, core % 2
        out[b, hf * TOK:(hf + 1) * TOK] = res[core]["OUT"]
    return out
```

```python
import math
from contextlib import ExitStack
import numpy as np
import concourse.bass as bass
import concourse.mybir as mybir
from concourse.bass_utils import run_bass_kernel_spmd

F32 = mybir.dt.float32
BF16 = mybir.dt.bfloat16
I32 = mybir.dt.int32
AF = mybir.ActivationFunctionType
ALU = mybir.AluOpType
AX = mybir.AxisListType

D = 2048
TOK = 2048
NT = TOK // 128
NE = 32
CAP = 512
NBLK = CAP // 128
NSLOT = NE * CAP
HID = 1024
EPS = 1e-6

ENGS = ("pe", "act", "dve", "pool", "sp")
SAME_ENG_SYNC = True
N_DMA_SEMS = 40
REPLICA = [[0, 1], [2, 3], [4, 5], [6, 7]]
NCORES_DEBUG = 0
STAGES = None


class Prog:
    def __init__(self, nc, stack):
        self.nc = nc
        self.stack = stack
        self.cnt = {e: 0 for e in ENGS}
        self.esem = {e: stack.enter_context(nc.semaphore("s_" + e)) for e in ENGS}
        self.dsem = [stack.enter_context(nc.semaphore("d%d" % i)) for i in range(N_DMA_SEMS)]
        self.dval = [0] * N_DMA_SEMS
        self.drr = 0
        self.csem = stack.enter_context(nc.semaphore("csem"))
        self.cval = 0
        self.known = {e: {} for e in ENGS}
        self._reset()
        self.uid = 0

    def _reset(self):
        self.ops = {e: [] for e in ENGS}
        self.last_w = {}
        self.readers = {}

    def sb(self, st, name, shape, dt):
        self.uid += 1
        return st.enter_context(self.nc.sbuf_tensor("%s_%d" % (name, self.uid), list(shape), dt))

    def _deps(self, eng, reads, writes):
        deps = []
        for r in reads:
            t = self.last_w.get(r)
            if t is not None:
                deps.append(t)
        for w in writes:
            t = self.last_w.get(w)
            if t is not None:
                deps.append(t)
            deps.extend(self.readers.get(w, ()))
        waits = []
        kn = self.known[eng]
        for (sem, val, deng, sid) in deps:
            if deng == eng and (eng == "pe" or not SAME_ENG_SYNC):
                continue
            if kn.get(sid, 0) >= val:
                continue
            kn[sid] = val
            waits.append((sem, val))
        return waits

    def _commit(self, tok, reads, writes):
        for w in writes:
            self.last_w[w] = tok
            self.readers[w] = []
        for r in reads:
            if r in writes:
                continue
            lst = self.readers.setdefault(r, [])
            lst.append(tok)
            if len(lst) > 48:
                latest = {}
                keep = []
                for t in lst:
                    if t[2] == "dma":
                        keep.append(t)
                    else:
                        latest[t[2]] = t
                self.readers[r] = keep[-40:] + list(latest.values())

    def op(self, eng, fn, reads=(), writes=(), inc=True):
        waits = self._deps(eng, reads, writes)
        if inc:
            self.cnt[eng] += 1
            tok = (self.esem[eng], self.cnt[eng], eng, "e_" + eng)
            self.ops[eng].append((waits, fn, (self.esem[eng], 1)))
        else:
            tok = (self.esem[eng], self.cnt[eng] + 1, eng, "e_" + eng)
            self.ops[eng].append((waits, fn, None))
        self._commit(tok, reads, writes)

    def coll(self, fn, reads=(), writes=()):
        waits = self._deps("pool", reads, writes)
        self.cval += 1
        tok = (self.csem, self.cval, "dma", "csem")
        self.ops["pool"].append((waits, fn, (self.csem, 1)))
        self._commit(tok, reads, writes)

    def dma(self, q, fn, reads=(), writes=()):
        s = self.drr
        self.drr = (self.drr + 1) % N_DMA_SEMS
        waits = self._deps(q, reads, writes)
        prev = self.dval[s]
        sid = "d%d" % s
        if prev > 0 and self.known[q].get(sid, 0) < prev:
            self.known[q][sid] = prev
            waits.append((self.dsem[s], prev))
        self.dval[s] += 16
        tok = (self.dsem[s], self.dval[s], "dma", sid)
        self.ops[q].append((waits, fn, (self.dsem[s], 16)))
        self._commit(tok, reads, writes)

    def barrier(self):
        for e in ENGS:
            waits = []
            for e2 in ENGS:
                if e2 != e and self.cnt[e2] > 0 and self.known[e].get("e_" + e2, 0) < self.cnt[e2]:
                    self.known[e]["e_" + e2] = self.cnt[e2]
                    waits.append((self.esem[e2], self.cnt[e2]))
            for i in range(N_DMA_SEMS):
                sid = "d%d" % i
                if self.dval[i] > 0 and self.known[e].get(sid, 0) < self.dval[i]:
                    self.known[e][sid] = self.dval[i]
                    waits.append((self.dsem[i], self.dval[i]))
            if self.cval > 0 and self.known[e].get("csem", 0) < self.cval:
                self.known[e]["csem"] = self.cval
                waits.append((self.csem, self.cval))
            if waits:
                self.ops[e].append((waits, None, None))

    def emit(self):
        self.barrier()
        ops = self.ops
        with self.nc.Block() as block:
            def run(engname):
                def body(e):
                    for (waits, fn, inc) in ops[engname]:
                        for (sem, val) in waits:
                            e.wait_ge(sem, val)
                        if fn is not None:
                            ins = fn(e)
                            if inc is not None:
                                ins.then_inc(inc[0], inc[1])
                return body
            block.tensor(run("pe"))
            block.scalar(run("act"))
            block.vector(run("dve"))
            block.gpsimd(run("pool"))
            block.sync(run("sp"))
        self._reset()


C_ID, C_LS, C_ON, C_ECAP, C_TRI, C_CM, C_PERM, C_INVF, C_SGN, C_IOTA, C_W = 0, 128, 256, 384, 416, 544, 672, 704, 705, 706, 738


def make_consts():
    c = np.zeros((128, C_W), np.float32)
    i = np.arange(128)
    c[:, C_ID:C_ID + 128] = np.eye(128)
    c[:, C_LS:C_LS + 128] = (i[:, None] < i[None, :])
    c[:, C_ON:C_ON + 128] = 1.0
    c[:, C_ECAP:C_ECAP + 32] = (np.arange(32) * CAP)[None, :]
    c[:, C_TRI:C_TRI + 128] = (i[:, None] <= i[None, :])
    c[:, C_CM:C_CM + 128] = (i[:, None] <= i[None, :])
    for pp in range(32):
        c[(pp + 16) % 32, C_PERM + pp] = 1.0
    half = 16
    invf = 500000.0 ** (-np.arange(half, dtype=np.float32) * 2.0 / 32)
    c[:32, C_INVF] = np.tile(invf, 2)
    c[:16, C_SGN] = -1.0
    c[16:32, C_SGN] = 1.0
    c[:, C_IOTA:C_IOTA + 32] = np.arange(32)[None, :]
    return c


class Ctx:
    pass


def bc(ap1d, n=128):
    return ap1d.partition_broadcast(n)


def load_consts(p, cx, st):
    cx.cf = p.sb(st, "cf", [128, C_W], F32)
    cx.cb = p.sb(st, "cb", [128, C_W], BF16)
    p.dma("sp", lambda e: e.dma_start(out=cx.cf[:], in_=cx.CONSTS.ap()), writes=["cf"])
    p.op("dve", lambda e: e.tensor_copy(out=cx.cb[:], in_=cx.cf[:]), reads=["cf"], writes=["cb"])
    p.emit()


def rms_mod_tile(p, cx, xs, G, SH, hf, hb, ss, nm, rd=(), wr=()):
    junk = cx.junk
    p.op("act", lambda e: e.activation(out=junk[:], in_=xs[:], func=AF.Square, accum_out=ss[:, 0:1]),
         reads=[nm + "xs"], writes=["junk", nm + "ss"])
    p.op("dve", lambda e: e.tensor_scalar(out=ss[:, 1:2], in0=ss[:, 0:1], scalar1=1.0 / D, scalar2=EPS, op0=ALU.mult, op1=ALU.add),
         reads=[nm + "ss"], writes=[nm + "ss"])
    p.op("act", lambda e: e.activation(out=ss[:, 2:3], in_=ss[:, 1:2], func=AF.Sqrt), reads=[nm + "ss"], writes=[nm + "ss"])
    p.op("dve", lambda e: e.reciprocal(out=ss[:, 3:4], in_=ss[:, 2:3]), reads=[nm + "ss"], writes=[nm + "ss"])
    tmp = cx.tmpf
    p.op("dve", lambda e: e.scalar_tensor_tensor(out=tmp[:], in0=xs[:], scalar=ss[:, 3:4], in1=G[:], op0=ALU.mult, op1=ALU.mult),
         reads=[nm + "xs", nm + "ss"] + list(rd), writes=["tmpf"])
    if hf is not None:
        p.op("pool", lambda e: e.tensor_tensor(out=hf[:], in0=tmp[:], in1=SH[:], op=ALU.add), reads=["tmpf"] + list(rd), writes=[nm + "hf"])
        p.op("act", lambda e: e.activation(out=hb[:], in_=hf[:], func=AF.Copy), reads=[nm + "hf"], writes=[nm + "hb"])
    else:
        p.op("pool", lambda e: e.tensor_tensor(out=hb[:], in0=tmp[:], in1=SH[:], op=ALU.add), reads=["tmpf"] + list(rd), writes=[nm + "hb"])


def load_mod_tiles(p, cx, st, L, which, gname):
    base = 3 * D * which
    G = p.sb(st, "G", [128, D], F32)
    SH = p.sb(st, "SH", [128, D], F32)
    GT = p.sb(st, "GT", [128, D], F32)
    gn = p.sb(st, "gn", [128, D], F32)
    md = cx.MODD.ap()
    p.dma("sp", lambda e: e.dma_start(out=SH[:], in_=bc(md[L, base:base + D])), reads=["MODD"], writes=["SH"])
    p.dma("act", lambda e: e.dma_start(out=G[:], in_=bc(md[L, base + D:base + 2 * D])), reads=["MODD"], writes=["G"])
    p.dma("sp", lambda e: e.dma_start(out=GT[:], in_=bc(md[L, base + 2 * D:base + 3 * D])), reads=["MODD"], writes=["GT"])
    p.dma("act", lambda e: e.dma_start(out=gn[:], in_=bc(gname.ap()[L, :])), writes=["gn"])
    p.op("dve", lambda e: e.scalar_tensor_tensor(out=G[:], in0=G[:], scalar=1.0, in1=gn[:], op0=ALU.add, op1=ALU.mult),
         reads=["G", "gn"], writes=["G"])
    return G, SH, GT


def mod_stage(p, cx, L):
    with ExitStack() as st:
        cT = p.sb(st, "cT", [128, 16], F32)
        cTb = p.sb(st, "cTb", [128, 16], BF16)
        ring = [p.sb(st, "aw%d" % i, [128, 4096], BF16) for i in range(4)]
        row = p.sb(st, "mrow", [1, 4096], F32)
        brow = p.sb(st, "brow", [1, 4096], F32)
        p.dma("sp", lambda e: e.dma_start(out=cT[:], in_=cx.CVT.ap()), writes=["cT"])
        p.op("act", lambda e: e.activation(out=cTb[:], in_=cT[:], func=AF.Silu), reads=["cT"], writes=["cTb"])
        aw = cx.ADA_W.ap()
        k = 0
        for ps_ in range(3):
            p.dma("sp", lambda e, ps_=ps_: e.dma_start(out=brow[:], in_=cx.ADA_B.ap()[L:L + 1, ps_ * 4096:(ps_ + 1) * 4096]), writes=["brow"])
            for kc in range(16):
                buf = ring[k % 4]
                bn = "aw%d" % (k % 4)
                k += 1
                p.dma("pool", lambda e, buf=buf, kc=kc, ps_=ps_: e.dma_start(out=buf[:], in_=aw[L, kc * 128:(kc + 1) * 128, ps_ * 4096:(ps_ + 1) * 4096]),
                      writes=[bn])
                for n in range(8):
                    p.op("pe", lambda e, buf=buf, kc=kc, n=n: e.matmul(cx.bank[n][0:1, :], lhsT=cTb[:, kc:kc + 1], rhs=buf[:, n * 512:(n + 1) * 512],
                                                                      start=(kc == 0), stop=(kc == 15)),
                         reads=[bn, "cTb"], writes=["bank%d" % n], inc=(n == 7))
            for n in range(8):
                p.op("dve", lambda e, n=n: e.tensor_tensor(out=row[:, n * 512:(n + 1) * 512], in0=cx.bank[n][0:1, :], in1=brow[:, n * 512:(n + 1) * 512], op=ALU.add),
                     reads=["bank%d" % n, "brow"], writes=["mrow"])
            p.dma("sp", lambda e, ps_=ps_: e.dma_start(out=cx.MODD.ap()[L:L + 1, ps_ * 4096:(ps_ + 1) * 4096], in_=row[:]), reads=["mrow"], writes=["MODD"])
        p.emit()


def moe_stage(p, cx, L, XIN, XOUT, final_g=None):
    with ExitStack() as st0:
        d1i = p.sb(st0, "d1i", [128, NT], I32)
        d2i = p.sb(st0, "d2i", [128, NT], I32)
        gts = p.sb(st0, "gts", [128, 2 * NT], F32)
        xin = XIN.ap()
        with ExitStack() as st:
            G, SH, GT_ = load_mod_tiles(p, cx, st, L, 1, cx.NORM_FFN_G)
            cf, cb = cx.cf, cx.cb
            xs2 = [p.sb(st, "xs%d" % i, [128, D], F32) for i in range(2)]
            hf2 = [p.sb(st, "hf%d" % i, [128, D], F32) for i in range(2)]
            hb2 = [p.sb(st, "hb%d" % i, [128, D], BF16) for i in range(2)]
            cx.junk = p.sb(st, "junk", [128, D], BF16)
            cx.tmpf = p.sb(st, "tmpf", [128, D], F32)
            hT = p.sb(st, "hTf", [128, 16, 128], F32)
            wr = p.sb(st, "wr", [128, 16, 36], F32)
            br = p.sb(st, "br", [128, 36], F32)
            macc = p.sb(st, "macc", [128, 32], BF16)
            sm = [p.sb(st, "sm%d" % i, [128, 160], F32) for i in range(2)]
            mk = [p.sb(st, "mk%d" % i, [128, 32], BF16) for i in range(2)]
            p.dma("sp", lambda e: e.dma_start(out=wr[:], in_=cx.WR.ap()[L]), writes=["wr"])
            p.dma("sp", lambda e: e.dma_start(out=br[:], in_=bc(cx.BRT.ap()[L, :])), writes=["br"])
            p.op("dve", lambda e: e.memset(macc[:], 0.0), writes=["macc"])
            for t in range(NT):
                b = t % 2
                nm = "r%d" % b
                xs, hf, hb, s, m = xs2[b], hf2[b], hb2[b], sm[b], mk[b]
                p.dma("sp", lambda e, xs=xs, t=t: e.dma_start(out=xs[:], in_=xin[t * 128:(t + 1) * 128, :]), reads=["XIN"], writes=[nm + "xs"])
                rms_mod_tile(p, cx, xs, G, SH, hf, hb, s, nm, rd=["G", "SH"])
                for q4 in range(4):
                    for j in range(4):
                        kc = q4 * 4 + j
                        p.op("pe", lambda e, hf=hf, kc=kc, j=j, q4=q4: e.transpose(out=cx.bank[q4][:, j * 128:(j + 1) * 128], in_=hf[:, kc * 128:(kc + 1) * 128], identity=cf[:, C_ID:C_ID + 128]),
                             reads=[nm + "hf"], writes=["bank%d" % q4])
                    eng = "act" if q4 % 2 == 0 else "dve"
                    if eng == "act":
                        p.op("act", lambda e, q4=q4: e.activation(out=hT[:, q4 * 4:(q4 + 1) * 4, :], in_=cx.bank[q4][:].rearrange("p (a b) -> p a b", a=4), func=AF.Copy),
                             reads=["bank%d" % q4], writes=["hT%d" % q4])
                    else:
                        p.op("dve", lambda e, q4=q4: e.tensor_copy(out=hT[:, q4 * 4:(q4 + 1) * 4, :], in_=cx.bank[q4][:].rearrange("p (a b) -> p a b", a=4)),
                             reads=["bank%d" % q4], writes=["hT%d" % q4])
                lgp = cx.bank[4]
                for kc in range(16):
                    p.op("pe", lambda e, kc=kc: e.matmul(lgp[:, 0:36], lhsT=hT[:, kc, :], rhs=wr[:, kc, :], start=(kc == 0), stop=(kc == 15)),
                         reads=["hT%d" % (kc // 4), "wr"], writes=["bank4"], inc=(kc == 15))
                lg = s[:, 8:44]
                sn = nm + "s"
                p.op("dve", lambda e, lg=lg: e.tensor_tensor(out=lg, in0=lgp[:, 0:36], in1=br[:], op=ALU.add), reads=["bank4", "br"], writes=[sn])
                p.op("dve", lambda e, s=s: e.reduce_max(out=s[:, 44:45], in_=s[:, 8:12], axis=AX.X), reads=[sn], writes=[sn])
                p.op("dve", lambda e, s=s: e.tensor_scalar(out=s[:, 45:46], in0=s[:, 44:45], scalar1=-1.0, scalar2=None, op0=ALU.mult), reads=[sn], writes=[sn])
                p.op("dve", lambda e, s=s: e.tensor_scalar(out=s[:, 48:52], in0=s[:, 8:12], scalar1=s[:, 44:45], scalar2=None, op0=ALU.is_equal), reads=[sn], writes=[sn])
                p.op("act", lambda e, s=s: e.activation(out=s[:, 100:104], in_=s[:, 8:12], func=AF.Exp, bias=s[:, 45:46], accum_out=s[:, 46:47]), reads=[sn], writes=[sn])
                p.op("dve", lambda e, s=s: e.reciprocal(out=s[:, 47:48], in_=s[:, 46:47]), reads=[sn], writes=[sn])
                p.op("dve", lambda e, s=s: e.tensor_scalar(out=s[:, 52:56], in0=s[:, 48:52], scalar1=1e30, scalar2=-1e30, op0=ALU.mult, op1=ALU.add), reads=[sn], writes=[sn])
                p.op("dve", lambda e, s=s: e.tensor_tensor(out=s[:, 56:88].rearrange("p (g k) -> p g k", g=4), in0=s[:, 12:44].rearrange("p (g k) -> p g k", g=4),
                                                           in1=s[:, 52:56].unsqueeze(2).to_broadcast([128, 4, 8]), op=ALU.add), reads=[sn], writes=[sn])
                p.op("dve", lambda e, s=s: e.max(out=s[:, 88:96], in_=s[:, 56:88]), reads=[sn], writes=[sn])
                p.op("dve", lambda e, s=s: e.tensor_tensor(out=s[:, 96:97], in0=s[:, 89:90], in1=s[:, 88:89], op=ALU.subtract), reads=[sn], writes=[sn])
                p.op("act", lambda e, s=s: e.activation(out=s[:, 97:98], in_=s[:, 96:97], func=AF.Exp), reads=[sn], writes=[sn])
                p.op("dve", lambda e, s=s: e.tensor_scalar(out=s[:, 98:99], in0=s[:, 97:98], scalar1=1.0, scalar2=None, op0=ALU.add), reads=[sn], writes=[sn])
                p.op("dve", lambda e, s=s: e.reciprocal(out=s[:, 99:100], in_=s[:, 98:99]), reads=[sn], writes=[sn])
                p.op("dve", lambda e, s=s, t=t: e.tensor_tensor(out=gts[:, 2 * t:2 * t + 1], in0=s[:, 99:100], in1=s[:, 47:48], op=ALU.mult), reads=[sn], writes=["gts"])
                p.op("dve", lambda e, s=s, t=t: e.tensor_tensor(out=gts[:, 2 * t + 1:2 * t + 2], in0=gts[:, 2 * t:2 * t + 1], in1=s[:, 97:98], op=ALU.mult), reads=[sn, "gts"], writes=["gts"])
                p.op("dve", lambda e, s=s: e.tensor_scalar(out=s[:, 100:132], in0=s[:, 56:88], scalar1=s[:, 88:89], scalar2=None, op0=ALU.is_equal), reads=[sn], writes=[sn])
                p.op("dve", lambda e, s=s: e.tensor_scalar(out=s[:, 8:40], in0=s[:, 56:88], scalar1=s[:, 89:90], scalar2=None, op0=ALU.is_equal), reads=[sn], writes=[sn])
                p.op("dve", lambda e, s=s, m=m: e.tensor_tensor(out=m[:], in0=s[:, 100:132], in1=s[:, 8:40], op=ALU.add), reads=[sn], writes=[nm + "mk"])
                pp = cx.bank[5]
                p.op("pe", lambda e, m=m: e.matmul(pp[:, 0:32], lhsT=cb[:, C_LS:C_LS + 128], rhs=m[:], start=True, stop=False), reads=[nm + "mk"], writes=["bank5"])
                p.op("pe", lambda e: e.matmul(pp[:, 0:32], lhsT=cb[:, C_ON:C_ON + 128], rhs=macc[:], start=False, stop=True), reads=["macc"], writes=["bank5"])
                p.op("dve", lambda e, s=s: e.tensor_tensor(out=s[:, 56:88], in0=pp[:, 0:32], in1=cf[:, C_ECAP:C_ECAP + 32], op=ALU.add), reads=["bank5", sn], writes=[sn])
                p.op("dve", lambda e, m=m: e.tensor_tensor(out=macc[:], in0=macc[:], in1=m[:], op=ALU.add), reads=["macc", nm + "mk", "bank5"], writes=["macc"])
                p.op("dve", lambda e, s=s: e.tensor_tensor(out=s[:, 100:132], in0=s[:, 100:132], in1=s[:, 56:88], op=ALU.mult), reads=[sn], writes=[sn])
                p.op("dve", lambda e, s=s: e.reduce_sum(out=s[:, 132:133], in_=s[:, 100:132], axis=AX.X), reads=[sn], writes=[sn])
                p.op("dve", lambda e, s=s: e.tensor_tensor(out=s[:, 8:40], in0=s[:, 8:40], in1=s[:, 56:88], op=ALU.mult), reads=[sn], writes=[sn])
                p.op("dve", lambda e, s=s: e.reduce_sum(out=s[:, 133:134], in_=s[:, 8:40], axis=AX.X), reads=[sn], writes=[sn])
                p.op("dve", lambda e, s=s, t=t: e.tensor_copy(out=d1i[:, t:t + 1], in_=s[:, 132:133]), reads=[sn], writes=["d1i"])
                p.op("dve", lambda e, s=s, t=t: e.tensor_copy(out=d2i[:, t:t + 1], in_=s[:, 133:134]), reads=[sn], writes=["d2i"])
                p.dma("pool", lambda e, hb=hb, t=t: e.indirect_dma_start(out=cx.XS.ap(), out_offset=bass.IndirectOffsetOnAxis(ap=d1i[:, t:t + 1], axis=0), in_=hb[:], in_offset=None),
                      reads=[nm + "hb", "d1i"], writes=["XSa%d" % t])
                p.dma("pool", lambda e, hb=hb, t=t: e.indirect_dma_start(out=cx.XS.ap(), out_offset=bass.IndirectOffsetOnAxis(ap=d2i[:, t:t + 1], axis=0), in_=hb[:], in_offset=None),
                      reads=[nm + "hb", "d2i"], writes=["XSb%d" % t])
            p.emit()
        with ExitStack() as st:
            cb = cx.cb
            NWG = 32
            NWD = 12
            wg = [p.sb(st, "wg%d" % i, [128, 2, 512], BF16) for i in range(NWG)]
            wd = [p.sb(st, "wd%d" % i, [128, D], BF16) for i in range(NWD)]
            xg = p.sb(st, "xg", [128, NBLK, D], BF16)
            xT = [p.sb(st, "xT%d" % i, [128, 16, CAP], BF16) for i in range(2)]
            hTb = p.sb(st, "hTb", [128, 8, CAP], BF16)
            sg = [p.sb(st, "sg%d" % i, [128, 256], F32) for i in range(2)]
            yb = [p.sb(st, "yb%d" % i, [128, D], F32) for i in range(2)]
            xsv = cx.XS.ap()[0:NSLOT].rearrange("(e b p) d -> e p b d", b=NBLK, p=128)
            ysv = cx.YS.ap()[0:NSLOT].rearrange("(e b p) d -> e b p d", b=NBLK, p=128)
            wguv = cx.W_GU.ap()
            wdv = cx.W_DN.ap()
            kg = 0
            kd = 0
            ky = 0
            for ex in range(NE):
                xb_ = ex % 2
                xTb = xT[xb_]
                p.dma("sp", lambda e, ex=ex: e.dma_start(out=xg[:], in_=xsv[ex]), writes=["xg"])
                tcnt = 0
                for blk in range(NBLK):
                    for hh in range(2):
                        bk = cx.bank[4 + tcnt % 4]
                        bkn = "bank%d" % (4 + tcnt % 4)
                        tcnt += 1
                        bkv = bk[:].bitcast(BF16)
                        for j in range(8):
                            kc = hh * 8 + j
                            p.op("pe", lambda e, bkv=bkv, j=j, kc=kc, blk=blk: e.transpose(out=bkv[:, j * 128:(j + 1) * 128], in_=xg[:, blk, kc * 128:(kc + 1) * 128], identity=cb[:, C_ID:C_ID + 128]),
                                 reads=["xg"], writes=[bkn], inc=(j == 7))
                        if tcnt % 2 == 0:
                            p.op("act", lambda e, bkv=bkv, hh=hh, blk=blk, xTb=xTb: e.activation(out=xTb[:, hh * 8:(hh + 1) * 8, blk * 128:(blk + 1) * 128], in_=bkv.rearrange("p (a b) -> p a b", a=8), func=AF.Copy),
                                 reads=[bkn], writes=["xT%d" % xb_])
                        else:
                            p.op("dve", lambda e, bkv=bkv, hh=hh, blk=blk, xTb=xTb: e.tensor_copy(out=xTb[:, hh * 8:(hh + 1) * 8, blk * 128:(blk + 1) * 128], in_=bkv.rearrange("p (a b) -> p a b", a=8)),
                                 reads=[bkn], writes=["xT%d" % xb_])
                for hp in range(2):
                    wgl = []
                    for kc in range(16):
                        i = kg % NWG
                        kg += 1
                        wgl.append(i)
                        p.dma("pool", lambda e, i=i, ex=ex, kc=kc, hp=hp: e.dma_start(out=wg[i][:], in_=wguv[cx.WL(L), ex, kc * 128:(kc + 1) * 128, :].rearrange("k (g c) -> k g c", g=2)[:, :, hp * 512:(hp + 1) * 512]),
                              writes=["wg%d" % i])
                    for sb_ in range(CAP // 256):
                        for kc in range(16):
                            i = wgl[kc]
                            for gi in range(2):
                                for j in range(4):
                                    bi = gi * 2 + j // 2
                                    p.op("pe", lambda e, i=i, gi=gi, j=j, bi=bi, kc=kc, xTb=xTb, sb_=sb_: e.matmul(cx.bank[bi][:, (j % 2) * 256:(j % 2 + 1) * 256], lhsT=wg[i][:, gi, j * 128:(j + 1) * 128], rhs=xTb[:, kc, sb_ * 256:(sb_ + 1) * 256],
                                                                                                                  start=(kc == 0 and j % 2 == 0), stop=(kc == 15), skip_group_check=True),
                                         reads=["wg%d" % i, "xT%d" % xb_], writes=["bank%d" % bi], inc=(kc == 15))
                        for j in range(4):
                            s_ = sg[j % 2]
                            p.op("act", lambda e, s_=s_, j=j: e.activation(out=s_[:], in_=cx.bank[j // 2][:, (j % 2) * 256:(j % 2 + 1) * 256], func=AF.Silu),
                                 reads=["bank%d" % (j // 2)], writes=["sg%d" % (j % 2)])
                            p.op("dve", lambda e, s_=s_, j=j, hp=hp, sb_=sb_: e.tensor_tensor(out=hTb[:, hp * 4 + j, sb_ * 256:(sb_ + 1) * 256], in0=s_[:], in1=cx.bank[2 + j // 2][:, (j % 2) * 256:(j % 2 + 1) * 256], op=ALU.mult),
                                 reads=["sg%d" % (j % 2), "bank%d" % (2 + j // 2)], writes=["hTb"])
                wdl = []
                for hc in range(8):
                    i = kd % NWD
                    kd += 1
                    wdl.append(i)
                    p.dma("pool", lambda e, i=i, ex=ex, hc=hc: e.dma_start(out=wd[i][:], in_=wdv[cx.WL(L), ex, hc * 128:(hc + 1) * 128, :]), writes=["wd%d" % i])
                for blk in range(NBLK):
                    y = yb[ky % 2]
                    yn = "yb%d" % (ky % 2)
                    ky += 1
                    for cg in range(4):
                        for hc in range(8):
                            i = wdl[hc]
                            p.op("pe", lambda e, i=i, cg=cg, hc=hc, blk=blk: e.matmul(cx.bank[4 + cg][:], lhsT=hTb[:, hc, blk * 128:(blk + 1) * 128], rhs=wd[i][:, cg * 512:(cg + 1) * 512], start=(hc == 0), stop=(hc == 7)),
                                 reads=["hTb", "wd%d" % i], writes=["bank%d" % (4 + cg)], inc=(hc == 7))
                        if cg % 2 == 0:
                            p.op("act", lambda e, y=y, cg=cg: e.activation(out=y[:, cg * 512:(cg + 1) * 512], in_=cx.bank[4 + cg][:], func=AF.Copy), reads=["bank%d" % (4 + cg)], writes=[yn])
                        else:
                            p.op("dve", lambda e, y=y, cg=cg: e.tensor_copy(out=y[:, cg * 512:(cg + 1) * 512], in_=cx.bank[4 + cg][:]), reads=["bank%d" % (4 + cg)], writes=[yn])
                    p.dma("act", lambda e, y=y, ex=ex, blk=blk: e.dma_start(out=ysv[ex, blk], in_=y[:]), reads=[yn], writes=["YS%d_%d" % (ex, blk)])
            p.emit()
        with ExitStack() as st:
            xs2 = [p.sb(st, "cxs%d" % i, [128, D], F32) for i in range(2)]
            y1 = [p.sb(st, "y1_%d" % i, [128, D], F32) for i in range(2)]
            y2 = [p.sb(st, "y2_%d" % i, [128, D], F32) for i in range(2)]
            GT = p.sb(st, "GT", [128, D], F32)
            p.dma("sp", lambda e: e.dma_start(out=GT[:], in_=bc(cx.MODD.ap()[L, 5 * D:6 * D])), writes=["GT"])
            if final_g is not None:
                FG = p.sb(st, "FG", [128, D], F32)
                p.dma("sp", lambda e: e.dma_start(out=FG[:], in_=bc(final_g.ap()[:])), writes=["FG"])
                cx.junk = p.sb(st, "junk", [128, D], BF16)
                fs = [p.sb(st, "fs%d" % i, [128, 4], F32) for i in range(2)]
            xout = XOUT.ap()
            for t in range(NT):
                b = t % 2
                xs, a1, a2 = xs2[b], y1[b], y2[b]
                p.dma("sp", lambda e, xs=xs, t=t: e.dma_start(out=xs[:], in_=xin[t * 128:(t + 1) * 128, :]), reads=["XIN"], writes=["cxs%d" % b])
                p.dma("pool", lambda e, a1=a1, t=t: e.indirect_dma_start(out=a1[:], out_offset=None, in_=cx.YS.ap(), in_offset=bass.IndirectOffsetOnAxis(ap=d1i[:, t:t + 1], axis=0)),
                      reads=["d1i"], writes=["y1_%d" % b])
                p.dma("pool", lambda e, a2=a2, t=t: e.indirect_dma_start(out=a2[:], out_offset=None, in_=cx.YS.ap(), in_offset=bass.IndirectOffsetOnAxis(ap=d2i[:, t:t + 1], axis=0)),
                      reads=["d2i"], writes=["y2_%d" % b])
                p.op("act", lambda e, a1=a1, t=t: e.activation(out=a1[:], in_=a1[:], func=AF.Copy, scale=gts[:, 2 * t:2 * t + 1]), reads=["y1_%d" % b, "gts"], writes=["y1_%d" % b])
                p.op("dve", lambda e, a1=a1, a2=a2, t=t: e.scalar_tensor_tensor(out=a2[:], in0=a2[:], scalar=gts[:, 2 * t + 1:2 * t + 2], in1=a1[:], op0=ALU.mult, op1=ALU.add),
                     reads=["y1_%d" % b, "y2_%d" % b, "gts"], writes=["y2_%d" % b])
                p.op("pool", lambda e, a2=a2: e.tensor_tensor(out=a2[:], in0=a2[:], in1=GT[:], op=ALU.mult), reads=["y2_%d" % b, "GT"], writes=["y2_%d" % b])
                p.op("dve", lambda e, a2=a2, xs=xs: e.tensor_tensor(out=xs[:], in0=a2[:], in1=xs[:], op=ALU.add), reads=["y2_%d" % b, "cxs%d" % b], writes=["cxs%d" % b])
                if final_g is not None:
                    f = fs[b]
                    p.op("act", lambda e, xs=xs, f=f: e.activation(out=cx.junk[:], in_=xs[:], func=AF.Square, accum_out=f[:, 0:1]), reads=["cxs%d" % b], writes=["junk", "fs%d" % b])
                    p.op("dve", lambda e, f=f: e.tensor_scalar(out=f[:, 1:2], in0=f[:, 0:1], scalar1=1.0 / D, scalar2=EPS, op0=ALU.mult, op1=ALU.add), reads=["fs%d" % b], writes=["fs%d" % b])
                    p.op("act", lambda e, f=f: e.activation(out=f[:, 2:3], in_=f[:, 1:2], func=AF.Sqrt), reads=["fs%d" % b], writes=["fs%d" % b])
                    p.op("dve", lambda e, f=f: e.reciprocal(out=f[:, 3:4], in_=f[:, 2:3]), reads=["fs%d" % b], writes=["fs%d" % b])
                    p.op("dve", lambda e, xs=xs, f=f: e.scalar_tensor_tensor(out=xs[:], in0=xs[:], scalar=f[:, 3:4], in1=FG[:], op0=ALU.mult, op1=ALU.mult),
                         reads=["cxs%d" % b, "fs%d" % b, "FG"], writes=["cxs%d" % b])
                p.dma("sp", lambda e, xs=xs, t=t: e.dma_start(out=xout[t * 128:(t + 1) * 128, :], in_=xs[:]), reads=["cxs%d" % b], writes=["XOUT"])
            p.emit()


def norm_to_hT(p, cx, XSRC, G, SH, hT, tb0=6):
    with ExitStack() as st:
        xs2 = [p.sb(st, "nxs%d" % i, [128, D], F32) for i in range(2)]
        hb2 = [p.sb(st, "nhb%d" % i, [128, D], BF16) for i in range(2)]
        ss2 = [p.sb(st, "nss%d" % i, [128, 4], F32) for i in range(2)]
        cx.junk = p.sb(st, "junk", [128, D], BF16)
        cx.tmpf = p.sb(st, "tmpf", [128, D], F32)
        cb = cx.cb
        for t in range(NT):
            b = t % 2
            nm = "n%d" % b
            xs, hb, ss = xs2[b], hb2[b], ss2[b]
            p.dma("sp", lambda e, xs=xs, t=t: e.dma_start(out=xs[:], in_=XSRC[t * 128:(t + 1) * 128, :]), writes=[nm + "xs"])
            rms_mod_tile(p, cx, xs, G, SH, None, hb, ss, nm, rd=["G", "SH"])
            for hh in range(2):
                bk = cx.bank[tb0 + hh]
                bkn = "bank%d" % (tb0 + hh)
                bkv = bk[:].bitcast(BF16)
                for j in range(8):
                    kc = hh * 8 + j
                    p.op("pe", lambda e, bkv=bkv, j=j, kc=kc, hb=hb: e.transpose(out=bkv[:, j * 128:(j + 1) * 128], in_=hb[:, kc * 128:(kc + 1) * 128], identity=cb[:, C_ID:C_ID + 128]),
                         reads=[nm + "hb"], writes=[bkn], inc=(j == 7))
                if hh == 0:
                    p.op("act", lambda e, bkv=bkv, hh=hh, t=t: e.activation(out=hT[:, hh * 8:(hh + 1) * 8, t * 128:(t + 1) * 128], in_=bkv.rearrange("p (a b) -> p a b", a=8), func=AF.Copy),
                         reads=[bkn], writes=["hT_%d_%d" % (t, hh)])
                else:
                    p.op("dve", lambda e, bkv=bkv, hh=hh, t=t: e.tensor_copy(out=hT[:, hh * 8:(hh + 1) * 8, t * 128:(t + 1) * 128], in_=bkv.rearrange("p (a b) -> p a b", a=8)),
                         reads=[bkn], writes=["hT_%d_%d" % (t, hh)])
        p.emit()


def proj_out_stage(p, cx, SRC, Wap, GT, XIN, XOUT):
    with ExitStack() as st:
        cb = cx.cb
        W = p.sb(st, "Wout", [128, 16, D], BF16)
        wv = Wap.rearrange("(kc k) c -> k kc c", k=128)
        for i in range(4):
            p.dma("pool", lambda e, i=i: e.dma_start(out=W[:, :, i * 512:(i + 1) * 512], in_=wv[:, :, i * 512:(i + 1) * 512]), writes=["Wout%d" % i])
        sr2 = [p.sb(st, "osr%d" % i, [128, D], BF16) for i in range(2)]
        sT2 = [p.sb(st, "osT%d" % i, [128, 16, 128], BF16) for i in range(2)]
        xs2 = [p.sb(st, "oxs%d" % i, [128, D], F32) for i in range(2)]
        tm2 = [p.sb(st, "otm%d" % i, [128, D], F32) for i in range(2)]
        for t in range(NT):
            b = t % 2
            sr, sT, xs, tm = sr2[b], sT2[b], xs2[b], tm2[b]
            p.dma("sp", lambda e, sr=sr, t=t: e.dma_start(out=sr[:], in_=SRC[t * 128:(t + 1) * 128, :]), writes=["osr%d" % b])
            p.dma("act", lambda e, xs=xs, t=t: e.dma_start(out=xs[:], in_=XIN[t * 128:(t + 1) * 128, :]), writes=["oxs%d" % b])
            for hh in range(2):
                bk = cx.bank[4 + hh]
                bkn = "bank%d" % (4 + hh)
                bkv = bk[:].bitcast(BF16)
                for j in range(8):
                    kc = hh * 8 + j
                    p.op("pe", lambda e, bkv=bkv, j=j, kc=kc, sr=sr: e.transpose(out=bkv[:, j * 128:(j + 1) * 128], in_=sr[:, kc * 128:(kc + 1) * 128], identity=cb[:, C_ID:C_ID + 128]),
                         reads=["osr%d" % b], writes=[bkn], inc=(j == 7))
                if hh == 0:
                    p.op("act", lambda e, bkv=bkv, hh=hh, sT=sT: e.activation(out=sT[:, hh * 8:(hh + 1) * 8, :], in_=bkv.rearrange("p (a b) -> p a b", a=8), func=AF.Copy),
                         reads=[bkn], writes=["osT%d" % b])
                else:
                    p.op("dve", lambda e, bkv=bkv, hh=hh, sT=sT: e.tensor_copy(out=sT[:, hh * 8:(hh + 1) * 8, :], in_=bkv.rearrange("p (a b) -> p a b", a=8)),
                         reads=[bkn], writes=["osT%d" % b])
            for cg in range(4):
                for kc in range(16):
                    p.op("pe", lambda e, cg=cg, kc=kc, sT=sT: e.matmul(cx.bank[cg][:], lhsT=sT[:, kc, :], rhs=W[:, kc, cg * 512:(cg + 1) * 512], start=(kc == 0), stop=(kc == 15)),
                         reads=["osT%d" % b, "Wout%d" % cg], writes=["bank%d" % cg], inc=(kc == 15))
                p.op("dve", lambda e, cg=cg, tm=tm: e.tensor_tensor(out=tm[:, cg * 512:(cg + 1) * 512], in0=cx.bank[cg][:], in1=GT[:, cg * 512:(cg + 1) * 512], op=ALU.mult),
                     reads=["bank%d" % cg, "GT"], writes=["otm%d_%d" % (b, cg)])
                p.op("pool", lambda e, cg=cg, tm=tm, xs=xs: e.tensor_tensor(out=tm[:, cg * 512:(cg + 1) * 512], in0=tm[:, cg * 512:(cg + 1) * 512], in1=xs[:, cg * 512:(cg + 1) * 512], op=ALU.add),
                     reads=["otm%d_%d" % (b, cg), "oxs%d" % b], writes=["otm%d_%d" % (b, cg)])
            p.dma("sp", lambda e, tm=tm, t=t: e.dma_start(out=XOUT[t * 128:(t + 1) * 128, :], in_=tm[:]), reads=["otm%d_%d" % (b, cg) for cg in range(4)], writes=["XO%d" % t])
        p.emit()


TWO_PI = 2.0 * math.pi
C1 = 6.28125
C2 = TWO_PI - C1


def rope_tables(p, cx, POSap, COS, SINS, tag):
    with ExitStack() as st:
        cf = cx.cf
        a = p.sb(st, "ra", [32, TOK], F32)
        k = p.sb(st, "rk", [32, TOK], F32)
        ki = p.sb(st, "rki", [32, TOK], I32)
        m = p.sb(st, "rm", [32, TOK], F32)
        p.dma("pool", lambda e: e.dma_start(out=a[:], in_=bc(POSap, 32)), writes=["ra"])
        p.op("dve", lambda e: e.tensor_scalar(out=a[:], in0=a[:], scalar1=cf[0:32, C_INVF:C_INVF + 1], scalar2=None, op0=ALU.mult), reads=["ra"], writes=["ra"])

        def reduce_(src, dst, shift):
            p.op("dve", lambda e: e.tensor_scalar(out=k[:], in0=src[:], scalar1=shift, scalar2=1.0 / TWO_PI, op0=ALU.add, op1=ALU.mult), reads=["ra", "rd"], writes=["rk"])
            p.op("dve", lambda e: e.tensor_copy(out=ki[:], in_=k[:]), reads=["rk"], writes=["rki"])
            p.op("dve", lambda e: e.tensor_copy(out=k[:], in_=ki[:]), reads=["rki"], writes=["rk"])
            p.op("dve", lambda e: e.scalar_tensor_tensor(out=dst[:], in0=k[:], scalar=-C1, in1=src[:], op0=ALU.mult, op1=ALU.add), reads=["rk", "ra"], writes=["rd"])
            p.op("dve", lambda e: e.scalar_tensor_tensor(out=dst[:], in0=k[:], scalar=-C2, in1=dst[:], op0=ALU.mult, op1=ALU.add), reads=["rk", "rd"], writes=["rd"])
            if shift != 0.0:
                p.op("dve", lambda e: e.tensor_scalar(out=dst[:], in0=dst[:], scalar1=shift, scalar2=None, op0=ALU.add), reads=["rd"], writes=["rd"])
            p.op("dve", lambda e: e.tensor_scalar(out=m[:], in0=dst[:], scalar1=math.pi, scalar2=-TWO_PI, op0=ALU.is_gt, op1=ALU.mult), reads=["rd"], writes=["rm"])
            p.op("dve", lambda e: e.tensor_tensor(out=dst[:], in0=dst[:], in1=m[:], op=ALU.add), reads=["rd", "rm"], writes=["rd"])
            p.op("dve", lambda e: e.tensor_scalar(out=m[:], in0=dst[:], scalar1=-math.pi, scalar2=TWO_PI, op0=ALU.is_lt, op1=ALU.mult), reads=["rd"], writes=["rm"])
            p.op("dve", lambda e: e.tensor_tensor(out=dst[:], in0=dst[:], in1=m[:], op=ALU.add), reads=["rd", "rm"], writes=["rd"])
            p.op("dve", lambda e: e.tensor_scalar(out=dst[:], in0=dst[:], scalar1=math.pi, scalar2=-math.pi, op0=ALU.min, op1=ALU.max), reads=["rd"], writes=["rd"])

        reduce_(a, SINS, 0.0)
        p.op("act", lambda e: e.activation(out=SINS[:], in_=SINS[:], func=AF.Sin), reads=["rd"], writes=["rd"])
        p.op("dve", lambda e: e.tensor_scalar(out=SINS[:], in0=SINS[:], scalar1=cf[0:32, C_SGN:C_SGN + 1], scalar2=None, op0=ALU.mult), reads=["rd"], writes=["rd"])
        reduce_(a, COS, math.pi / 2)
        p.op("act", lambda e: e.activation(out=COS[:], in_=COS[:], func=AF.Sin), reads=["rd"], writes=["rd"])
        p.emit()


def attn_proj(p, cx, hT, own, COS, SINS, nmax):
    with ExitStack() as st:
        cb = cx.cb
        wp = [p.sb(st, "wp%d" % i, [128, 16, 512], BF16) for i in range(2)]
        qk = [p.sb(st, "qk%d" % i, [128, 512], BF16) for i in range(4)]
        sq = [p.sb(st, "sq%d" % i, [128, 512], BF16) for i in range(2)]
        t1 = [p.sb(st, "t1_%d" % i, [32, 512], F32) for i in range(2)]
        t2 = [p.sb(st, "t2_%d" % i, [32, 512], F32) for i in range(2)]
        nm1 = p.sb(st, "nm1", [1, 2], F32)
        vs = [p.sb(st, "vs%d" % i, [128, 2, 257], BF16) for i in range(4)]
        for i in range(4):
            p.op("pool", lambda e, i=i: e.memset(vs[i][:], 1.0), writes=["vs%d" % i])
        wv = cx.ATTN_W_IN.ap().rearrange("(kc k) c -> k kc c", k=128)
        toff = TOK if own else 0
        pieces = range(12) if own else range(4, 12)
        cnt = 0
        vcnt = 0
        for pi, j in enumerate(pieces):
            w = wp[pi % 2]
            wn = "wp%d" % (pi % 2)
            p.dma("pool", lambda e, w=w, j=j: e.dma_start(out=w[:], in_=wv[:, :, j * 512:(j + 1) * 512]), writes=[wn])
            if j < 8:
                isq = j < 4
                for cc in range(4):
                    gcc = (j % 4) * 4 + cc
                    for tg in range(4):
                        bi = cnt % 4
                        bk = cx.bank[bi]
                        q_ = qk[cnt % 4]
                        qn = "qk%d" % (cnt % 4)
                        s_ = sq[cnt % 2]
                        sn = "sq%d" % (cnt % 2)
                        a1, a2 = t1[cnt % 2], t2[cnt % 2]
                        an = "t12_%d" % (cnt % 2)
                        nb = cx.bank[4 + cnt % 2]
                        nbn = "bank%d" % (4 + cnt % 2)
                        sb_ = cx.bank[6 + cnt % 2]
                        sbn = "bank%d" % (6 + cnt % 2)
                        cnt += 1
                        for kc in range(16):
                            p.op("pe", lambda e, bk=bk, w=w, kc=kc, cc=cc, tg=tg: e.matmul(bk[:], lhsT=w[:, kc, cc * 128:(cc + 1) * 128], rhs=hT[:, kc, tg * 512:(tg + 1) * 512], start=(kc == 0), stop=(kc == 15)),
                                 reads=[wn], writes=["bank%d" % bi], inc=(kc == 15))
                        p.op("act", lambda e, bk=bk, q_=q_: e.activation(out=q_[:], in_=bk[:], func=AF.Copy), reads=["bank%d" % bi], writes=[qn])
                        p.op("act", lambda e, bk=bk, s_=s_: e.activation(out=s_[:], in_=bk[:], func=AF.Square), reads=["bank%d" % bi], writes=[sn])
                        p.op("pe", lambda e, nb=nb, s_=s_: e.matmul(nb[0:1, :], lhsT=cb[:, C_ON:C_ON + 1], rhs=s_[:], start=True, stop=True), reads=[sn], writes=[nbn])
                        p.op("dve", lambda e, nb=nb: e.reduce_max(out=nm1[:, 0:1], in_=nb[0:1, :], axis=AX.X), reads=[nbn], writes=["nm1"])
                        ix = gcc if isq else 16 + gcc
                        p.op("dve", lambda e, ix=ix: e.tensor_tensor(out=nmax[:, ix:ix + 1], in0=nmax[:, ix:ix + 1], in1=nm1[:, 0:1], op=ALU.max), reads=["nm1", "nmax"], writes=["nmax"])
                        p.op("pe", lambda e, sb_=sb_, q_=q_: e.matmul(sb_[0:32, :], lhsT=cb[0:32, C_PERM:C_PERM + 32], rhs=q_[0:32, :], start=True, stop=True), reads=[qn], writes=[sbn])
                        p.op("dve", lambda e, a1=a1, q_=q_, tg=tg: e.tensor_tensor(out=a1[:], in0=q_[0:32, :], in1=COS[:, tg * 512:(tg + 1) * 512], op=ALU.mult), reads=[qn], writes=[an + "a"])
                        p.op("dve", lambda e, a2=a2, sb_=sb_, tg=tg: e.tensor_tensor(out=a2[:], in0=sb_[0:32, :], in1=SINS[:, tg * 512:(tg + 1) * 512], op=ALU.mult), reads=[sbn], writes=[an + "b"])
                        p.op("dve", lambda e, a1=a1, a2=a2, q_=q_: e.tensor_tensor(out=q_[0:32, :], in0=a1[:], in1=a2[:], op=ALU.add), reads=[an + "a", an + "b", qn], writes=[qn])
                        if isq:
                            dst = cx.QTD.ap()[gcc, :, tg * 512:(tg + 1) * 512]
                        else:
                            dst = cx.KTD.ap()[gcc, :, toff + tg * 512:toff + (tg + 1) * 512]
                        p.dma("sp", lambda e, dst=dst, q_=q_: e.dma_start(out=dst, in_=q_[:]), reads=[qn], writes=["qkd%d" % cnt])
            else:
                vj = j - 8
                for t in range(NT):
                    bi = cnt % 4
                    bk = cx.bank[bi]
                    cnt += 1
                    v_ = vs[vcnt % 4]
                    vn = "vs%d" % (vcnt % 4)
                    vcnt += 1
                    for kc in range(16):
                        p.op("pe", lambda e, bk=bk, w=w, kc=kc, t=t: e.matmul(bk[:], lhsT=hT[:, kc, t * 128:(t + 1) * 128], rhs=w[:, kc, :], start=(kc == 0), stop=(kc == 15)),
                             reads=[wn], writes=["bank%d" % bi], inc=(kc == 15))
                    if t % 2 == 0:
                        p.op("act", lambda e, bk=bk, v_=v_: e.activation(out=v_[:, :, 0:256], in_=bk[:].rearrange("p (a b) -> p a b", a=2), func=AF.Copy), reads=["bank%d" % bi], writes=[vn])
                    else:
                        p.op("dve", lambda e, bk=bk, v_=v_: e.tensor_copy(out=v_[:, :, 0:256], in_=bk[:].rearrange("p (a b) -> p a b", a=2)), reads=["bank%d" % bi], writes=[vn])
                    r0 = toff + t * 128
                    p.dma("act", lambda e, v_=v_, r0=r0, vj=vj: e.dma_start(out=cx.VD.ap()[r0:r0 + 128, 2 * vj:2 * vj + 2, :], in_=v_[:]), reads=[vn], writes=["vd%d" % cnt])
        p.emit()


def attn_consts(p, cx, st0, nmax):
    negc = p.sb(st0, "negc", [128, 16], F32)
    negcp = p.sb(st0, "negcp", [128, 16], F32)
    nlam = p.sb(st0, "nlam", [128, 1], F32)
    HG = p.sb(st0, "HG", [128, 256], F32)
    with ExitStack() as st:
        cf = cx.cf
        r = p.sb(st, "acr", [1, 64], F32)
        lm = p.sb(st, "lm", [1, 4, 128], F32)
        pf = p.sb(st, "pf", [128, 1], F32)
        for i, nm_ in enumerate([cx.LQ1, cx.LK1, cx.LQ2, cx.LK2]):
            p.dma("sp", lambda e, i=i, nm_=nm_: e.dma_start(out=lm[:, i, :], in_=nm_.ap()), writes=["lm"])
        p.dma("sp", lambda e: e.dma_start(out=pf[:], in_=cx.PREVFLAG.ap()), writes=["pf"])
        p.dma("sp", lambda e: e.dma_start(out=HG[:], in_=bc(cx.ATTN_HG.ap()[0, :])), writes=["HG"])
        p.op("dve", lambda e: e.tensor_scalar(out=HG[:], in0=HG[:], scalar1=0.8, scalar2=None, op0=ALU.mult), reads=["HG"], writes=["HG"])
        p.op("dve", lambda e: e.tensor_tensor(out=r[:, 0:16], in0=nmax[:, 0:16], in1=nmax[:, 16:32], op=ALU.mult), reads=["nmax"], writes=["acr"])
        p.op("act", lambda e: e.activation(out=r[:, 0:16], in_=r[:, 0:16], func=AF.Sqrt), reads=["acr"], writes=["acr"])
        p.op("dve", lambda e: e.tensor_scalar(out=r[:, 0:16], in0=r[:, 0:16], scalar1=-(128.0 ** -0.5), scalar2=None, op0=ALU.mult), reads=["acr"], writes=["acr"])
        p.op("dve", lambda e: e.tensor_tensor(out=lm[:, 0, :], in0=lm[:, 0, :], in1=lm[:, 1, :], op=ALU.mult), reads=["lm"], writes=["lm"])
        p.op("dve", lambda e: e.tensor_tensor(out=lm[:, 2, :], in0=lm[:, 2, :], in1=lm[:, 3, :], op=ALU.mult), reads=["lm"], writes=["lm"])
        p.op("dve", lambda e: e.reduce_sum(out=r[:, 32:33], in_=lm[:, 0, :], axis=AX.X), reads=["lm"], writes=["acr"])
        p.op("dve", lambda e: e.reduce_sum(out=r[:, 33:34], in_=lm[:, 2, :], axis=AX.X), reads=["lm"], writes=["acr"])
        p.op("act", lambda e: e.activation(out=r[:, 32:34], in_=r[:, 32:34], func=AF.Exp), reads=["acr"], writes=["acr"])
        p.op("dve", lambda e: e.tensor_tensor(out=r[:, 16:17], in0=r[:, 33:34], in1=r[:, 32:33], op=ALU.subtract), reads=["acr"], writes=["acr"])
        p.op("dve", lambda e: e.tensor_scalar(out=r[:, 16:17], in0=r[:, 16:17], scalar1=-0.2, scalar2=None, op0=ALU.add), reads=["acr"], writes=["acr"])
        p.op("pe", lambda e: e.matmul(cx.bank[0][:, 0:18], lhsT=cf[0:1, C_ON:C_ON + 128], rhs=r[0:1, 0:18], start=True, stop=True), reads=["acr"], writes=["bank0"])
        p.op("dve", lambda e: e.tensor_copy(out=negc[:], in_=cx.bank[0][:, 0:16]), reads=["bank0"], writes=["negc"])
        p.op("dve", lambda e: e.tensor_copy(out=nlam[:], in_=cx.bank[0][:, 16:17]), reads=["bank0"], writes=["nlam"])
        p.op("dve", lambda e: e.tensor_scalar(out=negcp[:], in0=negc[:], scalar1=pf[:, 0:1], scalar2=None, op0=ALU.add), reads=["negc", "pf"], writes=["negcp"])
        p.emit()
    return negc, negcp, nlam, HG


def attn_core(p, cx, negc, negcp, nlam, HG):
    SCALE = 128.0 ** -0.5
    with ExitStack() as st:
        QT = [[p.sb(st, "QT%d_%d" % (b, m), [128, TOK], BF16) for m in range(2)] for b in range(2)]
        KT = [[p.sb(st, "KT%d_%d" % (b, m), [128, 2 * TOK], BF16) for m in range(2)] for b in range(2)]
        V = [p.sb(st, "V%d" % b, [128, 32, 257], BF16) for b in range(2)]
        PT = [p.sb(st, "PT%d" % m, [128, 36, 512], BF16) for m in range(2)]
        o0 = p.sb(st, "o0", [128, 4, 256], F32)
        oa = [p.sb(st, "oa%d" % i, [128, 4, 256], BF16) for i in range(2)]
        rd = [p.sb(st, "rd%d" % i, [128, 8], F32) for i in range(4)]
        junk = p.sb(st, "cjunk", [128, 256], BF16)
        vdv = cx.VD.ap().rearrange("(t k) h e -> h k t e", k=128)

        def loads(h):
            b = h % 2
            for m in range(2):
                p.dma("sp", lambda e, b=b, m=m, h=h: e.dma_start(out=QT[b][m][:], in_=cx.QTD.ap()[2 * h + m]), writes=["QT%d_%d" % (b, m)])
                p.dma("sp", lambda e, b=b, m=m, h=h: e.dma_start(out=KT[b][m][:], in_=cx.KTD.ap()[2 * h + m]), writes=["KT%d_%d" % (b, m)])
            p.dma("act", lambda e, b=b, h=h: e.dma_start(out=V[b][:], in_=vdv[h]), writes=["V%d" % b])

        stb = [0]
        accb = [0]
        oac = [0]
        rdc = [0]

        def stageA(h, g, m):
            b = h % 2
            hm = 2 * h + m
            steps = []
            nk = 16 + 4 * g + 4
            for j in range(nk):
                def step(j=j):
                    qoff = max(0, j - 16 - 4 * g)
                    bi = stb[0] % 3
                    stb[0] += 1
                    bk = cx.bank[bi]
                    p.op("pe", lambda e: e.matmul(bk[:, qoff * 128:512], lhsT=KT[b][m][:, j * 128:(j + 1) * 128], rhs=QT[b][m][:, g * 512 + qoff * 128:(g + 1) * 512], start=True, stop=True),
                         reads=["KT%d_%d" % (b, m), "QT%d_%d" % (b, m)], writes=["bank%d" % bi])
                    bias = negcp[:, hm:hm + 1] if j < 16 else negc[:, hm:hm + 1]
                    p.op("act", lambda e: e.activation(out=PT[m][:, j, qoff * 128:512], in_=bk[:, qoff * 128:512], func=AF.Exp, bias=bias, scale=SCALE),
                         reads=["bank%d" % bi], writes=["PT%d_%d" % (m, j)])
                    if j >= 16 + 4 * g:
                        ii = j - 16 - 4 * g
                        p.op("pool", lambda e: e.memset(PT[m][64:128, j, ii * 128:ii * 128 + 64], 0.0), reads=[], writes=["PT%d_%d" % (m, j)])
                steps.append(step)
            return steps

        def stageB(h, g, m):
            b = h % 2
            steps = []
            for ii in range(4):
                ai = 3 + accb[0] % 5
                accb[0] += 1
                acc = cx.bank[ai]
                an = "bank%d" % ai
                nkk = 16 + 4 * g + ii + 1
                for j in range(nkk):
                    def step(j=j, ii=ii, acc=acc, an=an, nkk=nkk):
                        p.op("pe", lambda e: e.matmul(acc[:, 0:257], lhsT=PT[m][:, j, ii * 128:(ii + 1) * 128], rhs=V[b][:, j, :], start=(j == 0), stop=(j == nkk - 1)),
                             reads=["PT%d_%d" % (m, j), "V%d" % b], writes=[an], inc=(j == nkk - 1))
                    steps.append(step)

                def fin(ii=ii, acc=acc, an=an):
                    r = rd[rdc[0] % 4]
                    rn = "rd%d" % (rdc[0] % 4)
                    rdc[0] += 1
                    p.op("dve", lambda e: e.reciprocal(out=r[:, 0:1], in_=acc[:, 256:257]), reads=[an], writes=[rn])
                    if m == 0:
                        p.op("act", lambda e: e.activation(out=o0[:, ii, :], in_=acc[:, 0:256], func=AF.Copy, scale=r[:, 0:1]), reads=[an, rn], writes=["o0_%d" % ii])
                    else:
                        o = oa[oac[0] % 2]
                        on = "oa%d" % (oac[0] % 2)
                        p.op("dve", lambda e: e.tensor_tensor(out=r[:, 1:2], in0=r[:, 0:1], in1=nlam[:, 0:1], op=ALU.mult), reads=[rn], writes=[rn])
                        p.op("dve", lambda e: e.scalar_tensor_tensor(out=o0[:, ii, :], in0=acc[:, 0:256], scalar=r[:, 1:2], in1=o0[:, ii, :], op0=ALU.mult, op1=ALU.add),
                             reads=[an, rn, "o0_%d" % ii], writes=["o0_%d" % ii])
                        p.op("act", lambda e: e.activation(out=junk[:], in_=o0[:, ii, :], func=AF.Square, accum_out=r[:, 2:3]), reads=["o0_%d" % ii], writes=["cjunk", rn])
                        p.op("dve", lambda e: e.tensor_scalar(out=r[:, 3:4], in0=r[:, 2:3], scalar1=1.0 / 256, scalar2=EPS, op0=ALU.mult, op1=ALU.add), reads=[rn], writes=[rn])
                        p.op("act", lambda e: e.activation(out=r[:, 4:5], in_=r[:, 3:4], func=AF.Sqrt), reads=[rn], writes=[rn])
                        p.op("dve", lambda e: e.reciprocal(out=r[:, 5:6], in_=r[:, 4:5]), reads=[rn], writes=[rn])
                        p.op("dve", lambda e: e.scalar_tensor_tensor(out=o[:, ii, :], in0=o0[:, ii, :], scalar=r[:, 5:6], in1=HG[:], op0=ALU.mult, op1=ALU.mult),
                             reads=["o0_%d" % ii, rn], writes=[on + "_%d" % ii])
                        if ii == 3:
                            oac[0] += 1
                            dst = cx.OAD.ap()[g * 512:(g + 1) * 512, h * 256:(h + 1) * 256].rearrange("(i q) c -> q i c", q=128)
                            p.dma("sp", lambda e: e.dma_start(out=dst, in_=o[:]), reads=[on + "_%d" % i_ for i_ in range(4)], writes=["oad_%d_%d" % (h, g)])
                steps.append(fin)
            return steps

        def interleave(A, B):
            na, nb = len(A), len(B)
            ia = ib = 0
            while ia < na or ib < nb:
                if ib >= nb or (ia < na and ia * max(nb, 1) <= ib * max(na, 1)):
                    A[ia]()
                    ia += 1
                else:
                    B[ib]()
                    ib += 1

        loads(0)
        pend = []
        for h in range(8):
            for g in range(4):
                for m in range(2):
                    A = stageA(h, g, m)
                    interleave(A, pend)
                    pend = stageB(h, g, m)
                    if g == 0 and m == 0 and h + 1 < 8:
                        loads(h + 1)
        interleave([], pend)
        p.emit()


def attn_layer(p, cx, XIN, XPREV, XOUT):
    with ExitStack() as st0:
        nmax = p.sb(st0, "nmax", [1, 32], F32)
        p.op("dve", lambda e: e.memset(nmax[:], 0.0), writes=["nmax"])
        with ExitStack() as st1:
            G, SH, GT = load_mod_tiles(p, cx, st1, 0, 0, cx.NORM_MIX_G)
            COS = p.sb(st1, "COS", [32, TOK], F32)
            SINS = p.sb(st1, "SINS", [32, TOK], F32)
            hT = p.sb(st1, "hT", [128, 16, TOK], BF16)
            rope_tables(p, cx, cx.POS_PREV.ap()[0, :], COS, SINS, "p")
            norm_to_hT(p, cx, XPREV.ap(), G, SH, hT)
            attn_proj(p, cx, hT, False, COS, SINS, nmax)
            rope_tables(p, cx, cx.POS_OWN.ap()[0, :], COS, SINS, "o")
            norm_to_hT(p, cx, XIN.ap(), G, SH, hT)
            attn_proj(p, cx, hT, True, COS, SINS, nmax)
        negc, negcp, nlam, HG = attn_consts(p, cx, st0, nmax)
        attn_core(p, cx, negc, negcp, nlam, HG)
    with ExitStack() as st2:
        GT = p.sb(st2, "GT", [128, D], F32)
        p.dma("sp", lambda e: e.dma_start(out=GT[:], in_=bc(cx.MODD.ap()[0, 2 * D:3 * D])), writes=["GT"])
        proj_out_stage(p, cx, cx.OAD.ap(), cx.ATTN_W_OUT.ap(), GT, XIN.ap(), XOUT.ap())


def mlstm_proj(p, cx, hT, full=True):
    with ExitStack() as st:
        wp = [p.sb(st, "wp%d" % i, [128, 16, 512], BF16) for i in range(2)]
        wgt = p.sb(st, "wgt", [128, 16, 16], BF16)
        qk = [p.sb(st, "qk%d" % i, [128, 512], BF16) for i in range(4)]
        vs = [p.sb(st, "vs%d" % i, [128, 2, 257], BF16) for i in range(4)]
        ob = [p.sb(st, "ob%d" % i, [128, 512], BF16) for i in range(4)]
        gsb = [p.sb(st, "gsb%d" % i, [128, 16], F32) for i in range(2)]
        for i in range(4):
            p.op("pool", lambda e, i=i: e.memset(vs[i][:], 1.0), writes=["vs%d" % i])
        wv = cx.ML_W_IN.ap().rearrange("(kc k) c -> k kc c", k=128)
        p.dma("pool", lambda e: e.dma_start(out=wgt[:], in_=wv[:, :, 3 * D:3 * D + 16]), writes=["wgt"])
        cnt = 0
        for j in range(12 if full else 8):
            w = wp[j % 2]
            wn = "wp%d" % (j % 2)
            p.dma("pool", lambda e, w=w, j=j: e.dma_start(out=w[:], in_=wv[:, :, j * 512:(j + 1) * 512]), writes=[wn])
            if j < 4:
                for cc in range(4):
                    gcc = j * 4 + cc
                    for tg in range(4):
                        bi = cnt % 4
                        bk = cx.bank[bi]
                        q_ = qk[cnt % 4]
                        qn = "qk%d" % (cnt % 4)
                        cnt += 1
                        for kc in range(16):
                            p.op("pe", lambda e, bk=bk, w=w, kc=kc, cc=cc, tg=tg: e.matmul(bk[:], lhsT=w[:, kc, cc * 128:(cc + 1) * 128], rhs=hT[:, kc, tg * 512:(tg + 1) * 512], start=(kc == 0), stop=(kc == 15)),
                                 reads=[wn], writes=["bank%d" % bi], inc=(kc == 15))
                        if cnt % 2 == 0:
                            p.op("act", lambda e, bk=bk, q_=q_: e.activation(out=q_[:], in_=bk[:], func=AF.Copy), reads=["bank%d" % bi], writes=[qn])
                        else:
                            p.op("dve", lambda e, bk=bk, q_=q_: e.tensor_copy(out=q_[:], in_=bk[:]), reads=["bank%d" % bi], writes=[qn])
                        dst = cx.QKP.ap()[gcc, :, 3 + tg * 512:3 + (tg + 1) * 512]
                        p.dma("sp", lambda e, dst=dst, q_=q_: e.dma_start(out=dst, in_=q_[:]), reads=[qn], writes=["qkd%d" % cnt])
            else:
                for t in range(NT):
                    bi = cnt % 4
                    bk = cx.bank[bi]
                    cnt += 1
                    for kc in range(16):
                        p.op("pe", lambda e, bk=bk, w=w, kc=kc, t=t: e.matmul(bk[:], lhsT=hT[:, kc, t * 128:(t + 1) * 128], rhs=w[:, kc, :], start=(kc == 0), stop=(kc == 15)),
                             reads=[wn], writes=["bank%d" % bi], inc=(kc == 15))
                    if j < 8:
                        vj = j - 4
                        v_ = vs[cnt % 4]
                        vn = "vs%d" % (cnt % 4)
                        if t % 2 == 0:
                            p.op("act", lambda e, bk=bk, v_=v_: e.activation(out=v_[:, :, 0:256], in_=bk[:].rearrange("p (a b) -> p a b", a=2), func=AF.Copy), reads=["bank%d" % bi], writes=[vn])
                        else:
                            p.op("dve", lambda e, bk=bk, v_=v_: e.tensor_copy(out=v_[:, :, 0:256], in_=bk[:].rearrange("p (a b) -> p a b", a=2)), reads=["bank%d" % bi], writes=[vn])
                        p.dma("act", lambda e, v_=v_, t=t, vj=vj: e.dma_start(out=cx.VM.ap()[t * 128:(t + 1) * 128, 2 * vj:2 * vj + 2, :], in_=v_[:]), reads=[vn], writes=["vd%d" % cnt])
                    else:
                        oj = j - 8
                        o_ = ob[cnt % 4]
                        on = "ob%d" % (cnt % 4)
                        if t % 2 == 0:
                            p.op("act", lambda e, bk=bk, o_=o_: e.activation(out=o_[:], in_=bk[:], func=AF.Copy), reads=["bank%d" % bi], writes=[on])
                        else:
                            p.op("dve", lambda e, bk=bk, o_=o_: e.tensor_copy(out=o_[:], in_=bk[:]), reads=["bank%d" % bi], writes=[on])
                        p.dma("act", lambda e, o_=o_, t=t, oj=oj: e.dma_start(out=cx.OPRE.ap()[t * 128:(t + 1) * 128, oj * 512:(oj + 1) * 512], in_=o_[:]), reads=[on], writes=["od%d" % cnt])
        for t in range(NT):
            bk = cx.bank[4 + t % 2]
            g_ = gsb[t % 2]
            for kc in range(16):
                p.op("pe", lambda e, bk=bk, kc=kc, t=t: e.matmul(bk[:, 0:16], lhsT=hT[:, kc, t * 128:(t + 1) * 128], rhs=wgt[:, kc, :], start=(kc == 0), stop=(kc == 15)),
                     reads=["wgt"], writes=["bank%d" % (4 + t % 2)], inc=(kc == 15))
            p.op("dve", lambda e, bk=bk, g_=g_: e.tensor_copy(out=g_[:], in_=bk[:, 0:16]), reads=["bank%d" % (4 + t % 2)], writes=["gsb%d" % (t % 2)])
            p.dma("sp", lambda e, g_=g_, t=t: e.dma_start(out=cx.GATES.ap()[t * 128:(t + 1) * 128, :], in_=g_[:]), reads=["gsb%d" % (t % 2)], writes=["gd%d" % t])
        p.emit()


def mlstm_rec(p, cx, full=True, st_in="dram", st_out="dram"):
    RS = 128.0 ** -0.5
    with ExitStack() as st:
        cf, cb = cx.cf, cx.cb
        C = p.sb(st, "Cst", [128, 8, 257], F32)
        Cb = p.sb(st, "Cstb", [128, 8, 257], BF16)
        CW = p.sb(st, "CW", [128, 16, 4], F32)
        CB = p.sb(st, "CB", [128, 16], F32)
        GB = p.sb(st, "GB", [128, 16], F32)
        HGm = p.sb(st, "HGm", [128, D], F32)
        hl = p.sb(st, "hl", [128, 16, 3], F32)
        hlb = p.sb(st, "hlb", [128, 16, 3], BF16)
        qkp = [p.sb(st, "qkp%d" % i, [128, 16, 131], BF16) for i in range(2)]
        vt = [p.sb(st, "vt%d" % i, [128, 8, 257], BF16) for i in range(2)]
        gt_ = [p.sb(st, "gtl%d" % i, [128, 16], F32) for i in range(2)]
        op_ = [p.sb(st, "opl%d" % i, [128, D], BF16) for i in range(2)]
        sig = p.sb(st, "sig", [128, D], F32)
        cacc = [p.sb(st, "cacc%d" % i, [128, 128], F32) for i in range(4)]
        qs = p.sb(st, "qs", [128, 16, 128], BF16)
        gs = p.sb(st, "gs", [128, 64], F32)
        PTm = [p.sb(st, "PTm%d" % i, [128, 128], BF16) for i in range(2)]
        Kp = [p.sb(st, "Kp%d" % i, [128, 128], BF16) for i in range(2)]
        tmpC = [p.sb(st, "tmpC%d" % i, [128, 257], F32) for i in range(2)]
        ho = [p.sb(st, "ho%d" % i, [128, 256], F32) for i in range(2)]
        hn = [p.sb(st, "hn%d" % i, [128, D], BF16) for i in range(2)]
        rr = [p.sb(st, "rr%d" % i, [128, 8], F32) for i in range(4)]
        junk = p.sb(st, "mjunk", [128, 256], BF16)
        if st_in == "dram":
            p.dma("sp", lambda e: e.dma_start(out=C[:], in_=cx.STATE_IN.ap()), writes=["Cst"])
        elif st_in == "zero":
            p.op("dve", lambda e: e.memset(C[:], 0.0), writes=["Cst"])
        else:
            of = p.sb(st, "oddf", [128, 1], F32)
            p.dma("sp", lambda e: e.dma_start(out=of[:], in_=cx.ODDFLAG.ap()), writes=["oddf"])
            p.dma("sp", lambda e: e.dma_start(out=C[:].rearrange("p h e -> p (h e)"), in_=cx.ST_ALL.ap()[0:128, 0:8 * 257]), writes=["Cst"])
            p.op("dve", lambda e: e.tensor_scalar(out=C[:], in0=C[:], scalar1=of[:, 0:1], scalar2=None, op0=ALU.mult), reads=["Cst", "oddf"], writes=["Cst"])
        p.op("act", lambda e: e.activation(out=Cb[:], in_=C[:], func=AF.Copy), reads=["Cst"], writes=["Cstb"])
        p.dma("sp", lambda e: e.dma_start(out=CW[:], in_=cx.CONV_W.ap()), writes=["CW"])
        p.dma("sp", lambda e: e.dma_start(out=CB[:], in_=cx.CONV_B.ap()), writes=["CB"])
        p.dma("sp", lambda e: e.dma_start(out=GB[:], in_=bc(cx.GATE_B.ap()[0, :])), writes=["GB"])
        p.dma("sp", lambda e: e.dma_start(out=HGm[:], in_=bc(cx.ML_HG.ap()[0, :])), writes=["HGm"])
        if st_in == "dram":
            p.dma("sp", lambda e: e.dma_start(out=hl[:], in_=cx.HALO_IN.ap()), writes=["hl"])
        elif st_in == "zero":
            p.op("dve", lambda e: e.memset(hl[:], 0.0), writes=["hl"])
        else:
            p.dma("sp", lambda e: e.dma_start(out=hl[:].rearrange("p c t -> p (c t)"), in_=cx.ST_ALL.ap()[0:128, 8 * 257:8 * 257 + 48]), writes=["hl"])
            p.op("dve", lambda e: e.tensor_scalar(out=hl[:], in0=hl[:], scalar1=of[:, 0:1], scalar2=None, op0=ALU.mult), reads=["hl", "oddf"], writes=["hl"])
        p.op("dve", lambda e: e.tensor_copy(out=hlb[:], in_=hl[:]), reads=["hl"], writes=["hlb"])
        qkv = cx.QKP.ap().rearrange("c k t -> k c t")
        p.dma("sp", lambda e: e.dma_start(out=qkv[:, :, 0:3], in_=hlb[:], allow_slow_non_contiguous=True), reads=["hlb"], writes=["halo"])
        for c in range(NT):
            b = c % 2
            q_, v_, g_, o_ = qkp[b], vt[b], gt_[b], op_[b]
            rdh = ["halo"] if c == 0 else []
            p.dma("sp", lambda e, q_=q_, c=c: e.dma_start(out=q_[:], in_=qkv[:, :, c * 128:c * 128 + 131]), reads=rdh, writes=["qkp%d" % b])
            p.dma("act", lambda e, v_=v_, c=c: e.dma_start(out=v_[:], in_=cx.VM.ap()[c * 128:(c + 1) * 128]), writes=["vt%d" % b])
            p.dma("sp", lambda e, g_=g_, c=c: e.dma_start(out=g_[:], in_=cx.GATES.ap()[c * 128:(c + 1) * 128, :]), writes=["gtl%d" % b])
            if full:
                p.dma("act", lambda e, o_=o_, c=c: e.dma_start(out=o_[:], in_=cx.OPRE.ap()[c * 128:(c + 1) * 128, :]), writes=["opl%d" % b])
                p.op("act", lambda e, o_=o_: e.activation(out=sig[:], in_=o_[:], func=AF.Sigmoid), reads=["opl%d" % b], writes=["sig"])
            p.op("dve", lambda e, g_=g_: e.tensor_tensor(out=gs[:, 0:16], in0=g_[:], in1=GB[:], op=ALU.add), reads=["gtl%d" % b, "GB"], writes=["gs"])
            p.op("act", lambda e: e.activation(out=gs[:, 16:24], in_=gs[:, 8:16], func=AF.Exp, scale=-1.0), reads=["gs"], writes=["gs"])
            p.op("dve", lambda e: e.tensor_scalar(out=gs[:, 16:24], in0=gs[:, 16:24], scalar1=1.0, scalar2=None, op0=ALU.add), reads=["gs"], writes=["gs"])
            p.op("act", lambda e: e.activation(out=gs[:, 16:24], in_=gs[:, 16:24], func=AF.Ln), reads=["gs"], writes=["gs"])
            gbk = cx.bank[0]
            p.op("pe", lambda e: e.matmul(gbk[:, 0:8], lhsT=cf[:, C_TRI:C_TRI + 128], rhs=gs[:, 16:24], start=True, stop=True), reads=["gs"], writes=["bank0"])
            p.op("pe", lambda e: e.matmul(gbk[:, 8:16], lhsT=cf[:, C_ON:C_ON + 128], rhs=gs[:, 16:24], start=False, stop=True, skip_group_check=True), reads=["gs"], writes=["bank0"])
            p.op("dve", lambda e: e.tensor_tensor(out=gs[:, 32:40], in0=gs[:, 0:8], in1=gbk[:, 0:8], op=ALU.add), reads=["gs", "bank0"], writes=["gs"])
            p.op("act", lambda e: e.activation(out=gs[:, 32:40], in_=gs[:, 32:40], func=AF.Exp), reads=["gs"], writes=["gs"])
            p.op("dve", lambda e: e.tensor_scalar(out=gs[:, 32:40], in0=gs[:, 32:40], scalar1=RS, scalar2=None, op0=ALU.mult), reads=["gs"], writes=["gs"])
            p.op("act", lambda e: e.activation(out=gs[:, 40:56], in_=gbk[:, 0:16], func=AF.Exp, scale=-1.0), reads=["bank0"], writes=["gs"])
            for cc in range(16):
                if not full and cc < 8:
                    continue
                a_ = cacc[cc % 4]
                an = "cacc%d" % (cc % 4)
                p.op("act", lambda e, a_=a_, q_=q_, cc=cc: e.activation(out=a_[:], in_=q_[:, cc, 0:128], func=AF.Identity, scale=CW[:, cc, 0:1], bias=CB[:, cc:cc + 1]),
                     reads=["qkp%d" % b, "CW", "CB"], writes=[an])
                for j in range(1, 4):
                    p.op("dve", lambda e, a_=a_, q_=q_, cc=cc, j=j: e.scalar_tensor_tensor(out=a_[:], in0=q_[:, cc, j:j + 128], scalar=CW[:, cc, j:j + 1], in1=a_[:], op0=ALU.mult, op1=ALU.add),
                         reads=["qkp%d" % b, an], writes=[an])
                p.op("act", lambda e, a_=a_, cc=cc: e.activation(out=qs[:, cc, :], in_=a_[:], func=AF.Silu), reads=[an], writes=["qs%d" % cc])
            for h in range(8):
                s4 = (h % 2) * 4
                bS, bT, bA, bU = cx.bank[s4], cx.bank[s4 + 1], cx.bank[s4 + 2], cx.bank[s4 + 3]
                nS, nT, nA, nU = ["bank%d" % (s4 + i) for i in range(4)]
                kT = qs[:, 8 + h, :]
                qT = qs[:, h, :]
                pt = PTm[h % 2]
                kp = Kp[h % 2]
                r = rr[h % 4]
                rn = "rr%d" % (h % 4)
                if full:
                    p.op("pe", lambda e, bS=bS, kT=kT, qT=qT: e.matmul(bS[:, 0:128], lhsT=kT, rhs=qT, start=True, stop=True), reads=["qs%d" % (8 + h), "qs%d" % h], writes=[nS])
                    p.op("dve", lambda e, bS=bS, pt=pt, h=h: e.scalar_tensor_tensor(out=pt[:], in0=bS[:, 0:128], scalar=gs[:, 32 + h:33 + h], in1=cf[:, C_CM:C_CM + 128], op0=ALU.mult, op1=ALU.mult),
                         reads=[nS, "gs"], writes=["PTm%d" % (h % 2)])
                bTv = bT[:].bitcast(BF16)
                p.op("pe", lambda e, bTv=bTv, kT=kT: e.transpose(out=bTv[:, 0:128], in_=kT, identity=cb[:, C_ID:C_ID + 128]), reads=["qs%d" % (8 + h)], writes=[nT])
                p.op("act", lambda e, bTv=bTv, kp=kp, h=h: e.activation(out=kp[:], in_=bTv[:, 0:128], func=AF.Copy, scale=gs[:, 32 + h:33 + h]), reads=[nT, "gs"], writes=["Kp%d" % (h % 2)])
                if full:
                    p.op("pe", lambda e, bA=bA, pt=pt, v_=v_, h=h: e.matmul(bA[:, 0:257], lhsT=pt[:], rhs=v_[:, h, :], start=True, stop=False), reads=["PTm%d" % (h % 2), "vt%d" % b], writes=[nA])
                    p.op("pe", lambda e, bA=bA, qT=qT, h=h: e.matmul(bA[:, 0:257], lhsT=qT, rhs=Cb[:, h, :], start=False, stop=True), reads=["qs%d" % h, "Cstb%d" % h], writes=[nA])
                p.op("pe", lambda e, bU=bU, kp=kp, v_=v_, h=h: e.matmul(bU[:, 0:257], lhsT=kp[:], rhs=v_[:, h, :], start=True, stop=True), reads=["Kp%d" % (h % 2), "vt%d" % b], writes=[nU])
                tc_ = tmpC[h % 2]
                p.op("dve", lambda e, bU=bU, tc_=tc_, h=h: e.tensor_tensor(out=tc_[:], in0=bU[:, 0:257], in1=C[:, h, :], op=ALU.add), reads=[nU, "Cst%d" % h, nA], writes=["tmpC%d" % (h % 2)])
                p.op("dve", lambda e, tc_=tc_, h=h: e.tensor_scalar(out=C[:, h, :], in0=tc_[:], scalar1=gs[:, 48 + h:49 + h], scalar2=None, op0=ALU.mult), reads=["tmpC%d" % (h % 2), "gs"], writes=["Cst%d" % h])
                p.op("act", lambda e, h=h: e.activation(out=Cb[:, h, :], in_=C[:, h, :], func=AF.Copy), reads=["Cst%d" % h], writes=["Cstb%d" % h])
                if full:
                    o_h = ho[h % 2]
                    on = "ho%d" % (h % 2)
                    hn_ = hn[b]
                    p.op("dve", lambda e, bA=bA, r=r, h=h: e.tensor_tensor(out=r[:, 0:1], in0=bA[:, 256:257], in1=gs[:, 40 + h:41 + h], op=ALU.mult), reads=[nA, "gs"], writes=[rn])
                    p.op("dve", lambda e, r=r: e.tensor_scalar(out=r[:, 2:3], in0=r[:, 0:1], scalar1=-1.0, scalar2=None, op0=ALU.mult), reads=[rn], writes=[rn])
                    p.op("dve", lambda e, r=r: e.tensor_scalar(out=r[:, 1:2], in0=r[:, 0:1], scalar1=r[:, 2:3], scalar2=1.0, op0=ALU.max, op1=ALU.max), reads=[rn], writes=[rn])
                    p.op("dve", lambda e, r=r: e.reciprocal(out=r[:, 2:3], in_=r[:, 1:2]), reads=[rn], writes=[rn])
                    p.op("dve", lambda e, r=r, h=h: e.tensor_tensor(out=r[:, 3:4], in0=r[:, 2:3], in1=gs[:, 40 + h:41 + h], op=ALU.mult), reads=[rn, "gs"], writes=[rn])
                    p.op("act", lambda e, bA=bA, o_h=o_h, r=r: e.activation(out=o_h[:], in_=bA[:, 0:256], func=AF.Copy, scale=r[:, 3:4]), reads=[nA, rn], writes=[on])
                    p.op("act", lambda e, o_h=o_h, r=r: e.activation(out=junk[:], in_=o_h[:], func=AF.Square, accum_out=r[:, 4:5]), reads=[on], writes=["mjunk", rn])
                    p.op("dve", lambda e, r=r: e.tensor_scalar(out=r[:, 5:6], in0=r[:, 4:5], scalar1=1.0 / 256, scalar2=EPS, op0=ALU.mult, op1=ALU.add), reads=[rn], writes=[rn])
                    p.op("act", lambda e, r=r: e.activation(out=r[:, 6:7], in_=r[:, 5:6], func=AF.Sqrt), reads=[rn], writes=[rn])
                    p.op("dve", lambda e, r=r: e.reciprocal(out=r[:, 7:8], in_=r[:, 6:7]), reads=[rn], writes=[rn])
                    p.op("dve", lambda e, o_h=o_h, r=r, h=h: e.scalar_tensor_tensor(out=o_h[:], in0=o_h[:], scalar=r[:, 7:8], in1=HGm[:, h * 256:(h + 1) * 256], op0=ALU.mult, op1=ALU.mult),
                         reads=[on, rn, "HGm"], writes=[on])
                    p.op("pool", lambda e, o_h=o_h, hn_=hn_, h=h: e.tensor_tensor(out=hn_[:, h * 256:(h + 1) * 256], in0=o_h[:], in1=sig[:, h * 256:(h + 1) * 256], op=ALU.mult),
                         reads=[on, "sig"], writes=["hn%d_%d" % (b, h)])
            if full:
                p.dma("sp", lambda e, b=b, c=c: e.dma_start(out=cx.HN.ap()[c * 128:(c + 1) * 128, :], in_=hn[b][:]), reads=["hn%d_%d" % (b, h) for h in range(8)], writes=["hnd%d" % c])
        if st_out is not None:
            so = cx.STATE_OUT.ap() if st_out == "dram" else cx.ST_LOC.ap()[:, 0:8 * 257].rearrange("p (h e) -> p h e", h=8)
            ho_ = cx.HALO_OUT.ap() if st_out == "dram" else cx.ST_LOC.ap()[:, 8 * 257:8 * 257 + 48].rearrange("p (c t) -> p c t", c=16)
            p.dma("sp", lambda e: e.dma_start(out=so, in_=C[:]), reads=["Cst%d" % h for h in range(8)], writes=["so"])
            p.dma("sp", lambda e: e.dma_start(out=hlb[:], in_=qkv[:, :, TOK:TOK + 3], allow_slow_non_contiguous=True), reads=["halo"], writes=["hlb"])
            p.op("dve", lambda e: e.tensor_copy(out=hl[:], in_=hlb[:]), reads=["hlb"], writes=["hl"])
            p.dma("sp", lambda e: e.dma_start(out=ho_, in_=hl[:]), reads=["hl"], writes=["ho_"])
        p.emit()


def mlstm_layer(p, cx, XIN, XOUT, full=True, fused=False):
    with ExitStack() as st1:
        G, SH, GT = load_mod_tiles(p, cx, st1, 1, 0, cx.NORM_MIX_G)
        hT = p.sb(st1, "hT", [128, 16, TOK], BF16)
        norm_to_hT(p, cx, XIN.ap(), G, SH, hT)
        mlstm_proj(p, cx, hT, full)
    if fused:
        mlstm_rec(p, cx, False, st_in="zero", st_out="loc")
        p.coll(lambda e: e.collective_compute("AllGather", ALU.bypass, replica_groups=REPLICA,
                                              ins=[cx.ST_LOC.ap()], outs=[cx.ST_ALL.ap()]), writes=["ST_ALL"])
        p.emit()
        mlstm_rec(p, cx, True, st_in="gather", st_out=None)
    else:
        mlstm_rec(p, cx, full)
    if not full:
        return
    with ExitStack() as st2:
        GT = p.sb(st2, "GT", [128, D], F32)
        p.dma("sp", lambda e: e.dma_start(out=GT[:], in_=bc(cx.MODD.ap()[1, 2 * D:3 * D])), writes=["GT"])
        proj_out_stage(p, cx, cx.HN.ap(), cx.ML_W_OUT.ap(), GT, XIN.ap(), XOUT.ap())


def _common(nc, cx, st):
    cx.nc = nc
    cx.CONSTS = nc.dram_tensor("CONSTS", [128, C_W], F32, kind="ExternalInput")
    cx.NORM_MIX_G = nc.dram_tensor("NORM_MIX_G", [2, D], F32, kind="ExternalInput")
    cx.NORM_FFN_G = nc.dram_tensor("NORM_FFN_G", [2, D], F32, kind="ExternalInput")
    cx.WR = nc.dram_tensor("WR", [2, 128, 16, 36], F32, kind="ExternalInput")
    cx.BRT = nc.dram_tensor("BRT", [2, 36], F32, kind="ExternalInput")
    cx.XS = nc.dram_tensor("XS", [NSLOT + TOK, D], BF16, kind="Internal")
    cx.YS = nc.dram_tensor("YS", [NSLOT + TOK, D], F32, kind="Internal")
    p = Prog(nc, st)
    cx.bank = [st.enter_context(nc.psum_tensor("bank%d" % i, [128, 512], F32)) for i in range(8)]
    load_consts(p, cx, st)
    return p


def build_A():
    nc = bass.Bass("TRN2", target_bir_lowering=False)
    cx = Ctx()
    with ExitStack() as st:
        p = _common(nc, cx, st)
        cx.WL = lambda L: 0
        cx.MODD = nc.dram_tensor("MODD", [2, 6 * D], F32, kind="ExternalOutput")
        cx.CVT = nc.dram_tensor("CVT", [128, 16], F32, kind="ExternalInput")
        cx.ADA_W = nc.dram_tensor("ADA_W", [2, D, 6 * D], F32, kind="ExternalInput")
        cx.ADA_B = nc.dram_tensor("ADA_B", [2, 6 * D], F32, kind="ExternalInput")
        cx.ATTN_W_IN = nc.dram_tensor("ATTN_W_IN", [D, 3 * D], F32, kind="ExternalInput")
        cx.ATTN_W_OUT = nc.dram_tensor("ATTN_W_OUT", [D, D], F32, kind="ExternalInput")
        cx.LQ1 = nc.dram_tensor("LQ1", [1, 128], F32, kind="ExternalInput")
        cx.LK1 = nc.dram_tensor("LK1", [1, 128], F32, kind="ExternalInput")
        cx.LQ2 = nc.dram_tensor("LQ2", [1, 128], F32, kind="ExternalInput")
        cx.LK2 = nc.dram_tensor("LK2", [1, 128], F32, kind="ExternalInput")
        cx.ATTN_HG = nc.dram_tensor("ATTN_HG", [1, 256], F32, kind="ExternalInput")
        cx.PREVFLAG = nc.dram_tensor("PREVFLAG", [128, 1], F32, kind="ExternalInput")
        cx.POS_OWN = nc.dram_tensor("POS_OWN", [1, TOK], I32, kind="ExternalInput")
        cx.POS_PREV = nc.dram_tensor("POS_PREV", [1, TOK], I32, kind="ExternalInput")
        cx.W_GU = nc.dram_tensor("W_GU", [1, NE, D, 2 * HID], F32, kind="ExternalInput")
        cx.W_DN = nc.dram_tensor("W_DN", [1, NE, HID, D], F32, kind="ExternalInput")
        XIN = nc.dram_tensor("XIN", [TOK, D], F32, kind="ExternalInput")
        XPREV = nc.dram_tensor("XPREV", [TOK, D], F32, kind="ExternalInput")
        X1 = nc.dram_tensor("X1", [TOK, D], F32, kind="ExternalOutput")
        XMID = nc.dram_tensor("XMID", [TOK, D], F32, kind="Internal")
        cx.QTD = nc.dram_tensor("QTD", [16, 128, TOK], BF16, kind="Internal")
        cx.KTD = nc.dram_tensor("KTD", [16, 128, 2 * TOK], BF16, kind="Internal")
        cx.VD = nc.dram_tensor("VD", [2 * TOK, 8, 257], BF16, kind="Internal")
        cx.OAD = nc.dram_tensor("OAD", [TOK, D], BF16, kind="Internal")
        mod_stage(p, cx, 0)
        mod_stage(p, cx, 1)
        attn_layer(p, cx, XIN, XPREV, XMID)
        moe_stage(p, cx, 0, XMID, X1)
    return nc


def build_B(full=True):
    nc = bass.Bass("TRN2", target_bir_lowering=False)
    cx = Ctx()
    with ExitStack() as st:
        p = _common(nc, cx, st)
        cx.WL = lambda L: 0
        cx.MODD = nc.dram_tensor("MODD", [2, 6 * D], F32, kind="ExternalInput")
        cx.ML_W_IN = nc.dram_tensor("ML_W_IN", [D, 3 * D + 16], F32, kind="ExternalInput")
        cx.CONV_W = nc.dram_tensor("CONV_W", [128, 16, 4], F32, kind="ExternalInput")
        cx.CONV_B = nc.dram_tensor("CONV_B", [128, 16], F32, kind="ExternalInput")
        cx.GATE_B = nc.dram_tensor("GATE_B", [1, 16], F32, kind="ExternalInput")
        cx.ML_HG = nc.dram_tensor("ML_HG", [1, D], F32, kind="ExternalInput")
        if full:
            cx.ML_W_OUT = nc.dram_tensor("ML_W_OUT", [D, D], F32, kind="ExternalInput")
            cx.FINAL_G = nc.dram_tensor("FINAL_G", [D], F32, kind="ExternalInput")
            cx.W_GU = nc.dram_tensor("W_GU", [1, NE, D, 2 * HID], F32, kind="ExternalInput")
            cx.W_DN = nc.dram_tensor("W_DN", [1, NE, HID, D], F32, kind="ExternalInput")
            OUT = nc.dram_tensor("OUT", [TOK, D], F32, kind="ExternalOutput")
        cx.STATE_IN = nc.dram_tensor("STATE_IN", [128, 8, 257], F32, kind="ExternalInput")
        cx.HALO_IN = nc.dram_tensor("HALO_IN", [128, 16, 3], F32, kind="ExternalInput")
        cx.STATE_OUT = nc.dram_tensor("STATE_OUT", [128, 8, 257], F32, kind="ExternalOutput")
        cx.HALO_OUT = nc.dram_tensor("HALO_OUT", [128, 16, 3], F32, kind="ExternalOutput")
        XIN = nc.dram_tensor("XIN", [TOK, D], F32, kind="ExternalInput")
        XMID = nc.dram_tensor("XMID", [TOK, D], F32, kind="Internal")
        cx.QKP = nc.dram_tensor("QKP", [16, 128, 3 + TOK], BF16, kind="Internal")
        cx.VM = nc.dram_tensor("VM", [TOK, 8, 257], BF16, kind="Internal")
        cx.OPRE = nc.dram_tensor("OPRE", [TOK, D], BF16, kind="Internal")
        cx.GATES = nc.dram_tensor("GATES", [TOK, 16], F32, kind="Internal")
        cx.HN = nc.dram_tensor("HN", [TOK, D], BF16, kind="Internal")
        mlstm_layer(p, cx, XIN, XMID, full)
        if full:
            moe_stage(p, cx, 1, XMID, OUT, final_g=cx.FINAL_G)
    return nc


def build_fused():
    nc = bass.Bass("TRN2", target_bir_lowering=False)
    cx = Ctx()
    with ExitStack() as st:
        p = _common(nc, cx, st)
        cx.WL = lambda L: L
        ei = lambda name, shape, dt=F32: nc.dram_tensor(name, list(shape), dt, kind="ExternalInput")
        it = lambda name, shape, dt=F32: nc.dram_tensor(name, list(shape), dt, kind="Internal")
        cx.MODD = it("MODD", [2, 6 * D])
        cx.CVT = ei("CVT", [128, 16])
        cx.ADA_W = ei("ADA_W", [2, D, 6 * D])
        cx.ADA_B = ei("ADA_B", [2, 6 * D])
        cx.ATTN_W_IN = ei("ATTN_W_IN", [D, 3 * D])
        cx.ATTN_W_OUT = ei("ATTN_W_OUT", [D, D])
        cx.LQ1 = ei("LQ1", [1, 128]); cx.LK1 = ei("LK1", [1, 128]); cx.LQ2 = ei("LQ2", [1, 128]); cx.LK2 = ei("LK2", [1, 128])
        cx.ATTN_HG = ei("ATTN_HG", [1, 256])
        cx.PREVFLAG = ei("PREVFLAG", [128, 1])
        cx.ODDFLAG = ei("ODDFLAG", [128, 1])
        cx.POS_OWN = ei("POS_OWN", [1, TOK], I32)
        cx.POS_PREV = ei("POS_PREV", [1, TOK], I32)
        cx.W_GU = ei("W_GU", [2, NE, D, 2 * HID])
        cx.W_DN = ei("W_DN", [2, NE, HID, D])
        cx.ML_W_IN = ei("ML_W_IN", [D, 3 * D + 16])
        cx.ML_W_OUT = ei("ML_W_OUT", [D, D])
        cx.CONV_W = ei("CONV_W", [128, 16, 4]); cx.CONV_B = ei("CONV_B", [128, 16]); cx.GATE_B = ei("GATE_B", [1, 16])
        cx.ML_HG = ei("ML_HG", [1, D]); cx.FINAL_G = ei("FINAL_G", [D])
        XIN = ei("XIN", [TOK, D]); XPREV = ei("XPREV", [TOK, D])
        OUT = nc.dram_tensor("OUT", [TOK, D], F32, kind="ExternalOutput")
        XMID0 = it("XMID0", [TOK, D]); X1 = it("X1", [TOK, D]); XMID1 = it("XMID1", [TOK, D])
        cx.QTD = it("QTD", [16, 128, TOK], BF16); cx.KTD = it("KTD", [16, 128, 2 * TOK], BF16)
        cx.VD = it("VD", [2 * TOK, 8, 257], BF16); cx.OAD = it("OAD", [TOK, D], BF16)
        cx.QKP = it("QKP", [16, 128, 3 + TOK], BF16); cx.VM = it("VM", [TOK, 8, 257], BF16)
        cx.OPRE = it("OPRE", [TOK, D], BF16); cx.GATES = it("GATES", [TOK, 16]); cx.HN = it("HN", [TOK, D], BF16)
        cx.ST_LOC = it("ST_LOC", [128, 8 * 257 + 48]); cx.ST_ALL = it("ST_ALL", [256, 8 * 257 + 48])
        S = STAGES or ("mod", "attn", "moe0", "ml", "moe1")
        if "mod" in S:
            mod_stage(p, cx, 0)
            mod_stage(p, cx, 1)
        if "attn" in S:
            attn_layer(p, cx, XIN, XPREV, XMID0)
        if "moe0" in S:
            moe_stage(p, cx, 0, XMID0, X1)
        if "ml" in S:
            mlstm_layer(p, cx, X1, XMID1, True, fused=True)
        if "moe1" in S:
            moe_stage(p, cx, 1, XMID1, OUT, final_g=cx.FINAL_G)
    return nc


def kernel(**inp):
    f32 = np.float32
    x = np.asarray(inp["x"], f32)
    c = np.asarray(inp["c"], f32)
    pos = np.asarray(inp["positions"], np.int32)
    wr = np.concatenate([inp["moe_w_group"], inp["moe_w_expert"]], axis=-1).reshape(2, 16, 128, 36).transpose(0, 2, 1, 3)
    wr = np.ascontiguousarray(wr, f32)
    brt = np.ascontiguousarray(np.concatenate([inp["moe_b_group"], inp["moe_b_expert"]], axis=-1), f32)
    cw = np.ascontiguousarray(np.asarray(inp["mlstm_conv_w"][0], f32).reshape(4, 16, 128).transpose(2, 1, 0))
    cbias = np.ascontiguousarray(np.asarray(inp["mlstm_conv_b"][0], f32).reshape(16, 128).T)
    n = 8
    common = {"CONSTS": make_consts(), "NORM_MIX_G": np.ascontiguousarray(inp["norm_mix_g"], f32), "NORM_FFN_G": np.ascontiguousarray(inp["norm_ffn_g"], f32),
              "WR": wr, "BRT": brt, "ADA_W": inp["ada_w"], "ADA_B": inp["ada_b"],
              "ATTN_W_IN": inp["attn_w_in"][0], "ATTN_W_OUT": inp["attn_w_out"][0],
              "LQ1": inp["attn_lambda_q1"], "LK1": inp["attn_lambda_k1"], "LQ2": inp["attn_lambda_q2"], "LK2": inp["attn_lambda_k2"],
              "ATTN_HG": inp["attn_head_norm_g"], "W_GU": inp["moe_w_gu"], "W_DN": inp["moe_w_down"],
              "ML_W_IN": inp["mlstm_w_in"][0], "ML_W_OUT": inp["mlstm_w_out"][0], "CONV_W": cw, "CONV_B": cbias,
              "GATE_B": inp["mlstm_gate_b"], "ML_HG": inp["mlstm_head_norm_g"], "FINAL_G": inp["final_norm_g"]}
    zx = np.zeros((TOK, D), f32)
    zp = np.zeros((1, TOK), np.int32)
    maps = []
    for core in range(n):
        b, hf = core // 2, core % 2
        sl = slice(hf * TOK, (hf + 1) * TOK)
        m = dict(common)
        m.update({
            "CVT": np.ascontiguousarray(c[b].reshape(16, 128).T),
            "PREVFLAG": np.full((128, 1), 0.0 if hf == 1 else -30000.0, f32),
            "ODDFLAG": np.full((128, 1), float(hf), f32),
            "POS_OWN": np.ascontiguousarray(pos[b, sl].reshape(1, TOK)),
            "POS_PREV": np.ascontiguousarray(pos[b, :TOK].reshape(1, TOK)) if hf == 1 else zp,
            "XIN": np.ascontiguousarray(x[b, sl]),
            "XPREV": np.ascontiguousarray(x[b, :TOK]) if hf == 1 else zx,
        })
        maps.append(m)
    nc = build_fused()
    if NCORES_DEBUG:
        res = run_bass_kernel_spmd(nc, maps[:NCORES_DEBUG], core_ids=list(range(NCORES_DEBUG))).results
        return res
    res = run_bass_kernel_spmd(nc, maps, core_ids=list(range(n))).results
    out = np.empty((4, 2 * TOK, D), f32)
    for core in range(n):
        b, hf = core // 2, core % 2
        out[b, hf * TOK:(hf + 1) * TOK] = res[core]["OUT"]
    return out
```

```python
import math
from contextlib import ExitStack
import numpy as np
import concourse.bass as bass
import concourse.mybir as mybir
from concourse.bass_utils import run_bass_kernel_spmd

F32 = mybir.dt.float32
BF16 = mybir.dt.bfloat16
I32 = mybir.dt.int32
AF = mybir.ActivationFunctionType
ALU = mybir.AluOpType
AX = mybir.AxisListType

D = 2048
TOK = 2048
NT = TOK // 128
NE = 32
CAP = 512
NBLK = CAP // 128
NSLOT = NE * CAP
HID = 1024
EPS = 1e-6

ENGS = ("pe", "act", "dve", "pool", "sp")
SAME_ENG_SYNC = True
N_DMA_SEMS = 40
REPLICA = [[0, 1], [2, 3], [4, 5], [6, 7]]
NCORES_DEBUG = 0
STAGES = None


class Prog:
    def __init__(self, nc, stack):
        self.nc = nc
        self.stack = stack
        self.cnt = {e: 0 for e in ENGS}
        self.esem = {e: stack.enter_context(nc.semaphore("s_" + e)) for e in ENGS}
        self.dsem = [stack.enter_context(nc.semaphore("d%d" % i)) for i in range(N_DMA_SEMS)]
        self.dval = [0] * N_DMA_SEMS
        self.drr = 0
        self.csem = stack.enter_context(nc.semaphore("csem"))
        self.cval = 0
        self.known = {e: {} for e in ENGS}
        self._reset()
        self.uid = 0

    def _reset(self):
        self.ops = {e: [] for e in ENGS}
        self.last_w = {}
        self.readers = {}

    def sb(self, st, name, shape, dt):
        self.uid += 1
        return st.enter_context(self.nc.sbuf_tensor("%s_%d" % (name, self.uid), list(shape), dt))

    def _deps(self, eng, reads, writes):
        deps = []
        for r in reads:
            t = self.last_w.get(r)
            if t is not None:
                deps.append(t)
        for w in writes:
            t = self.last_w.get(w)
            if t is not None:
                deps.append(t)
            deps.extend(self.readers.get(w, ()))
        waits = []
        kn = self.known[eng]
        for (sem, val, deng, sid) in deps:
            if deng == eng and (eng == "pe" or not SAME_ENG_SYNC):
                continue
            if kn.get(sid, 0) >= val:
                continue
            kn[sid] = val
            waits.append((sem, val))
        return waits

    def _commit(self, tok, reads, writes):
        for w in writes:
            self.last_w[w] = tok
            self.readers[w] = []
        for r in reads:
            if r in writes:
                continue
            lst = self.readers.setdefault(r, [])
            lst.append(tok)
            if len(lst) > 48:
                latest = {}
                keep = []
                for t in lst:
                    if t[2] == "dma":
                        keep.append(t)
                    else:
                        latest[t[2]] = t
                self.readers[r] = keep[-40:] + list(latest.values())

    def op(self, eng, fn, reads=(), writes=(), inc=True):
        waits = self._deps(eng, reads, writes)
        if inc:
            self.cnt[eng] += 1
            tok = (self.esem[eng], self.cnt[eng], eng, "e_" + eng)
            self.ops[eng].append((waits, fn, (self.esem[eng], 1)))
        else:
            tok = (self.esem[eng], self.cnt[eng] + 1, eng, "e_" + eng)
            self.ops[eng].append((waits, fn, None))
        self._commit(tok, reads, writes)

    def coll(self, fn, reads=(), writes=()):
        waits = self._deps("pool", reads, writes)
        self.cval += 1
        tok = (self.csem, self.cval, "dma", "csem")
        self.ops["pool"].append((waits, fn, (self.csem, 1)))
        self._commit(tok, reads, writes)

    def dma(self, q, fn, reads=(), writes=()):
        s = self.drr
        self.drr = (self.drr + 1) % N_DMA_SEMS
        waits = self._deps(q, reads, writes)
        prev = self.dval[s]
        sid = "d%d" % s
        if prev > 0 and self.known[q].get(sid, 0) < prev:
            self.known[q][sid] = prev
            waits.append((self.dsem[s], prev))
        self.dval[s] += 16
        tok = (self.dsem[s], self.dval[s], "dma", sid)
        self.ops[q].append((waits, fn, (self.dsem[s], 16)))
        self._commit(tok, reads, writes)

    def barrier(self):
        for e in ENGS:
            waits = []
            for e2 in ENGS:
                if e2 != e and self.cnt[e2] > 0 and self.known[e].get("e_" + e2, 0) < self.cnt[e2]:
                    self.known[e]["e_" + e2] = self.cnt[e2]
                    waits.append((self.esem[e2], self.cnt[e2]))
            for i in range(N_DMA_SEMS):
                sid = "d%d" % i
                if self.dval[i] > 0 and self.known[e].get(sid, 0) < self.dval[i]:
                    self.known[e][sid] = self.dval[i]
                    waits.append((self.dsem[i], self.dval[i]))
            if self.cval > 0 and self.known[e].get("csem", 0) < self.cval:
                self.known[e]["csem"] = self.cval
                waits.append((self.csem, self.cval))
            if waits:
                self.ops[e].append((waits, None, None))

    def emit(self):
        self.barrier()
        ops = self.ops
        with self.nc.Block() as block:
            def run(engname):
                def body(e):
                    for (waits, fn, inc) in ops[engname]:
                        for (sem, val) in waits:
                            e.wait_ge(sem, val)
                        if fn is not None:
                            ins = fn(e)
                            if inc is not None:
                                ins.then_inc(inc[0], inc[1])
                return body
            block.tensor(run("pe"))
            block.scalar(run("act"))
            block.vector(run("dve"))
            block.gpsimd(run("pool"))
            block.sync(run("sp"))
        self._reset()


C_ID, C_LS, C_ON, C_ECAP, C_TRI, C_CM, C_PERM, C_INVF, C_SGN, C_IOTA, C_W = 0, 128, 256, 384, 416, 544, 672, 704, 705, 706, 738


def make_consts():
    c = np.zeros((128, C_W), np.float32)
    i = np.arange(128)
    c[:, C_ID:C_ID + 128] = np.eye(128)
    c[:, C_LS:C_LS + 128] = (i[:, None] < i[None, :])
    c[:, C_ON:C_ON + 128] = 1.0
    c[:, C_ECAP:C_ECAP + 32] = (np.arange(32) * CAP)[None, :]
    c[:, C_TRI:C_TRI + 128] = (i[:, None] <= i[None, :])
    c[:, C_CM:C_CM + 128] = (i[:, None] <= i[None, :])
    for pp in range(32):
        c[(pp + 16) % 32, C_PERM + pp] = 1.0
    half = 16
    invf = 500000.0 ** (-np.arange(half, dtype=np.float32) * 2.0 / 32)
    c[:32, C_INVF] = np.tile(invf, 2)
    c[:16, C_SGN] = -1.0
    c[16:32, C_SGN] = 1.0
    c[:, C_IOTA:C_IOTA + 32] = np.arange(32)[None, :]
    return c


class Ctx:
    pass


def bc(ap1d, n=128):
    return ap1d.partition_broadcast(n)


def load_consts(p, cx, st):
    cx.cf = p.sb(st, "cf", [128, C_W], F32)
    cx.cb = p.sb(st, "cb", [128, C_W], BF16)
    p.dma("sp", lambda e: e.dma_start(out=cx.cf[:], in_=cx.CONSTS.ap()), writes=["cf"])
    p.op("dve", lambda e: e.tensor_copy(out=cx.cb[:], in_=cx.cf[:]), reads=["cf"], writes=["cb"])
    p.emit()


def rms_mod_tile(p, cx, xs, G, SH, hf, hb, ss, nm, rd=(), wr=()):
    junk = cx.junk
    p.op("act", lambda e: e.activation(out=junk[:], in_=xs[:], func=AF.Square, accum_out=ss[:, 0:1]),
         reads=[nm + "xs"], writes=["junk", nm + "ss"])
    p.op("dve", lambda e: e.tensor_scalar(out=ss[:, 1:2], in0=ss[:, 0:1], scalar1=1.0 / D, scalar2=EPS, op0=ALU.mult, op1=ALU.add),
         reads=[nm + "ss"], writes=[nm + "ss"])
    p.op("act", lambda e: e.activation(out=ss[:, 2:3], in_=ss[:, 1:2], func=AF.Sqrt), reads=[nm + "ss"], writes=[nm + "ss"])
    p.op("dve", lambda e: e.reciprocal(out=ss[:, 3:4], in_=ss[:, 2:3]), reads=[nm + "ss"], writes=[nm + "ss"])
    tmp = cx.tmpf
    p.op("dve", lambda e: e.scalar_tensor_tensor(out=tmp[:], in0=xs[:], scalar=ss[:, 3:4], in1=G[:], op0=ALU.mult, op1=ALU.mult),
         reads=[nm + "xs", nm + "ss"] + list(rd), writes=["tmpf"])
    if hf is not None:
        p.op("pool", lambda e: e.tensor_tensor(out=hf[:], in0=tmp[:], in1=SH[:], op=ALU.add), reads=["tmpf"] + list(rd), writes=[nm + "hf"])
        p.op("act", lambda e: e.activation(out=hb[:], in_=hf[:], func=AF.Copy), reads=[nm + "hf"], writes=[nm + "hb"])
    else:
        p.op("pool", lambda e: e.tensor_tensor(out=hb[:], in0=tmp[:], in1=SH[:], op=ALU.add), reads=["tmpf"] + list(rd), writes=[nm + "hb"])


def load_mod_tiles(p, cx, st, L, which, gname):
    base = 3 * D * which
    G = p.sb(st, "G", [128, D], F32)
    SH = p.sb(st, "SH", [128, D], F32)
    GT = p.sb(st, "GT", [128, D], F32)
    gn = p.sb(st, "gn", [128, D], F32)
    md = cx.MODD.ap()
    p.dma("sp", lambda e: e.dma_start(out=SH[:], in_=bc(md[L, base:base + D])), reads=["MODD"], writes=["SH"])
    p.dma("act", lambda e: e.dma_start(out=G[:], in_=bc(md[L, base + D:base + 2 * D])), reads=["MODD"], writes=["G"])
    p.dma("sp", lambda e: e.dma_start(out=GT[:], in_=bc(md[L, base + 2 * D:base + 3 * D])), reads=["MODD"], writes=["GT"])
    p.dma("act", lambda e: e.dma_start(out=gn[:], in_=bc(gname.ap()[L, :])), writes=["gn"])
    p.op("dve", lambda e: e.scalar_tensor_tensor(out=G[:], in0=G[:], scalar=1.0, in1=gn[:], op0=ALU.add, op1=ALU.mult),
         reads=["G", "gn"], writes=["G"])
    return G, SH, GT


def mod_stage(p, cx, L):
    with ExitStack() as st:
        cT = p.sb(st, "cT", [128, 16], F32)
        cTb = p.sb(st, "cTb", [128, 16], BF16)
        ring = [p.sb(st, "aw%d" % i, [128, 4096], BF16) for i in range(4)]
        row = p.sb(st, "mrow", [1, 4096], F32)
        brow = p.sb(st, "brow", [1, 4096], F32)
        p.dma("sp", lambda e: e.dma_start(out=cT[:], in_=cx.CVT.ap()), writes=["cT"])
        p.op("act", lambda e: e.activation(out=cTb[:], in_=cT[:], func=AF.Silu), reads=["cT"], writes=["cTb"])
        aw = cx.ADA_W.ap()
        k = 0
        for ps_ in range(3):
            p.dma("sp", lambda e, ps_=ps_: e.dma_start(out=brow[:], in_=cx.ADA_B.ap()[L:L + 1, ps_ * 4096:(ps_ + 1) * 4096]), writes=["brow"])
            for kc in range(16):
                buf = ring[k % 4]
                bn = "aw%d" % (k % 4)
                k += 1
                p.dma("pool", lambda e, buf=buf, kc=kc, ps_=ps_: e.dma_start(out=buf[:], in_=aw[L, kc * 128:(kc + 1) * 128, ps_ * 4096:(ps_ + 1) * 4096]),
                      writes=[bn])
                for n in range(8):
                    p.op("pe", lambda e, buf=buf, kc=kc, n=n: e.matmul(cx.bank[n][0:1, :], lhsT=cTb[:, kc:kc + 1], rhs=buf[:, n * 512:(n + 1) * 512],
                                                                      start=(kc == 0), stop=(kc == 15)),
                         reads=[bn, "cTb"], writes=["bank%d" % n], inc=(n == 7))
            for n in range(8):
                p.op("dve", lambda e, n=n: e.tensor_tensor(out=row[:, n * 512:(n + 1) * 512], in0=cx.bank[n][0:1, :], in1=brow[:, n * 512:(n + 1) * 512], op=ALU.add),
                     reads=["bank%d" % n, "brow"], writes=["mrow"])
            p.dma("sp", lambda e, ps_=ps_: e.dma_start(out=cx.MODD.ap()[L:L + 1, ps_ * 4096:(ps_ + 1) * 4096], in_=row[:]), reads=["mrow"], writes=["MODD"])
        p.emit()


def moe_stage(p, cx, L, XIN, XOUT, final_g=None):
    with ExitStack() as st0:
        d1i = p.sb(st0, "d1i", [128, NT], I32)
        d2i = p.sb(st0, "d2i", [128, NT], I32)
        gts = p.sb(st0, "gts", [128, 2 * NT], F32)
        xin = XIN.ap()
        with ExitStack() as st:
            G, SH, GT_ = load_mod_tiles(p, cx, st, L, 1, cx.NORM_FFN_G)
            cf, cb = cx.cf, cx.cb
            xs2 = [p.sb(st, "xs%d" % i, [128, D], F32) for i in range(2)]
            hf2 = [p.sb(st, "hf%d" % i, [128, D], F32) for i in range(2)]
            hb2 = [p.sb(st, "hb%d" % i, [128, D], BF16) for i in range(2)]
            cx.junk = p.sb(st, "junk", [128, D], BF16)
            cx.tmpf = p.sb(st, "tmpf", [128, D], F32)
            hT = p.sb(st, "hTf", [128, 16, 128], F32)
            wr = p.sb(st, "wr", [128, 16, 36], F32)
            br = p.sb(st, "br", [128, 36], F32)
            macc = p.sb(st, "macc", [128, 32], BF16)
            sm = [p.sb(st, "sm%d" % i, [128, 160], F32) for i in range(2)]
            mk = [p.sb(st, "mk%d" % i, [128, 32], BF16) for i in range(2)]
            p.dma("sp", lambda e: e.dma_start(out=wr[:], in_=cx.WR.ap()[L]), writes=["wr"])
            p.dma("sp", lambda e: e.dma_start(out=br[:], in_=bc(cx.BRT.ap()[L, :])), writes=["br"])
            p.op("dve", lambda e: e.memset(macc[:], 0.0), writes=["macc"])
            for t in range(NT):
                b = t % 2
                nm = "r%d" % b
                xs, hf, hb, s, m = xs2[b], hf2[b], hb2[b], sm[b], mk[b]
                p.dma("sp", lambda e, xs=xs, t=t: e.dma_start(out=xs[:], in_=xin[t * 128:(t + 1) * 128, :]), reads=["XIN"], writes=[nm + "xs"])
                rms_mod_tile(p, cx, xs, G, SH, hf, hb, s, nm, rd=["G", "SH"])
                for q4 in range(4):
                    for j in range(4):
                        kc = q4 * 4 + j
                        p.op("pe", lambda e, hf=hf, kc=kc, j=j, q4=q4: e.transpose(out=cx.bank[q4][:, j * 128:(j + 1) * 128], in_=hf[:, kc * 128:(kc + 1) * 128], identity=cf[:, C_ID:C_ID + 128]),
                             reads=[nm + "hf"], writes=["bank%d" % q4])
                    eng = "act" if q4 % 2 == 0 else "dve"
                    if eng == "act":
                        p.op("act", lambda e, q4=q4: e.activation(out=hT[:, q4 * 4:(q4 + 1) * 4, :], in_=cx.bank[q4][:].rearrange("p (a b) -> p a b", a=4), func=AF.Copy),
                             reads=["bank%d" % q4], writes=["hT%d" % q4])
                    else:
                        p.op("dve", lambda e, q4=q4: e.tensor_copy(out=hT[:, q4 * 4:(q4 + 1) * 4, :], in_=cx.bank[q4][:].rearrange("p (a b) -> p a b", a=4)),
                             reads=["bank%d" % q4], writes=["hT%d" % q4])
                lgp = cx.bank[4]
                for kc in range(16):
                    p.op("pe", lambda e, kc=kc: e.matmul(lgp[:, 0:36], lhsT=hT[:, kc, :], rhs=wr[:, kc, :], start=(kc == 0), stop=(kc == 15)),
                         reads=["hT%d" % (kc // 4), "wr"], writes=["bank4"], inc=(kc == 15))
                lg = s[:, 8:44]
                sn = nm + "s"
                p.op("dve", lambda e, lg=lg: e.tensor_tensor(out=lg, in0=lgp[:, 0:36], in1=br[:], op=ALU.add), reads=["bank4", "br"], writes=[sn])
                p.op("dve", lambda e, s=s: e.reduce_max(out=s[:, 44:45], in_=s[:, 8:12], axis=AX.X), reads=[sn], writes=[sn])
                p.op("dve", lambda e, s=s: e.tensor_scalar(out=s[:, 45:46], in0=s[:, 44:45], scalar1=-1.0, scalar2=None, op0=ALU.mult), reads=[sn], writes=[sn])
                p.op("dve", lambda e, s=s: e.tensor_scalar(out=s[:, 48:52], in0=s[:, 8:12], scalar1=s[:, 44:45], scalar2=None, op0=ALU.is_equal), reads=[sn], writes=[sn])
                p.op("act", lambda e, s=s: e.activation(out=s[:, 100:104], in_=s[:, 8:12], func=AF.Exp, bias=s[:, 45:46], accum_out=s[:, 46:47]), reads=[sn], writes=[sn])
                p.op("dve", lambda e, s=s: e.reciprocal(out=s[:, 47:48], in_=s[:, 46:47]), reads=[sn], writes=[sn])
                p.op("dve", lambda e, s=s: e.tensor_scalar(out=s[:, 52:56], in0=s[:, 48:52], scalar1=1e30, scalar2=-1e30, op0=ALU.mult, op1=ALU.add), reads=[sn], writes=[sn])
                p.op("dve", lambda e, s=s: e.tensor_tensor(out=s[:, 56:88].rearrange("p (g k) -> p g k", g=4), in0=s[:, 12:44].rearrange("p (g k) -> p g k", g=4),
                                                           in1=s[:, 52:56].unsqueeze(2).to_broadcast([128, 4, 8]), op=ALU.add), reads=[sn], writes=[sn])
                p.op("dve", lambda e, s=s: e.max(out=s[:, 88:96], in_=s[:, 56:88]), reads=[sn], writes=[sn])
                p.op("dve", lambda e, s=s: e.tensor_tensor(out=s[:, 96:97], in0=s[:, 89:90], in1=s[:, 88:89], op=ALU.subtract), reads=[sn], writes=[sn])
                p.op("act", lambda e, s=s: e.activation(out=s[:, 97:98], in_=s[:, 96:97], func=AF.Exp), reads=[sn], writes=[sn])
                p.op("dve", lambda e, s=s: e.tensor_scalar(out=s[:, 98:99], in0=s[:, 97:98], scalar1=1.0, scalar2=None, op0=ALU.add), reads=[sn], writes=[sn])
                p.op("dve", lambda e, s=s: e.reciprocal(out=s[:, 99:100], in_=s[:, 98:99]), reads=[sn], writes=[sn])
                p.op("dve", lambda e, s=s, t=t: e.tensor_tensor(out=gts[:, 2 * t:2 * t + 1], in0=s[:, 99:100], in1=s[:, 47:48], op=ALU.mult), reads=[sn], writes=["gts"])
                p.op("dve", lambda e, s=s, t=t: e.tensor_tensor(out=gts[:, 2 * t + 1:2 * t + 2], in0=gts[:, 2 * t:2 * t + 1], in1=s[:, 97:98], op=ALU.mult), reads=[sn, "gts"], writes=["gts"])
                p.op("dve", lambda e, s=s: e.tensor_scalar(out=s[:, 100:132], in0=s[:, 56:88], scalar1=s[:, 88:89], scalar2=None, op0=ALU.is_equal), reads=[sn], writes=[sn])
                p.op("dve", lambda e, s=s: e.tensor_scalar(out=s[:, 8:40], in0=s[:, 56:88], scalar1=s[:, 89:90], scalar2=None, op0=ALU.is_equal), reads=[sn], writes=[sn])
                p.op("dve", lambda e, s=s, m=m: e.tensor_tensor(out=m[:], in0=s[:, 100:132], in1=s[:, 8:40], op=ALU.add), reads=[sn], writes=[nm + "mk"])
                pp = cx.bank[5]
                p.op("pe", lambda e, m=m: e.matmul(pp[:, 0:32], lhsT=cb[:, C_LS:C_LS + 128], rhs=m[:], start=True, stop=False), reads=[nm + "mk"], writes=["bank5"])
                p.op("pe", lambda e: e.matmul(pp[:, 0:32], lhsT=cb[:, C_ON:C_ON + 128], rhs=macc[:], start=False, stop=True), reads=["macc"], writes=["bank5"])
                p.op("dve", lambda e, s=s: e.tensor_tensor(out=s[:, 56:88], in0=pp[:, 0:32], in1=cf[:, C_ECAP:C_ECAP + 32], op=ALU.add), reads=["bank5", sn], writes=[sn])
                p.op("dve", lambda e, m=m: e.tensor_tensor(out=macc[:], in0=macc[:], in1=m[:], op=ALU.add), reads=["macc", nm + "mk", "bank5"], writes=["macc"])
                p.op("dve", lambda e, s=s: e.tensor_tensor(out=s[:, 100:132], in0=s[:, 100:132], in1=s[:, 56:88], op=ALU.mult), reads=[sn], writes=[sn])
                p.op("dve", lambda e, s=s: e.reduce_sum(out=s[:, 132:133], in_=s[:, 100:132], axis=AX.X), reads=[sn], writes=[sn])
                p.op("dve", lambda e, s=s: e.tensor_tensor(out=s[:, 8:40], in0=s[:, 8:40], in1=s[:, 56:88], op=ALU.mult), reads=[sn], writes=[sn])
                p.op("dve", lambda e, s=s: e.reduce_sum(out=s[:, 133:134], in_=s[:, 8:40], axis=AX.X), reads=[sn], writes=[sn])
                p.op("dve", lambda e, s=s, t=t: e.tensor_copy(out=d1i[:, t:t + 1], in_=s[:, 132:133]), reads=[sn], writes=["d1i"])
                p.op("dve", lambda e, s=s, t=t: e.tensor_copy(out=d2i[:, t:t + 1], in_=s[:, 133:134]), reads=[sn], writes=["d2i"])
                p.dma("pool", lambda e, hb=hb, t=t: e.indirect_dma_start(out=cx.XS.ap(), out_offset=bass.IndirectOffsetOnAxis(ap=d1i[:, t:t + 1], axis=0), in_=hb[:], in_offset=None),
                      reads=[nm + "hb", "d1i"], writes=["XSa%d" % t])
                p.dma("pool", lambda e, hb=hb, t=t: e.indirect_dma_start(out=cx.XS.ap(), out_offset=bass.IndirectOffsetOnAxis(ap=d2i[:, t:t + 1], axis=0), in_=hb[:], in_offset=None),
                      reads=[nm + "hb", "d2i"], writes=["XSb%d" % t])
            p.emit()
        with ExitStack() as st:
            cb = cx.cb
            NWG = 32
            NWD = 12
            wg = [p.sb(st, "wg%d" % i, [128, 2, 512], BF16) for i in range(NWG)]
            wd = [p.sb(st, "wd%d" % i, [128, D], BF16) for i in range(NWD)]
            xg = p.sb(st, "xg", [128, NBLK, D], BF16)
            xT = [p.sb(st, "xT%d" % i, [128, 16, CAP], BF16) for i in range(2)]
            hTb = p.sb(st, "hTb", [128, 8, CAP], BF16)
            sg = [p.sb(st, "sg%d" % i, [128, 256], F32) for i in range(2)]
            yb = [p.sb(st, "yb%d" % i, [128, D], F32) for i in range(2)]
            xsv = cx.XS.ap()[0:NSLOT].rearrange("(e b p) d -> e p b d", b=NBLK, p=128)
            ysv = cx.YS.ap()[0:NSLOT].rearrange("(e b p) d -> e b p d", b=NBLK, p=128)
            wguv = cx.W_GU.ap()
            wdv = cx.W_DN.ap()
            kg = 0
            kd = 0
            ky = 0
            for ex in range(NE):
                xb_ = ex % 2
                xTb = xT[xb_]
                p.dma("sp", lambda e, ex=ex: e.dma_start(out=xg[:], in_=xsv[ex]), writes=["xg"])
                tcnt = 0
                for blk in range(NBLK):
                    for hh in range(2):
                        bk = cx.bank[4 + tcnt % 4]
                        bkn = "bank%d" % (4 + tcnt % 4)
                        tcnt += 1
                        bkv = bk[:].bitcast(BF16)
                        for j in range(8):
                            kc = hh * 8 + j
                            p.op("pe", lambda e, bkv=bkv, j=j, kc=kc, blk=blk: e.transpose(out=bkv[:, j * 128:(j + 1) * 128], in_=xg[:, blk, kc * 128:(kc + 1) * 128], identity=cb[:, C_ID:C_ID + 128]),
                                 reads=["xg"], writes=[bkn], inc=(j == 7))
                        if tcnt % 2 == 0:
                            p.op("act", lambda e, bkv=bkv, hh=hh, blk=blk, xTb=xTb: e.activation(out=xTb[:, hh * 8:(hh + 1) * 8, blk * 128:(blk + 1) * 128], in_=bkv.rearrange("p (a b) -> p a b", a=8), func=AF.Copy),
                                 reads=[bkn], writes=["xT%d" % xb_])
                        else:
                            p.op("dve", lambda e, bkv=bkv, hh=hh, blk=blk, xTb=xTb: e.tensor_copy(out=xTb[:, hh * 8:(hh + 1) * 8, blk * 128:(blk + 1) * 128], in_=bkv.rearrange("p (a b) -> p a b", a=8)),
                                 reads=[bkn], writes=["xT%d" % xb_])
                for hp in range(2):
                    wgl = []
                    for kc in range(16):
                        i = kg % NWG
                        kg += 1
                        wgl.append(i)
                        p.dma("pool", lambda e, i=i, ex=ex, kc=kc, hp=hp: e.dma_start(out=wg[i][:], in_=wguv[cx.WL(L), ex, kc * 128:(kc + 1) * 128, :].rearrange("k (g c) -> k g c", g=2)[:, :, hp * 512:(hp + 1) * 512]),
                              writes=["wg%d" % i])
                    for sb_ in range(CAP // 256):
                        for kc in range(16):
                            i = wgl[kc]
                            for gi in range(2):
                                for j in range(4):
                                    bi = gi * 2 + j // 2
                                    p.op("pe", lambda e, i=i, gi=gi, j=j, bi=bi, kc=kc, xTb=xTb, sb_=sb_: e.matmul(cx.bank[bi][:, (j % 2) * 256:(j % 2 + 1) * 256], lhsT=wg[i][:, gi, j * 128:(j + 1) * 128], rhs=xTb[:, kc, sb_ * 256:(sb_ + 1) * 256],
                                                                                                                  start=(kc == 0 and j % 2 == 0), stop=(kc == 15), skip_group_check=True),
                                         reads=["wg%d" % i, "xT%d" % xb_], writes=["bank%d" % bi], inc=(kc == 15))
                        for j in range(4):
                            s_ = sg[j % 2]
                            p.op("act", lambda e, s_=s_, j=j: e.activation(out=s_[:], in_=cx.bank[j // 2][:, (j % 2) * 256:(j % 2 + 1) * 256], func=AF.Silu),
                                 reads=["bank%d" % (j // 2)], writes=["sg%d" % (j % 2)])
                            p.op("dve", lambda e, s_=s_, j=j, hp=hp, sb_=sb_: e.tensor_tensor(out=hTb[:, hp * 4 + j, sb_ * 256:(sb_ + 1) * 256], in0=s_[:], in1=cx.bank[2 + j // 2][:, (j % 2) * 256:(j % 2 + 1) * 256], op=ALU.mult),
                                 reads=["sg%d" % (j % 2), "bank%d" % (2 + j // 2)], writes=["hTb"])
                wdl = []
                for hc in range(8):
                    i = kd % NWD
                    kd += 1
                    wdl.append(i)
                    p.dma("pool", lambda e, i=i, ex=ex, hc=hc: e.dma_start(out=wd[i][:], in_=wdv[cx.WL(L), ex, hc * 128:(hc + 1) * 128, :]), writes=["wd%d" % i])
                for blk in range(NBLK):
                    y = yb[ky % 2]
                    yn = "yb%d" % (ky % 2)
                    ky += 1
                    for cg in range(4):
                        for hc in range(8):
                            i = wdl[hc]
                            p.op("pe", lambda e, i=i, cg=cg, hc=hc, blk=blk: e.matmul(cx.bank[4 + cg][:], lhsT=hTb[:, hc, blk * 128:(blk + 1) * 128], rhs=wd[i][:, cg * 512:(cg + 1) * 512], start=(hc == 0), stop=(hc == 7)),
                                 reads=["hTb", "wd%d" % i], writes=["bank%d" % (4 + cg)], inc=(hc == 7))
                        if cg % 2 == 0:
                            p.op("act", lambda e, y=y, cg=cg: e.activation(out=y[:, cg * 512:(cg + 1) * 512], in_=cx.bank[4 + cg][:], func=AF.Copy), reads=["bank%d" % (4 + cg)], writes=[yn])
                        else:
                            p.op("dve", lambda e, y=y, cg=cg: e.tensor_copy(out=y[:, cg * 512:(cg + 1) * 512], in_=cx.bank[4 + cg][:]), reads=["bank%d" % (4 + cg)], writes=[yn])
                    p.dma("act", lambda e, y=y, ex=ex, blk=blk: e.dma_start(out=ysv[ex, blk], in_=y[:]), reads=[yn], writes=["YS%d_%d" % (ex, blk)])
            p.emit()
        with ExitStack() as st:
            xs2 = [p.sb(st, "cxs%d" % i, [128, D], F32) for i in range(2)]
            y1 = [p.sb(st, "y1_%d" % i, [128, D], F32) for i in range(2)]
            y2 = [p.sb(st, "y2_%d" % i, [128, D], F32) for i in range(2)]
            GT = p.sb(st, "GT", [128, D], F32)
            p.dma("sp", lambda e: e.dma_start(out=GT[:], in_=bc(cx.MODD.ap()[L, 5 * D:6 * D])), writes=["GT"])
            if final_g is not None:
                FG = p.sb(st, "FG", [128, D], F32)
                p.dma("sp", lambda e: e.dma_start(out=FG[:], in_=bc(final_g.ap()[:])), writes=["FG"])
                cx.junk = p.sb(st, "junk", [128, D], BF16)
                fs = [p.sb(st, "fs%d" % i, [128, 4], F32) for i in range(2)]
            xout = XOUT.ap()
            for t in range(NT):
                b = t % 2
                xs, a1, a2 = xs2[b], y1[b], y2[b]
                p.dma("sp", lambda e, xs=xs, t=t: e.dma_start(out=xs[:], in_=xin[t * 128:(t + 1) * 128, :]), reads=["XIN"], writes=["cxs%d" % b])
                p.dma("pool", lambda e, a1=a1, t=t: e.indirect_dma_start(out=a1[:], out_offset=None, in_=cx.YS.ap(), in_offset=bass.IndirectOffsetOnAxis(ap=d1i[:, t:t + 1], axis=0)),
                      reads=["d1i"], writes=["y1_%d" % b])
                p.dma("pool", lambda e, a2=a2, t=t: e.indirect_dma_start(out=a2[:], out_offset=None, in_=cx.YS.ap(), in_offset=bass.IndirectOffsetOnAxis(ap=d2i[:, t:t + 1], axis=0)),
                      reads=["d2i"], writes=["y2_%d" % b])
                p.op("act", lambda e, a1=a1, t=t: e.activation(out=a1[:], in_=a1[:], func=AF.Copy, scale=gts[:, 2 * t:2 * t + 1]), reads=["y1_%d" % b, "gts"], writes=["y1_%d" % b])
                p.op("dve", lambda e, a1=a1, a2=a2, t=t: e.scalar_tensor_tensor(out=a2[:], in0=a2[:], scalar=gts[:, 2 * t + 1:2 * t + 2], in1=a1[:], op0=ALU.mult, op1=ALU.add),
                     reads=["y1_%d" % b, "y2_%d" % b, "gts"], writes=["y2_%d" % b])
                p.op("pool", lambda e, a2=a2: e.tensor_tensor(out=a2[:], in0=a2[:], in1=GT[:], op=ALU.mult), reads=["y2_%d" % b, "GT"], writes=["y2_%d" % b])
                p.op("dve", lambda e, a2=a2, xs=xs: e.tensor_tensor(out=xs[:], in0=a2[:], in1=xs[:], op=ALU.add), reads=["y2_%d" % b, "cxs%d" % b], writes=["cxs%d" % b])
                if final_g is not None:
                    f = fs[b]
                    p.op("act", lambda e, xs=xs, f=f: e.activation(out=cx.junk[:], in_=xs[:], func=AF.Square, accum_out=f[:, 0:1]), reads=["cxs%d" % b], writes=["junk", "fs%d" % b])
                    p.op("dve", lambda e, f=f: e.tensor_scalar(out=f[:, 1:2], in0=f[:, 0:1], scalar1=1.0 / D, scalar2=EPS, op0=ALU.mult, op1=ALU.add), reads=["fs%d" % b], writes=["fs%d" % b])
                    p.op("act", lambda e, f=f: e.activation(out=f[:, 2:3], in_=f[:, 1:2], func=AF.Sqrt), reads=["fs%d" % b], writes=["fs%d" % b])
                    p.op("dve", lambda e, f=f: e.reciprocal(out=f[:, 3:4], in_=f[:, 2:3]), reads=["fs%d" % b], writes=["fs%d" % b])
                    p.op("dve", lambda e, xs=xs, f=f: e.scalar_tensor_tensor(out=xs[:], in0=xs[:], scalar=f[:, 3:4], in1=FG[:], op0=ALU.mult, op1=ALU.mult),
                         reads=["cxs%d" % b, "fs%d" % b, "FG"], writes=["cxs%d" % b])
                p.dma("sp", lambda e, xs=xs, t=t: e.dma_start(out=xout[t * 128:(t + 1) * 128, :], in_=xs[:]), reads=["cxs%d" % b], writes=["XOUT"])
            p.emit()


def norm_to_hT(p, cx, XSRC, G, SH, hT, tb0=6):
    with ExitStack() as st:
        xs2 = [p.sb(st, "nxs%d" % i, [128, D], F32) for i in range(2)]
        hb2 = [p.sb(st, "nhb%d" % i, [128, D], BF16) for i in range(2)]
        ss2 = [p.sb(st, "nss%d" % i, [128, 4], F32) for i in range(2)]
        cx.junk = p.sb(st, "junk", [128, D], BF16)
        cx.tmpf = p.sb(st, "tmpf", [128, D], F32)
        cb = cx.cb
        for t in range(NT):
            b = t % 2
            nm = "n%d" % b
            xs, hb, ss = xs2[b], hb2[b], ss2[b]
            p.dma("sp", lambda e, xs=xs, t=t: e.dma_start(out=xs[:], in_=XSRC[t * 128:(t + 1) * 128, :]), writes=[nm + "xs"])
            rms_mod_tile(p, cx, xs, G, SH, None, hb, ss, nm, rd=["G", "SH"])
            for hh in range(2):
                bk = cx.bank[tb0 + hh]
                bkn = "bank%d" % (tb0 + hh)
                bkv = bk[:].bitcast(BF16)
                for j in range(8):
                    kc = hh * 8 + j
                    p.op("pe", lambda e, bkv=bkv, j=j, kc=kc, hb=hb: e.transpose(out=bkv[:, j * 128:(j + 1) * 128], in_=hb[:, kc * 128:(kc + 1) * 128], identity=cb[:, C_ID:C_ID + 128]),
                         reads=[nm + "hb"], writes=[bkn], inc=(j == 7))
                if hh == 0:
                    p.op("act", lambda e, bkv=bkv, hh=hh, t=t: e.activation(out=hT[:, hh * 8:(hh + 1) * 8, t * 128:(t + 1) * 128], in_=bkv.rearrange("p (a b) -> p a b", a=8), func=AF.Copy),
                         reads=[bkn], writes=["hT_%d_%d" % (t, hh)])
                else:
                    p.op("dve", lambda e, bkv=bkv, hh=hh, t=t: e.tensor_copy(out=hT[:, hh * 8:(hh + 1) * 8, t * 128:(t + 1) * 128], in_=bkv.rearrange("p (a b) -> p a b", a=8)),
                         reads=[bkn], writes=["hT_%d_%d" % (t, hh)])
        p.emit()


def proj_out_stage(p, cx, SRC, Wap, GT, XIN, XOUT):
    with ExitStack() as st:
        cb = cx.cb
        W = p.sb(st, "Wout", [128, 16, D], BF16)
        wv = Wap.rearrange("(kc k) c -> k kc c", k=128)
        for i in range(4):
            p.dma("pool", lambda e, i=i: e.dma_start(out=W[:, :, i * 512:(i + 1) * 512], in_=wv[:, :, i * 512:(i + 1) * 512]), writes=["Wout%d" % i])
        sr2 = [p.sb(st, "osr%d" % i, [128, D], BF16) for i in range(2)]
        sT2 = [p.sb(st, "osT%d" % i, [128, 16, 128], BF16) for i in range(2)]
        xs2 = [p.sb(st, "oxs%d" % i, [128, D], F32) for i in range(2)]
        tm2 = [p.sb(st, "otm%d" % i, [128, D], F32) for i in range(2)]
        for t in range(NT):
            b = t % 2
            sr, sT, xs, tm = sr2[b], sT2[b], xs2[b], tm2[b]
            p.dma("sp", lambda e, sr=sr, t=t: e.dma_start(out=sr[:], in_=SRC[t * 128:(t + 1) * 128, :]), writes=["osr%d" % b])
            p.dma("act", lambda e, xs=xs, t=t: e.dma_start(out=xs[:], in_=XIN[t * 128:(t + 1) * 128, :]), writes=["oxs%d" % b])
            for hh in range(2):
                bk = cx.bank[4 + hh]
                bkn = "bank%d" % (4 + hh)
                bkv = bk[:].bitcast(BF16)
                for j in range(8):
                    kc = hh * 8 + j
                    p.op("pe", lambda e, bkv=bkv, j=j, kc=kc, sr=sr: e.transpose(out=bkv[:, j * 128:(j + 1) * 128], in_=sr[:, kc * 128:(kc + 1) * 128], identity=cb[:, C_ID:C_ID + 128]),
                         reads=["osr%d" % b], writes=[bkn], inc=(j == 7))
                if hh == 0:
                    p.op("act", lambda e, bkv=bkv, hh=hh, sT=sT: e.activation(out=sT[:, hh * 8:(hh + 1) * 8, :], in_=bkv.rearrange("p (a b) -> p a b", a=8), func=AF.Copy),
                         reads=[bkn], writes=["osT%d" % b])
                else:
                    p.op("dve", lambda e, bkv=bkv, hh=hh, sT=sT: e.tensor_copy(out=sT[:, hh * 8:(hh + 1) * 8, :], in_=bkv.rearrange("p (a b) -> p a b", a=8)),
                         reads=[bkn], writes=["osT%d" % b])
            for cg in range(4):
                for kc in range(16):
                    p.op("pe", lambda e, cg=cg, kc=kc, sT=sT: e.matmul(cx.bank[cg][:], lhsT=sT[:, kc, :], rhs=W[:, kc, cg * 512:(cg + 1) * 512], start=(kc == 0), stop=(kc == 15)),
                         reads=["osT%d" % b, "Wout%d" % cg], writes=["bank%d" % cg], inc=(kc == 15))
                p.op("dve", lambda e, cg=cg, tm=tm: e.tensor_tensor(out=tm[:, cg * 512:(cg + 1) * 512], in0=cx.bank[cg][:], in1=GT[:, cg * 512:(cg + 1) * 512], op=ALU.mult),
                     reads=["bank%d" % cg, "GT"], writes=["otm%d_%d" % (b, cg)])
                p.op("pool", lambda e, cg=cg, tm=tm, xs=xs: e.tensor_tensor(out=tm[:, cg * 512:(cg + 1) * 512], in0=tm[:, cg * 512:(cg + 1) * 512], in1=xs[:, cg * 512:(cg + 1) * 512], op=ALU.add),
                     reads=["otm%d_%d" % (b, cg), "oxs%d" % b], writes=["otm%d_%d" % (b, cg)])
            p.dma("sp", lambda e, tm=tm, t=t: e.dma_start(out=XOUT[t * 128:(t + 1) * 128, :], in_=tm[:]), reads=["otm%d_%d" % (b, cg) for cg in range(4)], writes=["XO%d" % t])
        p.emit()


TWO_PI = 2.0 * math.pi
C1 = 6.28125
C2 = TWO_PI - C1


def rope_tables(p, cx, POSap, COS, SINS, tag):
    with ExitStack() as st:
        cf = cx.cf
        a = p.sb(st, "ra", [32, TOK], F32)
        k = p.sb(st, "rk", [32, TOK], F32)
        ki = p.sb(st, "rki", [32, TOK], I32)
        m = p.sb(st, "rm", [32, TOK], F32)
        p.dma("pool", lambda e: e.dma_start(out=a[:], in_=bc(POSap, 32)), writes=["ra"])
        p.op("dve", lambda e: e.tensor_scalar(out=a[:], in0=a[:], scalar1=cf[0:32, C_INVF:C_INVF + 1], scalar2=None, op0=ALU.mult), reads=["ra"], writes=["ra"])

        def reduce_(src, dst, shift):
            p.op("dve", lambda e: e.tensor_scalar(out=k[:], in0=src[:], scalar1=shift, scalar2=1.0 / TWO_PI, op0=ALU.add, op1=ALU.mult), reads=["ra", "rd"], writes=["rk"])
            p.op("dve", lambda e: e.tensor_copy(out=ki[:], in_=k[:]), reads=["rk"], writes=["rki"])
            p.op("dve", lambda e: e.tensor_copy(out=k[:], in_=ki[:]), reads=["rki"], writes=["rk"])
            p.op("dve", lambda e: e.scalar_tensor_tensor(out=dst[:], in0=k[:], scalar=-C1, in1=src[:], op0=ALU.mult, op1=ALU.add), reads=["rk", "ra"], writes=["rd"])
            p.op("dve", lambda e: e.scalar_tensor_tensor(out=dst[:], in0=k[:], scalar=-C2, in1=dst[:], op0=ALU.mult, op1=ALU.add), reads=["rk", "rd"], writes=["rd"])
            if shift != 0.0:
                p.op("dve", lambda e: e.tensor_scalar(out=dst[:], in0=dst[:], scalar1=shift, scalar2=None, op0=ALU.add), reads=["rd"], writes=["rd"])
            p.op("dve", lambda e: e.tensor_scalar(out=m[:], in0=dst[:], scalar1=math.pi, scalar2=-TWO_PI, op0=ALU.is_gt, op1=ALU.mult), reads=["rd"], writes=["rm"])
            p.op("dve", lambda e: e.tensor_tensor(out=dst[:], in0=dst[:], in1=m[:], op=ALU.add), reads=["rd", "rm"], writes=["rd"])
            p.op("dve", lambda e: e.tensor_scalar(out=m[:], in0=dst[:], scalar1=-math.pi, scalar2=TWO_PI, op0=ALU.is_lt, op1=ALU.mult), reads=["rd"], writes=["rm"])
            p.op("dve", lambda e: e.tensor_tensor(out=dst[:], in0=dst[:], in1=m[:], op=ALU.add), reads=["rd", "rm"], writes=["rd"])
            p.op("dve", lambda e: e.tensor_scalar(out=dst[:], in0=dst[:], scalar1=math.pi, scalar2=-math.pi, op0=ALU.min, op1=ALU.max), reads=["rd"], writes=["rd"])

        reduce_(a, SINS, 0.0)
        p.op("act", lambda e: e.activation(out=SINS[:], in_=SINS[:], func=AF.Sin), reads=["rd"], writes=["rd"])
        p.op("dve", lambda e: e.tensor_scalar(out=SINS[:], in0=SINS[:], scalar1=cf[0:32, C_SGN:C_SGN + 1], scalar2=None, op0=ALU.mult), reads=["rd"], writes=["rd"])
        reduce_(a, COS, math.pi / 2)
        p.op("act", lambda e: e.activation(out=COS[:], in_=COS[:], func=AF.Sin), reads=["rd"], writes=["rd"])
        p.emit()


def attn_proj(p, cx, hT, own, COS, SINS, nmax):
    with ExitStack() as st:
        cb = cx.cb
        wp = [p.sb(st, "wp%d" % i, [128, 16, 512], BF16) for i in range(2)]
        qk = [p.sb(st, "qk%d" % i, [128, 512], BF16) for i in range(4)]
        sq = [p.sb(st, "sq%d" % i, [128, 512], BF16) for i in range(2)]
        t1 = [p.sb(st, "t1_%d" % i, [32, 512], F32) for i in range(2)]
        t2 = [p.sb(st, "t2_%d" % i, [32, 512], F32) for i in range(2)]
        nm1 = p.sb(st, "nm1", [1, 2], F32)
        vs = [p.sb(st, "vs%d" % i, [128, 2, 257], BF16) for i in range(4)]
        for i in range(4):
            p.op("pool", lambda e, i=i: e.memset(vs[i][:], 1.0), writes=["vs%d" % i])
        wv = cx.ATTN_W_IN.ap().rearrange("(kc k) c -> k kc c", k=128)
        toff = TOK if own else 0
        pieces = range(12) if own else range(4, 12)
        cnt = 0
        vcnt = 0
        for pi, j in enumerate(pieces):
            w = wp[pi % 2]
            wn = "wp%d" % (pi % 2)
            p.dma("pool", lambda e, w=w, j=j: e.dma_start(out=w[:], in_=wv[:, :, j * 512:(j + 1) * 512]), writes=[wn])
            if j < 8:
                isq = j < 4
                for cc in range(4):
                    gcc = (j % 4) * 4 + cc
                    for tg in range(4):
                        bi = cnt % 4
                        bk = cx.bank[bi]
                        q_ = qk[cnt % 4]
                        qn = "qk%d" % (cnt % 4)
                        s_ = sq[cnt % 2]
                        sn = "sq%d" % (cnt % 2)
                        a1, a2 = t1[cnt % 2], t2[cnt % 2]
                        an = "t12_%d" % (cnt % 2)
                        nb = cx.bank[4 + cnt % 2]
                        nbn = "bank%d" % (4 + cnt % 2)
                        sb_ = cx.bank[6 + cnt % 2]
                        sbn = "bank%d" % (6 + cnt % 2)
                        cnt += 1
                        for kc in range(16):
                            p.op("pe", lambda e, bk=bk, w=w, kc=kc, cc=cc, tg=tg: e.matmul(bk[:], lhsT=w[:, kc, cc * 128:(cc + 1) * 128], rhs=hT[:, kc, tg * 512:(tg + 1) * 512], start=(kc == 0), stop=(kc == 15)),
                                 reads=[wn], writes=["bank%d" % bi], inc=(kc == 15))
                        p.op("act", lambda e, bk=bk, q_=q_: e.activation(out=q_[:], in_=bk[:], func=AF.Copy), reads=["bank%d" % bi], writes=[qn])
                        p.op("act", lambda e, bk=bk, s_=s_: e.activation(out=s_[:], in_=bk[:], func=AF.Square), reads=["bank%d" % bi], writes=[sn])
                        p.op("pe", lambda e, nb=nb, s_=s_: e.matmul(nb[0:1, :], lhsT=cb[:, C_ON:C_ON + 1], rhs=s_[:], start=True, stop=True), reads=[sn], writes=[nbn])
                        p.op("dve", lambda e, nb=nb: e.reduce_max(out=nm1[:, 0:1], in_=nb[0:1, :], axis=AX.X), reads=[nbn], writes=["nm1"])
                        ix = gcc if isq else 16 + gcc
                        p.op("dve", lambda e, ix=ix: e.tensor_tensor(out=nmax[:, ix:ix + 1], in0=nmax[:, ix:ix + 1], in1=nm1[:, 0:1], op=ALU.max), reads=["nm1", "nmax"], writes=["nmax"])
                        p.op("pe", lambda e, sb_=sb_, q_=q_: e.matmul(sb_[0:32, :], lhsT=cb[0:32, C_PERM:C_PERM + 32], rhs=q_[0:32, :], start=True, stop=True), reads=[qn], writes=[sbn])
                        p.op("dve", lambda e, a1=a1, q_=q_, tg=tg: e.tensor_tensor(out=a1[:], in0=q_[0:32, :], in1=COS[:, tg * 512:(tg + 1) * 512], op=ALU.mult), reads=[qn], writes=[an + "a"])
                        p.op("dve", lambda e, a2=a2, sb_=sb_, tg=tg: e.tensor_tensor(out=a2[:], in0=sb_[0:32, :], in1=SINS[:, tg * 512:(tg + 1) * 512], op=ALU.mult), reads=[sbn], writes=[an + "b"])
                        p.op("dve", lambda e, a1=a1, a2=a2, q_=q_: e.tensor_tensor(out=q_[0:32, :], in0=a1[:], in1=a2[:], op=ALU.add), reads=[an + "a", an + "b", qn], writes=[qn])
                        if isq:
                            dst = cx.QTD.ap()[gcc, :, tg * 512:(tg + 1) * 512]
                        else:
                            dst = cx.KTD.ap()[gcc, :, toff + tg * 512:toff + (tg + 1) * 512]
                        p.dma("sp", lambda e, dst=dst, q_=q_: e.dma_start(out=dst, in_=q_[:]), reads=[qn], writes=["qkd%d" % cnt])
            else:
                vj = j - 8
                for t in range(NT):
                    bi = cnt % 4
                    bk = cx.bank[bi]
                    cnt += 1
                    v_ = vs[vcnt % 4]
                    vn = "vs%d" % (vcnt % 4)
                    vcnt += 1
                    for kc in range(16):
                        p.op("pe", lambda e, bk=bk, w=w, kc=kc, t=t: e.matmul(bk[:], lhsT=hT[:, kc, t * 128:(t + 1) * 128], rhs=w[:, kc, :], start=(kc == 0), stop=(kc == 15)),
                             reads=[wn], writes=["bank%d" % bi], inc=(kc == 15))
                    if t % 2 == 0:
                        p.op("act", lambda e, bk=bk, v_=v_: e.activation(out=v_[:, :, 0:256], in_=bk[:].rearrange("p (a b) -> p a b", a=2), func=AF.Copy), reads=["bank%d" % bi], writes=[vn])
                    else:
                        p.op("dve", lambda e, bk=bk, v_=v_: e.tensor_copy(out=v_[:, :, 0:256], in_=bk[:].rearrange("p (a b) -> p a b", a=2)), reads=["bank%d" % bi], writes=[vn])
                    r0 = toff + t * 128
                    p.dma("act", lambda e, v_=v_, r0=r0, vj=vj: e.dma_start(out=cx.VD.ap()[r0:r0 + 128, 2 * vj:2 * vj + 2, :], in_=v_[:]), reads=[vn], writes=["vd%d" % cnt])
        p.emit()


def attn_consts(p, cx, st0, nmax):
    negc = p.sb(st0, "negc", [128, 16], F32)
    negcp = p.sb(st0, "negcp", [128, 16], F32)
    nlam = p.sb(st0, "nlam", [128, 1], F32)
    HG = p.sb(st0, "HG", [128, 256], F32)
    with ExitStack() as st:
        cf = cx.cf
        r = p.sb(st, "acr", [1, 64], F32)
        lm = p.sb(st, "lm", [1, 4, 128], F32)
        pf = p.sb(st, "pf", [128, 1], F32)
        for i, nm_ in enumerate([cx.LQ1, cx.LK1, cx.LQ2, cx.LK2]):
            p.dma("sp", lambda e, i=i, nm_=nm_: e.dma_start(out=lm[:, i, :], in_=nm_.ap()), writes=["lm"])
        p.dma("sp", lambda e: e.dma_start(out=pf[:], in_=cx.PREVFLAG.ap()), writes=["pf"])
        p.dma("sp", lambda e: e.dma_start(out=HG[:], in_=bc(cx.ATTN_HG.ap()[0, :])), writes=["HG"])
        p.op("dve", lambda e: e.tensor_scalar(out=HG[:], in0=HG[:], scalar1=0.8, scalar2=None, op0=ALU.mult), reads=["HG"], writes=["HG"])
        p.op("dve", lambda e: e.tensor_tensor(out=r[:, 0:16], in0=nmax[:, 0:16], in1=nmax[:, 16:32], op=ALU.mult), reads=["nmax"], writes=["acr"])
        p.op("act", lambda e: e.activation(out=r[:, 0:16], in_=r[:, 0:16], func=AF.Sqrt), reads=["acr"], writes=["acr"])
        p.op("dve", lambda e: e.tensor_scalar(out=r[:, 0:16], in0=r[:, 0:16], scalar1=-(128.0 ** -0.5), scalar2=None, op0=ALU.mult), reads=["acr"], writes=["acr"])
        p.op("dve", lambda e: e.tensor_tensor(out=lm[:, 0, :], in0=lm[:, 0, :], in1=lm[:, 1, :], op=ALU.mult), reads=["lm"], writes=["lm"])
        p.op("dve", lambda e: e.tensor_tensor(out=lm[:, 2, :], in0=lm[:, 2, :], in1=lm[:, 3, :], op=ALU.mult), reads=["lm"], writes=["lm"])
        p.op("dve", lambda e: e.reduce_sum(out=r[:, 32:33], in_=lm[:, 0, :], axis=AX.X), reads=["lm"], writes=["acr"])
        p.op("dve", lambda e: e.reduce_sum(out=r[:, 33:34], in_=lm[:, 2, :], axis=AX.X), reads=["lm"], writes=["acr"])
        p.op("act", lambda e: e.activation(out=r[:, 32:34], in_=r[:, 32:34], func=AF.Exp), reads=["acr"], writes=["acr"])
        p.op("dve", lambda e: e.tensor_tensor(out=r[:, 16:17], in0=r[:, 33:34], in1=r[:, 32:33], op=ALU.subtract), reads=["acr"], writes=["acr"])
        p.op("dve", lambda e: e.tensor_scalar(out=r[:, 16:17], in0=r[:, 16:17], scalar1=-0.2, scalar2=None, op0=ALU.add), reads=["acr"], writes=["acr"])
        p.op("pe", lambda e: e.matmul(cx.bank[0][:, 0:18], lhsT=cf[0:1, C_ON:C_ON + 128], rhs=r[0:1, 0:18], start=True, stop=True), reads=["acr"], writes=["bank0"])
        p.op("dve", lambda e: e.tensor_copy(out=negc[:], in_=cx.bank[0][:, 0:16]), reads=["bank0"], writes=["negc"])
        p.op("dve", lambda e: e.tensor_copy(out=nlam[:], in_=cx.bank[0][:, 16:17]), reads=["bank0"], writes=["nlam"])
        p.op("dve", lambda e: e.tensor_scalar(out=negcp[:], in0=negc[:], scalar1=pf[:, 0:1], scalar2=None, op0=ALU.add), reads=["negc", "pf"], writes=["negcp"])
        p.emit()
    return negc, negcp, nlam, HG


def attn_core(p, cx, negc, negcp, nlam, HG):
    SCALE = 128.0 ** -0.5
    with ExitStack() as st:
        QT = [[p.sb(st, "QT%d_%d" % (b, m), [128, TOK], BF16) for m in range(2)] for b in range(2)]
        KT = [[p.sb(st, "KT%d_%d" % (b, m), [128, 2 * TOK], BF16) for m in range(2)] for b in range(2)]
        V = [p.sb(st, "V%d" % b, [128, 32, 257], BF16) for b in range(2)]
        PT = [p.sb(st, "PT%d" % m, [128, 36, 512], BF16) for m in range(2)]
        o0 = p.sb(st, "o0", [128, 4, 256], F32)
        oa = [p.sb(st, "oa%d" % i, [128, 4, 256], BF16) for i in range(2)]
        rd = [p.sb(st, "rd%d" % i, [128, 8], F32) for i in range(4)]
        junk = p.sb(st, "cjunk", [128, 256], BF16)
        vdv = cx.VD.ap().rearrange("(t k) h e -> h k t e", k=128)

        def loads(h):
            b = h % 2
            for m in range(2):
                p.dma("sp", lambda e, b=b, m=m, h=h: e.dma_start(out=QT[b][m][:], in_=cx.QTD.ap()[2 * h + m]), writes=["QT%d_%d" % (b, m)])
                p.dma("sp", lambda e, b=b, m=m, h=h: e.dma_start(out=KT[b][m][:], in_=cx.KTD.ap()[2 * h + m]), writes=["KT%d_%d" % (b, m)])
            p.dma("act", lambda e, b=b, h=h: e.dma_start(out=V[b][:], in_=vdv[h]), writes=["V%d" % b])

        stb = [0]
        accb = [0]
        oac = [0]
        rdc = [0]

        def stageA(h, g, m):
            b = h % 2
            hm = 2 * h + m
            steps = []
            nk = 16 + 4 * g + 4
            for j in range(nk):
                def step(j=j):
                    qoff = max(0, j - 16 - 4 * g)
                    bi = stb[0] % 3
                    stb[0] += 1
                    bk = cx.bank[bi]
                    p.op("pe", lambda e: e.matmul(bk[:, qoff * 128:512], lhsT=KT[b][m][:, j * 128:(j + 1) * 128], rhs=QT[b][m][:, g * 512 + qoff * 128:(g + 1) * 512], start=True, stop=True),
                         reads=["KT%d_%d" % (b, m), "QT%d_%d" % (b, m)], writes=["bank%d" % bi])
                    bias = negcp[:, hm:hm + 1] if j < 16 else negc[:, hm:hm + 1]
                    p.op("act", lambda e: e.activation(out=PT[m][:, j, qoff * 128:512], in_=bk[:, qoff * 128:512], func=AF.Exp, bias=bias, scale=SCALE),
                         reads=["bank%d" % bi], writes=["PT%d_%d" % (m, j)])
                    if j >= 16 + 4 * g:
                        ii = j - 16 - 4 * g
                        p.op("pool", lambda e: e.memset(PT[m][64:128, j, ii * 128:ii * 128 + 64], 0.0), reads=[], writes=["PT%d_%d" % (m, j)])
                steps.append(step)
            return steps

        def stageB(h, g, m):
            b = h % 2
            steps = []
            for ii in range(4):
                ai = 3 + accb[0] % 5
                accb[0] += 1
                acc = cx.bank[ai]
                an = "bank%d" % ai
                nkk = 16 + 4 * g + ii + 1
                for j in range(nkk):
                    def step(j=j, ii=ii, acc=acc, an=an, nkk=nkk):
                        p.op("pe", lambda e: e.matmul(acc[:, 0:257], lhsT=PT[m][:, j, ii * 128:(ii + 1) * 128], rhs=V[b][:, j, :], start=(j == 0), stop=(j == nkk - 1)),
                             reads=["PT%d_%d" % (m, j), "V%d" % b], writes=[an], inc=(j == nkk - 1))
                    steps.append(step)

                def fin(ii=ii, acc=acc, an=an):
                    r = rd[rdc[0] % 4]
                    rn = "rd%d" % (rdc[0] % 4)
                    rdc[0] += 1
                    p.op("dve", lambda e: e.reciprocal(out=r[:, 0:1], in_=acc[:, 256:257]), reads=[an], writes=[rn])
                    if m == 0:
                        p.op("act", lambda e: e.activation(out=o0[:, ii, :], in_=acc[:, 0:256], func=AF.Copy, scale=r[:, 0:1]), reads=[an, rn], writes=["o0_%d" % ii])
                    else:
                        o = oa[oac[0] % 2]
                        on = "oa%d" % (oac[0] % 2)
                        p.op("dve", lambda e: e.tensor_tensor(out=r[:, 1:2], in0=r[:, 0:1], in1=nlam[:, 0:1], op=ALU.mult), reads=[rn], writes=[rn])
                        p.op("dve", lambda e: e.scalar_tensor_tensor(out=o0[:, ii, :], in0=acc[:, 0:256], scalar=r[:, 1:2], in1=o0[:, ii, :], op0=ALU.mult, op1=ALU.add),
                             reads=[an, rn, "o0_%d" % ii], writes=["o0_%d" % ii])
                        p.op("act", lambda e: e.activation(out=junk[:], in_=o0[:, ii, :], func=AF.Square, accum_out=r[:, 2:3]), reads=["o0_%d" % ii], writes=["cjunk", rn])
                        p.op("dve", lambda e: e.tensor_scalar(out=r[:, 3:4], in0=r[:, 2:3], scalar1=1.0 / 256, scalar2=EPS, op0=ALU.mult, op1=ALU.add), reads=[rn], writes=[rn])
                        p.op("act", lambda e: e.activation(out=r[:, 4:5], in_=r[:, 3:4], func=AF.Sqrt), reads=[rn], writes=[rn])
                        p.op("dve", lambda e: e.reciprocal(out=r[:, 5:6], in_=r[:, 4:5]), reads=[rn], writes=[rn])
                        p.op("dve", lambda e: e.scalar_tensor_tensor(out=o[:, ii, :], in0=o0[:, ii, :], scalar=r[:, 5:6], in1=HG[:], op0=ALU.mult, op1=ALU.mult),
                             reads=["o0_%d" % ii, rn], writes=[on + "_%d" % ii])
                        if ii == 3:
                            oac[0] += 1
                            dst = cx.OAD.ap()[g * 512:(g + 1) * 512, h * 256:(h + 1) * 256].rearrange("(i q) c -> q i c", q=128)
                            p.dma("sp", lambda e: e.dma_start(out=dst, in_=o[:]), reads=[on + "_%d" % i_ for i_ in range(4)], writes=["oad_%d_%d" % (h, g)])
                steps.append(fin)
            return steps

        def interleave(A, B):
            na, nb = len(A), len(B)
            ia = ib = 0
            while ia < na or ib < nb:
                if ib >= nb or (ia < na and ia * max(nb, 1) <= ib * max(na, 1)):
                    A[ia]()
                    ia += 1
                else:
                    B[ib]()
                    ib += 1

        loads(0)
        pend = []
        for h in range(8):
            for g in range(4):
                for m in range(2):
                    A = stageA(h, g, m)
                    interleave(A, pend)
                    pend = stageB(h, g, m)
                    if g == 0 and m == 0 and h + 1 < 8:
                        loads(h + 1)
        interleave([], pend)
        p.emit()


def attn_layer(p, cx, XIN, XPREV, XOUT):
    with ExitStack() as st0:
        nmax = p.sb(st0, "nmax", [1, 32], F32)
        p.op("dve", lambda e: e.memset(nmax[:], 0.0), writes=["nmax"])
        with ExitStack() as st1:
            G, SH, GT = load_mod_tiles(p, cx, st1, 0, 0, cx.NORM_MIX_G)
            COS = p.sb(st1, "COS", [32, TOK], F32)
            SINS = p.sb(st1, "SINS", [32, TOK], F32)
            hT = p.sb(st1, "hT", [128, 16, TOK], BF16)
            rope_tables(p, cx, cx.POS_PREV.ap()[0, :], COS, SINS, "p")
            norm_to_hT(p, cx, XPREV.ap(), G, SH, hT)
            attn_proj(p, cx, hT, False, COS, SINS, nmax)
            rope_tables(p, cx, cx.POS_OWN.ap()[0, :], COS, SINS, "o")
            norm_to_hT(p, cx, XIN.ap(), G, SH, hT)
            attn_proj(p, cx, hT, True, COS, SINS, nmax)
        negc, negcp, nlam, HG = attn_consts(p, cx, st0, nmax)
        attn_core(p, cx, negc, negcp, nlam, HG)
    with ExitStack() as st2:
        GT = p.sb(st2, "GT", [128, D], F32)
        p.dma("sp", lambda e: e.dma_start(out=GT[:], in_=bc(cx.MODD.ap()[0, 2 * D:3 * D])), writes=["GT"])
        proj_out_stage(p, cx, cx.OAD.ap(), cx.ATTN_W_OUT.ap(), GT, XIN.ap(), XOUT.ap())


def mlstm_proj(p, cx, hT, full=True):
    with ExitStack() as st:
        wp = [p.sb(st, "wp%d" % i, [128, 16, 512], BF16) for i in range(2)]
        wgt = p.sb(st, "wgt", [128, 16, 16], BF16)
        qk = [p.sb(st, "qk%d" % i, [128, 512], BF16) for i in range(4)]
        vs = [p.sb(st, "vs%d" % i, [128, 2, 257], BF16) for i in range(4)]
        ob = [p.sb(st, "ob%d" % i, [128, 512], BF16) for i in range(4)]
        gsb = [p.sb(st, "gsb%d" % i, [128, 16], F32) for i in range(2)]
        for i in range(4):
            p.op("pool", lambda e, i=i: e.memset(vs[i][:], 1.0), writes=["vs%d" % i])
        wv = cx.ML_W_IN.ap().rearrange("(kc k) c -> k kc c", k=128)
        p.dma("pool", lambda e: e.dma_start(out=wgt[:], in_=wv[:, :, 3 * D:3 * D + 16]), writes=["wgt"])
        cnt = 0
        for j in range(12 if full else 8):
            w = wp[j % 2]
            wn = "wp%d" % (j % 2)
            p.dma("pool", lambda e, w=w, j=j: e.dma_start(out=w[:], in_=wv[:, :, j * 512:(j + 1) * 512]), writes=[wn])
            if j < 4:
                for cc in range(4):
                    gcc = j * 4 + cc
                    for tg in range(4):
                        bi = cnt % 4
                        bk = cx.bank[bi]
                        q_ = qk[cnt % 4]
                        qn = "qk%d" % (cnt % 4)
                        cnt += 1
                        for kc in range(16):
                            p.op("pe", lambda e, bk=bk, w=w, kc=kc, cc=cc, tg=tg: e.matmul(bk[:], lhsT=w[:, kc, cc * 128:(cc + 1) * 128], rhs=hT[:, kc, tg * 512:(tg + 1) * 512], start=(kc == 0), stop=(kc == 15)),
                                 reads=[wn], writes=["bank%d" % bi], inc=(kc == 15))
                        if cnt % 2 == 0:
                            p.op("act", lambda e, bk=bk, q_=q_: e.activation(out=q_[:], in_=bk[:], func=AF.Copy), reads=["bank%d" % bi], writes=[qn])
                        else:
                            p.op("dve", lambda e, bk=bk, q_=q_: e.tensor_copy(out=q_[:], in_=bk[:]), reads=["bank%d" % bi], writes=[qn])
                        dst = cx.QKP.ap()[gcc, :, 3 + tg * 512:3 + (tg + 1) * 512]
                        p.dma("sp", lambda e, dst=dst, q_=q_: e.dma_start(out=dst, in_=q_[:]), reads=[qn], writes=["qkd%d" % cnt])
            else:
                for t in range(NT):
                    bi = cnt % 4
                    bk = cx.bank[bi]
                    cnt += 1
                    for kc in range(16):
                        p.op("pe", lambda e, bk=bk, w=w, kc=kc, t=t: e.matmul(bk[:], lhsT=hT[:, kc, t * 128:(t + 1) * 128], rhs=w[:, kc, :], start=(kc == 0), stop=(kc == 15)),
                             reads=[wn], writes=["bank%d" % bi], inc=(kc == 15))
                    if j < 8:
                        vj = j - 4
                        v_ = vs[cnt % 4]
                        vn = "vs%d" % (cnt % 4)
                        if t % 2 == 0:
                            p.op("act", lambda e, bk=bk, v_=v_: e.activation(out=v_[:, :, 0:256], in_=bk[:].rearrange("p (a b) -> p a b", a=2), func=AF.Copy), reads=["bank%d" % bi], writes=[vn])
                        else:
                            p.op("dve", lambda e, bk=bk, v_=v_: e.tensor_copy(out=v_[:, :, 0:256], in_=bk[:].rearrange("p (a b) -> p a b", a=2)), reads=["bank%d" % bi], writes=[vn])
                        p.dma("act", lambda e, v_=v_, t=t, vj=vj: e.dma_start(out=cx.VM.ap()[t * 128:(t + 1) * 128, 2 * vj:2 * vj + 2, :], in_=v_[:]), reads=[vn], writes=["vd%d" % cnt])
                    else:
                        oj = j - 8
                        o_ = ob[cnt % 4]
                        on = "ob%d" % (cnt % 4)
                        if t % 2 == 0:
                            p.op("act", lambda e, bk=bk, o_=o_: e.activation(out=o_[:], in_=bk[:], func=AF.Copy), reads=["bank%d" % bi], writes=[on])
                        else:
                            p.op("dve", lambda e, bk=bk, o_=o_: e.tensor_copy(out=o_[:], in_=bk[:]), reads=["bank%d" % bi], writes=[on])
                        p.dma("act", lambda e, o_=o_, t=t, oj=oj: e.dma_start(out=cx.OPRE.ap()[t * 128:(t + 1) * 128, oj * 512:(oj + 1) * 512], in_=o_[:]), reads=[on], writes=["od%d" % cnt])
        for t in range(NT):
            bk = cx.bank[4 + t % 2]
            g_ = gsb[t % 2]
            for kc in range(16):
                p.op("pe", lambda e, bk=bk, kc=kc, t=t: e.matmul(bk[:, 0:16], lhsT=hT[:, kc, t * 128:(t + 1) * 128], rhs=wgt[:, kc, :], start=(kc == 0), stop=(kc == 15)),
                     reads=["wgt"], writes=["bank%d" % (4 + t % 2)], inc=(kc == 15))
            p.op("dve", lambda e, bk=bk, g_=g_: e.tensor_copy(out=g_[:], in_=bk[:, 0:16]), reads=["bank%d" % (4 + t % 2)], writes=["gsb%d" % (t % 2)])
            p.dma("sp", lambda e, g_=g_, t=t: e.dma_start(out=cx.GATES.ap()[t * 128:(t + 1) * 128, :], in_=g_[:]), reads=["gsb%d" % (t % 2)], writes=["gd%d" % t])
        p.emit()


def mlstm_rec(p, cx, full=True, st_in="dram", st_out="dram"):
    RS = 128.0 ** -0.5
    with ExitStack() as st:
        cf, cb = cx.cf, cx.cb
        C = p.sb(st, "Cst", [128, 8, 257], F32)
        Cb = p.sb(st, "Cstb", [128, 8, 257], BF16)
        CW = p.sb(st, "CW", [128, 16, 4], F32)
        CB = p.sb(st, "CB", [128, 16], F32)
        GB = p.sb(st, "GB", [128, 16], F32)
        HGm = p.sb(st, "HGm", [128, D], F32)
        hl = p.sb(st, "hl", [128, 16, 3], F32)
        hlb = p.sb(st, "hlb", [128, 16, 3], BF16)
        qkp = [p.sb(st, "qkp%d" % i, [128, 16, 131], BF16) for i in range(2)]
        vt = [p.sb(st, "vt%d" % i, [128, 8, 257], BF16) for i in range(2)]
        gt_ = [p.sb(st, "gtl%d" % i, [128, 16], F32) for i in range(2)]
        op_ = [p.sb(st, "opl%d" % i, [128, D], BF16) for i in range(2)]
        sig = p.sb(st, "sig", [128, D], F32)
        cacc = [p.sb(st, "cacc%d" % i, [128, 128], F32) for i in range(4)]
        qs = p.sb(st, "qs", [128, 16, 128], BF16)
        gs = p.sb(st, "gs", [128, 64], F32)
        PTm = [p.sb(st, "PTm%d" % i, [128, 128], BF16) for i in range(2)]
        Kp = [p.sb(st, "Kp%d" % i, [128, 128], BF16) for i in range(2)]
        tmpC = [p.sb(st, "tmpC%d" % i, [128, 257], F32) for i in range(2)]
        ho = [p.sb(st, "ho%d" % i, [128, 256], F32) for i in range(2)]
        hn = [p.sb(st, "hn%d" % i, [128, D], BF16) for i in range(2)]
        rr = [p.sb(st, "rr%d" % i, [128, 8], F32) for i in range(4)]
        junk = p.sb(st, "mjunk", [128, 256], BF16)
        if st_in == "dram":
            p.dma("sp", lambda e: e.dma_start(out=C[:], in_=cx.STATE_IN.ap()), writes=["Cst"])
        elif st_in == "zero":
            p.op("dve", lambda e: e.memset(C[:], 0.0), writes=["Cst"])
        else:
            of = p.sb(st, "oddf", [128, 1], F32)
            p.dma("sp", lambda e: e.dma_start(out=of[:], in_=cx.ODDFLAG.ap()), writes=["oddf"])
            p.dma("sp", lambda e: e.dma_start(out=C[:].rearrange("p h e -> p (h e)"), in_=cx.ST_ALL.ap()[0:128, 0:8 * 257]), writes=["Cst"])
            p.op("dve", lambda e: e.tensor_scalar(out=C[:], in0=C[:], scalar1=of[:, 0:1], scalar2=None, op0=ALU.mult), reads=["Cst", "oddf"], writes=["Cst"])
        p.op("act", lambda e: e.activation(out=Cb[:], in_=C[:], func=AF.Copy), reads=["Cst"], writes=["Cstb"])
        p.dma("sp", lambda e: e.dma_start(out=CW[:], in_=cx.CONV_W.ap()), writes=["CW"])
        p.dma("sp", lambda e: e.dma_start(out=CB[:], in_=cx.CONV_B.ap()), writes=["CB"])
        p.dma("sp", lambda e: e.dma_start(out=GB[:], in_=bc(cx.GATE_B.ap()[0, :])), writes=["GB"])
        p.dma("sp", lambda e: e.dma_start(out=HGm[:], in_=bc(cx.ML_HG.ap()[0, :])), writes=["HGm"])
        if st_in == "dram":
            p.dma("sp", lambda e: e.dma_start(out=hl[:], in_=cx.HALO_IN.ap()), writes=["hl"])
        elif st_in == "zero":
            p.op("dve", lambda e: e.memset(hl[:], 0.0), writes=["hl"])
        else:
            p.dma("sp", lambda e: e.dma_start(out=hl[:].rearrange("p c t -> p (c t)"), in_=cx.ST_ALL.ap()[0:128, 8 * 257:8 * 257 + 48]), writes=["hl"])
            p.op("dve", lambda e: e.tensor_scalar(out=hl[:], in0=hl[:], scalar1=of[:, 0:1], scalar2=None, op0=ALU.mult), reads=["hl", "oddf"], writes=["hl"])
        p.op("dve", lambda e: e.tensor_copy(out=hlb[:], in_=hl[:]), reads=["hl"], writes=["hlb"])
        qkv = cx.QKP.ap().rearrange("c k t -> k c t")
        p.dma("sp", lambda e: e.dma_start(out=qkv[:, :, 0:3], in_=hlb[:], allow_slow_non_contiguous=True), reads=["hlb"], writes=["halo"])
        for c in range(NT):
            b = c % 2
            q_, v_, g_, o_ = qkp[b], vt[b], gt_[b], op_[b]
            rdh = ["halo"] if c == 0 else []
            p.dma("sp", lambda e, q_=q_, c=c: e.dma_start(out=q_[:], in_=qkv[:, :, c * 128:c * 128 + 131]), reads=rdh, writes=["qkp%d" % b])
            p.dma("act", lambda e, v_=v_, c=c: e.dma_start(out=v_[:], in_=cx.VM.ap()[c * 128:(c + 1) * 128]), writes=["vt%d" % b])
            p.dma("sp", lambda e, g_=g_, c=c: e.dma_start(out=g_[:], in_=cx.GATES.ap()[c * 128:(c + 1) * 128, :]), writes=["gtl%d" % b])
            if full:
                p.dma("act", lambda e, o_=o_, c=c: e.dma_start(out=o_[:], in_=cx.OPRE.ap()[c * 128:(c + 1) * 128, :]), writes=["opl%d" % b])
                p.op("act", lambda e, o_=o_: e.activation(out=sig[:], in_=o_[:], func=AF.Sigmoid), reads=["opl%d" % b], writes=["sig"])
            p.op("dve", lambda e, g_=g_: e.tensor_tensor(out=gs[:, 0:16], in0=g_[:], in1=GB[:], op=ALU.add), reads=["gtl%d" % b, "GB"], writes=["gs"])
            p.op("act", lambda e: e.activation(out=gs[:, 16:24], in_=gs[:, 8:16], func=AF.Exp, scale=-1.0), reads=["gs"], writes=["gs"])
            p.op("dve", lambda e: e.tensor_scalar(out=gs[:, 16:24], in0=gs[:, 16:24], scalar1=1.0, scalar2=None, op0=ALU.add), reads=["gs"], writes=["gs"])
            p.op("act", lambda e: e.activation(out=gs[:, 16:24], in_=gs[:, 16:24], func=AF.Ln), reads=["gs"], writes=["gs"])
            gbk = cx.bank[0]
            p.op("pe", lambda e: e.matmul(gbk[:, 0:8], lhsT=cf[:, C_TRI:C_TRI + 128], rhs=gs[:, 16:24], start=True, stop=True), reads=["gs"], writes=["bank0"])
            p.op("pe", lambda e: e.matmul(gbk[:, 8:16], lhsT=cf[:, C_ON:C_ON + 128], rhs=gs[:, 16:24], start=False, stop=True, skip_group_check=True), reads=["gs"], writes=["bank0"])
            p.op("dve", lambda e: e.tensor_tensor(out=gs[:, 32:40], in0=gs[:, 0:8], in1=gbk[:, 0:8], op=ALU.add), reads=["gs", "bank0"], writes=["gs"])
            p.op("act", lambda e: e.activation(out=gs[:, 32:40], in_=gs[:, 32:40], func=AF.Exp), reads=["gs"], writes=["gs"])
            p.op("dve", lambda e: e.tensor_scalar(out=gs[:, 32:40], in0=gs[:, 32:40], scalar1=RS, scalar2=None, op0=ALU.mult), reads=["gs"], writes=["gs"])
            p.op("act", lambda e: e.activation(out=gs[:, 40:56], in_=gbk[:, 0:16], func=AF.Exp, scale=-1.0), reads=["bank0"], writes=["gs"])
            for g4 in range(0, 16, 4):
                ccs = [cc for cc in range(g4, g4 + 4) if full or cc >= 8]
                for cc in ccs:
                    a_ = cacc[cc % 4]
                    an = "cacc%d" % (cc % 4)
                    p.op("act", lambda e, a_=a_, q_=q_, cc=cc: e.activation(out=a_[:], in_=q_[:, cc, 0:128], func=AF.Identity, scale=CW[:, cc, 0:1], bias=CB[:, cc:cc + 1]),
                         reads=["qkp%d" % b, "CW", "CB"], writes=[an])
                for j in range(1, 4):
                    for cc in ccs:
                        a_ = cacc[cc % 4]
                        an = "cacc%d" % (cc % 4)
                        p.op("dve", lambda e, a_=a_, q_=q_, cc=cc, j=j: e.scalar_tensor_tensor(out=a_[:], in0=q_[:, cc, j:j + 128], scalar=CW[:, cc, j:j + 1], in1=a_[:], op0=ALU.mult, op1=ALU.add),
                             reads=["qkp%d" % b, an], writes=[an])
                for cc in ccs:
                    a_ = cacc[cc % 4]
                    an = "cacc%d" % (cc % 4)
                    p.op("act", lambda e, a_=a_, cc=cc: e.activation(out=qs[:, cc, :], in_=a_[:], func=AF.Silu), reads=[an], writes=["qs%d" % cc])
            def head_ops(h, c=c, b=b, q_=q_, v_=v_):
                s4 = (h % 2) * 4
                bS, bT, bA, bU = cx.bank[s4], cx.bank[s4 + 1], cx.bank[s4 + 2], cx.bank[s4 + 3]
                nS, nT, nA, nU = ["bank%d" % (s4 + i) for i in range(4)]
                kT = qs[:, 8 + h, :]
                qT = qs[:, h, :]
                pt = PTm[h % 2]
                kp = Kp[h % 2]
                r = rr[h % 4]
                rn = "rr%d" % (h % 4)
                if full:
                    p.op("pe", lambda e, bS=bS, kT=kT, qT=qT: e.matmul(bS[:, 0:128], lhsT=kT, rhs=qT, start=True, stop=True), reads=["qs%d" % (8 + h), "qs%d" % h], writes=[nS])
                    yield
                    p.op("dve", lambda e, bS=bS, pt=pt, h=h: e.scalar_tensor_tensor(out=pt[:], in0=bS[:, 0:128], scalar=gs[:, 32 + h:33 + h], in1=cf[:, C_CM:C_CM + 128], op0=ALU.mult, op1=ALU.mult),
                         reads=[nS, "gs"], writes=["PTm%d" % (h % 2)])
                    yield
                bTv = bT[:].bitcast(BF16)
                p.op("pe", lambda e, bTv=bTv, kT=kT: e.transpose(out=bTv[:, 0:128], in_=kT, identity=cb[:, C_ID:C_ID + 128]), reads=["qs%d" % (8 + h)], writes=[nT])
                yield
                p.op("act", lambda e, bTv=bTv, kp=kp, h=h: e.activation(out=kp[:], in_=bTv[:, 0:128], func=AF.Copy, scale=gs[:, 32 + h:33 + h]), reads=[nT, "gs"], writes=["Kp%d" % (h % 2)])
                yield
                if full:
                    p.op("pe", lambda e, bA=bA, pt=pt, v_=v_, h=h: e.matmul(bA[:, 0:257], lhsT=pt[:], rhs=v_[:, h, :], start=True, stop=False), reads=["PTm%d" % (h % 2), "vt%d" % b], writes=[nA])
                    yield
                    p.op("pe", lambda e, bA=bA, qT=qT, h=h: e.matmul(bA[:, 0:257], lhsT=qT, rhs=Cb[:, h, :], start=False, stop=True), reads=["qs%d" % h, "Cstb%d" % h], writes=[nA])
                    yield
                p.op("pe", lambda e, bU=bU, kp=kp, v_=v_, h=h: e.matmul(bU[:, 0:257], lhsT=kp[:], rhs=v_[:, h, :], start=True, stop=True), reads=["Kp%d" % (h % 2), "vt%d" % b], writes=[nU])
                yield
                tc_ = tmpC[h % 2]
                p.op("dve", lambda e, bU=bU, tc_=tc_, h=h: e.tensor_tensor(out=tc_[:], in0=bU[:, 0:257], in1=C[:, h, :], op=ALU.add), reads=[nU, "Cst%d" % h, nA], writes=["tmpC%d" % (h % 2)])
                yield
                p.op("dve", lambda e, tc_=tc_, h=h: e.tensor_scalar(out=C[:, h, :], in0=tc_[:], scalar1=gs[:, 48 + h:49 + h], scalar2=None, op0=ALU.mult), reads=["tmpC%d" % (h % 2), "gs"], writes=["Cst%d" % h])
                yield
                p.op("act", lambda e, h=h: e.activation(out=Cb[:, h, :], in_=C[:, h, :], func=AF.Copy), reads=["Cst%d" % h], writes=["Cstb%d" % h])
                yield
                if full:
                    o_h = ho[h % 2]
                    on = "ho%d" % (h % 2)
                    hn_ = hn[b]
                    p.op("dve", lambda e, bA=bA, r=r, h=h: e.tensor_tensor(out=r[:, 0:1], in0=bA[:, 256:257], in1=gs[:, 40 + h:41 + h], op=ALU.mult), reads=[nA, "gs"], writes=[rn])
                    yield
                    p.op("dve", lambda e, r=r: e.tensor_scalar(out=r[:, 2:3], in0=r[:, 0:1], scalar1=-1.0, scalar2=None, op0=ALU.mult), reads=[rn], writes=[rn])
                    yield
                    p.op("dve", lambda e, r=r: e.tensor_scalar(out=r[:, 1:2], in0=r[:, 0:1], scalar1=r[:, 2:3], scalar2=1.0, op0=ALU.max, op1=ALU.max), reads=[rn], writes=[rn])
                    yield
                    p.op("dve", lambda e, r=r: e.reciprocal(out=r[:, 2:3], in_=r[:, 1:2]), reads=[rn], writes=[rn])
                    yield
                    p.op("dve", lambda e, r=r, h=h: e.tensor_tensor(out=r[:, 3:4], in0=r[:, 2:3], in1=gs[:, 40 + h:41 + h], op=ALU.mult), reads=[rn, "gs"], writes=[rn])
                    yield
                    p.op("act", lambda e, bA=bA, o_h=o_h, r=r: e.activation(out=o_h[:], in_=bA[:, 0:256], func=AF.Copy, scale=r[:, 3:4]), reads=[nA, rn], writes=[on])
                    yield
                    p.op("act", lambda e, o_h=o_h, r=r: e.activation(out=junk[:], in_=o_h[:], func=AF.Square, accum_out=r[:, 4:5]), reads=[on], writes=["mjunk", rn])
                    yield
                    p.op("dve", lambda e, r=r: e.tensor_scalar(out=r[:, 5:6], in0=r[:, 4:5], scalar1=1.0 / 256, scalar2=EPS, op0=ALU.mult, op1=ALU.add), reads=[rn], writes=[rn])
                    yield
                    p.op("act", lambda e, r=r: e.activation(out=r[:, 6:7], in_=r[:, 5:6], func=AF.Sqrt), reads=[rn], writes=[rn])
                    yield
                    p.op("dve", lambda e, r=r: e.reciprocal(out=r[:, 7:8], in_=r[:, 6:7]), reads=[rn], writes=[rn])
                    yield
                    p.op("dve", lambda e, o_h=o_h, r=r, h=h: e.scalar_tensor_tensor(out=o_h[:], in0=o_h[:], scalar=r[:, 7:8], in1=HGm[:, h * 256:(h + 1) * 256], op0=ALU.mult, op1=ALU.mult),
                         reads=[on, rn, "HGm"], writes=[on])
                    yield
                    p.op("pool", lambda e, o_h=o_h, hn_=hn_, h=h: e.tensor_tensor(out=hn_[:, h * 256:(h + 1) * 256], in0=o_h[:], in1=sig[:, h * 256:(h + 1) * 256], op=ALU.mult),
                         reads=[on, "sig"], writes=["hn%d_%d" % (b, h)])
                    yield
            for hp2 in range(4):
                g0, g1 = head_ops(2 * hp2), head_ops(2 * hp2 + 1)
                alive = [g0, g1]
                while alive:
                    for g_ in list(alive):
                        try:
                            next(g_)
                        except StopIteration:
                            alive.remove(g_)
            if full:
                p.dma("sp", lambda e, b=b, c=c: e.dma_start(out=cx.HN.ap()[c * 128:(c + 1) * 128, :], in_=hn[b][:]), reads=["hn%d_%d" % (b, h) for h in range(8)], writes=["hnd%d" % c])
        if st_out is not None:
            so = cx.STATE_OUT.ap() if st_out == "dram" else cx.ST_LOC.ap()[:, 0:8 * 257].rearrange("p (h e) -> p h e", h=8)
            ho_ = cx.HALO_OUT.ap() if st_out == "dram" else cx.ST_LOC.ap()[:, 8 * 257:8 * 257 + 48].rearrange("p (c t) -> p c t", c=16)
            p.dma("sp", lambda e: e.dma_start(out=so, in_=C[:]), reads=["Cst%d" % h for h in range(8)], writes=["so"])
            p.dma("sp", lambda e: e.dma_start(out=hlb[:], in_=qkv[:, :, TOK:TOK + 3], allow_slow_non_contiguous=True), reads=["halo"], writes=["hlb"])
            p.op("dve", lambda e: e.tensor_copy(out=hl[:], in_=hlb[:]), reads=["hlb"], writes=["hl"])
            p.dma("sp", lambda e: e.dma_start(out=ho_, in_=hl[:]), reads=["hl"], writes=["ho_"])
        p.emit()


def mlstm_layer(p, cx, XIN, XOUT, full=True, fused=False):
    with ExitStack() as st1:
        G, SH, GT = load_mod_tiles(p, cx, st1, 1, 0, cx.NORM_MIX_G)
        hT = p.sb(st1, "hT", [128, 16, TOK], BF16)
        norm_to_hT(p, cx, XIN.ap(), G, SH, hT)
        mlstm_proj(p, cx, hT, full)
    if fused:
        mlstm_rec(p, cx, False, st_in="zero", st_out="loc")
        p.coll(lambda e: e.collective_compute("AllGather", ALU.bypass, replica_groups=REPLICA,
                                              ins=[cx.ST_LOC.ap()], outs=[cx.ST_ALL.ap()]), writes=["ST_ALL"])
        p.emit()
        mlstm_rec(p, cx, True, st_in="gather", st_out=None)
    else:
        mlstm_rec(p, cx, full)
    if not full:
        return
    with ExitStack() as st2:
        GT = p.sb(st2, "GT", [128, D], F32)
        p.dma("sp", lambda e: e.dma_start(out=GT[:], in_=bc(cx.MODD.ap()[1, 2 * D:3 * D])), writes=["GT"])
        proj_out_stage(p, cx, cx.HN.ap(), cx.ML_W_OUT.ap(), GT, XIN.ap(), XOUT.ap())


def _common(nc, cx, st):
    cx.nc = nc
    cx.CONSTS = nc.dram_tensor("CONSTS", [128, C_W], F32, kind="ExternalInput")
    cx.NORM_MIX_G = nc.dram_tensor("NORM_MIX_G", [2, D], F32, kind="ExternalInput")
    cx.NORM_FFN_G = nc.dram_tensor("NORM_FFN_G", [2, D], F32, kind="ExternalInput")
    cx.WR = nc.dram_tensor("WR", [2, 128, 16, 36], F32, kind="ExternalInput")
    cx.BRT = nc.dram_tensor("BRT", [2, 36], F32, kind="ExternalInput")
    cx.XS = nc.dram_tensor("XS", [NSLOT + TOK, D], BF16, kind="Internal")
    cx.YS = nc.dram_tensor("YS", [NSLOT + TOK, D], F32, kind="Internal")
    p = Prog(nc, st)
    cx.bank = [st.enter_context(nc.psum_tensor("bank%d" % i, [128, 512], F32)) for i in range(8)]
    load_consts(p, cx, st)
    return p


def build_A():
    nc = bass.Bass("TRN2", target_bir_lowering=False)
    cx = Ctx()
    with ExitStack() as st:
        p = _common(nc, cx, st)
        cx.WL = lambda L: 0
        cx.MODD = nc.dram_tensor("MODD", [2, 6 * D], F32, kind="ExternalOutput")
        cx.CVT = nc.dram_tensor("CVT", [128, 16], F32, kind="ExternalInput")
        cx.ADA_W = nc.dram_tensor("ADA_W", [2, D, 6 * D], F32, kind="ExternalInput")
        cx.ADA_B = nc.dram_tensor("ADA_B", [2, 6 * D], F32, kind="ExternalInput")
        cx.ATTN_W_IN = nc.dram_tensor("ATTN_W_IN", [D, 3 * D], F32, kind="ExternalInput")
        cx.ATTN_W_OUT = nc.dram_tensor("ATTN_W_OUT", [D, D], F32, kind="ExternalInput")
        cx.LQ1 = nc.dram_tensor("LQ1", [1, 128], F32, kind="ExternalInput")
        cx.LK1 = nc.dram_tensor("LK1", [1, 128], F32, kind="ExternalInput")
        cx.LQ2 = nc.dram_tensor("LQ2", [1, 128], F32, kind="ExternalInput")
        cx.LK2 = nc.dram_tensor("LK2", [1, 128], F32, kind="ExternalInput")
        cx.ATTN_HG = nc.dram_tensor("ATTN_HG", [1, 256], F32, kind="ExternalInput")
        cx.PREVFLAG = nc.dram_tensor("PREVFLAG", [128, 1], F32, kind="ExternalInput")
        cx.POS_OWN = nc.dram_tensor("POS_OWN", [1, TOK], I32, kind="ExternalInput")
        cx.POS_PREV = nc.dram_tensor("POS_PREV", [1, TOK], I32, kind="ExternalInput")
        cx.W_GU = nc.dram_tensor("W_GU", [1, NE, D, 2 * HID], F32, kind="ExternalInput")
        cx.W_DN = nc.dram_tensor("W_DN", [1, NE, HID, D], F32, kind="ExternalInput")
        XIN = nc.dram_tensor("XIN", [TOK, D], F32, kind="ExternalInput")
        XPREV = nc.dram_tensor("XPREV", [TOK, D], F32, kind="ExternalInput")
        X1 = nc.dram_tensor("X1", [TOK, D], F32, kind="ExternalOutput")
        XMID = nc.dram_tensor("XMID", [TOK, D], F32, kind="Internal")
        cx.QTD = nc.dram_tensor("QTD", [16, 128, TOK], BF16, kind="Internal")
        cx.KTD = nc.dram_tensor("KTD", [16, 128, 2 * TOK], BF16, kind="Internal")
        cx.VD = nc.dram_tensor("VD", [2 * TOK, 8, 257], BF16, kind="Internal")
        cx.OAD = nc.dram_tensor("OAD", [TOK, D], BF16, kind="Internal")
        mod_stage(p, cx, 0)
        mod_stage(p, cx, 1)
        attn_layer(p, cx, XIN, XPREV, XMID)
        moe_stage(p, cx, 0, XMID, X1)
    return nc


def build_B(full=True):
    nc = bass.Bass("TRN2", target_bir_lowering=False)
    cx = Ctx()
    with ExitStack() as st:
        p = _common(nc, cx, st)
        cx.WL = lambda L: 0
        cx.MODD = nc.dram_tensor("MODD", [2, 6 * D], F32, kind="ExternalInput")
        cx.ML_W_IN = nc.dram_tensor("ML_W_IN", [D, 3 * D + 16], F32, kind="ExternalInput")
        cx.CONV_W = nc.dram_tensor("CONV_W", [128, 16, 4], F32, kind="ExternalInput")
        cx.CONV_B = nc.dram_tensor("CONV_B", [128, 16], F32, kind="ExternalInput")
        cx.GATE_B = nc.dram_tensor("GATE_B", [1, 16], F32, kind="ExternalInput")
        cx.ML_HG = nc.dram_tensor("ML_HG", [1, D], F32, kind="ExternalInput")
        if full:
            cx.ML_W_OUT = nc.dram_tensor("ML_W_OUT", [D, D], F32, kind="ExternalInput")
            cx.FINAL_G = nc.dram_tensor("FINAL_G", [D], F32, kind="ExternalInput")
            cx.W_GU = nc.dram_tensor("W_GU", [1, NE, D, 2 * HID], F32, kind="ExternalInput")
            cx.W_DN = nc.dram_tensor("W_DN", [1, NE, HID, D], F32, kind="ExternalInput")
            OUT = nc.dram_tensor("OUT", [TOK, D], F32, kind="ExternalOutput")
        cx.STATE_IN = nc.dram_tensor("STATE_IN", [128, 8, 257], F32, kind="ExternalInput")
        cx.HALO_IN = nc.dram_tensor("HALO_IN", [128, 16, 3], F32, kind="ExternalInput")
        cx.STATE_OUT = nc.dram_tensor("STATE_OUT", [128, 8, 257], F32, kind="ExternalOutput")
        cx.HALO_OUT = nc.dram_tensor("HALO_OUT", [128, 16, 3], F32, kind="ExternalOutput")
        XIN = nc.dram_tensor("XIN", [TOK, D], F32, kind="ExternalInput")
        XMID = nc.dram_tensor("XMID", [TOK, D], F32, kind="Internal")
        cx.QKP = nc.dram_tensor("QKP", [16, 128, 3 + TOK], BF16, kind="Internal")
        cx.VM = nc.dram_tensor("VM", [TOK, 8, 257], BF16, kind="Internal")
        cx.OPRE = nc.dram_tensor("OPRE", [TOK, D], BF16, kind="Internal")
        cx.GATES = nc.dram_tensor("GATES", [TOK, 16], F32, kind="Internal")
        cx.HN = nc.dram_tensor("HN", [TOK, D], BF16, kind="Internal")
        mlstm_layer(p, cx, XIN, XMID, full)
        if full:
            moe_stage(p, cx, 1, XMID, OUT, final_g=cx.FINAL_G)
    return nc


def build_fused():
    nc = bass.Bass("TRN2", target_bir_lowering=False)
    cx = Ctx()
    with ExitStack() as st:
        p = _common(nc, cx, st)
        cx.WL = lambda L: L
        ei = lambda name, shape, dt=F32: nc.dram_tensor(name, list(shape), dt, kind="ExternalInput")
        it = lambda name, shape, dt=F32: nc.dram_tensor(name, list(shape), dt, kind="Internal")
        cx.MODD = it("MODD", [2, 6 * D])
        cx.CVT = ei("CVT", [128, 16])
        cx.ADA_W = ei("ADA_W", [2, D, 6 * D])
        cx.ADA_B = ei("ADA_B", [2, 6 * D])
        cx.ATTN_W_IN = ei("ATTN_W_IN", [D, 3 * D])
        cx.ATTN_W_OUT = ei("ATTN_W_OUT", [D, D])
        cx.LQ1 = ei("LQ1", [1, 128]); cx.LK1 = ei("LK1", [1, 128]); cx.LQ2 = ei("LQ2", [1, 128]); cx.LK2 = ei("LK2", [1, 128])
        cx.ATTN_HG = ei("ATTN_HG", [1, 256])
        cx.PREVFLAG = ei("PREVFLAG", [128, 1])
        cx.ODDFLAG = ei("ODDFLAG", [128, 1])
        cx.POS_OWN = ei("POS_OWN", [1, TOK], I32)
        cx.POS_PREV = ei("POS_PREV", [1, TOK], I32)
        cx.W_GU = ei("W_GU", [2, NE, D, 2 * HID])
        cx.W_DN = ei("W_DN", [2, NE, HID, D])
        cx.ML_W_IN = ei("ML_W_IN", [D, 3 * D + 16])
        cx.ML_W_OUT = ei("ML_W_OUT", [D, D])
        cx.CONV_W = ei("CONV_W", [128, 16, 4]); cx.CONV_B = ei("CONV_B", [128, 16]); cx.GATE_B = ei("GATE_B", [1, 16])
        cx.ML_HG = ei("ML_HG", [1, D]); cx.FINAL_G = ei("FINAL_G", [D])
        XIN = ei("XIN", [TOK, D]); XPREV = ei("XPREV", [TOK, D])
        OUT = nc.dram_tensor("OUT", [TOK, D], F32, kind="ExternalOutput")
        XMID0 = it("XMID0", [TOK, D]); X1 = it("X1", [TOK, D]); XMID1 = it("XMID1", [TOK, D])
        cx.QTD = it("QTD", [16, 128, TOK], BF16); cx.KTD = it("KTD", [16, 128, 2 * TOK], BF16)
        cx.VD = it("VD", [2 * TOK, 8, 257], BF16); cx.OAD = it("OAD", [TOK, D], BF16)
        cx.QKP = it("QKP", [16, 128, 3 + TOK], BF16); cx.VM = it("VM", [TOK, 8, 257], BF16)
        cx.OPRE = it("OPRE", [TOK, D], BF16); cx.GATES = it("GATES", [TOK, 16]); cx.HN = it("HN", [TOK, D], BF16)
        cx.ST_LOC = it("ST_LOC", [128, 8 * 257 + 48]); cx.ST_ALL = it("ST_ALL", [256, 8 * 257 + 48])
        S = STAGES or ("mod", "attn", "moe0", "ml", "moe1")
        if "mod" in S:
            mod_stage(p, cx, 0)
            mod_stage(p, cx, 1)
        if "attn" in S:
            attn_layer(p, cx, XIN, XPREV, XMID0)
        if "moe0" in S:
            moe_stage(p, cx, 0, XMID0, X1)
        if "ml" in S:
            mlstm_layer(p, cx, X1, XMID1, True, fused=True)
        if "moe1" in S:
            moe_stage(p, cx, 1, XMID1, OUT, final_g=cx.FINAL_G)
    return nc


def kernel(**inp):
    f32 = np.float32
    x = np.asarray(inp["x"], f32)
    c = np.asarray(inp["c"], f32)
    pos = np.asarray(inp["positions"], np.int32)
    wr = np.concatenate([inp["moe_w_group"], inp["moe_w_expert"]], axis=-1).reshape(2, 16, 128, 36).transpose(0, 2, 1, 3)
    wr = np.ascontiguousarray(wr, f32)
    brt = np.ascontiguousarray(np.concatenate([inp["moe_b_group"], inp["moe_b_expert"]], axis=-1), f32)
    cw = np.ascontiguousarray(np.asarray(inp["mlstm_conv_w"][0], f32).reshape(4, 16, 128).transpose(2, 1, 0))
    cbias = np.ascontiguousarray(np.asarray(inp["mlstm_conv_b"][0], f32).reshape(16, 128).T)
    n = 8
    common = {"CONSTS": make_consts(), "NORM_MIX_G": np.ascontiguousarray(inp["norm_mix_g"], f32), "NORM_FFN_G": np.ascontiguousarray(inp["norm_ffn_g"], f32),
              "WR": wr, "BRT": brt, "ADA_W": inp["ada_w"], "ADA_B": inp["ada_b"],
              "ATTN_W_IN": inp["attn_w_in"][0], "ATTN_W_OUT": inp["attn_w_out"][0],
              "LQ1": inp["attn_lambda_q1"], "LK1": inp["attn_lambda_k1"], "LQ2": inp["attn_lambda_q2"], "LK2": inp["attn_lambda_k2"],
              "ATTN_HG": inp["attn_head_norm_g"], "W_GU": inp["moe_w_gu"], "W_DN": inp["moe_w_down"],
              "ML_W_IN": inp["mlstm_w_in"][0], "ML_W_OUT": inp["mlstm_w_out"][0], "CONV_W": cw, "CONV_B": cbias,
              "GATE_B": inp["mlstm_gate_b"], "ML_HG": inp["mlstm_head_norm_g"], "FINAL_G": inp["final_norm_g"]}
    zx = np.zeros((TOK, D), f32)
    zp = np.zeros((1, TOK), np.int32)
    maps = []
    for core in range(n):
        b, hf = core // 2, core % 2
        sl = slice(hf * TOK, (hf + 1) * TOK)
        m = dict(common)
        m.update({
            "CVT": np.ascontiguousarray(c[b].reshape(16, 128).T),
            "PREVFLAG": np.full((128, 1), 0.0 if hf == 1 else -30000.0, f32),
            "ODDFLAG": np.full((128, 1), float(hf), f32),
            "POS_OWN": np.ascontiguousarray(pos[b, sl].reshape(1, TOK)),
            "POS_PREV": np.ascontiguousarray(pos[b, :TOK].reshape(1, TOK)) if hf == 1 else zp,
            "XIN": np.ascontiguousarray(x[b, sl]),
            "XPREV": np.ascontiguousarray(x[b, :TOK]) if hf == 1 else zx,
        })
        maps.append(m)
    nc = build_fused()
    if NCORES_DEBUG:
        res = run_bass_kernel_spmd(nc, maps[:NCORES_DEBUG], core_ids=list(range(NCORES_DEBUG))).results
        return res
    res = run_bass_kernel_spmd(nc, maps, core_ids=list(range(n))).results
    out = np.empty((4, 2 * TOK, D), f32)
    for core in range(n):
        b, hf = core // 2, core % 2
        out[b, hf * TOK:(hf + 1) * TOK] = res[core]["OUT"]
    return out
```

```python
import math
from contextlib import ExitStack
import numpy as np
import concourse.bass as bass
import concourse.mybir as mybir
from concourse.bass_utils import run_bass_kernel_spmd

F32 = mybir.dt.float32
BF16 = mybir.dt.bfloat16
I32 = mybir.dt.int32
AF = mybir.ActivationFunctionType
ALU = mybir.AluOpType
AX = mybir.AxisListType

D = 2048
TOK = 2048
NT = TOK // 128
NE = 32
CAP = 512
NBLK = CAP // 128
NSLOT = NE * CAP
HID = 1024
EPS = 1e-6

ENGS = ("pe", "act", "dve", "pool", "sp")
SAME_ENG_SYNC = True
N_DMA_SEMS = 40
REPLICA = [[0, 1], [2, 3], [4, 5], [6, 7]]
NCORES_DEBUG = 0
STAGES = None


class Prog:
    def __init__(self, nc, stack):
        self.nc = nc
        self.stack = stack
        self.cnt = {e: 0 for e in ENGS}
        self.esem = {e: stack.enter_context(nc.semaphore("s_" + e)) for e in ENGS}
        self.dsem = [stack.enter_context(nc.semaphore("d%d" % i)) for i in range(N_DMA_SEMS)]
        self.dval = [0] * N_DMA_SEMS
        self.drr = 0
        self.csem = stack.enter_context(nc.semaphore("csem"))
        self.cval = 0
        self.known = {e: {} for e in ENGS}
        self._reset()
        self.uid = 0

    def _reset(self):
        self.ops = {e: [] for e in ENGS}
        self.last_w = {}
        self.readers = {}

    def sb(self, st, name, shape, dt):
        self.uid += 1
        return st.enter_context(self.nc.sbuf_tensor("%s_%d" % (name, self.uid), list(shape), dt))

    def _deps(self, eng, reads, writes):
        deps = []
        for r in reads:
            t = self.last_w.get(r)
            if t is not None:
                deps.append(t)
        for w in writes:
            t = self.last_w.get(w)
            if t is not None:
                deps.append(t)
            deps.extend(self.readers.get(w, ()))
        waits = []
        kn = self.known[eng]
        for (sem, val, deng, sid) in deps:
            if deng == eng and (eng == "pe" or not SAME_ENG_SYNC):
                continue
            if kn.get(sid, 0) >= val:
                continue
            kn[sid] = val
            waits.append((sem, val))
        return waits

    def _commit(self, tok, reads, writes):
        for w in writes:
            self.last_w[w] = tok
            self.readers[w] = []
        for r in reads:
            if r in writes:
                continue
            lst = self.readers.setdefault(r, [])
            lst.append(tok)
            if len(lst) > 48:
                latest = {}
                keep = []
                for t in lst:
                    if t[2] == "dma":
                        keep.append(t)
                    else:
                        latest[t[2]] = t
                self.readers[r] = keep[-40:] + list(latest.values())

    def op(self, eng, fn, reads=(), writes=(), inc=True):
        waits = self._deps(eng, reads, writes)
        if inc:
            self.cnt[eng] += 1
            tok = (self.esem[eng], self.cnt[eng], eng, "e_" + eng)
            self.ops[eng].append((waits, fn, (self.esem[eng], 1)))
        else:
            tok = (self.esem[eng], self.cnt[eng] + 1, eng, "e_" + eng)
            self.ops[eng].append((waits, fn, None))
        self._commit(tok, reads, writes)

    def coll(self, fn, reads=(), writes=()):
        waits = self._deps("pool", reads, writes)
        self.cval += 1
        tok = (self.csem, self.cval, "dma", "csem")
        self.ops["pool"].append((waits, fn, (self.csem, 1)))
        self._commit(tok, reads, writes)

    def dma(self, q, fn, reads=(), writes=()):
        s = self.drr
        self.drr = (self.drr + 1) % N_DMA_SEMS
        waits = self._deps(q, reads, writes)
        prev = self.dval[s]
        sid = "d%d" % s
        if prev > 0 and self.known[q].get(sid, 0) < prev:
            self.known[q][sid] = prev
            waits.append((self.dsem[s], prev))
        self.dval[s] += 16
        tok = (self.dsem[s], self.dval[s], "dma", sid)
        self.ops[q].append((waits, fn, (self.dsem[s], 16)))
        self._commit(tok, reads, writes)

    def barrier(self):
        for e in ENGS:
            waits = []
            for e2 in ENGS:
                if e2 != e and self.cnt[e2] > 0 and self.known[e].get("e_" + e2, 0) < self.cnt[e2]:
                    self.known[e]["e_" + e2] = self.cnt[e2]
                    waits.append((self.esem[e2], self.cnt[e2]))
            for i in range(N_DMA_SEMS):
                sid = "d%d" % i
                if self.dval[i] > 0 and self.known[e].get(sid, 0) < self.dval[i]:
                    self.known[e][sid] = self.dval[i]
                    waits.append((self.dsem[i], self.dval[i]))
            if self.cval > 0 and self.known[e].get("csem", 0) < self.cval:
                self.known[e]["csem"] = self.cval
                waits.append((self.csem, self.cval))
            if waits:
                self.ops[e].append((waits, None, None))

    def emit(self):
        self.barrier()
        ops = self.ops
        with self.nc.Block() as block:
            def run(engname):
                def body(e):
                    for (waits, fn, inc) in ops[engname]:
                        for (sem, val) in waits:
                            e.wait_ge(sem, val)
                        if fn is not None:
                            ins = fn(e)
                            if inc is not None:
                                ins.then_inc(inc[0], inc[1])
                return body
            block.tensor(run("pe"))
            block.scalar(run("act"))
            block.vector(run("dve"))
            block.gpsimd(run("pool"))
            block.sync(run("sp"))
        self._reset()


C_ID, C_LS, C_ON, C_ECAP, C_TRI, C_CM, C_PERM, C_INVF, C_SGN, C_IOTA, C_W = 0, 128, 256, 384, 416, 544, 672, 704, 705, 706, 738


def make_consts():
    c = np.zeros((128, C_W), np.float32)
    i = np.arange(128)
    c[:, C_ID:C_ID + 128] = np.eye(128)
    c[:, C_LS:C_LS + 128] = (i[:, None] < i[None, :])
    c[:, C_ON:C_ON + 128] = 1.0
    c[:, C_ECAP:C_ECAP + 32] = (np.arange(32) * CAP)[None, :]
    c[:, C_TRI:C_TRI + 128] = (i[:, None] <= i[None, :])
    c[:, C_CM:C_CM + 128] = (i[:, None] <= i[None, :])
    for pp in range(32):
        c[(pp + 16) % 32, C_PERM + pp] = 1.0
    half = 16
    invf = 500000.0 ** (-np.arange(half, dtype=np.float32) * 2.0 / 32)
    c[:32, C_INVF] = np.tile(invf, 2)
    c[:16, C_SGN] = -1.0
    c[16:32, C_SGN] = 1.0
    c[:, C_IOTA:C_IOTA + 32] = np.arange(32)[None, :]
    return c


class Ctx:
    pass


def bc(ap1d, n=128):
    return ap1d.partition_broadcast(n)


def load_consts(p, cx, st):
    cx.cf = p.sb(st, "cf", [128, C_W], F32)
    cx.cb = p.sb(st, "cb", [128, C_W], BF16)
    p.dma("sp", lambda e: e.dma_start(out=cx.cf[:], in_=cx.CONSTS.ap()), writes=["cf"])
    p.op("dve", lambda e: e.tensor_copy(out=cx.cb[:], in_=cx.cf[:]), reads=["cf"], writes=["cb"])
    p.emit()


def rms_mod_tile(p, cx, xs, G, SH, hf, hb, ss, nm, rd=(), wr=()):
    junk = cx.junk
    p.op("act", lambda e: e.activation(out=junk[:], in_=xs[:], func=AF.Square, accum_out=ss[:, 0:1]),
         reads=[nm + "xs"], writes=["junk", nm + "ss"])
    p.op("dve", lambda e: e.tensor_scalar(out=ss[:, 1:2], in0=ss[:, 0:1], scalar1=1.0 / D, scalar2=EPS, op0=ALU.mult, op1=ALU.add),
         reads=[nm + "ss"], writes=[nm + "ss"])
    p.op("act", lambda e: e.activation(out=ss[:, 2:3], in_=ss[:, 1:2], func=AF.Sqrt), reads=[nm + "ss"], writes=[nm + "ss"])
    p.op("dve", lambda e: e.reciprocal(out=ss[:, 3:4], in_=ss[:, 2:3]), reads=[nm + "ss"], writes=[nm + "ss"])
    tmp = cx.tmpf
    p.op("dve", lambda e: e.scalar_tensor_tensor(out=tmp[:], in0=xs[:], scalar=ss[:, 3:4], in1=G[:], op0=ALU.mult, op1=ALU.mult),
         reads=[nm + "xs", nm + "ss"] + list(rd), writes=["tmpf"])
    if hf is not None:
        p.op("pool", lambda e: e.tensor_tensor(out=hf[:], in0=tmp[:], in1=SH[:], op=ALU.add), reads=["tmpf"] + list(rd), writes=[nm + "hf"])
        p.op("act", lambda e: e.activation(out=hb[:], in_=hf[:], func=AF.Copy), reads=[nm + "hf"], writes=[nm + "hb"])
    else:
        p.op("pool", lambda e: e.tensor_tensor(out=hb[:], in0=tmp[:], in1=SH[:], op=ALU.add), reads=["tmpf"] + list(rd), writes=[nm + "hb"])


def load_mod_tiles(p, cx, st, L, which, gname):
    base = 3 * D * which
    G = p.sb(st, "G", [128, D], F32)
    SH = p.sb(st, "SH", [128, D], F32)
    GT = p.sb(st, "GT", [128, D], F32)
    gn = p.sb(st, "gn", [128, D], F32)
    md = cx.MODD.ap()
    p.dma("sp", lambda e: e.dma_start(out=SH[:], in_=bc(md[L, base:base + D])), reads=["MODD"], writes=["SH"])
    p.dma("act", lambda e: e.dma_start(out=G[:], in_=bc(md[L, base + D:base + 2 * D])), reads=["MODD"], writes=["G"])
    p.dma("sp", lambda e: e.dma_start(out=GT[:], in_=bc(md[L, base + 2 * D:base + 3 * D])), reads=["MODD"], writes=["GT"])
    p.dma("act", lambda e: e.dma_start(out=gn[:], in_=bc(gname.ap()[L, :])), writes=["gn"])
    p.op("dve", lambda e: e.scalar_tensor_tensor(out=G[:], in0=G[:], scalar=1.0, in1=gn[:], op0=ALU.add, op1=ALU.mult),
         reads=["G", "gn"], writes=["G"])
    return G, SH, GT


def mod_stage(p, cx, L):
    with ExitStack() as st:
        cT = p.sb(st, "cT", [128, 16], F32)
        cTb = p.sb(st, "cTb", [128, 16], BF16)
        ring = [p.sb(st, "aw%d" % i, [128, 4096], BF16) for i in range(4)]
        row = p.sb(st, "mrow", [1, 4096], F32)
        brow = p.sb(st, "brow", [1, 4096], F32)
        p.dma("sp", lambda e: e.dma_start(out=cT[:], in_=cx.CVT.ap()), writes=["cT"])
        p.op("act", lambda e: e.activation(out=cTb[:], in_=cT[:], func=AF.Silu), reads=["cT"], writes=["cTb"])
        aw = cx.ADA_W.ap()
        k = 0
        for ps_ in range(3):
            p.dma("sp", lambda e, ps_=ps_: e.dma_start(out=brow[:], in_=cx.ADA_B.ap()[L:L + 1, ps_ * 4096:(ps_ + 1) * 4096]), writes=["brow"])
            for kc in range(16):
                buf = ring[k % 4]
                bn = "aw%d" % (k % 4)
                k += 1
                p.dma("pool", lambda e, buf=buf, kc=kc, ps_=ps_: e.dma_start(out=buf[:], in_=aw[L, kc * 128:(kc + 1) * 128, ps_ * 4096:(ps_ + 1) * 4096]),
                      writes=[bn])
                for n in range(8):
                    p.op("pe", lambda e, buf=buf, kc=kc, n=n: e.matmul(cx.bank[n][0:1, :], lhsT=cTb[:, kc:kc + 1], rhs=buf[:, n * 512:(n + 1) * 512],
                                                                      start=(kc == 0), stop=(kc == 15)),
                         reads=[bn, "cTb"], writes=["bank%d" % n], inc=(n == 7))
            for n in range(8):
                p.op("dve", lambda e, n=n: e.tensor_tensor(out=row[:, n * 512:(n + 1) * 512], in0=cx.bank[n][0:1, :], in1=brow[:, n * 512:(n + 1) * 512], op=ALU.add),
                     reads=["bank%d" % n, "brow"], writes=["mrow"])
            p.dma("sp", lambda e, ps_=ps_: e.dma_start(out=cx.MODD.ap()[L:L + 1, ps_ * 4096:(ps_ + 1) * 4096], in_=row[:]), reads=["mrow"], writes=["MODD"])
        p.emit()


def moe_stage(p, cx, L, XIN, XOUT, final_g=None):
    with ExitStack() as st0:
        d1i = p.sb(st0, "d1i", [128, NT], I32)
        d2i = p.sb(st0, "d2i", [128, NT], I32)
        gts = p.sb(st0, "gts", [128, 2 * NT], F32)
        xin = XIN.ap()
        with ExitStack() as st:
            G, SH, GT_ = load_mod_tiles(p, cx, st, L, 1, cx.NORM_FFN_G)
            cf, cb = cx.cf, cx.cb
            xs2 = [p.sb(st, "xs%d" % i, [128, D], F32) for i in range(2)]
            hf2 = [p.sb(st, "hf%d" % i, [128, D], F32) for i in range(2)]
            hb2 = [p.sb(st, "hb%d" % i, [128, D], BF16) for i in range(2)]
            cx.junk = p.sb(st, "junk", [128, D], BF16)
            cx.tmpf = p.sb(st, "tmpf", [128, D], F32)
            hT = p.sb(st, "hTf", [128, 16, 128], F32)
            wr = p.sb(st, "wr", [128, 16, 36], F32)
            br = p.sb(st, "br", [128, 36], F32)
            macc = p.sb(st, "macc", [128, 32], BF16)
            sm = [p.sb(st, "sm%d" % i, [128, 160], F32) for i in range(2)]
            mk = [p.sb(st, "mk%d" % i, [128, 32], BF16) for i in range(2)]
            p.dma("sp", lambda e: e.dma_start(out=wr[:], in_=cx.WR.ap()[L]), writes=["wr"])
            p.dma("sp", lambda e: e.dma_start(out=br[:], in_=bc(cx.BRT.ap()[L, :])), writes=["br"])
            p.op("dve", lambda e: e.memset(macc[:], 0.0), writes=["macc"])
            for t in range(NT):
                b = t % 2
                nm = "r%d" % b
                xs, hf, hb, s, m = xs2[b], hf2[b], hb2[b], sm[b], mk[b]
                p.dma("sp", lambda e, xs=xs, t=t: e.dma_start(out=xs[:], in_=xin[t * 128:(t + 1) * 128, :]), reads=["XIN"], writes=[nm + "xs"])
                rms_mod_tile(p, cx, xs, G, SH, hf, hb, s, nm, rd=["G", "SH"])
                for q4 in range(4):
                    for j in range(4):
                        kc = q4 * 4 + j
                        p.op("pe", lambda e, hf=hf, kc=kc, j=j, q4=q4: e.transpose(out=cx.bank[q4][:, j * 128:(j + 1) * 128], in_=hf[:, kc * 128:(kc + 1) * 128], identity=cf[:, C_ID:C_ID + 128]),
                             reads=[nm + "hf"], writes=["bank%d" % q4])
                    eng = "act" if q4 % 2 == 0 else "dve"
                    if eng == "act":
                        p.op("act", lambda e, q4=q4: e.activation(out=hT[:, q4 * 4:(q4 + 1) * 4, :], in_=cx.bank[q4][:].rearrange("p (a b) -> p a b", a=4), func=AF.Copy),
                             reads=["bank%d" % q4], writes=["hT%d" % q4])
                    else:
                        p.op("dve", lambda e, q4=q4: e.tensor_copy(out=hT[:, q4 * 4:(q4 + 1) * 4, :], in_=cx.bank[q4][:].rearrange("p (a b) -> p a b", a=4)),
                             reads=["bank%d" % q4], writes=["hT%d" % q4])
                lgp = cx.bank[4]
                for kc in range(16):
                    p.op("pe", lambda e, kc=kc: e.matmul(lgp[:, 0:36], lhsT=hT[:, kc, :], rhs=wr[:, kc, :], start=(kc == 0), stop=(kc == 15)),
                         reads=["hT%d" % (kc // 4), "wr"], writes=["bank4"], inc=(kc == 15))
                lg = s[:, 8:44]
                sn = nm + "s"
                p.op("dve", lambda e, lg=lg: e.tensor_tensor(out=lg, in0=lgp[:, 0:36], in1=br[:], op=ALU.add), reads=["bank4", "br"], writes=[sn])
                p.op("dve", lambda e, s=s: e.reduce_max(out=s[:, 44:45], in_=s[:, 8:12], axis=AX.X), reads=[sn], writes=[sn])
                p.op("dve", lambda e, s=s: e.tensor_scalar(out=s[:, 45:46], in0=s[:, 44:45], scalar1=-1.0, scalar2=None, op0=ALU.mult), reads=[sn], writes=[sn])
                p.op("dve", lambda e, s=s: e.tensor_scalar(out=s[:, 48:52], in0=s[:, 8:12], scalar1=s[:, 44:45], scalar2=None, op0=ALU.is_equal), reads=[sn], writes=[sn])
                p.op("act", lambda e, s=s: e.activation(out=s[:, 100:104], in_=s[:, 8:12], func=AF.Exp, bias=s[:, 45:46], accum_out=s[:, 46:47]), reads=[sn], writes=[sn])
                p.op("dve", lambda e, s=s: e.reciprocal(out=s[:, 47:48], in_=s[:, 46:47]), reads=[sn], writes=[sn])
                p.op("dve", lambda e, s=s: e.tensor_scalar(out=s[:, 52:56], in0=s[:, 48:52], scalar1=1e30, scalar2=-1e30, op0=ALU.mult, op1=ALU.add), reads=[sn], writes=[sn])
                p.op("dve", lambda e, s=s: e.tensor_tensor(out=s[:, 56:88].rearrange("p (g k) -> p g k", g=4), in0=s[:, 12:44].rearrange("p (g k) -> p g k", g=4),
                                                           in1=s[:, 52:56].unsqueeze(2).to_broadcast([128, 4, 8]), op=ALU.add), reads=[sn], writes=[sn])
                p.op("dve", lambda e, s=s: e.max(out=s[:, 88:96], in_=s[:, 56:88]), reads=[sn], writes=[sn])
                p.op("dve", lambda e, s=s: e.tensor_tensor(out=s[:, 96:97], in0=s[:, 89:90], in1=s[:, 88:89], op=ALU.subtract), reads=[sn], writes=[sn])
                p.op("act", lambda e, s=s: e.activation(out=s[:, 97:98], in_=s[:, 96:97], func=AF.Exp), reads=[sn], writes=[sn])
                p.op("dve", lambda e, s=s: e.tensor_scalar(out=s[:, 98:99], in0=s[:, 97:98], scalar1=1.0, scalar2=None, op0=ALU.add), reads=[sn], writes=[sn])
                p.op("dve", lambda e, s=s: e.reciprocal(out=s[:, 99:100], in_=s[:, 98:99]), reads=[sn], writes=[sn])
                p.op("dve", lambda e, s=s, t=t: e.tensor_tensor(out=gts[:, 2 * t:2 * t + 1], in0=s[:, 99:100], in1=s[:, 47:48], op=ALU.mult), reads=[sn], writes=["gts"])
                p.op("dve", lambda e, s=s, t=t: e.tensor_tensor(out=gts[:, 2 * t + 1:2 * t + 2], in0=gts[:, 2 * t:2 * t + 1], in1=s[:, 97:98], op=ALU.mult), reads=[sn, "gts"], writes=["gts"])
                p.op("dve", lambda e, s=s: e.tensor_scalar(out=s[:, 100:132], in0=s[:, 56:88], scalar1=s[:, 88:89], scalar2=None, op0=ALU.is_equal), reads=[sn], writes=[sn])
                p.op("dve", lambda e, s=s: e.tensor_scalar(out=s[:, 8:40], in0=s[:, 56:88], scalar1=s[:, 89:90], scalar2=None, op0=ALU.is_equal), reads=[sn], writes=[sn])
                p.op("dve", lambda e, s=s, m=m: e.tensor_tensor(out=m[:], in0=s[:, 100:132], in1=s[:, 8:40], op=ALU.add), reads=[sn], writes=[nm + "mk"])
                pp = cx.bank[5]
                p.op("pe", lambda e, m=m: e.matmul(pp[:, 0:32], lhsT=cb[:, C_LS:C_LS + 128], rhs=m[:], start=True, stop=False), reads=[nm + "mk"], writes=["bank5"])
                p.op("pe", lambda e: e.matmul(pp[:, 0:32], lhsT=cb[:, C_ON:C_ON + 128], rhs=macc[:], start=False, stop=True), reads=["macc"], writes=["bank5"])
                p.op("dve", lambda e, s=s: e.tensor_tensor(out=s[:, 56:88], in0=pp[:, 0:32], in1=cf[:, C_ECAP:C_ECAP + 32], op=ALU.add), reads=["bank5", sn], writes=[sn])
                p.op("dve", lambda e, m=m: e.tensor_tensor(out=macc[:], in0=macc[:], in1=m[:], op=ALU.add), reads=["macc", nm + "mk", "bank5"], writes=["macc"])
                p.op("dve", lambda e, s=s: e.tensor_tensor(out=s[:, 100:132], in0=s[:, 100:132], in1=s[:, 56:88], op=ALU.mult), reads=[sn], writes=[sn])
                p.op("dve", lambda e, s=s: e.reduce_sum(out=s[:, 132:133], in_=s[:, 100:132], axis=AX.X), reads=[sn], writes=[sn])
                p.op("dve", lambda e, s=s: e.tensor_tensor(out=s[:, 8:40], in0=s[:, 8:40], in1=s[:, 56:88], op=ALU.mult), reads=[sn], writes=[sn])
                p.op("dve", lambda e, s=s: e.reduce_sum(out=s[:, 133:134], in_=s[:, 8:40], axis=AX.X), reads=[sn], writes=[sn])
                p.op("dve", lambda e, s=s, t=t: e.tensor_copy(out=d1i[:, t:t + 1], in_=s[:, 132:133]), reads=[sn], writes=["d1i"])
                p.op("dve", lambda e, s=s, t=t: e.tensor_copy(out=d2i[:, t:t + 1], in_=s[:, 133:134]), reads=[sn], writes=["d2i"])
                p.dma("pool", lambda e, hb=hb, t=t: e.indirect_dma_start(out=cx.XS.ap(), out_offset=bass.IndirectOffsetOnAxis(ap=d1i[:, t:t + 1], axis=0), in_=hb[:], in_offset=None),
                      reads=[nm + "hb", "d1i"], writes=["XSa%d" % t])
                p.dma("pool", lambda e, hb=hb, t=t: e.indirect_dma_start(out=cx.XS.ap(), out_offset=bass.IndirectOffsetOnAxis(ap=d2i[:, t:t + 1], axis=0), in_=hb[:], in_offset=None),
                      reads=[nm + "hb", "d2i"], writes=["XSb%d" % t])
            p.emit()
        with ExitStack() as st:
            cb = cx.cb
            NWG = 32
            NWD = 12
            wg = [p.sb(st, "wg%d" % i, [128, 2, 512], BF16) for i in range(NWG)]
            wd = [p.sb(st, "wd%d" % i, [128, D], BF16) for i in range(NWD)]
            xg = p.sb(st, "xg", [128, NBLK, D], BF16)
            xT = [p.sb(st, "xT%d" % i, [128, 16, CAP], BF16) for i in range(2)]
            hTb = p.sb(st, "hTb", [128, 8, CAP], BF16)
            sg = [p.sb(st, "sg%d" % i, [128, 256], F32) for i in range(2)]
            yb = [p.sb(st, "yb%d" % i, [128, D], F32) for i in range(2)]
            xsv = cx.XS.ap()[0:NSLOT].rearrange("(e b p) d -> e p b d", b=NBLK, p=128)
            ysv = cx.YS.ap()[0:NSLOT].rearrange("(e b p) d -> e b p d", b=NBLK, p=128)
            wguv = cx.W_GU.ap()
            wdv = cx.W_DN.ap()
            kg = 0
            kd = 0
            ky = 0
            for ex in range(NE):
                xb_ = ex % 2
                xTb = xT[xb_]
                p.dma("sp", lambda e, ex=ex: e.dma_start(out=xg[:], in_=xsv[ex]), writes=["xg"])
                tcnt = 0
                for blk in range(NBLK):
                    for hh in range(2):
                        bk = cx.bank[4 + tcnt % 4]
                        bkn = "bank%d" % (4 + tcnt % 4)
                        tcnt += 1
                        bkv = bk[:].bitcast(BF16)
                        for j in range(8):
                            kc = hh * 8 + j
                            p.op("pe", lambda e, bkv=bkv, j=j, kc=kc, blk=blk: e.transpose(out=bkv[:, j * 128:(j + 1) * 128], in_=xg[:, blk, kc * 128:(kc + 1) * 128], identity=cb[:, C_ID:C_ID + 128]),
                                 reads=["xg"], writes=[bkn], inc=(j == 7))
                        if tcnt % 2 == 0:
                            p.op("act", lambda e, bkv=bkv, hh=hh, blk=blk, xTb=xTb: e.activation(out=xTb[:, hh * 8:(hh + 1) * 8, blk * 128:(blk + 1) * 128], in_=bkv.rearrange("p (a b) -> p a b", a=8), func=AF.Copy),
                                 reads=[bkn], writes=["xT%d" % xb_])
                        else:
                            p.op("dve", lambda e, bkv=bkv, hh=hh, blk=blk, xTb=xTb: e.tensor_copy(out=xTb[:, hh * 8:(hh + 1) * 8, blk * 128:(blk + 1) * 128], in_=bkv.rearrange("p (a b) -> p a b", a=8)),
                                 reads=[bkn], writes=["xT%d" % xb_])
                for hp in range(2):
                    wgl = []
                    for kc in range(16):
                        i = kg % NWG
                        kg += 1
                        wgl.append(i)
                        p.dma("pool", lambda e, i=i, ex=ex, kc=kc, hp=hp: e.dma_start(out=wg[i][:], in_=wguv[cx.WL(L), ex, kc * 128:(kc + 1) * 128, :].rearrange("k (g c) -> k g c", g=2)[:, :, hp * 512:(hp + 1) * 512]),
                              writes=["wg%d" % i])
                    for sb_ in range(CAP // 256):
                        bo = 4 * ((hp * (CAP // 256) + sb_) % 2)
                        for kc in range(16):
                            i = wgl[kc]
                            for gi in range(2):
                                for j in range(4):
                                    bi = bo + gi * 2 + j // 2
                                    p.op("pe", lambda e, i=i, gi=gi, j=j, bi=bi, kc=kc, xTb=xTb, sb_=sb_: e.matmul(cx.bank[bi][:, (j % 2) * 256:(j % 2 + 1) * 256], lhsT=wg[i][:, gi, j * 128:(j + 1) * 128], rhs=xTb[:, kc, sb_ * 256:(sb_ + 1) * 256],
                                                                                                                  start=(kc == 0 and j % 2 == 0), stop=(kc == 15), skip_group_check=True),
                                         reads=["wg%d" % i, "xT%d" % xb_], writes=["bank%d" % bi], inc=(kc == 15))
                        for j in range(4):
                            s_ = sg[j % 2]
                            p.op("act", lambda e, s_=s_, j=j, bo=bo: e.activation(out=s_[:], in_=cx.bank[bo + j // 2][:, (j % 2) * 256:(j % 2 + 1) * 256], func=AF.Silu),
                                 reads=["bank%d" % (bo + j // 2)], writes=["sg%d" % (j % 2)])
                            p.op("dve", lambda e, s_=s_, j=j, hp=hp, sb_=sb_, bo=bo: e.tensor_tensor(out=hTb[:, hp * 4 + j, sb_ * 256:(sb_ + 1) * 256], in0=s_[:], in1=cx.bank[bo + 2 + j // 2][:, (j % 2) * 256:(j % 2 + 1) * 256], op=ALU.mult),
                                 reads=["sg%d" % (j % 2), "bank%d" % (bo + 2 + j // 2)], writes=["hTb"])
                wdl = []
                for hc in range(8):
                    i = kd % NWD
                    kd += 1
                    wdl.append(i)
                    p.dma("pool", lambda e, i=i, ex=ex, hc=hc: e.dma_start(out=wd[i][:], in_=wdv[cx.WL(L), ex, hc * 128:(hc + 1) * 128, :]), writes=["wd%d" % i])
                for blk in range(NBLK):
                    y = yb[ky % 2]
                    yn = "yb%d" % (ky % 2)
                    ky += 1
                    db = 4 * ((blk + 1) % 2)
                    for cg in range(4):
                        for hc in range(8):
                            i = wdl[hc]
                            p.op("pe", lambda e, i=i, cg=cg, hc=hc, blk=blk, db=db: e.matmul(cx.bank[db + cg][:], lhsT=hTb[:, hc, blk * 128:(blk + 1) * 128], rhs=wd[i][:, cg * 512:(cg + 1) * 512], start=(hc == 0), stop=(hc == 7)),
                                 reads=["hTb", "wd%d" % i], writes=["bank%d" % (db + cg)], inc=(hc == 7))
                        if cg % 2 == 0:
                            p.op("act", lambda e, y=y, cg=cg, db=db: e.activation(out=y[:, cg * 512:(cg + 1) * 512], in_=cx.bank[db + cg][:], func=AF.Copy), reads=["bank%d" % (db + cg)], writes=[yn])
                        else:
                            p.op("dve", lambda e, y=y, cg=cg, db=db: e.tensor_copy(out=y[:, cg * 512:(cg + 1) * 512], in_=cx.bank[db + cg][:]), reads=["bank%d" % (db + cg)], writes=[yn])
                    p.dma("act", lambda e, y=y, ex=ex, blk=blk: e.dma_start(out=ysv[ex, blk], in_=y[:]), reads=[yn], writes=["YS%d_%d" % (ex, blk)])
            p.emit()
        with ExitStack() as st:
            xs2 = [p.sb(st, "cxs%d" % i, [128, D], F32) for i in range(2)]
            y1 = [p.sb(st, "y1_%d" % i, [128, D], F32) for i in range(2)]
            y2 = [p.sb(st, "y2_%d" % i, [128, D], F32) for i in range(2)]
            GT = p.sb(st, "GT", [128, D], F32)
            p.dma("sp", lambda e: e.dma_start(out=GT[:], in_=bc(cx.MODD.ap()[L, 5 * D:6 * D])), writes=["GT"])
            if final_g is not None:
                FG = p.sb(st, "FG", [128, D], F32)
                p.dma("sp", lambda e: e.dma_start(out=FG[:], in_=bc(final_g.ap()[:])), writes=["FG"])
                cx.junk = p.sb(st, "junk", [128, D], BF16)
                fs = [p.sb(st, "fs%d" % i, [128, 4], F32) for i in range(2)]
            xout = XOUT.ap()
            for t in range(NT):
                b = t % 2
                xs, a1, a2 = xs2[b], y1[b], y2[b]
                p.dma("sp", lambda e, xs=xs, t=t: e.dma_start(out=xs[:], in_=xin[t * 128:(t + 1) * 128, :]), reads=["XIN"], writes=["cxs%d" % b])
                p.dma("pool", lambda e, a1=a1, t=t: e.indirect_dma_start(out=a1[:], out_offset=None, in_=cx.YS.ap(), in_offset=bass.IndirectOffsetOnAxis(ap=d1i[:, t:t + 1], axis=0)),
                      reads=["d1i"], writes=["y1_%d" % b])
                p.dma("pool", lambda e, a2=a2, t=t: e.indirect_dma_start(out=a2[:], out_offset=None, in_=cx.YS.ap(), in_offset=bass.IndirectOffsetOnAxis(ap=d2i[:, t:t + 1], axis=0)),
                      reads=["d2i"], writes=["y2_%d" % b])
                p.op("act", lambda e, a1=a1, t=t: e.activation(out=a1[:], in_=a1[:], func=AF.Copy, scale=gts[:, 2 * t:2 * t + 1]), reads=["y1_%d" % b, "gts"], writes=["y1_%d" % b])
                p.op("dve", lambda e, a1=a1, a2=a2, t=t: e.scalar_tensor_tensor(out=a2[:], in0=a2[:], scalar=gts[:, 2 * t + 1:2 * t + 2], in1=a1[:], op0=ALU.mult, op1=ALU.add),
                     reads=["y1_%d" % b, "y2_%d" % b, "gts"], writes=["y2_%d" % b])
                p.op("pool", lambda e, a2=a2: e.tensor_tensor(out=a2[:], in0=a2[:], in1=GT[:], op=ALU.mult), reads=["y2_%d" % b, "GT"], writes=["y2_%d" % b])
                p.op("dve", lambda e, a2=a2, xs=xs: e.tensor_tensor(out=xs[:], in0=a2[:], in1=xs[:], op=ALU.add), reads=["y2_%d" % b, "cxs%d" % b], writes=["cxs%d" % b])
                if final_g is not None:
                    f = fs[b]
                    p.op("act", lambda e, xs=xs, f=f: e.activation(out=cx.junk[:], in_=xs[:], func=AF.Square, accum_out=f[:, 0:1]), reads=["cxs%d" % b], writes=["junk", "fs%d" % b])
                    p.op("dve", lambda e, f=f: e.tensor_scalar(out=f[:, 1:2], in0=f[:, 0:1], scalar1=1.0 / D, scalar2=EPS, op0=ALU.mult, op1=ALU.add), reads=["fs%d" % b], writes=["fs%d" % b])
                    p.op("act", lambda e, f=f: e.activation(out=f[:, 2:3], in_=f[:, 1:2], func=AF.Sqrt), reads=["fs%d" % b], writes=["fs%d" % b])
                    p.op("dve", lambda e, f=f: e.reciprocal(out=f[:, 3:4], in_=f[:, 2:3]), reads=["fs%d" % b], writes=["fs%d" % b])
                    p.op("dve", lambda e, xs=xs, f=f: e.scalar_tensor_tensor(out=xs[:], in0=xs[:], scalar=f[:, 3:4], in1=FG[:], op0=ALU.mult, op1=ALU.mult),
                         reads=["cxs%d" % b, "fs%d" % b, "FG"], writes=["cxs%d" % b])
                p.dma("sp", lambda e, xs=xs, t=t: e.dma_start(out=xout[t * 128:(t + 1) * 128, :], in_=xs[:]), reads=["cxs%d" % b], writes=["XOUT"])
            p.emit()


def norm_to_hT(p, cx, XSRC, G, SH, hT, tb0=6):
    with ExitStack() as st:
        xs2 = [p.sb(st, "nxs%d" % i, [128, D], F32) for i in range(2)]
        hb2 = [p.sb(st, "nhb%d" % i, [128, D], BF16) for i in range(2)]
        ss2 = [p.sb(st, "nss%d" % i, [128, 4], F32) for i in range(2)]
        cx.junk = p.sb(st, "junk", [128, D], BF16)
        cx.tmpf = p.sb(st, "tmpf", [128, D], F32)
        cb = cx.cb
        for t in range(NT):
            b = t % 2
            nm = "n%d" % b
            xs, hb, ss = xs2[b], hb2[b], ss2[b]
            p.dma("sp", lambda e, xs=xs, t=t: e.dma_start(out=xs[:], in_=XSRC[t * 128:(t + 1) * 128, :]), writes=[nm + "xs"])
            rms_mod_tile(p, cx, xs, G, SH, None, hb, ss, nm, rd=["G", "SH"])
            for hh in range(2):
                bk = cx.bank[tb0 + hh]
                bkn = "bank%d" % (tb0 + hh)
                bkv = bk[:].bitcast(BF16)
                for j in range(8):
                    kc = hh * 8 + j
                    p.op("pe", lambda e, bkv=bkv, j=j, kc=kc, hb=hb: e.transpose(out=bkv[:, j * 128:(j + 1) * 128], in_=hb[:, kc * 128:(kc + 1) * 128], identity=cb[:, C_ID:C_ID + 128]),
                         reads=[nm + "hb"], writes=[bkn], inc=(j == 7))
                if hh == 0:
                    p.op("act", lambda e, bkv=bkv, hh=hh, t=t: e.activation(out=hT[:, hh * 8:(hh + 1) * 8, t * 128:(t + 1) * 128], in_=bkv.rearrange("p (a b) -> p a b", a=8), func=AF.Copy),
                         reads=[bkn], writes=["hT_%d_%d" % (t, hh)])
                else:
                    p.op("dve", lambda e, bkv=bkv, hh=hh, t=t: e.tensor_copy(out=hT[:, hh * 8:(hh + 1) * 8, t * 128:(t + 1) * 128], in_=bkv.rearrange("p (a b) -> p a b", a=8)),
                         reads=[bkn], writes=["hT_%d_%d" % (t, hh)])
        p.emit()


def proj_out_stage(p, cx, SRC, Wap, GT, XIN, XOUT):
    with ExitStack() as st:
        cb = cx.cb
        W = p.sb(st, "Wout", [128, 16, D], BF16)
        wv = Wap.rearrange("(kc k) c -> k kc c", k=128)
        for i in range(4):
            p.dma("pool", lambda e, i=i: e.dma_start(out=W[:, :, i * 512:(i + 1) * 512], in_=wv[:, :, i * 512:(i + 1) * 512]), writes=["Wout%d" % i])
        sr2 = [p.sb(st, "osr%d" % i, [128, D], BF16) for i in range(2)]
        sT2 = [p.sb(st, "osT%d" % i, [128, 16, 128], BF16) for i in range(2)]
        xs2 = [p.sb(st, "oxs%d" % i, [128, D], F32) for i in range(2)]
        tm2 = [p.sb(st, "otm%d" % i, [128, D], F32) for i in range(2)]
        for t in range(NT):
            b = t % 2
            sr, sT, xs, tm = sr2[b], sT2[b], xs2[b], tm2[b]
            p.dma("sp", lambda e, sr=sr, t=t: e.dma_start(out=sr[:], in_=SRC[t * 128:(t + 1) * 128, :]), writes=["osr%d" % b])
            p.dma("act", lambda e, xs=xs, t=t: e.dma_start(out=xs[:], in_=XIN[t * 128:(t + 1) * 128, :]), writes=["oxs%d" % b])
            for hh in range(2):
                bk = cx.bank[4 + hh]
                bkn = "bank%d" % (4 + hh)
                bkv = bk[:].bitcast(BF16)
                for j in range(8):
                    kc = hh * 8 + j
                    p.op("pe", lambda e, bkv=bkv, j=j, kc=kc, sr=sr: e.transpose(out=bkv[:, j * 128:(j + 1) * 128], in_=sr[:, kc * 128:(kc + 1) * 128], identity=cb[:, C_ID:C_ID + 128]),
                         reads=["osr%d" % b], writes=[bkn], inc=(j == 7))
                if hh == 0:
                    p.op("act", lambda e, bkv=bkv, hh=hh, sT=sT: e.activation(out=sT[:, hh * 8:(hh + 1) * 8, :], in_=bkv.rearrange("p (a b) -> p a b", a=8), func=AF.Copy),
                         reads=[bkn], writes=["osT%d" % b])
                else:
                    p.op("dve", lambda e, bkv=bkv, hh=hh, sT=sT: e.tensor_copy(out=sT[:, hh * 8:(hh + 1) * 8, :], in_=bkv.rearrange("p (a b) -> p a b", a=8)),
                         reads=[bkn], writes=["osT%d" % b])
            for cg in range(4):
                for kc in range(16):
                    p.op("pe", lambda e, cg=cg, kc=kc, sT=sT: e.matmul(cx.bank[cg][:], lhsT=sT[:, kc, :], rhs=W[:, kc, cg * 512:(cg + 1) * 512], start=(kc == 0), stop=(kc == 15)),
                         reads=["osT%d" % b, "Wout%d" % cg], writes=["bank%d" % cg], inc=(kc == 15))
                p.op("dve", lambda e, cg=cg, tm=tm: e.tensor_tensor(out=tm[:, cg * 512:(cg + 1) * 512], in0=cx.bank[cg][:], in1=GT[:, cg * 512:(cg + 1) * 512], op=ALU.mult),
                     reads=["bank%d" % cg, "GT"], writes=["otm%d_%d" % (b, cg)])
                p.op("pool", lambda e, cg=cg, tm=tm, xs=xs: e.tensor_tensor(out=tm[:, cg * 512:(cg + 1) * 512], in0=tm[:, cg * 512:(cg + 1) * 512], in1=xs[:, cg * 512:(cg + 1) * 512], op=ALU.add),
                     reads=["otm%d_%d" % (b, cg), "oxs%d" % b], writes=["otm%d_%d" % (b, cg)])
            p.dma("sp", lambda e, tm=tm, t=t: e.dma_start(out=XOUT[t * 128:(t + 1) * 128, :], in_=tm[:]), reads=["otm%d_%d" % (b, cg) for cg in range(4)], writes=["XO%d" % t])
        p.emit()


TWO_PI = 2.0 * math.pi
C1 = 6.28125
C2 = TWO_PI - C1


def rope_tables(p, cx, POSap, COS, SINS, tag):
    with ExitStack() as st:
        cf = cx.cf
        a = p.sb(st, "ra", [32, TOK], F32)
        k = p.sb(st, "rk", [32, TOK], F32)
        ki = p.sb(st, "rki", [32, TOK], I32)
        m = p.sb(st, "rm", [32, TOK], F32)
        p.dma("pool", lambda e: e.dma_start(out=a[:], in_=bc(POSap, 32)), writes=["ra"])
        p.op("dve", lambda e: e.tensor_scalar(out=a[:], in0=a[:], scalar1=cf[0:32, C_INVF:C_INVF + 1], scalar2=None, op0=ALU.mult), reads=["ra"], writes=["ra"])

        def reduce_(src, dst, shift):
            p.op("dve", lambda e: e.tensor_scalar(out=k[:], in0=src[:], scalar1=shift, scalar2=1.0 / TWO_PI, op0=ALU.add, op1=ALU.mult), reads=["ra", "rd"], writes=["rk"])
            p.op("dve", lambda e: e.tensor_copy(out=ki[:], in_=k[:]), reads=["rk"], writes=["rki"])
            p.op("dve", lambda e: e.tensor_copy(out=k[:], in_=ki[:]), reads=["rki"], writes=["rk"])
            p.op("dve", lambda e: e.scalar_tensor_tensor(out=dst[:], in0=k[:], scalar=-C1, in1=src[:], op0=ALU.mult, op1=ALU.add), reads=["rk", "ra"], writes=["rd"])
            p.op("dve", lambda e: e.scalar_tensor_tensor(out=dst[:], in0=k[:], scalar=-C2, in1=dst[:], op0=ALU.mult, op1=ALU.add), reads=["rk", "rd"], writes=["rd"])
            if shift != 0.0:
                p.op("dve", lambda e: e.tensor_scalar(out=dst[:], in0=dst[:], scalar1=shift, scalar2=None, op0=ALU.add), reads=["rd"], writes=["rd"])
            p.op("dve", lambda e: e.tensor_scalar(out=m[:], in0=dst[:], scalar1=math.pi, scalar2=-TWO_PI, op0=ALU.is_gt, op1=ALU.mult), reads=["rd"], writes=["rm"])
            p.op("dve", lambda e: e.tensor_tensor(out=dst[:], in0=dst[:], in1=m[:], op=ALU.add), reads=["rd", "rm"], writes=["rd"])
            p.op("dve", lambda e: e.tensor_scalar(out=m[:], in0=dst[:], scalar1=-math.pi, scalar2=TWO_PI, op0=ALU.is_lt, op1=ALU.mult), reads=["rd"], writes=["rm"])
            p.op("dve", lambda e: e.tensor_tensor(out=dst[:], in0=dst[:], in1=m[:], op=ALU.add), reads=["rd", "rm"], writes=["rd"])
            p.op("dve", lambda e: e.tensor_scalar(out=dst[:], in0=dst[:], scalar1=math.pi, scalar2=-math.pi, op0=ALU.min, op1=ALU.max), reads=["rd"], writes=["rd"])

        reduce_(a, SINS, 0.0)
        p.op("act", lambda e: e.activation(out=SINS[:], in_=SINS[:], func=AF.Sin), reads=["rd"], writes=["rd"])
        p.op("dve", lambda e: e.tensor_scalar(out=SINS[:], in0=SINS[:], scalar1=cf[0:32, C_SGN:C_SGN + 1], scalar2=None, op0=ALU.mult), reads=["rd"], writes=["rd"])
        reduce_(a, COS, math.pi / 2)
        p.op("act", lambda e: e.activation(out=COS[:], in_=COS[:], func=AF.Sin), reads=["rd"], writes=["rd"])
        p.emit()


def attn_proj(p, cx, hT, own, COS, SINS, nmax):
    with ExitStack() as st:
        cb = cx.cb
        wp = [p.sb(st, "wp%d" % i, [128, 16, 512], BF16) for i in range(2)]
        qk = [p.sb(st, "qk%d" % i, [128, 512], BF16) for i in range(4)]
        sq = [p.sb(st, "sq%d" % i, [128, 512], BF16) for i in range(2)]
        t1 = [p.sb(st, "t1_%d" % i, [32, 512], F32) for i in range(2)]
        t2 = [p.sb(st, "t2_%d" % i, [32, 512], F32) for i in range(2)]
        nm1 = p.sb(st, "nm1", [1, 2], F32)
        vs = [p.sb(st, "vs%d" % i, [128, 2, 257], BF16) for i in range(4)]
        for i in range(4):
            p.op("pool", lambda e, i=i: e.memset(vs[i][:], 1.0), writes=["vs%d" % i])
        wv = cx.ATTN_W_IN.ap().rearrange("(kc k) c -> k kc c", k=128)
        toff = TOK if own else 0
        pieces = range(12) if own else range(4, 12)
        cnt = 0
        vcnt = 0
        for pi, j in enumerate(pieces):
            w = wp[pi % 2]
            wn = "wp%d" % (pi % 2)
            p.dma("pool", lambda e, w=w, j=j: e.dma_start(out=w[:], in_=wv[:, :, j * 512:(j + 1) * 512]), writes=[wn])
            if j < 8:
                isq = j < 4
                for cc in range(4):
                    gcc = (j % 4) * 4 + cc
                    for tg in range(4):
                        bi = cnt % 4
                        bk = cx.bank[bi]
                        q_ = qk[cnt % 4]
                        qn = "qk%d" % (cnt % 4)
                        s_ = sq[cnt % 2]
                        sn = "sq%d" % (cnt % 2)
                        a1, a2 = t1[cnt % 2], t2[cnt % 2]
                        an = "t12_%d" % (cnt % 2)
                        nb = cx.bank[4 + cnt % 2]
                        nbn = "bank%d" % (4 + cnt % 2)
                        sb_ = cx.bank[6 + cnt % 2]
                        sbn = "bank%d" % (6 + cnt % 2)
                        cnt += 1
                        for kc in range(16):
                            p.op("pe", lambda e, bk=bk, w=w, kc=kc, cc=cc, tg=tg: e.matmul(bk[:], lhsT=w[:, kc, cc * 128:(cc + 1) * 128], rhs=hT[:, kc, tg * 512:(tg + 1) * 512], start=(kc == 0), stop=(kc == 15)),
                                 reads=[wn], writes=["bank%d" % bi], inc=(kc == 15))
                        p.op("act", lambda e, bk=bk, q_=q_: e.activation(out=q_[:], in_=bk[:], func=AF.Copy), reads=["bank%d" % bi], writes=[qn])
                        p.op("act", lambda e, bk=bk, s_=s_: e.activation(out=s_[:], in_=bk[:], func=AF.Square), reads=["bank%d" % bi], writes=[sn])
                        p.op("pe", lambda e, nb=nb, s_=s_: e.matmul(nb[0:1, :], lhsT=cb[:, C_ON:C_ON + 1], rhs=s_[:], start=True, stop=True), reads=[sn], writes=[nbn])
                        p.op("dve", lambda e, nb=nb: e.reduce_max(out=nm1[:, 0:1], in_=nb[0:1, :], axis=AX.X), reads=[nbn], writes=["nm1"])
                        ix = gcc if isq else 16 + gcc
                        p.op("dve", lambda e, ix=ix: e.tensor_tensor(out=nmax[:, ix:ix + 1], in0=nmax[:, ix:ix + 1], in1=nm1[:, 0:1], op=ALU.max), reads=["nm1", "nmax"], writes=["nmax"])
                        p.op("pe", lambda e, sb_=sb_, q_=q_: e.matmul(sb_[0:32, :], lhsT=cb[0:32, C_PERM:C_PERM + 32], rhs=q_[0:32, :], start=True, stop=True), reads=[qn], writes=[sbn])
                        p.op("dve", lambda e, a1=a1, q_=q_, tg=tg: e.tensor_tensor(out=a1[:], in0=q_[0:32, :], in1=COS[:, tg * 512:(tg + 1) * 512], op=ALU.mult), reads=[qn], writes=[an + "a"])
                        p.op("dve", lambda e, a2=a2, sb_=sb_, tg=tg: e.tensor_tensor(out=a2[:], in0=sb_[0:32, :], in1=SINS[:, tg * 512:(tg + 1) * 512], op=ALU.mult), reads=[sbn], writes=[an + "b"])
                        p.op("dve", lambda e, a1=a1, a2=a2, q_=q_: e.tensor_tensor(out=q_[0:32, :], in0=a1[:], in1=a2[:], op=ALU.add), reads=[an + "a", an + "b", qn], writes=[qn])
                        if isq:
                            dst = cx.QTD.ap()[gcc, :, tg * 512:(tg + 1) * 512]
                        else:
                            dst = cx.KTD.ap()[gcc, :, toff + tg * 512:toff + (tg + 1) * 512]
                        p.dma("sp", lambda e, dst=dst, q_=q_: e.dma_start(out=dst, in_=q_[:]), reads=[qn], writes=["qkd%d" % cnt])
            else:
                vj = j - 8
                for t in range(NT):
                    bi = cnt % 4
                    bk = cx.bank[bi]
                    cnt += 1
                    v_ = vs[vcnt % 4]
                    vn = "vs%d" % (vcnt % 4)
                    vcnt += 1
                    for kc in range(16):
                        p.op("pe", lambda e, bk=bk, w=w, kc=kc, t=t: e.matmul(bk[:], lhsT=hT[:, kc, t * 128:(t + 1) * 128], rhs=w[:, kc, :], start=(kc == 0), stop=(kc == 15)),
                             reads=[wn], writes=["bank%d" % bi], inc=(kc == 15))
                    if t % 2 == 0:
                        p.op("act", lambda e, bk=bk, v_=v_: e.activation(out=v_[:, :, 0:256], in_=bk[:].rearrange("p (a b) -> p a b", a=2), func=AF.Copy), reads=["bank%d" % bi], writes=[vn])
                    else:
                        p.op("dve", lambda e, bk=bk, v_=v_: e.tensor_copy(out=v_[:, :, 0:256], in_=bk[:].rearrange("p (a b) -> p a b", a=2)), reads=["bank%d" % bi], writes=[vn])
                    r0 = toff + t * 128
                    p.dma("act", lambda e, v_=v_, r0=r0, vj=vj: e.dma_start(out=cx.VD.ap()[r0:r0 + 128, 2 * vj:2 * vj + 2, :], in_=v_[:]), reads=[vn], writes=["vd%d" % cnt])
        p.emit()


def attn_consts(p, cx, st0, nmax):
    negc = p.sb(st0, "negc", [128, 16], F32)
    negcp = p.sb(st0, "negcp", [128, 16], F32)
    nlam = p.sb(st0, "nlam", [128, 1], F32)
    HG = p.sb(st0, "HG", [128, 256], F32)
    with ExitStack() as st:
        cf = cx.cf
        r = p.sb(st, "acr", [1, 64], F32)
        lm = p.sb(st, "lm", [1, 4, 128], F32)
        pf = p.sb(st, "pf", [128, 1], F32)
        for i, nm_ in enumerate([cx.LQ1, cx.LK1, cx.LQ2, cx.LK2]):
            p.dma("sp", lambda e, i=i, nm_=nm_: e.dma_start(out=lm[:, i, :], in_=nm_.ap()), writes=["lm"])
        p.dma("sp", lambda e: e.dma_start(out=pf[:], in_=cx.PREVFLAG.ap()), writes=["pf"])
        p.dma("sp", lambda e: e.dma_start(out=HG[:], in_=bc(cx.ATTN_HG.ap()[0, :])), writes=["HG"])
        p.op("dve", lambda e: e.tensor_scalar(out=HG[:], in0=HG[:], scalar1=0.8, scalar2=None, op0=ALU.mult), reads=["HG"], writes=["HG"])
        p.op("dve", lambda e: e.tensor_tensor(out=r[:, 0:16], in0=nmax[:, 0:16], in1=nmax[:, 16:32], op=ALU.mult), reads=["nmax"], writes=["acr"])
        p.op("act", lambda e: e.activation(out=r[:, 0:16], in_=r[:, 0:16], func=AF.Sqrt), reads=["acr"], writes=["acr"])
        p.op("dve", lambda e: e.tensor_scalar(out=r[:, 0:16], in0=r[:, 0:16], scalar1=-(128.0 ** -0.5), scalar2=None, op0=ALU.mult), reads=["acr"], writes=["acr"])
        p.op("dve", lambda e: e.tensor_tensor(out=lm[:, 0, :], in0=lm[:, 0, :], in1=lm[:, 1, :], op=ALU.mult), reads=["lm"], writes=["lm"])
        p.op("dve", lambda e: e.tensor_tensor(out=lm[:, 2, :], in0=lm[:, 2, :], in1=lm[:, 3, :], op=ALU.mult), reads=["lm"], writes=["lm"])
        p.op("dve", lambda e: e.reduce_sum(out=r[:, 32:33], in_=lm[:, 0, :], axis=AX.X), reads=["lm"], writes=["acr"])
        p.op("dve", lambda e: e.reduce_sum(out=r[:, 33:34], in_=lm[:, 2, :], axis=AX.X), reads=["lm"], writes=["acr"])
        p.op("act", lambda e: e.activation(out=r[:, 32:34], in_=r[:, 32:34], func=AF.Exp), reads=["acr"], writes=["acr"])
        p.op("dve", lambda e: e.tensor_tensor(out=r[:, 16:17], in0=r[:, 33:34], in1=r[:, 32:33], op=ALU.subtract), reads=["acr"], writes=["acr"])
        p.op("dve", lambda e: e.tensor_scalar(out=r[:, 16:17], in0=r[:, 16:17], scalar1=-0.2, scalar2=None, op0=ALU.add), reads=["acr"], writes=["acr"])
        p.op("pe", lambda e: e.matmul(cx.bank[0][:, 0:18], lhsT=cf[0:1, C_ON:C_ON + 128], rhs=r[0:1, 0:18], start=True, stop=True), reads=["acr"], writes=["bank0"])
        p.op("dve", lambda e: e.tensor_copy(out=negc[:], in_=cx.bank[0][:, 0:16]), reads=["bank0"], writes=["negc"])
        p.op("dve", lambda e: e.tensor_copy(out=nlam[:], in_=cx.bank[0][:, 16:17]), reads=["bank0"], writes=["nlam"])
        p.op("dve", lambda e: e.tensor_scalar(out=negcp[:], in0=negc[:], scalar1=pf[:, 0:1], scalar2=None, op0=ALU.add), reads=["negc", "pf"], writes=["negcp"])
        p.emit()
    return negc, negcp, nlam, HG


def attn_core(p, cx, negc, negcp, nlam, HG):
    SCALE = 128.0 ** -0.5
    with ExitStack() as st:
        QT = [[p.sb(st, "QT%d_%d" % (b, m), [128, TOK], BF16) for m in range(2)] for b in range(2)]
        KT = [[p.sb(st, "KT%d_%d" % (b, m), [128, 2 * TOK], BF16) for m in range(2)] for b in range(2)]
        V = [p.sb(st, "V%d" % b, [128, 32, 257], BF16) for b in range(2)]
        PT = [p.sb(st, "PT%d" % m, [128, 36, 512], BF16) for m in range(2)]
        o0 = p.sb(st, "o0", [128, 4, 256], F32)
        oa = [p.sb(st, "oa%d" % i, [128, 4, 256], BF16) for i in range(2)]
        rd = [p.sb(st, "rd%d" % i, [128, 8], F32) for i in range(4)]
        junk = p.sb(st, "cjunk", [128, 256], BF16)
        vdv = cx.VD.ap().rearrange("(t k) h e -> h k t e", k=128)

        def loads(h):
            b = h % 2
            for m in range(2):
                p.dma("sp", lambda e, b=b, m=m, h=h: e.dma_start(out=QT[b][m][:], in_=cx.QTD.ap()[2 * h + m]), writes=["QT%d_%d" % (b, m)])
                p.dma("sp", lambda e, b=b, m=m, h=h: e.dma_start(out=KT[b][m][:], in_=cx.KTD.ap()[2 * h + m]), writes=["KT%d_%d" % (b, m)])
            p.dma("act", lambda e, b=b, h=h: e.dma_start(out=V[b][:], in_=vdv[h]), writes=["V%d" % b])

        stb = [0]
        accb = [0]
        oac = [0]
        rdc = [0]

        def stageA(h, g, m):
            b = h % 2
            hm = 2 * h + m
            steps = []
            nk = 16 + 4 * g + 4
            for j in range(nk):
                def step(j=j):
                    qoff = max(0, j - 16 - 4 * g)
                    bi = stb[0] % 3
                    stb[0] += 1
                    bk = cx.bank[bi]
                    p.op("pe", lambda e: e.matmul(bk[:, qoff * 128:512], lhsT=KT[b][m][:, j * 128:(j + 1) * 128], rhs=QT[b][m][:, g * 512 + qoff * 128:(g + 1) * 512], start=True, stop=True),
                         reads=["KT%d_%d" % (b, m), "QT%d_%d" % (b, m)], writes=["bank%d" % bi])
                    bias = negcp[:, hm:hm + 1] if j < 16 else negc[:, hm:hm + 1]
                    p.op("act", lambda e: e.activation(out=PT[m][:, j, qoff * 128:512], in_=bk[:, qoff * 128:512], func=AF.Exp, bias=bias, scale=SCALE),
                         reads=["bank%d" % bi], writes=["PT%d_%d" % (m, j)])
                    if j >= 16 + 4 * g:
                        ii = j - 16 - 4 * g
                        p.op("pool", lambda e: e.memset(PT[m][64:128, j, ii * 128:ii * 128 + 64], 0.0), reads=[], writes=["PT%d_%d" % (m, j)])
                steps.append(step)
            return steps

        def stageB(h, g, m):
            b = h % 2
            steps = []
            for ii in range(4):
                ai = 3 + accb[0] % 5
                accb[0] += 1
                acc = cx.bank[ai]
                an = "bank%d" % ai
                nkk = 16 + 4 * g + ii + 1
                for j in range(nkk):
                    def step(j=j, ii=ii, acc=acc, an=an, nkk=nkk):
                        p.op("pe", lambda e: e.matmul(acc[:, 0:257], lhsT=PT[m][:, j, ii * 128:(ii + 1) * 128], rhs=V[b][:, j, :], start=(j == 0), stop=(j == nkk - 1)),
                             reads=["PT%d_%d" % (m, j), "V%d" % b], writes=[an], inc=(j == nkk - 1))
                    steps.append(step)

                def fin(ii=ii, acc=acc, an=an):
                    r = rd[rdc[0] % 4]
                    rn = "rd%d" % (rdc[0] % 4)
                    rdc[0] += 1
                    p.op("dve", lambda e: e.reciprocal(out=r[:, 0:1], in_=acc[:, 256:257]), reads=[an], writes=[rn])
                    if m == 0:
                        p.op("act", lambda e: e.activation(out=o0[:, ii, :], in_=acc[:, 0:256], func=AF.Copy, scale=r[:, 0:1]), reads=[an, rn], writes=["o0_%d" % ii])
                    else:
                        o = oa[oac[0] % 2]
                        on = "oa%d" % (oac[0] % 2)
                        p.op("dve", lambda e: e.tensor_tensor(out=r[:, 1:2], in0=r[:, 0:1], in1=nlam[:, 0:1], op=ALU.mult), reads=[rn], writes=[rn])
                        p.op("dve", lambda e: e.scalar_tensor_tensor(out=o0[:, ii, :], in0=acc[:, 0:256], scalar=r[:, 1:2], in1=o0[:, ii, :], op0=ALU.mult, op1=ALU.add),
                             reads=[an, rn, "o0_%d" % ii], writes=["o0_%d" % ii])
                        p.op("act", lambda e: e.activation(out=junk[:], in_=o0[:, ii, :], func=AF.Square, accum_out=r[:, 2:3]), reads=["o0_%d" % ii], writes=["cjunk", rn])
                        p.op("dve", lambda e: e.tensor_scalar(out=r[:, 3:4], in0=r[:, 2:3], scalar1=1.0 / 256, scalar2=EPS, op0=ALU.mult, op1=ALU.add), reads=[rn], writes=[rn])
                        p.op("act", lambda e: e.activation(out=r[:, 4:5], in_=r[:, 3:4], func=AF.Sqrt), reads=[rn], writes=[rn])
                        p.op("dve", lambda e: e.reciprocal(out=r[:, 5:6], in_=r[:, 4:5]), reads=[rn], writes=[rn])
                        p.op("dve", lambda e: e.scalar_tensor_tensor(out=o[:, ii, :], in0=o0[:, ii, :], scalar=r[:, 5:6], in1=HG[:], op0=ALU.mult, op1=ALU.mult),
                             reads=["o0_%d" % ii, rn], writes=[on + "_%d" % ii])
                        if ii == 3:
                            oac[0] += 1
                            dst = cx.OAD.ap()[g * 512:(g + 1) * 512, h * 256:(h + 1) * 256].rearrange("(i q) c -> q i c", q=128)
                            p.dma("sp", lambda e: e.dma_start(out=dst, in_=o[:]), reads=[on + "_%d" % i_ for i_ in range(4)], writes=["oad_%d_%d" % (h, g)])
                steps.append(fin)
            return steps

        def interleave(A, B):
            na, nb = len(A), len(B)
            ia = ib = 0
            while ia < na or ib < nb:
                if ib >= nb or (ia < na and ia * max(nb, 1) <= ib * max(na, 1)):
                    A[ia]()
                    ia += 1
                else:
                    B[ib]()
                    ib += 1

        loads(0)
        pend = []
        for h in range(8):
            for g in range(4):
                for m in range(2):
                    A = stageA(h, g, m)
                    interleave(A, pend)
                    pend = stageB(h, g, m)
                    if g == 0 and m == 0 and h + 1 < 8:
                        loads(h + 1)
        interleave([], pend)
        p.emit()


def attn_layer(p, cx, XIN, XPREV, XOUT):
    with ExitStack() as st0:
        nmax = p.sb(st0, "nmax", [1, 32], F32)
        p.op("dve", lambda e: e.memset(nmax[:], 0.0), writes=["nmax"])
        with ExitStack() as st1:
            G, SH, GT = load_mod_tiles(p, cx, st1, 0, 0, cx.NORM_MIX_G)
            COS = p.sb(st1, "COS", [32, TOK], F32)
            SINS = p.sb(st1, "SINS", [32, TOK], F32)
            hT = p.sb(st1, "hT", [128, 16, TOK], BF16)
            rope_tables(p, cx, cx.POS_PREV.ap()[0, :], COS, SINS, "p")
            norm_to_hT(p, cx, XPREV.ap(), G, SH, hT)
            attn_proj(p, cx, hT, False, COS, SINS, nmax)
            rope_tables(p, cx, cx.POS_OWN.ap()[0, :], COS, SINS, "o")
            norm_to_hT(p, cx, XIN.ap(), G, SH, hT)
            attn_proj(p, cx, hT, True, COS, SINS, nmax)
        negc, negcp, nlam, HG = attn_consts(p, cx, st0, nmax)
        attn_core(p, cx, negc, negcp, nlam, HG)
    with ExitStack() as st2:
        GT = p.sb(st2, "GT", [128, D], F32)
        p.dma("sp", lambda e: e.dma_start(out=GT[:], in_=bc(cx.MODD.ap()[0, 2 * D:3 * D])), writes=["GT"])
        proj_out_stage(p, cx, cx.OAD.ap(), cx.ATTN_W_OUT.ap(), GT, XIN.ap(), XOUT.ap())


def mlstm_proj(p, cx, hT, full=True):
    with ExitStack() as st:
        wp = [p.sb(st, "wp%d" % i, [128, 16, 512], BF16) for i in range(2)]
        wgt = p.sb(st, "wgt", [128, 16, 16], BF16)
        qk = [p.sb(st, "qk%d" % i, [128, 512], BF16) for i in range(4)]
        vs = [p.sb(st, "vs%d" % i, [128, 2, 257], BF16) for i in range(4)]
        ob = [p.sb(st, "ob%d" % i, [128, 512], BF16) for i in range(4)]
        gsb = [p.sb(st, "gsb%d" % i, [128, 16], F32) for i in range(2)]
        for i in range(4):
            p.op("pool", lambda e, i=i: e.memset(vs[i][:], 1.0), writes=["vs%d" % i])
        wv = cx.ML_W_IN.ap().rearrange("(kc k) c -> k kc c", k=128)
        p.dma("pool", lambda e: e.dma_start(out=wgt[:], in_=wv[:, :, 3 * D:3 * D + 16]), writes=["wgt"])
        cnt = 0
        for j in range(12 if full else 8):
            w = wp[j % 2]
            wn = "wp%d" % (j % 2)
            p.dma("pool", lambda e, w=w, j=j: e.dma_start(out=w[:], in_=wv[:, :, j * 512:(j + 1) * 512]), writes=[wn])
            if j < 4:
                for cc in range(4):
                    gcc = j * 4 + cc
                    for tg in range(4):
                        bi = cnt % 4
                        bk = cx.bank[bi]
                        q_ = qk[cnt % 4]
                        qn = "qk%d" % (cnt % 4)
                        cnt += 1
                        for kc in range(16):
                            p.op("pe", lambda e, bk=bk, w=w, kc=kc, cc=cc, tg=tg: e.matmul(bk[:], lhsT=w[:, kc, cc * 128:(cc + 1) * 128], rhs=hT[:, kc, tg * 512:(tg + 1) * 512], start=(kc == 0), stop=(kc == 15)),
                                 reads=[wn], writes=["bank%d" % bi], inc=(kc == 15))
                        if cnt % 2 == 0:
                            p.op("act", lambda e, bk=bk, q_=q_: e.activation(out=q_[:], in_=bk[:], func=AF.Copy), reads=["bank%d" % bi], writes=[qn])
                        else:
                            p.op("dve", lambda e, bk=bk, q_=q_: e.tensor_copy(out=q_[:], in_=bk[:]), reads=["bank%d" % bi], writes=[qn])
                        dst = cx.QKP.ap()[gcc, :, 3 + tg * 512:3 + (tg + 1) * 512]
                        p.dma("sp", lambda e, dst=dst, q_=q_: e.dma_start(out=dst, in_=q_[:]), reads=[qn], writes=["qkd%d" % cnt])
            else:
                for t in range(NT):
                    bi = cnt % 4
                    bk = cx.bank[bi]
                    cnt += 1
                    for kc in range(16):
                        p.op("pe", lambda e, bk=bk, w=w, kc=kc, t=t: e.matmul(bk[:], lhsT=hT[:, kc, t * 128:(t + 1) * 128], rhs=w[:, kc, :], start=(kc == 0), stop=(kc == 15)),
                             reads=[wn], writes=["bank%d" % bi], inc=(kc == 15))
                    if j < 8:
                        vj = j - 4
                        v_ = vs[cnt % 4]
                        vn = "vs%d" % (cnt % 4)
                        if t % 2 == 0:
                            p.op("act", lambda e, bk=bk, v_=v_: e.activation(out=v_[:, :, 0:256], in_=bk[:].rearrange("p (a b) -> p a b", a=2), func=AF.Copy), reads=["bank%d" % bi], writes=[vn])
                        else:
                            p.op("dve", lambda e, bk=bk, v_=v_: e.tensor_copy(out=v_[:, :, 0:256], in_=bk[:].rearrange("p (a b) -> p a b", a=2)), reads=["bank%d" % bi], writes=[vn])
                        p.dma("act", lambda e, v_=v_, t=t, vj=vj: e.dma_start(out=cx.VM.ap()[t * 128:(t + 1) * 128, 2 * vj:2 * vj + 2, :], in_=v_[:]), reads=[vn], writes=["vd%d" % cnt])
                    else:
                        oj = j - 8
                        o_ = ob[cnt % 4]
                        on = "ob%d" % (cnt % 4)
                        if t % 2 == 0:
                            p.op("act", lambda e, bk=bk, o_=o_: e.activation(out=o_[:], in_=bk[:], func=AF.Copy), reads=["bank%d" % bi], writes=[on])
                        else:
                            p.op("dve", lambda e, bk=bk, o_=o_: e.tensor_copy(out=o_[:], in_=bk[:]), reads=["bank%d" % bi], writes=[on])
                        p.dma("act", lambda e, o_=o_, t=t, oj=oj: e.dma_start(out=cx.OPRE.ap()[t * 128:(t + 1) * 128, oj * 512:(oj + 1) * 512], in_=o_[:]), reads=[on], writes=["od%d" % cnt])
        for t in range(NT):
            bk = cx.bank[4 + t % 2]
            g_ = gsb[t % 2]
            for kc in range(16):
                p.op("pe", lambda e, bk=bk, kc=kc, t=t: e.matmul(bk[:, 0:16], lhsT=hT[:, kc, t * 128:(t + 1) * 128], rhs=wgt[:, kc, :], start=(kc == 0), stop=(kc == 15)),
                     reads=["wgt"], writes=["bank%d" % (4 + t % 2)], inc=(kc == 15))
            p.op("dve", lambda e, bk=bk, g_=g_: e.tensor_copy(out=g_[:], in_=bk[:, 0:16]), reads=["bank%d" % (4 + t % 2)], writes=["gsb%d" % (t % 2)])
            p.dma("sp", lambda e, g_=g_, t=t: e.dma_start(out=cx.GATES.ap()[t * 128:(t + 1) * 128, :], in_=g_[:]), reads=["gsb%d" % (t % 2)], writes=["gd%d" % t])
        p.emit()


def mlstm_rec(p, cx, full=True, st_in="dram", st_out="dram"):
    RS = 128.0 ** -0.5
    with ExitStack() as st:
        cf, cb = cx.cf, cx.cb
        C = p.sb(st, "Cst", [128, 8, 257], F32)
        Cb = p.sb(st, "Cstb", [128, 8, 257], BF16)
        CW = p.sb(st, "CW", [128, 16, 4], F32)
        CB = p.sb(st, "CB", [128, 16], F32)
        GB = p.sb(st, "GB", [128, 16], F32)
        HGm = p.sb(st, "HGm", [128, D], F32)
        hl = p.sb(st, "hl", [128, 16, 3], F32)
        hlb = p.sb(st, "hlb", [128, 16, 3], BF16)
        qkp = [p.sb(st, "qkp%d" % i, [128, 16, 131], BF16) for i in range(2)]
        vt = [p.sb(st, "vt%d" % i, [128, 8, 257], BF16) for i in range(2)]
        gt_ = [p.sb(st, "gtl%d" % i, [128, 16], F32) for i in range(2)]
        op_ = [p.sb(st, "opl%d" % i, [128, D], BF16) for i in range(2)]
        sig = p.sb(st, "sig", [128, D], F32)
        cacc = [p.sb(st, "cacc%d" % i, [128, 128], F32) for i in range(4)]
        qs = p.sb(st, "qs", [128, 16, 128], BF16)
        gs = p.sb(st, "gs", [128, 64], F32)
        PTm = [p.sb(st, "PTm%d" % i, [128, 128], BF16) for i in range(2)]
        Kp = [p.sb(st, "Kp%d" % i, [128, 128], BF16) for i in range(2)]
        tmpC = [p.sb(st, "tmpC%d" % i, [128, 257], F32) for i in range(2)]
        ho = [p.sb(st, "ho%d" % i, [128, 256], F32) for i in range(2)]
        hn = [p.sb(st, "hn%d" % i, [128, D], BF16) for i in range(2)]
        rr = [p.sb(st, "rr%d" % i, [128, 8], F32) for i in range(4)]
        junk = p.sb(st, "mjunk", [128, 256], BF16)
        if st_in == "dram":
            p.dma("sp", lambda e: e.dma_start(out=C[:], in_=cx.STATE_IN.ap()), writes=["Cst"])
        elif st_in == "zero":
            p.op("dve", lambda e: e.memset(C[:], 0.0), writes=["Cst"])
        else:
            of = p.sb(st, "oddf", [128, 1], F32)
            p.dma("sp", lambda e: e.dma_start(out=of[:], in_=cx.ODDFLAG.ap()), writes=["oddf"])
            p.dma("sp", lambda e: e.dma_start(out=C[:].rearrange("p h e -> p (h e)"), in_=cx.ST_ALL.ap()[0:128, 0:8 * 257]), writes=["Cst"])
            p.op("dve", lambda e: e.tensor_scalar(out=C[:], in0=C[:], scalar1=of[:, 0:1], scalar2=None, op0=ALU.mult), reads=["Cst", "oddf"], writes=["Cst"])
        p.op("act", lambda e: e.activation(out=Cb[:], in_=C[:], func=AF.Copy), reads=["Cst"], writes=["Cstb"])
        p.dma("sp", lambda e: e.dma_start(out=CW[:], in_=cx.CONV_W.ap()), writes=["CW"])
        p.dma("sp", lambda e: e.dma_start(out=CB[:], in_=cx.CONV_B.ap()), writes=["CB"])
        p.dma("sp", lambda e: e.dma_start(out=GB[:], in_=bc(cx.GATE_B.ap()[0, :])), writes=["GB"])
        p.dma("sp", lambda e: e.dma_start(out=HGm[:], in_=bc(cx.ML_HG.ap()[0, :])), writes=["HGm"])
        if st_in == "dram":
            p.dma("sp", lambda e: e.dma_start(out=hl[:], in_=cx.HALO_IN.ap()), writes=["hl"])
        elif st_in == "zero":
            p.op("dve", lambda e: e.memset(hl[:], 0.0), writes=["hl"])
        else:
            p.dma("sp", lambda e: e.dma_start(out=hl[:].rearrange("p c t -> p (c t)"), in_=cx.ST_ALL.ap()[0:128, 8 * 257:8 * 257 + 48]), writes=["hl"])
            p.op("dve", lambda e: e.tensor_scalar(out=hl[:], in0=hl[:], scalar1=of[:, 0:1], scalar2=None, op0=ALU.mult), reads=["hl", "oddf"], writes=["hl"])
        p.op("dve", lambda e: e.tensor_copy(out=hlb[:], in_=hl[:]), reads=["hl"], writes=["hlb"])
        qkv = cx.QKP.ap().rearrange("c k t -> k c t")
        p.dma("sp", lambda e: e.dma_start(out=qkv[:, :, 0:3], in_=hlb[:], allow_slow_non_contiguous=True), reads=["hlb"], writes=["halo"])
        for c in range(NT):
            b = c % 2
            q_, v_, g_, o_ = qkp[b], vt[b], gt_[b], op_[b]
            rdh = ["halo"] if c == 0 else []
            p.dma("sp", lambda e, q_=q_, c=c: e.dma_start(out=q_[:], in_=qkv[:, :, c * 128:c * 128 + 131]), reads=rdh, writes=["qkp%d" % b])
            p.dma("act", lambda e, v_=v_, c=c: e.dma_start(out=v_[:], in_=cx.VM.ap()[c * 128:(c + 1) * 128]), writes=["vt%d" % b])
            p.dma("sp", lambda e, g_=g_, c=c: e.dma_start(out=g_[:], in_=cx.GATES.ap()[c * 128:(c + 1) * 128, :]), writes=["gtl%d" % b])
            if full:
                p.dma("act", lambda e, o_=o_, c=c: e.dma_start(out=o_[:], in_=cx.OPRE.ap()[c * 128:(c + 1) * 128, :]), writes=["opl%d" % b])
                p.op("act", lambda e, o_=o_: e.activation(out=sig[:], in_=o_[:], func=AF.Sigmoid), reads=["opl%d" % b], writes=["sig"])
            p.op("dve", lambda e, g_=g_: e.tensor_tensor(out=gs[:, 0:16], in0=g_[:], in1=GB[:], op=ALU.add), reads=["gtl%d" % b, "GB"], writes=["gs"])
            p.op("act", lambda e: e.activation(out=gs[:, 16:24], in_=gs[:, 8:16], func=AF.Exp, scale=-1.0), reads=["gs"], writes=["gs"])
            p.op("dve", lambda e: e.tensor_scalar(out=gs[:, 16:24], in0=gs[:, 16:24], scalar1=1.0, scalar2=None, op0=ALU.add), reads=["gs"], writes=["gs"])
            p.op("act", lambda e: e.activation(out=gs[:, 16:24], in_=gs[:, 16:24], func=AF.Ln), reads=["gs"], writes=["gs"])
            gbk = cx.bank[0]
            p.op("pe", lambda e: e.matmul(gbk[:, 0:8], lhsT=cf[:, C_TRI:C_TRI + 128], rhs=gs[:, 16:24], start=True, stop=True), reads=["gs"], writes=["bank0"])
            p.op("pe", lambda e: e.matmul(gbk[:, 8:16], lhsT=cf[:, C_ON:C_ON + 128], rhs=gs[:, 16:24], start=False, stop=True, skip_group_check=True), reads=["gs"], writes=["bank0"])
            p.op("dve", lambda e: e.tensor_tensor(out=gs[:, 32:40], in0=gs[:, 0:8], in1=gbk[:, 0:8], op=ALU.add), reads=["gs", "bank0"], writes=["gs"])
            p.op("act", lambda e: e.activation(out=gs[:, 32:40], in_=gs[:, 32:40], func=AF.Exp), reads=["gs"], writes=["gs"])
            p.op("dve", lambda e: e.tensor_scalar(out=gs[:, 32:40], in0=gs[:, 32:40], scalar1=RS, scalar2=None, op0=ALU.mult), reads=["gs"], writes=["gs"])
            p.op("act", lambda e: e.activation(out=gs[:, 40:56], in_=gbk[:, 0:16], func=AF.Exp, scale=-1.0), reads=["bank0"], writes=["gs"])
            for g4 in range(0, 16, 4):
                ccs = [cc for cc in range(g4, g4 + 4) if full or cc >= 8]
                for cc in ccs:
                    a_ = cacc[cc % 4]
                    an = "cacc%d" % (cc % 4)
                    p.op("act", lambda e, a_=a_, q_=q_, cc=cc: e.activation(out=a_[:], in_=q_[:, cc, 0:128], func=AF.Identity, scale=CW[:, cc, 0:1], bias=CB[:, cc:cc + 1]),
                         reads=["qkp%d" % b, "CW", "CB"], writes=[an])
                for j in range(1, 4):
                    for cc in ccs:
                        a_ = cacc[cc % 4]
                        an = "cacc%d" % (cc % 4)
                        p.op("dve", lambda e, a_=a_, q_=q_, cc=cc, j=j: e.scalar_tensor_tensor(out=a_[:], in0=q_[:, cc, j:j + 128], scalar=CW[:, cc, j:j + 1], in1=a_[:], op0=ALU.mult, op1=ALU.add),
                             reads=["qkp%d" % b, an], writes=[an])
                for cc in ccs:
                    a_ = cacc[cc % 4]
                    an = "cacc%d" % (cc % 4)
                    p.op("act", lambda e, a_=a_, cc=cc: e.activation(out=qs[:, cc, :], in_=a_[:], func=AF.Silu), reads=[an], writes=["qs%d" % cc])
            def head_ops(h, c=c, b=b, q_=q_, v_=v_):
                s4 = (h % 2) * 4
                bS, bT, bA, bU = cx.bank[s4], cx.bank[s4 + 1], cx.bank[s4 + 2], cx.bank[s4 + 3]
                nS, nT, nA, nU = ["bank%d" % (s4 + i) for i in range(4)]
                kT = qs[:, 8 + h, :]
                qT = qs[:, h, :]
                pt = PTm[h % 2]
                kp = Kp[h % 2]
                r = rr[h % 4]
                rn = "rr%d" % (h % 4)
                if full:
                    p.op("pe", lambda e, bS=bS, kT=kT, qT=qT: e.matmul(bS[:, 0:128], lhsT=kT, rhs=qT, start=True, stop=True), reads=["qs%d" % (8 + h), "qs%d" % h], writes=[nS])
                    yield
                    p.op("dve", lambda e, bS=bS, pt=pt, h=h: e.scalar_tensor_tensor(out=pt[:], in0=bS[:, 0:128], scalar=gs[:, 32 + h:33 + h], in1=cf[:, C_CM:C_CM + 128], op0=ALU.mult, op1=ALU.mult),
                         reads=[nS, "gs"], writes=["PTm%d" % (h % 2)])
                    yield
                bTv = bT[:].bitcast(BF16)
                p.op("pe", lambda e, bTv=bTv, kT=kT: e.transpose(out=bTv[:, 0:128], in_=kT, identity=cb[:, C_ID:C_ID + 128]), reads=["qs%d" % (8 + h)], writes=[nT])
                yield
                p.op("act", lambda e, bTv=bTv, kp=kp, h=h: e.activation(out=kp[:], in_=bTv[:, 0:128], func=AF.Copy, scale=gs[:, 32 + h:33 + h]), reads=[nT, "gs"], writes=["Kp%d" % (h % 2)])
                yield
                if full:
                    p.op("pe", lambda e, bA=bA, pt=pt, v_=v_, h=h: e.matmul(bA[:, 0:257], lhsT=pt[:], rhs=v_[:, h, :], start=True, stop=False), reads=["PTm%d" % (h % 2), "vt%d" % b], writes=[nA])
                    yield
                    p.op("pe", lambda e, bA=bA, qT=qT, h=h: e.matmul(bA[:, 0:257], lhsT=qT, rhs=Cb[:, h, :], start=False, stop=True), reads=["qs%d" % h, "Cstb%d" % h], writes=[nA])
                    yield
                p.op("pe", lambda e, bU=bU, kp=kp, v_=v_, h=h: e.matmul(bU[:, 0:257], lhsT=kp[:], rhs=v_[:, h, :], start=True, stop=True), reads=["Kp%d" % (h % 2), "vt%d" % b], writes=[nU])
                yield
                tc_ = tmpC[h % 2]
                p.op("dve", lambda e, bU=bU, tc_=tc_, h=h: e.tensor_tensor(out=tc_[:], in0=bU[:, 0:257], in1=C[:, h, :], op=ALU.add), reads=[nU, "Cst%d" % h, nA], writes=["tmpC%d" % (h % 2)])
                yield
                p.op("dve", lambda e, tc_=tc_, h=h: e.tensor_scalar(out=C[:, h, :], in0=tc_[:], scalar1=gs[:, 48 + h:49 + h], scalar2=None, op0=ALU.mult), reads=["tmpC%d" % (h % 2), "gs"], writes=["Cst%d" % h])
                yield
                p.op("act", lambda e, h=h: e.activation(out=Cb[:, h, :], in_=C[:, h, :], func=AF.Copy), reads=["Cst%d" % h], writes=["Cstb%d" % h])
                yield
                if full:
                    o_h = ho[h % 2]
                    on = "ho%d" % (h % 2)
                    hn_ = hn[b]
                    p.op("dve", lambda e, bA=bA, r=r, h=h: e.tensor_tensor(out=r[:, 0:1], in0=bA[:, 256:257], in1=gs[:, 40 + h:41 + h], op=ALU.mult), reads=[nA, "gs"], writes=[rn])
                    yield
                    p.op("dve", lambda e, r=r: e.tensor_scalar(out=r[:, 2:3], in0=r[:, 0:1], scalar1=-1.0, scalar2=None, op0=ALU.mult), reads=[rn], writes=[rn])
                    yield
                    p.op("dve", lambda e, r=r: e.tensor_scalar(out=r[:, 1:2], in0=r[:, 0:1], scalar1=r[:, 2:3], scalar2=1.0, op0=ALU.max, op1=ALU.max), reads=[rn], writes=[rn])
                    yield
                    p.op("dve", lambda e, r=r: e.reciprocal(out=r[:, 2:3], in_=r[:, 1:2]), reads=[rn], writes=[rn])
                    yield
                    p.op("dve", lambda e, r=r, h=h: e.tensor_tensor(out=r[:, 3:4], in0=r[:, 2:3], in1=gs[:, 40 + h:41 + h], op=ALU.mult), reads=[rn, "gs"], writes=[rn])
                    yield
                    p.op("act", lambda e, bA=bA, o_h=o_h, r=r: e.activation(out=o_h[:], in_=bA[:, 0:256], func=AF.Copy, scale=r[:, 3:4]), reads=[nA, rn], writes=[on])
                    yield
                    p.op("act", lambda e, o_h=o_h, r=r: e.activation(out=junk[:], in_=o_h[:], func=AF.Square, accum_out=r[:, 4:5]), reads=[on], writes=["mjunk", rn])
                    yield
                    p.op("dve", lambda e, r=r: e.tensor_scalar(out=r[:, 5:6], in0=r[:, 4:5], scalar1=1.0 / 256, scalar2=EPS, op0=ALU.mult, op1=ALU.add), reads=[rn], writes=[rn])
                    yield
                    p.op("act", lambda e, r=r: e.activation(out=r[:, 6:7], in_=r[:, 5:6], func=AF.Sqrt), reads=[rn], writes=[rn])
                    yield
                    p.op("dve", lambda e, r=r: e.reciprocal(out=r[:, 7:8], in_=r[:, 6:7]), reads=[rn], writes=[rn])
                    yield
                    p.op("dve", lambda e, o_h=o_h, r=r, h=h: e.scalar_tensor_tensor(out=o_h[:], in0=o_h[:], scalar=r[:, 7:8], in1=HGm[:, h * 256:(h + 1) * 256], op0=ALU.mult, op1=ALU.mult),
                         reads=[on, rn, "HGm"], writes=[on])
                    yield
                    p.op("pool", lambda e, o_h=o_h, hn_=hn_, h=h: e.tensor_tensor(out=hn_[:, h * 256:(h + 1) * 256], in0=o_h[:], in1=sig[:, h * 256:(h + 1) * 256], op=ALU.mult),
                         reads=[on, "sig"], writes=["hn%d_%d" % (b, h)])
                    yield
            for hp2 in range(4):
                g0, g1 = head_ops(2 * hp2), head_ops(2 * hp2 + 1)
                alive = [g0, g1]
                while alive:
                    for g_ in list(alive):
                        try:
                            next(g_)
                        except StopIteration:
                            alive.remove(g_)
            if full:
                p.dma("sp", lambda e, b=b, c=c: e.dma_start(out=cx.HN.ap()[c * 128:(c + 1) * 128, :], in_=hn[b][:]), reads=["hn%d_%d" % (b, h) for h in range(8)], writes=["hnd%d" % c])
        if st_out is not None:
            so = cx.STATE_OUT.ap() if st_out == "dram" else cx.ST_LOC.ap()[:, 0:8 * 257].rearrange("p (h e) -> p h e", h=8)
            ho_ = cx.HALO_OUT.ap() if st_out == "dram" else cx.ST_LOC.ap()[:, 8 * 257:8 * 257 + 48].rearrange("p (c t) -> p c t", c=16)
            p.dma("sp", lambda e: e.dma_start(out=so, in_=C[:]), reads=["Cst%d" % h for h in range(8)], writes=["so"])
            p.dma("sp", lambda e: e.dma_start(out=hlb[:], in_=qkv[:, :, TOK:TOK + 3], allow_slow_non_contiguous=True), reads=["halo"], writes=["hlb"])
            p.op("dve", lambda e: e.tensor_copy(out=hl[:], in_=hlb[:]), reads=["hlb"], writes=["hl"])
            p.dma("sp", lambda e: e.dma_start(out=ho_, in_=hl[:]), reads=["hl"], writes=["ho_"])
        p.emit()


def mlstm_layer(p, cx, XIN, XOUT, full=True, fused=False):
    with ExitStack() as st1:
        G, SH, GT = load_mod_tiles(p, cx, st1, 1, 0, cx.NORM_MIX_G)
        hT = p.sb(st1, "hT", [128, 16, TOK], BF16)
        norm_to_hT(p, cx, XIN.ap(), G, SH, hT)
        mlstm_proj(p, cx, hT, full)
    if fused:
        mlstm_rec(p, cx, False, st_in="zero", st_out="loc")
        p.coll(lambda e: e.collective_compute("AllGather", ALU.bypass, replica_groups=REPLICA,
                                              ins=[cx.ST_LOC.ap()], outs=[cx.ST_ALL.ap()]), writes=["ST_ALL"])
        p.emit()
        mlstm_rec(p, cx, True, st_in="gather", st_out=None)
    else:
        mlstm_rec(p, cx, full)
    if not full:
        return
    with ExitStack() as st2:
        GT = p.sb(st2, "GT", [128, D], F32)
        p.dma("sp", lambda e: e.dma_start(out=GT[:], in_=bc(cx.MODD.ap()[1, 2 * D:3 * D])), writes=["GT"])
        proj_out_stage(p, cx, cx.HN.ap(), cx.ML_W_OUT.ap(), GT, XIN.ap(), XOUT.ap())


def _common(nc, cx, st):
    cx.nc = nc
    cx.CONSTS = nc.dram_tensor("CONSTS", [128, C_W], F32, kind="ExternalInput")
    cx.NORM_MIX_G = nc.dram_tensor("NORM_MIX_G", [2, D], F32, kind="ExternalInput")
    cx.NORM_FFN_G = nc.dram_tensor("NORM_FFN_G", [2, D], F32, kind="ExternalInput")
    cx.WR = nc.dram_tensor("WR", [2, 128, 16, 36], F32, kind="ExternalInput")
    cx.BRT = nc.dram_tensor("BRT", [2, 36], F32, kind="ExternalInput")
    cx.XS = nc.dram_tensor("XS", [NSLOT + TOK, D], BF16, kind="Internal")
    cx.YS = nc.dram_tensor("YS", [NSLOT + TOK, D], F32, kind="Internal")
    p = Prog(nc, st)
    cx.bank = [st.enter_context(nc.psum_tensor("bank%d" % i, [128, 512], F32)) for i in range(8)]
    load_consts(p, cx, st)
    return p


def build_A():
    nc = bass.Bass("TRN2", target_bir_lowering=False)
    cx = Ctx()
    with ExitStack() as st:
        p = _common(nc, cx, st)
        cx.WL = lambda L: 0
        cx.MODD = nc.dram_tensor("MODD", [2, 6 * D], F32, kind="ExternalOutput")
        cx.CVT = nc.dram_tensor("CVT", [128, 16], F32, kind="ExternalInput")
        cx.ADA_W = nc.dram_tensor("ADA_W", [2, D, 6 * D], F32, kind="ExternalInput")
        cx.ADA_B = nc.dram_tensor("ADA_B", [2, 6 * D], F32, kind="ExternalInput")
        cx.ATTN_W_IN = nc.dram_tensor("ATTN_W_IN", [D, 3 * D], F32, kind="ExternalInput")
        cx.ATTN_W_OUT = nc.dram_tensor("ATTN_W_OUT", [D, D], F32, kind="ExternalInput")
        cx.LQ1 = nc.dram_tensor("LQ1", [1, 128], F32, kind="ExternalInput")
        cx.LK1 = nc.dram_tensor("LK1", [1, 128], F32, kind="ExternalInput")
        cx.LQ2 = nc.dram_tensor("LQ2", [1, 128], F32, kind="ExternalInput")
        cx.LK2 = nc.dram_tensor("LK2", [1, 128], F32, kind="ExternalInput")
        cx.ATTN_HG = nc.dram_tensor("ATTN_HG", [1, 256], F32, kind="ExternalInput")
        cx.PREVFLAG = nc.dram_tensor("PREVFLAG", [128, 1], F32, kind="ExternalInput")
        cx.POS_OWN = nc.dram_tensor("POS_OWN", [1, TOK], I32, kind="ExternalInput")
        cx.POS_PREV = nc.dram_tensor("POS_PREV", [1, TOK], I32, kind="ExternalInput")
        cx.W_GU = nc.dram_tensor("W_GU", [1, NE, D, 2 * HID], F32, kind="ExternalInput")
        cx.W_DN = nc.dram_tensor("W_DN", [1, NE, HID, D], F32, kind="ExternalInput")
        XIN = nc.dram_tensor("XIN", [TOK, D], F32, kind="ExternalInput")
        XPREV = nc.dram_tensor("XPREV", [TOK, D], F32, kind="ExternalInput")
        X1 = nc.dram_tensor("X1", [TOK, D], F32, kind="ExternalOutput")
        XMID = nc.dram_tensor("XMID", [TOK, D], F32, kind="Internal")
        cx.QTD = nc.dram_tensor("QTD", [16, 128, TOK], BF16, kind="Internal")
        cx.KTD = nc.dram_tensor("KTD", [16, 128, 2 * TOK], BF16, kind="Internal")
        cx.VD = nc.dram_tensor("VD", [2 * TOK, 8, 257], BF16, kind="Internal")
        cx.OAD = nc.dram_tensor("OAD", [TOK, D], BF16, kind="Internal")
        mod_stage(p, cx, 0)
        mod_stage(p, cx, 1)
        attn_layer(p, cx, XIN, XPREV, XMID)
        moe_stage(p, cx, 0, XMID, X1)
    return nc


def build_B(full=True):
    nc = bass.Bass("TRN2", target_bir_lowering=False)
    cx = Ctx()
    with ExitStack() as st:
        p = _common(nc, cx, st)
        cx.WL = lambda L: 0
        cx.MODD = nc.dram_tensor("MODD", [2, 6 * D], F32, kind="ExternalInput")
        cx.ML_W_IN = nc.dram_tensor("ML_W_IN", [D, 3 * D + 16], F32, kind="ExternalInput")
        cx.CONV_W = nc.dram_tensor("CONV_W", [128, 16, 4], F32, kind="ExternalInput")
        cx.CONV_B = nc.dram_tensor("CONV_B", [128, 16], F32, kind="ExternalInput")
        cx.GATE_B = nc.dram_tensor("GATE_B", [1, 16], F32, kind="ExternalInput")
        cx.ML_HG = nc.dram_tensor("ML_HG", [1, D], F32, kind="ExternalInput")
        if full:
            cx.ML_W_OUT = nc.dram_tensor("ML_W_OUT", [D, D], F32, kind="ExternalInput")
            cx.FINAL_G = nc.dram_tensor("FINAL_G", [D], F32, kind="ExternalInput")
            cx.W_GU = nc.dram_tensor("W_GU", [1, NE, D, 2 * HID], F32, kind="ExternalInput")
            cx.W_DN = nc.dram_tensor("W_DN", [1, NE, HID, D], F32, kind="ExternalInput")
            OUT = nc.dram_tensor("OUT", [TOK, D], F32, kind="ExternalOutput")
        cx.STATE_IN = nc.dram_tensor("STATE_IN", [128, 8, 257], F32, kind="ExternalInput")
        cx.HALO_IN = nc.dram_tensor("HALO_IN", [128, 16, 3], F32, kind="ExternalInput")
        cx.STATE_OUT = nc.dram_tensor("STATE_OUT", [128, 8, 257], F32, kind="ExternalOutput")
        cx.HALO_OUT = nc.dram_tensor("HALO_OUT", [128, 16, 3], F32, kind="ExternalOutput")
        XIN = nc.dram_tensor("XIN", [TOK, D], F32, kind="ExternalInput")
        XMID = nc.dram_tensor("XMID", [TOK, D], F32, kind="Internal")
        cx.QKP = nc.dram_tensor("QKP", [16, 128, 3 + TOK], BF16, kind="Internal")
        cx.VM = nc.dram_tensor("VM", [TOK, 8, 257], BF16, kind="Internal")
        cx.OPRE = nc.dram_tensor("OPRE", [TOK, D], BF16, kind="Internal")
        cx.GATES = nc.dram_tensor("GATES", [TOK, 16], F32, kind="Internal")
        cx.HN = nc.dram_tensor("HN", [TOK, D], BF16, kind="Internal")
        mlstm_layer(p, cx, XIN, XMID, full)
        if full:
            moe_stage(p, cx, 1, XMID, OUT, final_g=cx.FINAL_G)
    return nc


def build_fused():
    nc = bass.Bass("TRN2", target_bir_lowering=False)
    cx = Ctx()
    with ExitStack() as st:
        p = _common(nc, cx, st)
        cx.WL = lambda L: L
        ei = lambda name, shape, dt=F32: nc.dram_tensor(name, list(shape), dt, kind="ExternalInput")
        it = lambda name, shape, dt=F32: nc.dram_tensor(name, list(shape), dt, kind="Internal")
        cx.MODD = it("MODD", [2, 6 * D])
        cx.CVT = ei("CVT", [128, 16])
        cx.ADA_W = ei("ADA_W", [2, D, 6 * D])
        cx.ADA_B = ei("ADA_B", [2, 6 * D])
        cx.ATTN_W_IN = ei("ATTN_W_IN", [D, 3 * D])
        cx.ATTN_W_OUT = ei("ATTN_W_OUT", [D, D])
        cx.LQ1 = ei("LQ1", [1, 128]); cx.LK1 = ei("LK1", [1, 128]); cx.LQ2 = ei("LQ2", [1, 128]); cx.LK2 = ei("LK2", [1, 128])
        cx.ATTN_HG = ei("ATTN_HG", [1, 256])
        cx.PREVFLAG = ei("PREVFLAG", [128, 1])
        cx.ODDFLAG = ei("ODDFLAG", [128, 1])
        cx.POS_OWN = ei("POS_OWN", [1, TOK], I32)
        cx.POS_PREV = ei("POS_PREV", [1, TOK], I32)
        cx.W_GU = ei("W_GU", [2, NE, D, 2 * HID])
        cx.W_DN = ei("W_DN", [2, NE, HID, D])
        cx.ML_W_IN = ei("ML_W_IN", [D, 3 * D + 16])
        cx.ML_W_OUT = ei("ML_W_OUT", [D, D])
        cx.CONV_W = ei("CONV_W", [128, 16, 4]); cx.CONV_B = ei("CONV_B", [128, 16]); cx.GATE_B = ei("GATE_B", [1, 16])
        cx.ML_HG = ei("ML_HG", [1, D]); cx.FINAL_G = ei("FINAL_G", [D])
        XIN = ei("XIN", [TOK, D]); XPREV = ei("XPREV", [TOK, D])
        OUT = nc.dram_tensor("OUT", [TOK, D], F32, kind="ExternalOutput")
        XMID0 = it("XMID0", [TOK, D]); X1 = it("X1", [TOK, D]); XMID1 = it("XMID1", [TOK, D])
        cx.QTD = it("QTD", [16, 128, TOK], BF16); cx.KTD = it("KTD", [16, 128, 2 * TOK], BF16)
        cx.VD = it("VD", [2 * TOK, 8, 257], BF16); cx.OAD = it("OAD", [TOK, D], BF16)
        cx.QKP = it("QKP", [16, 128, 3 + TOK], BF16); cx.VM = it("VM", [TOK, 8, 257], BF16)
        cx.OPRE = it("OPRE", [TOK, D], BF16); cx.GATES = it("GATES", [TOK, 16]); cx.HN = it("HN", [TOK, D], BF16)
        cx.ST_LOC = it("ST_LOC", [128, 8 * 257 + 48]); cx.ST_ALL = it("ST_ALL", [256, 8 * 257 + 48])
        S = STAGES or ("mod", "attn", "moe0", "ml", "moe1")
        if "mod" in S:
            mod_stage(p, cx, 0)
            mod_stage(p, cx, 1)
        if "attn" in S:
            attn_layer(p, cx, XIN, XPREV, XMID0)
        if "moe0" in S:
            moe_stage(p, cx, 0, XMID0, X1)
        if "ml" in S:
            mlstm_layer(p, cx, X1, XMID1, True, fused=True)
        if "moe1" in S:
            moe_stage(p, cx, 1, XMID1, OUT, final_g=cx.FINAL_G)
    return nc


def kernel(**inp):
    f32 = np.float32
    x = np.asarray(inp["x"], f32)
    c = np.asarray(inp["c"], f32)
    pos = np.asarray(inp["positions"], np.int32)
    wr = np.concatenate([inp["moe_w_group"], inp["moe_w_expert"]], axis=-1).reshape(2, 16, 128, 36).transpose(0, 2, 1, 3)
    wr = np.ascontiguousarray(wr, f32)
    brt = np.ascontiguousarray(np.concatenate([inp["moe_b_group"], inp["moe_b_expert"]], axis=-1), f32)
    cw = np.ascontiguousarray(np.asarray(inp["mlstm_conv_w"][0], f32).reshape(4, 16, 128).transpose(2, 1, 0))
    cbias = np.ascontiguousarray(np.asarray(inp["mlstm_conv_b"][0], f32).reshape(16, 128).T)
    n = 8
    common = {"CONSTS": make_consts(), "NORM_MIX_G": np.ascontiguousarray(inp["norm_mix_g"], f32), "NORM_FFN_G": np.ascontiguousarray(inp["norm_ffn_g"], f32),
              "WR": wr, "BRT": brt, "ADA_W": inp["ada_w"], "ADA_B": inp["ada_b"],
              "ATTN_W_IN": inp["attn_w_in"][0], "ATTN_W_OUT": inp["attn_w_out"][0],
              "LQ1": inp["attn_lambda_q1"], "LK1": inp["attn_lambda_k1"], "LQ2": inp["attn_lambda_q2"], "LK2": inp["attn_lambda_k2"],
              "ATTN_HG": inp["attn_head_norm_g"], "W_GU": inp["moe_w_gu"], "W_DN": inp["moe_w_down"],
              "ML_W_IN": inp["mlstm_w_in"][0], "ML_W_OUT": inp["mlstm_w_out"][0], "CONV_W": cw, "CONV_B": cbias,
              "GATE_B": inp["mlstm_gate_b"], "ML_HG": inp["mlstm_head_norm_g"], "FINAL_G": inp["final_norm_g"]}
    zx = np.zeros((TOK, D), f32)
    zp = np.zeros((1, TOK), np.int32)
    maps = []
    for core in range(n):
        b, hf = core // 2, core % 2
        sl = slice(hf * TOK, (hf + 1) * TOK)
        m = dict(common)
        m.update({
            "CVT": np.ascontiguousarray(c[b].reshape(16, 128).T),
            "PREVFLAG": np.full((128, 1), 0.0 if hf == 1 else -30000.0, f32),
            "ODDFLAG": np.full((128, 1), float(hf), f32),
            "POS_OWN": np.ascontiguousarray(pos[b, sl].reshape(1, TOK)),
            "POS_PREV": np.ascontiguousarray(pos[b, :TOK].reshape(1, TOK)) if hf == 1 else zp,
            "XIN": np.ascontiguousarray(x[b, sl]),
            "XPREV": np.ascontiguousarray(x[b, :TOK]) if hf == 1 else zx,
        })
        maps.append(m)
    nc = build_fused()
    if NCORES_DEBUG:
        res = run_bass_kernel_spmd(nc, maps[:NCORES_DEBUG], core_ids=list(range(NCORES_DEBUG))).results
        return res
    res = run_bass_kernel_spmd(nc, maps, core_ids=list(range(n))).results
    out = np.empty((4, 2 * TOK, D), f32)
    for core in range(n):
        b, hf = core // 2, core % 2
        out[b, hf * TOK:(hf + 1) * TOK] = res[core]["OUT"]
    return out
```

```python
import math
from contextlib import ExitStack
import numpy as np
import concourse.bass as bass
import concourse.mybir as mybir
from concourse.bass_utils import run_bass_kernel_spmd

F32 = mybir.dt.float32
BF16 = mybir.dt.bfloat16
I32 = mybir.dt.int32
AF = mybir.ActivationFunctionType
ALU = mybir.AluOpType
AX = mybir.AxisListType

D = 2048
TOK = 2048
NT = TOK // 128
NE = 32
CAP = 512
NBLK = CAP // 128
NSLOT = NE * CAP
HID = 1024
EPS = 1e-6

ENGS = ("pe", "act", "dve", "pool", "sp")
SAME_ENG_SYNC = True
N_DMA_SEMS = 40
REPLICA = [[0, 1], [2, 3], [4, 5], [6, 7]]
NCORES_DEBUG = 0
STAGES = None


class Prog:
    def __init__(self, nc, stack):
        self.nc = nc
        self.stack = stack
        self.cnt = {e: 0 for e in ENGS}
        self.esem = {e: stack.enter_context(nc.semaphore("s_" + e)) for e in ENGS}
        self.dsem = [stack.enter_context(nc.semaphore("d%d" % i)) for i in range(N_DMA_SEMS)]
        self.dval = [0] * N_DMA_SEMS
        self.drr = 0
        self.csem = stack.enter_context(nc.semaphore("csem"))
        self.cval = 0
        self.known = {e: {} for e in ENGS}
        self._reset()
        self.uid = 0

    def _reset(self):
        self.ops = {e: [] for e in ENGS}
        self.last_w = {}
        self.readers = {}

    def sb(self, st, name, shape, dt):
        self.uid += 1
        return st.enter_context(self.nc.sbuf_tensor("%s_%d" % (name, self.uid), list(shape), dt))

    def _deps(self, eng, reads, writes):
        deps = []
        for r in reads:
            t = self.last_w.get(r)
            if t is not None:
                deps.append(t)
        for w in writes:
            t = self.last_w.get(w)
            if t is not None:
                deps.append(t)
            deps.extend(self.readers.get(w, ()))
        waits = []
        kn = self.known[eng]
        for (sem, val, deng, sid) in deps:
            if deng == eng and (eng == "pe" or not SAME_ENG_SYNC):
                continue
            if kn.get(sid, 0) >= val:
                continue
            kn[sid] = val
            waits.append((sem, val))
        return waits

    def _commit(self, tok, reads, writes):
        for w in writes:
            self.last_w[w] = tok
            self.readers[w] = []
        for r in reads:
            if r in writes:
                continue
            lst = self.readers.setdefault(r, [])
            lst.append(tok)
            if len(lst) > 48:
                latest = {}
                keep = []
                for t in lst:
                    if t[2] == "dma":
                        keep.append(t)
                    else:
                        latest[t[2]] = t
                self.readers[r] = keep[-40:] + list(latest.values())

    def op(self, eng, fn, reads=(), writes=(), inc=True):
        waits = self._deps(eng, reads, writes)
        if inc:
            self.cnt[eng] += 1
            tok = (self.esem[eng], self.cnt[eng], eng, "e_" + eng)
            self.ops[eng].append((waits, fn, (self.esem[eng], 1)))
        else:
            tok = (self.esem[eng], self.cnt[eng] + 1, eng, "e_" + eng)
            self.ops[eng].append((waits, fn, None))
        self._commit(tok, reads, writes)

    def coll(self, fn, reads=(), writes=()):
        waits = self._deps("pool", reads, writes)
        self.cval += 1
        tok = (self.csem, self.cval, "dma", "csem")
        self.ops["pool"].append((waits, fn, (self.csem, 1)))
        self._commit(tok, reads, writes)

    def dma(self, q, fn, reads=(), writes=()):
        s = self.drr
        self.drr = (self.drr + 1) % N_DMA_SEMS
        waits = self._deps(q, reads, writes)
        prev = self.dval[s]
        sid = "d%d" % s
        if prev > 0 and self.known[q].get(sid, 0) < prev:
            self.known[q][sid] = prev
            waits.append((self.dsem[s], prev))
        self.dval[s] += 16
        tok = (self.dsem[s], self.dval[s], "dma", sid)
        self.ops[q].append((waits, fn, (self.dsem[s], 16)))
        self._commit(tok, reads, writes)

    def barrier(self):
        for e in ENGS:
            waits = []
            for e2 in ENGS:
                if e2 != e and self.cnt[e2] > 0 and self.known[e].get("e_" + e2, 0) < self.cnt[e2]:
                    self.known[e]["e_" + e2] = self.cnt[e2]
                    waits.append((self.esem[e2], self.cnt[e2]))
            for i in range(N_DMA_SEMS):
                sid = "d%d" % i
                if self.dval[i] > 0 and self.known[e].get(sid, 0) < self.dval[i]:
                    self.known[e][sid] = self.dval[i]
                    waits.append((self.dsem[i], self.dval[i]))
            if self.cval > 0 and self.known[e].get("csem", 0) < self.cval:
                self.known[e]["csem"] = self.cval
                waits.append((self.csem, self.cval))
            if waits:
                self.ops[e].append((waits, None, None))

    def emit(self):
        self.barrier()
        ops = self.ops
        with self.nc.Block() as block:
            def run(engname):
                def body(e):
                    for (waits, fn, inc) in ops[engname]:
                        for (sem, val) in waits:
                            e.wait_ge(sem, val)
                        if fn is not None:
                            ins = fn(e)
                            if inc is not None:
                                ins.then_inc(inc[0], inc[1])
                return body
            block.tensor(run("pe"))
            block.scalar(run("act"))
            block.vector(run("dve"))
            block.gpsimd(run("pool"))
            block.sync(run("sp"))
        self._reset()


C_ID, C_LS, C_ON, C_ECAP, C_TRI, C_CM, C_PERM, C_INVF, C_SGN, C_IOTA, C_W = 0, 128, 256, 384, 416, 544, 672, 704, 705, 706, 738


def make_consts():
    c = np.zeros((128, C_W), np.float32)
    i = np.arange(128)
    c[:, C_ID:C_ID + 128] = np.eye(128)
    c[:, C_LS:C_LS + 128] = (i[:, None] < i[None, :])
    c[:, C_ON:C_ON + 128] = 1.0
    c[:, C_ECAP:C_ECAP + 32] = (np.arange(32) * CAP)[None, :]
    c[:, C_TRI:C_TRI + 128] = (i[:, None] <= i[None, :])
    c[:, C_CM:C_CM + 128] = (i[:, None] <= i[None, :])
    for pp in range(32):
        c[(pp + 16) % 32, C_PERM + pp] = 1.0
    half = 16
    invf = 500000.0 ** (-np.arange(half, dtype=np.float32) * 2.0 / 32)
    c[:32, C_INVF] = np.tile(invf, 2)
    c[:16, C_SGN] = -1.0
    c[16:32, C_SGN] = 1.0
    c[:, C_IOTA:C_IOTA + 32] = np.arange(32)[None, :]
    return c


class Ctx:
    pass


def bc(ap1d, n=128):
    return ap1d.partition_broadcast(n)


def load_consts(p, cx, st):
    cx.cf = p.sb(st, "cf", [128, C_W], F32)
    cx.cb = p.sb(st, "cb", [128, C_W], BF16)
    p.dma("sp", lambda e: e.dma_start(out=cx.cf[:], in_=cx.CONSTS.ap()), writes=["cf"])
    p.op("dve", lambda e: e.tensor_copy(out=cx.cb[:], in_=cx.cf[:]), reads=["cf"], writes=["cb"])
    p.emit()


def rms_mod_tile(p, cx, xs, G, SH, hf, hb, ss, nm, rd=(), wr=()):
    junk = cx.junk
    p.op("act", lambda e: e.activation(out=junk[:], in_=xs[:], func=AF.Square, accum_out=ss[:, 0:1]),
         reads=[nm + "xs"], writes=["junk", nm + "ss"])
    p.op("dve", lambda e: e.tensor_scalar(out=ss[:, 1:2], in0=ss[:, 0:1], scalar1=1.0 / D, scalar2=EPS, op0=ALU.mult, op1=ALU.add),
         reads=[nm + "ss"], writes=[nm + "ss"])
    p.op("act", lambda e: e.activation(out=ss[:, 2:3], in_=ss[:, 1:2], func=AF.Sqrt), reads=[nm + "ss"], writes=[nm + "ss"])
    p.op("dve", lambda e: e.reciprocal(out=ss[:, 3:4], in_=ss[:, 2:3]), reads=[nm + "ss"], writes=[nm + "ss"])
    tmp = cx.tmpf
    p.op("dve", lambda e: e.scalar_tensor_tensor(out=tmp[:], in0=xs[:], scalar=ss[:, 3:4], in1=G[:], op0=ALU.mult, op1=ALU.mult),
         reads=[nm + "xs", nm + "ss"] + list(rd), writes=["tmpf"])
    if hf is not None:
        p.op("pool", lambda e: e.tensor_tensor(out=hf[:], in0=tmp[:], in1=SH[:], op=ALU.add), reads=["tmpf"] + list(rd), writes=[nm + "hf"])
        p.op("act", lambda e: e.activation(out=hb[:], in_=hf[:], func=AF.Copy), reads=[nm + "hf"], writes=[nm + "hb"])
    else:
        p.op("pool", lambda e: e.tensor_tensor(out=hb[:], in0=tmp[:], in1=SH[:], op=ALU.add), reads=["tmpf"] + list(rd), writes=[nm + "hb"])


def load_mod_tiles(p, cx, st, L, which, gname):
    base = 3 * D * which
    G = p.sb(st, "G", [128, D], F32)
    SH = p.sb(st, "SH", [128, D], F32)
    GT = p.sb(st, "GT", [128, D], F32)
    gn = p.sb(st, "gn", [128, D], F32)
    md = cx.MODD.ap()
    p.dma("sp", lambda e: e.dma_start(out=SH[:], in_=bc(md[L, base:base + D])), reads=["MODD"], writes=["SH"])
    p.dma("act", lambda e: e.dma_start(out=G[:], in_=bc(md[L, base + D:base + 2 * D])), reads=["MODD"], writes=["G"])
    p.dma("sp", lambda e: e.dma_start(out=GT[:], in_=bc(md[L, base + 2 * D:base + 3 * D])), reads=["MODD"], writes=["GT"])
    p.dma("act", lambda e: e.dma_start(out=gn[:], in_=bc(gname.ap()[L, :])), writes=["gn"])
    p.op("dve", lambda e: e.scalar_tensor_tensor(out=G[:], in0=G[:], scalar=1.0, in1=gn[:], op0=ALU.add, op1=ALU.mult),
         reads=["G", "gn"], writes=["G"])
    return G, SH, GT


def mod_stage(p, cx, L):
    with ExitStack() as st:
        cT = p.sb(st, "cT", [128, 16], F32)
        cTb = p.sb(st, "cTb", [128, 16], BF16)
        ring = [p.sb(st, "aw%d" % i, [128, 4096], BF16) for i in range(4)]
        row = p.sb(st, "mrow", [1, 4096], F32)
        brow = p.sb(st, "brow", [1, 4096], F32)
        p.dma("sp", lambda e: e.dma_start(out=cT[:], in_=cx.CVT.ap()), writes=["cT"])
        p.op("act", lambda e: e.activation(out=cTb[:], in_=cT[:], func=AF.Silu), reads=["cT"], writes=["cTb"])
        aw = cx.ADA_W.ap()
        k = 0
        for ps_ in range(3):
            p.dma("sp", lambda e, ps_=ps_: e.dma_start(out=brow[:], in_=cx.ADA_B.ap()[L:L + 1, ps_ * 4096:(ps_ + 1) * 4096]), writes=["brow"])
            for kc in range(16):
                buf = ring[k % 4]
                bn = "aw%d" % (k % 4)
                k += 1
                p.dma("pool", lambda e, buf=buf, kc=kc, ps_=ps_: e.dma_start(out=buf[:], in_=aw[L, kc * 128:(kc + 1) * 128, ps_ * 4096:(ps_ + 1) * 4096]),
                      writes=[bn])
                for n in range(8):
                    p.op("pe", lambda e, buf=buf, kc=kc, n=n: e.matmul(cx.bank[n][0:1, :], lhsT=cTb[:, kc:kc + 1], rhs=buf[:, n * 512:(n + 1) * 512],
                                                                      start=(kc == 0), stop=(kc == 15)),
                         reads=[bn, "cTb"], writes=["bank%d" % n], inc=(n == 7))
            for n in range(8):
                p.op("dve", lambda e, n=n: e.tensor_tensor(out=row[:, n * 512:(n + 1) * 512], in0=cx.bank[n][0:1, :], in1=brow[:, n * 512:(n + 1) * 512], op=ALU.add),
                     reads=["bank%d" % n, "brow"], writes=["mrow"])
            p.dma("sp", lambda e, ps_=ps_: e.dma_start(out=cx.MODD.ap()[L:L + 1, ps_ * 4096:(ps_ + 1) * 4096], in_=row[:]), reads=["mrow"], writes=["MODD"])
        p.emit()


def moe_stage(p, cx, L, XIN, XOUT, final_g=None):
    with ExitStack() as st0:
        d1i = p.sb(st0, "d1i", [128, NT], I32)
        d2i = p.sb(st0, "d2i", [128, NT], I32)
        gts = p.sb(st0, "gts", [128, 2 * NT], F32)
        xin = XIN.ap()
        with ExitStack() as st:
            G, SH, GT_ = load_mod_tiles(p, cx, st, L, 1, cx.NORM_FFN_G)
            cf, cb = cx.cf, cx.cb
            xs2 = [p.sb(st, "xs%d" % i, [128, D], F32) for i in range(2)]
            hf2 = [p.sb(st, "hf%d" % i, [128, D], F32) for i in range(2)]
            hb2 = [p.sb(st, "hb%d" % i, [128, D], BF16) for i in range(2)]
            cx.junk = p.sb(st, "junk", [128, D], BF16)
            cx.tmpf = p.sb(st, "tmpf", [128, D], F32)
            hT = p.sb(st, "hTf", [128, 16, 128], F32)
            wr = p.sb(st, "wr", [128, 16, 36], F32)
            br = p.sb(st, "br", [128, 36], F32)
            macc = p.sb(st, "macc", [128, 32], BF16)
            sm = [p.sb(st, "sm%d" % i, [128, 160], F32) for i in range(2)]
            mk = [p.sb(st, "mk%d" % i, [128, 32], BF16) for i in range(2)]
            p.dma("sp", lambda e: e.dma_start(out=wr[:], in_=cx.WR.ap()[L]), writes=["wr"])
            p.dma("sp", lambda e: e.dma_start(out=br[:], in_=bc(cx.BRT.ap()[L, :])), writes=["br"])
            p.op("dve", lambda e: e.memset(macc[:], 0.0), writes=["macc"])
            for t in range(NT):
                b = t % 2
                nm = "r%d" % b
                xs, hf, hb, s, m = xs2[b], hf2[b], hb2[b], sm[b], mk[b]
                p.dma("sp", lambda e, xs=xs, t=t: e.dma_start(out=xs[:], in_=xin[t * 128:(t + 1) * 128, :]), reads=["XIN"], writes=[nm + "xs"])
                rms_mod_tile(p, cx, xs, G, SH, hf, hb, s, nm, rd=["G", "SH"])
                for q4 in range(4):
                    for j in range(4):
                        kc = q4 * 4 + j
                        p.op("pe", lambda e, hf=hf, kc=kc, j=j, q4=q4: e.transpose(out=cx.bank[q4][:, j * 128:(j + 1) * 128], in_=hf[:, kc * 128:(kc + 1) * 128], identity=cf[:, C_ID:C_ID + 128]),
                             reads=[nm + "hf"], writes=["bank%d" % q4])
                    eng = "act" if q4 % 2 == 0 else "dve"
                    if eng == "act":
                        p.op("act", lambda e, q4=q4: e.activation(out=hT[:, q4 * 4:(q4 + 1) * 4, :], in_=cx.bank[q4][:].rearrange("p (a b) -> p a b", a=4), func=AF.Copy),
                             reads=["bank%d" % q4], writes=["hT%d" % q4])
                    else:
                        p.op("dve", lambda e, q4=q4: e.tensor_copy(out=hT[:, q4 * 4:(q4 + 1) * 4, :], in_=cx.bank[q4][:].rearrange("p (a b) -> p a b", a=4)),
                             reads=["bank%d" % q4], writes=["hT%d" % q4])
                lgp = cx.bank[4]
                for kc in range(16):
                    p.op("pe", lambda e, kc=kc: e.matmul(lgp[:, 0:36], lhsT=hT[:, kc, :], rhs=wr[:, kc, :], start=(kc == 0), stop=(kc == 15)),
                         reads=["hT%d" % (kc // 4), "wr"], writes=["bank4"], inc=(kc == 15))
                lg = s[:, 8:44]
                sn = nm + "s"
                p.op("dve", lambda e, lg=lg: e.tensor_tensor(out=lg, in0=lgp[:, 0:36], in1=br[:], op=ALU.add), reads=["bank4", "br"], writes=[sn])
                p.op("dve", lambda e, s=s: e.reduce_max(out=s[:, 44:45], in_=s[:, 8:12], axis=AX.X), reads=[sn], writes=[sn])
                p.op("dve", lambda e, s=s: e.tensor_scalar(out=s[:, 45:46], in0=s[:, 44:45], scalar1=-1.0, scalar2=None, op0=ALU.mult), reads=[sn], writes=[sn])
                p.op("dve", lambda e, s=s: e.tensor_scalar(out=s[:, 48:52], in0=s[:, 8:12], scalar1=s[:, 44:45], scalar2=None, op0=ALU.is_equal), reads=[sn], writes=[sn])
                p.op("act", lambda e, s=s: e.activation(out=s[:, 100:104], in_=s[:, 8:12], func=AF.Exp, bias=s[:, 45:46], accum_out=s[:, 46:47]), reads=[sn], writes=[sn])
                p.op("dve", lambda e, s=s: e.reciprocal(out=s[:, 47:48], in_=s[:, 46:47]), reads=[sn], writes=[sn])
                p.op("dve", lambda e, s=s: e.tensor_scalar(out=s[:, 52:56], in0=s[:, 48:52], scalar1=1e30, scalar2=-1e30, op0=ALU.mult, op1=ALU.add), reads=[sn], writes=[sn])
                p.op("dve", lambda e, s=s: e.tensor_tensor(out=s[:, 56:88].rearrange("p (g k) -> p g k", g=4), in0=s[:, 12:44].rearrange("p (g k) -> p g k", g=4),
                                                           in1=s[:, 52:56].unsqueeze(2).to_broadcast([128, 4, 8]), op=ALU.add), reads=[sn], writes=[sn])
                p.op("dve", lambda e, s=s: e.max(out=s[:, 88:96], in_=s[:, 56:88]), reads=[sn], writes=[sn])
                p.op("dve", lambda e, s=s: e.tensor_tensor(out=s[:, 96:97], in0=s[:, 89:90], in1=s[:, 88:89], op=ALU.subtract), reads=[sn], writes=[sn])
                p.op("act", lambda e, s=s: e.activation(out=s[:, 97:98], in_=s[:, 96:97], func=AF.Exp), reads=[sn], writes=[sn])
                p.op("dve", lambda e, s=s: e.tensor_scalar(out=s[:, 98:99], in0=s[:, 97:98], scalar1=1.0, scalar2=None, op0=ALU.add), reads=[sn], writes=[sn])
                p.op("dve", lambda e, s=s: e.reciprocal(out=s[:, 99:100], in_=s[:, 98:99]), reads=[sn], writes=[sn])
                p.op("dve", lambda e, s=s, t=t: e.tensor_tensor(out=gts[:, 2 * t:2 * t + 1], in0=s[:, 99:100], in1=s[:, 47:48], op=ALU.mult), reads=[sn], writes=["gts"])
                p.op("dve", lambda e, s=s, t=t: e.tensor_tensor(out=gts[:, 2 * t + 1:2 * t + 2], in0=gts[:, 2 * t:2 * t + 1], in1=s[:, 97:98], op=ALU.mult), reads=[sn, "gts"], writes=["gts"])
                p.op("dve", lambda e, s=s: e.tensor_scalar(out=s[:, 100:132], in0=s[:, 56:88], scalar1=s[:, 88:89], scalar2=None, op0=ALU.is_equal), reads=[sn], writes=[sn])
                p.op("dve", lambda e, s=s: e.tensor_scalar(out=s[:, 8:40], in0=s[:, 56:88], scalar1=s[:, 89:90], scalar2=None, op0=ALU.is_equal), reads=[sn], writes=[sn])
                p.op("dve", lambda e, s=s, m=m: e.tensor_tensor(out=m[:], in0=s[:, 100:132], in1=s[:, 8:40], op=ALU.add), reads=[sn], writes=[nm + "mk"])
                pp = cx.bank[5]
                p.op("pe", lambda e, m=m: e.matmul(pp[:, 0:32], lhsT=cb[:, C_LS:C_LS + 128], rhs=m[:], start=True, stop=False), reads=[nm + "mk"], writes=["bank5"])
                p.op("pe", lambda e: e.matmul(pp[:, 0:32], lhsT=cb[:, C_ON:C_ON + 128], rhs=macc[:], start=False, stop=True), reads=["macc"], writes=["bank5"])
                p.op("dve", lambda e, s=s: e.tensor_tensor(out=s[:, 56:88], in0=pp[:, 0:32], in1=cf[:, C_ECAP:C_ECAP + 32], op=ALU.add), reads=["bank5", sn], writes=[sn])
                p.op("dve", lambda e, m=m: e.tensor_tensor(out=macc[:], in0=macc[:], in1=m[:], op=ALU.add), reads=["macc", nm + "mk", "bank5"], writes=["macc"])
                p.op("dve", lambda e, s=s: e.tensor_tensor(out=s[:, 100:132], in0=s[:, 100:132], in1=s[:, 56:88], op=ALU.mult), reads=[sn], writes=[sn])
                p.op("dve", lambda e, s=s: e.reduce_sum(out=s[:, 132:133], in_=s[:, 100:132], axis=AX.X), reads=[sn], writes=[sn])
                p.op("dve", lambda e, s=s: e.tensor_tensor(out=s[:, 8:40], in0=s[:, 8:40], in1=s[:, 56:88], op=ALU.mult), reads=[sn], writes=[sn])
                p.op("dve", lambda e, s=s: e.reduce_sum(out=s[:, 133:134], in_=s[:, 8:40], axis=AX.X), reads=[sn], writes=[sn])
                p.op("dve", lambda e, s=s, t=t: e.tensor_copy(out=d1i[:, t:t + 1], in_=s[:, 132:133]), reads=[sn], writes=["d1i"])
                p.op("dve", lambda e, s=s, t=t: e.tensor_copy(out=d2i[:, t:t + 1], in_=s[:, 133:134]), reads=[sn], writes=["d2i"])
                p.dma("pool", lambda e, hb=hb, t=t: e.indirect_dma_start(out=cx.XS.ap(), out_offset=bass.IndirectOffsetOnAxis(ap=d1i[:, t:t + 1], axis=0), in_=hb[:], in_offset=None),
                      reads=[nm + "hb", "d1i"], writes=["XSa%d" % t])
                p.dma("pool", lambda e, hb=hb, t=t: e.indirect_dma_start(out=cx.XS.ap(), out_offset=bass.IndirectOffsetOnAxis(ap=d2i[:, t:t + 1], axis=0), in_=hb[:], in_offset=None),
                      reads=[nm + "hb", "d2i"], writes=["XSb%d" % t])
            p.emit()
        with ExitStack() as st:
            cb = cx.cb
            NWG = 32
            NWD = 12
            wg = [p.sb(st, "wg%d" % i, [128, 2, 512], BF16) for i in range(NWG)]
            wd = [p.sb(st, "wd%d" % i, [128, D], BF16) for i in range(NWD)]
            xg = p.sb(st, "xg", [128, NBLK, D], BF16)
            xT = [p.sb(st, "xT%d" % i, [128, 16, CAP], BF16) for i in range(2)]
            hTb = p.sb(st, "hTb", [128, 8, CAP], BF16)
            sg = [p.sb(st, "sg%d" % i, [128, 256], F32) for i in range(2)]
            yb = [p.sb(st, "yb%d" % i, [128, D], F32) for i in range(2)]
            xsv = cx.XS.ap()[0:NSLOT].rearrange("(e b p) d -> e p b d", b=NBLK, p=128)
            ysv = cx.YS.ap()[0:NSLOT].rearrange("(e b p) d -> e b p d", b=NBLK, p=128)
            wguv = cx.W_GU.ap()
            wdv = cx.W_DN.ap()
            kg = 0
            kd = 0
            ky = 0
            for ex in range(NE):
                xb_ = ex % 2
                xTb = xT[xb_]
                p.dma("sp", lambda e, ex=ex: e.dma_start(out=xg[:], in_=xsv[ex]), writes=["xg"])
                tcnt = 0
                for blk in range(NBLK):
                    for hh in range(2):
                        bk = cx.bank[4 + tcnt % 4]
                        bkn = "bank%d" % (4 + tcnt % 4)
                        tcnt += 1
                        bkv = bk[:].bitcast(BF16)
                        for j in range(8):
                            kc = hh * 8 + j
                            p.op("pe", lambda e, bkv=bkv, j=j, kc=kc, blk=blk: e.transpose(out=bkv[:, j * 128:(j + 1) * 128], in_=xg[:, blk, kc * 128:(kc + 1) * 128], identity=cb[:, C_ID:C_ID + 128]),
                                 reads=["xg"], writes=[bkn], inc=(j == 7))
                        if tcnt % 2 == 0:
                            p.op("act", lambda e, bkv=bkv, hh=hh, blk=blk, xTb=xTb: e.activation(out=xTb[:, hh * 8:(hh + 1) * 8, blk * 128:(blk + 1) * 128], in_=bkv.rearrange("p (a b) -> p a b", a=8), func=AF.Copy),
                                 reads=[bkn], writes=["xT%d" % xb_])
                        else:
                            p.op("dve", lambda e, bkv=bkv, hh=hh, blk=blk, xTb=xTb: e.tensor_copy(out=xTb[:, hh * 8:(hh + 1) * 8, blk * 128:(blk + 1) * 128], in_=bkv.rearrange("p (a b) -> p a b", a=8)),
                                 reads=[bkn], writes=["xT%d" % xb_])
                for hp in range(2):
                    wgl = []
                    for kc in range(16):
                        i = kg % NWG
                        kg += 1
                        wgl.append(i)
                        p.dma("pool", lambda e, i=i, ex=ex, kc=kc, hp=hp: e.dma_start(out=wg[i][:], in_=wguv[cx.WL(L), ex, kc * 128:(kc + 1) * 128, :].rearrange("k (g c) -> k g c", g=2)[:, :, hp * 512:(hp + 1) * 512]),
                              writes=["wg%d" % i])
                    for sb_ in range(CAP // 256):
                        bo = 4 * ((hp * (CAP // 256) + sb_) % 2)
                        for kc in range(16):
                            i = wgl[kc]
                            for gi in range(2):
                                for j in range(4):
                                    bi = bo + gi * 2 + j // 2
                                    p.op("pe", lambda e, i=i, gi=gi, j=j, bi=bi, kc=kc, xTb=xTb, sb_=sb_: e.matmul(cx.bank[bi][:, (j % 2) * 256:(j % 2 + 1) * 256], lhsT=wg[i][:, gi, j * 128:(j + 1) * 128], rhs=xTb[:, kc, sb_ * 256:(sb_ + 1) * 256],
                                                                                                                  start=(kc == 0 and j % 2 == 0), stop=(kc == 15), skip_group_check=True),
                                         reads=["wg%d" % i, "xT%d" % xb_], writes=["bank%d" % bi], inc=(kc == 15))
                        for j in range(4):
                            s_ = sg[j % 2]
                            p.op("act", lambda e, s_=s_, j=j, bo=bo: e.activation(out=s_[:], in_=cx.bank[bo + j // 2][:, (j % 2) * 256:(j % 2 + 1) * 256], func=AF.Silu),
                                 reads=["bank%d" % (bo + j // 2)], writes=["sg%d" % (j % 2)])
                            p.op("dve", lambda e, s_=s_, j=j, hp=hp, sb_=sb_, bo=bo: e.tensor_tensor(out=hTb[:, hp * 4 + j, sb_ * 256:(sb_ + 1) * 256], in0=s_[:], in1=cx.bank[bo + 2 + j // 2][:, (j % 2) * 256:(j % 2 + 1) * 256], op=ALU.mult),
                                 reads=["sg%d" % (j % 2), "bank%d" % (bo + 2 + j // 2)], writes=["hTb"])
                wdl = []
                for hc in range(8):
                    i = kd % NWD
                    kd += 1
                    wdl.append(i)
                    p.dma("pool", lambda e, i=i, ex=ex, hc=hc: e.dma_start(out=wd[i][:], in_=wdv[cx.WL(L), ex, hc * 128:(hc + 1) * 128, :]), writes=["wd%d" % i])
                for blk in range(NBLK):
                    y = yb[ky % 2]
                    yn = "yb%d" % (ky % 2)
                    ky += 1
                    db = 4 * ((blk + 1) % 2)
                    for cg in range(4):
                        for hc in range(8):
                            i = wdl[hc]
                            p.op("pe", lambda e, i=i, cg=cg, hc=hc, blk=blk, db=db: e.matmul(cx.bank[db + cg][:], lhsT=hTb[:, hc, blk * 128:(blk + 1) * 128], rhs=wd[i][:, cg * 512:(cg + 1) * 512], start=(hc == 0), stop=(hc == 7)),
                                 reads=["hTb", "wd%d" % i], writes=["bank%d" % (db + cg)], inc=(hc == 7))
                        if cg % 2 == 0:
                            p.op("act", lambda e, y=y, cg=cg, db=db: e.activation(out=y[:, cg * 512:(cg + 1) * 512], in_=cx.bank[db + cg][:], func=AF.Copy), reads=["bank%d" % (db + cg)], writes=[yn])
                        else:
                            p.op("dve", lambda e, y=y, cg=cg, db=db: e.tensor_copy(out=y[:, cg * 512:(cg + 1) * 512], in_=cx.bank[db + cg][:]), reads=["bank%d" % (db + cg)], writes=[yn])
                    p.dma("act", lambda e, y=y, ex=ex, blk=blk: e.dma_start(out=ysv[ex, blk], in_=y[:]), reads=[yn], writes=["YS%d_%d" % (ex, blk)])
            p.emit()
        with ExitStack() as st:
            xs2 = [p.sb(st, "cxs%d" % i, [128, D], F32) for i in range(2)]
            y1 = [p.sb(st, "y1_%d" % i, [128, D], F32) for i in range(2)]
            y2 = [p.sb(st, "y2_%d" % i, [128, D], F32) for i in range(2)]
            GT = p.sb(st, "GT", [128, D], F32)
            p.dma("sp", lambda e: e.dma_start(out=GT[:], in_=bc(cx.MODD.ap()[L, 5 * D:6 * D])), writes=["GT"])
            if final_g is not None:
                FG = p.sb(st, "FG", [128, D], F32)
                p.dma("sp", lambda e: e.dma_start(out=FG[:], in_=bc(final_g.ap()[:])), writes=["FG"])
                cx.junk = p.sb(st, "junk", [128, D], BF16)
                fs = [p.sb(st, "fs%d" % i, [128, 4], F32) for i in range(2)]
            xout = XOUT.ap()
            for t in range(NT):
                b = t % 2
                xs, a1, a2 = xs2[b], y1[b], y2[b]
                p.dma("sp", lambda e, xs=xs, t=t: e.dma_start(out=xs[:], in_=xin[t * 128:(t + 1) * 128, :]), reads=["XIN"], writes=["cxs%d" % b])
                p.dma("pool", lambda e, a1=a1, t=t: e.indirect_dma_start(out=a1[:], out_offset=None, in_=cx.YS.ap(), in_offset=bass.IndirectOffsetOnAxis(ap=d1i[:, t:t + 1], axis=0)),
                      reads=["d1i"], writes=["y1_%d" % b])
                p.dma("pool", lambda e, a2=a2, t=t: e.indirect_dma_start(out=a2[:], out_offset=None, in_=cx.YS.ap(), in_offset=bass.IndirectOffsetOnAxis(ap=d2i[:, t:t + 1], axis=0)),
                      reads=["d2i"], writes=["y2_%d" % b])
                p.op("act", lambda e, a1=a1, t=t: e.activation(out=a1[:], in_=a1[:], func=AF.Copy, scale=gts[:, 2 * t:2 * t + 1]), reads=["y1_%d" % b, "gts"], writes=["y1_%d" % b])
                p.op("dve", lambda e, a1=a1, a2=a2, t=t: e.scalar_tensor_tensor(out=a2[:], in0=a2[:], scalar=gts[:, 2 * t + 1:2 * t + 2], in1=a1[:], op0=ALU.mult, op1=ALU.add),
                     reads=["y1_%d" % b, "y2_%d" % b, "gts"], writes=["y2_%d" % b])
                p.op("pool", lambda e, a2=a2: e.tensor_tensor(out=a2[:], in0=a2[:], in1=GT[:], op=ALU.mult), reads=["y2_%d" % b, "GT"], writes=["y2_%d" % b])
                p.op("dve", lambda e, a2=a2, xs=xs: e.tensor_tensor(out=xs[:], in0=a2[:], in1=xs[:], op=ALU.add), reads=["y2_%d" % b, "cxs%d" % b], writes=["cxs%d" % b])
                if final_g is not None:
                    f = fs[b]
                    p.op("act", lambda e, xs=xs, f=f: e.activation(out=cx.junk[:], in_=xs[:], func=AF.Square, accum_out=f[:, 0:1]), reads=["cxs%d" % b], writes=["junk", "fs%d" % b])
                    p.op("dve", lambda e, f=f: e.tensor_scalar(out=f[:, 1:2], in0=f[:, 0:1], scalar1=1.0 / D, scalar2=EPS, op0=ALU.mult, op1=ALU.add), reads=["fs%d" % b], writes=["fs%d" % b])
                    p.op("act", lambda e, f=f: e.activation(out=f[:, 2:3], in_=f[:, 1:2], func=AF.Sqrt), reads=["fs%d" % b], writes=["fs%d" % b])
                    p.op("dve", lambda e, f=f: e.reciprocal(out=f[:, 3:4], in_=f[:, 2:3]), reads=["fs%d" % b], writes=["fs%d" % b])
                    p.op("dve", lambda e, xs=xs, f=f: e.scalar_tensor_tensor(out=xs[:], in0=xs[:], scalar=f[:, 3:4], in1=FG[:], op0=ALU.mult, op1=ALU.mult),
                         reads=["cxs%d" % b, "fs%d" % b, "FG"], writes=["cxs%d" % b])
                p.dma("sp", lambda e, xs=xs, t=t: e.dma_start(out=xout[t * 128:(t + 1) * 128, :], in_=xs[:]), reads=["cxs%d" % b], writes=["XOUT"])
            p.emit()


def norm_to_hT(p, cx, XSRC, G, SH, hT, tb0=6):
    with ExitStack() as st:
        xs2 = [p.sb(st, "nxs%d" % i, [128, D], F32) for i in range(2)]
        hb2 = [p.sb(st, "nhb%d" % i, [128, D], BF16) for i in range(2)]
        ss2 = [p.sb(st, "nss%d" % i, [128, 4], F32) for i in range(2)]
        jk2 = [p.sb(st, "njk%d" % i, [128, D], BF16) for i in range(2)]
        tm2 = [p.sb(st, "ntm%d" % i, [128, D], F32) for i in range(2)]
        cb = cx.cb

        def tile_ops(t):
            b = t % 2
            nm = "n%d" % b
            xs, hb, ss, jk, tm = xs2[b], hb2[b], ss2[b], jk2[b], tm2[b]
            p.dma("sp", lambda e: e.dma_start(out=xs[:], in_=XSRC[t * 128:(t + 1) * 128, :]), writes=[nm + "xs"])
            yield
            p.op("act", lambda e: e.activation(out=jk[:], in_=xs[:], func=AF.Square, accum_out=ss[:, 0:1]), reads=[nm + "xs"], writes=[nm + "jk", nm + "ss"])
            yield
            p.op("dve", lambda e: e.tensor_scalar(out=ss[:, 1:2], in0=ss[:, 0:1], scalar1=1.0 / D, scalar2=EPS, op0=ALU.mult, op1=ALU.add), reads=[nm + "ss"], writes=[nm + "ss"])
            yield
            p.op("act", lambda e: e.activation(out=ss[:, 2:3], in_=ss[:, 1:2], func=AF.Sqrt), reads=[nm + "ss"], writes=[nm + "ss"])
            yield
            p.op("dve", lambda e: e.reciprocal(out=ss[:, 3:4], in_=ss[:, 2:3]), reads=[nm + "ss"], writes=[nm + "ss"])
            yield
            p.op("dve", lambda e: e.scalar_tensor_tensor(out=tm[:], in0=xs[:], scalar=ss[:, 3:4], in1=G[:], op0=ALU.mult, op1=ALU.mult),
                 reads=[nm + "xs", nm + "ss", "G", "SH"], writes=[nm + "tm"])
            yield
            p.op("pool", lambda e: e.tensor_tensor(out=hb[:], in0=tm[:], in1=SH[:], op=ALU.add), reads=[nm + "tm", "G", "SH"], writes=[nm + "hb"])
            yield
            for hh in range(2):
                bk = cx.bank[tb0 + hh]
                bkn = "bank%d" % (tb0 + hh)
                bkv = bk[:].bitcast(BF16)
                for j in range(8):
                    kc = hh * 8 + j
                    p.op("pe", lambda e, bkv=bkv, j=j, kc=kc: e.transpose(out=bkv[:, j * 128:(j + 1) * 128], in_=hb[:, kc * 128:(kc + 1) * 128], identity=cb[:, C_ID:C_ID + 128]),
                         reads=[nm + "hb"], writes=[bkn], inc=(j == 7))
                yield
                if hh == 0:
                    p.op("act", lambda e, bkv=bkv, hh=hh: e.activation(out=hT[:, hh * 8:(hh + 1) * 8, t * 128:(t + 1) * 128], in_=bkv.rearrange("p (a b) -> p a b", a=8), func=AF.Copy),
                         reads=[bkn], writes=["hT_%d_%d" % (t, hh)])
                else:
                    p.op("dve", lambda e, bkv=bkv, hh=hh: e.tensor_copy(out=hT[:, hh * 8:(hh + 1) * 8, t * 128:(t + 1) * 128], in_=bkv.rearrange("p (a b) -> p a b", a=8)),
                         reads=[bkn], writes=["hT_%d_%d" % (t, hh)])
                yield

        active = []
        nxt = 0
        while active or nxt < NT:
            if nxt < NT and (not active or (len(active) < 2 and active[0][1] >= 5)):
                active.append([tile_ops(nxt), 0])
                nxt += 1
            for a_ in list(active):
                try:
                    next(a_[0])
                    a_[1] += 1
                except StopIteration:
                    active.remove(a_)
        p.emit()


def proj_out_stage(p, cx, SRC, Wap, GT, XIN, XOUT):
    with ExitStack() as st:
        cb = cx.cb
        W = p.sb(st, "Wout", [128, 16, D], BF16)
        wv = Wap.rearrange("(kc k) c -> k kc c", k=128)
        for i in range(4):
            p.dma("pool", lambda e, i=i: e.dma_start(out=W[:, :, i * 512:(i + 1) * 512], in_=wv[:, :, i * 512:(i + 1) * 512]), writes=["Wout%d" % i])
        sr2 = [p.sb(st, "osr%d" % i, [128, D], BF16) for i in range(2)]
        sT2 = [p.sb(st, "osT%d" % i, [128, 16, 128], BF16) for i in range(2)]
        xs2 = [p.sb(st, "oxs%d" % i, [128, D], F32) for i in range(2)]
        tm2 = [p.sb(st, "otm%d" % i, [128, D], F32) for i in range(2)]
        for t in range(NT):
            b = t % 2
            sr, sT, xs, tm = sr2[b], sT2[b], xs2[b], tm2[b]
            p.dma("sp", lambda e, sr=sr, t=t: e.dma_start(out=sr[:], in_=SRC[t * 128:(t + 1) * 128, :]), writes=["osr%d" % b])
            p.dma("act", lambda e, xs=xs, t=t: e.dma_start(out=xs[:], in_=XIN[t * 128:(t + 1) * 128, :]), writes=["oxs%d" % b])
            for hh in range(2):
                bk = cx.bank[4 + hh]
                bkn = "bank%d" % (4 + hh)
                bkv = bk[:].bitcast(BF16)
                for j in range(8):
                    kc = hh * 8 + j
                    p.op("pe", lambda e, bkv=bkv, j=j, kc=kc, sr=sr: e.transpose(out=bkv[:, j * 128:(j + 1) * 128], in_=sr[:, kc * 128:(kc + 1) * 128], identity=cb[:, C_ID:C_ID + 128]),
                         reads=["osr%d" % b], writes=[bkn], inc=(j == 7))
                if hh == 0:
                    p.op("act", lambda e, bkv=bkv, hh=hh, sT=sT: e.activation(out=sT[:, hh * 8:(hh + 1) * 8, :], in_=bkv.rearrange("p (a b) -> p a b", a=8), func=AF.Copy),
                         reads=[bkn], writes=["osT%d" % b])
                else:
                    p.op("dve", lambda e, bkv=bkv, hh=hh, sT=sT: e.tensor_copy(out=sT[:, hh * 8:(hh + 1) * 8, :], in_=bkv.rearrange("p (a b) -> p a b", a=8)),
                         reads=[bkn], writes=["osT%d" % b])
            for cg in range(4):
                for kc in range(16):
                    p.op("pe", lambda e, cg=cg, kc=kc, sT=sT: e.matmul(cx.bank[cg][:], lhsT=sT[:, kc, :], rhs=W[:, kc, cg * 512:(cg + 1) * 512], start=(kc == 0), stop=(kc == 15)),
                         reads=["osT%d" % b, "Wout%d" % cg], writes=["bank%d" % cg], inc=(kc == 15))
                p.op("dve", lambda e, cg=cg, tm=tm: e.tensor_tensor(out=tm[:, cg * 512:(cg + 1) * 512], in0=cx.bank[cg][:], in1=GT[:, cg * 512:(cg + 1) * 512], op=ALU.mult),
                     reads=["bank%d" % cg, "GT"], writes=["otm%d_%d" % (b, cg)])
                p.op("pool", lambda e, cg=cg, tm=tm, xs=xs: e.tensor_tensor(out=tm[:, cg * 512:(cg + 1) * 512], in0=tm[:, cg * 512:(cg + 1) * 512], in1=xs[:, cg * 512:(cg + 1) * 512], op=ALU.add),
                     reads=["otm%d_%d" % (b, cg), "oxs%d" % b], writes=["otm%d_%d" % (b, cg)])
            p.dma("sp", lambda e, tm=tm, t=t: e.dma_start(out=XOUT[t * 128:(t + 1) * 128, :], in_=tm[:]), reads=["otm%d_%d" % (b, cg) for cg in range(4)], writes=["XO%d" % t])
        p.emit()


TWO_PI = 2.0 * math.pi
C1 = 6.28125
C2 = TWO_PI - C1


def rope_tables(p, cx, POSap, COS, SINS, tag):
    with ExitStack() as st:
        cf = cx.cf
        a = p.sb(st, "ra", [32, TOK], F32)
        k = p.sb(st, "rk", [32, TOK], F32)
        ki = p.sb(st, "rki", [32, TOK], I32)
        m = p.sb(st, "rm", [32, TOK], F32)
        p.dma("pool", lambda e: e.dma_start(out=a[:], in_=bc(POSap, 32)), writes=["ra"])
        p.op("dve", lambda e: e.tensor_scalar(out=a[:], in0=a[:], scalar1=cf[0:32, C_INVF:C_INVF + 1], scalar2=None, op0=ALU.mult), reads=["ra"], writes=["ra"])

        def reduce_(src, dst, shift):
            p.op("dve", lambda e: e.tensor_scalar(out=k[:], in0=src[:], scalar1=shift, scalar2=1.0 / TWO_PI, op0=ALU.add, op1=ALU.mult), reads=["ra", "rd"], writes=["rk"])
            p.op("dve", lambda e: e.tensor_copy(out=ki[:], in_=k[:]), reads=["rk"], writes=["rki"])
            p.op("dve", lambda e: e.tensor_copy(out=k[:], in_=ki[:]), reads=["rki"], writes=["rk"])
            p.op("dve", lambda e: e.scalar_tensor_tensor(out=dst[:], in0=k[:], scalar=-C1, in1=src[:], op0=ALU.mult, op1=ALU.add), reads=["rk", "ra"], writes=["rd"])
            p.op("dve", lambda e: e.scalar_tensor_tensor(out=dst[:], in0=k[:], scalar=-C2, in1=dst[:], op0=ALU.mult, op1=ALU.add), reads=["rk", "rd"], writes=["rd"])
            if shift != 0.0:
                p.op("dve", lambda e: e.tensor_scalar(out=dst[:], in0=dst[:], scalar1=shift, scalar2=None, op0=ALU.add), reads=["rd"], writes=["rd"])
            p.op("dve", lambda e: e.tensor_scalar(out=m[:], in0=dst[:], scalar1=math.pi, scalar2=-TWO_PI, op0=ALU.is_gt, op1=ALU.mult), reads=["rd"], writes=["rm"])
            p.op("dve", lambda e: e.tensor_tensor(out=dst[:], in0=dst[:], in1=m[:], op=ALU.add), reads=["rd", "rm"], writes=["rd"])
            p.op("dve", lambda e: e.tensor_scalar(out=m[:], in0=dst[:], scalar1=-math.pi, scalar2=TWO_PI, op0=ALU.is_lt, op1=ALU.mult), reads=["rd"], writes=["rm"])
            p.op("dve", lambda e: e.tensor_tensor(out=dst[:], in0=dst[:], in1=m[:], op=ALU.add), reads=["rd", "rm"], writes=["rd"])
            p.op("dve", lambda e: e.tensor_scalar(out=dst[:], in0=dst[:], scalar1=math.pi, scalar2=-math.pi, op0=ALU.min, op1=ALU.max), reads=["rd"], writes=["rd"])

        reduce_(a, SINS, 0.0)
        p.op("act", lambda e: e.activation(out=SINS[:], in_=SINS[:], func=AF.Sin), reads=["rd"], writes=["rd"])
        p.op("dve", lambda e: e.tensor_scalar(out=SINS[:], in0=SINS[:], scalar1=cf[0:32, C_SGN:C_SGN + 1], scalar2=None, op0=ALU.mult), reads=["rd"], writes=["rd"])
        reduce_(a, COS, math.pi / 2)
        p.op("act", lambda e: e.activation(out=COS[:], in_=COS[:], func=AF.Sin), reads=["rd"], writes=["rd"])
        p.emit()


def attn_proj(p, cx, hT, own, COS, SINS, nmax):
    with ExitStack() as st:
        cb = cx.cb
        wp = [p.sb(st, "wp%d" % i, [128, 16, 512], BF16) for i in range(2)]
        qk = [p.sb(st, "qk%d" % i, [128, 512], BF16) for i in range(4)]
        sq = [p.sb(st, "sq%d" % i, [128, 512], BF16) for i in range(2)]
        t1 = [p.sb(st, "t1_%d" % i, [32, 512], F32) for i in range(2)]
        t2 = [p.sb(st, "t2_%d" % i, [32, 512], F32) for i in range(2)]
        nm1 = p.sb(st, "nm1", [1, 2], F32)
        vs = [p.sb(st, "vs%d" % i, [128, 2, 257], BF16) for i in range(4)]
        for i in range(4):
            p.op("pool", lambda e, i=i: e.memset(vs[i][:], 1.0), writes=["vs%d" % i])
        wv = cx.ATTN_W_IN.ap().rearrange("(kc k) c -> k kc c", k=128)
        toff = TOK if own else 0
        pieces = range(12) if own else range(4, 12)
        cnt = 0
        vcnt = 0
        for pi, j in enumerate(pieces):
            w = wp[pi % 2]
            wn = "wp%d" % (pi % 2)
            p.dma("pool", lambda e, w=w, j=j: e.dma_start(out=w[:], in_=wv[:, :, j * 512:(j + 1) * 512]), writes=[wn])
            if j < 8:
                isq = j < 4
                for cc in range(4):
                    gcc = (j % 4) * 4 + cc
                    for tg in range(4):
                        bi = cnt % 4
                        bk = cx.bank[bi]
                        q_ = qk[cnt % 4]
                        qn = "qk%d" % (cnt % 4)
                        s_ = sq[cnt % 2]
                        sn = "sq%d" % (cnt % 2)
                        a1, a2 = t1[cnt % 2], t2[cnt % 2]
                        an = "t12_%d" % (cnt % 2)
                        nb = cx.bank[4 + cnt % 2]
                        nbn = "bank%d" % (4 + cnt % 2)
                        sb_ = cx.bank[6 + cnt % 2]
                        sbn = "bank%d" % (6 + cnt % 2)
                        cnt += 1
                        for kc in range(16):
                            p.op("pe", lambda e, bk=bk, w=w, kc=kc, cc=cc, tg=tg: e.matmul(bk[:], lhsT=w[:, kc, cc * 128:(cc + 1) * 128], rhs=hT[:, kc, tg * 512:(tg + 1) * 512], start=(kc == 0), stop=(kc == 15)),
                                 reads=[wn], writes=["bank%d" % bi], inc=(kc == 15))
                        p.op("act", lambda e, bk=bk, q_=q_: e.activation(out=q_[:], in_=bk[:], func=AF.Copy), reads=["bank%d" % bi], writes=[qn])
                        p.op("act", lambda e, bk=bk, s_=s_: e.activation(out=s_[:], in_=bk[:], func=AF.Square), reads=["bank%d" % bi], writes=[sn])
                        p.op("pe", lambda e, nb=nb, s_=s_: e.matmul(nb[0:1, :], lhsT=cb[:, C_ON:C_ON + 1], rhs=s_[:], start=True, stop=True), reads=[sn], writes=[nbn])
                        p.op("dve", lambda e, nb=nb: e.reduce_max(out=nm1[:, 0:1], in_=nb[0:1, :], axis=AX.X), reads=[nbn], writes=["nm1"])
                        ix = gcc if isq else 16 + gcc
                        p.op("dve", lambda e, ix=ix: e.tensor_tensor(out=nmax[:, ix:ix + 1], in0=nmax[:, ix:ix + 1], in1=nm1[:, 0:1], op=ALU.max), reads=["nm1", "nmax"], writes=["nmax"])
                        p.op("pe", lambda e, sb_=sb_, q_=q_: e.matmul(sb_[0:32, :], lhsT=cb[0:32, C_PERM:C_PERM + 32], rhs=q_[0:32, :], start=True, stop=True), reads=[qn], writes=[sbn])
                        p.op("dve", lambda e, a1=a1, q_=q_, tg=tg: e.tensor_tensor(out=a1[:], in0=q_[0:32, :], in1=COS[:, tg * 512:(tg + 1) * 512], op=ALU.mult), reads=[qn], writes=[an + "a"])
                        p.op("dve", lambda e, a2=a2, sb_=sb_, tg=tg: e.tensor_tensor(out=a2[:], in0=sb_[0:32, :], in1=SINS[:, tg * 512:(tg + 1) * 512], op=ALU.mult), reads=[sbn], writes=[an + "b"])
                        p.op("dve", lambda e, a1=a1, a2=a2, q_=q_: e.tensor_tensor(out=q_[0:32, :], in0=a1[:], in1=a2[:], op=ALU.add), reads=[an + "a", an + "b", qn], writes=[qn])
                        if isq:
                            dst = cx.QTD.ap()[gcc, :, tg * 512:(tg + 1) * 512]
                        else:
                            dst = cx.KTD.ap()[gcc, :, toff + tg * 512:toff + (tg + 1) * 512]
                        p.dma("sp", lambda e, dst=dst, q_=q_: e.dma_start(out=dst, in_=q_[:]), reads=[qn], writes=["qkd%d" % cnt])
            else:
                vj = j - 8
                for t in range(NT):
                    bi = cnt % 4
                    bk = cx.bank[bi]
                    cnt += 1
                    v_ = vs[vcnt % 4]
                    vn = "vs%d" % (vcnt % 4)
                    vcnt += 1
                    for kc in range(16):
                        p.op("pe", lambda e, bk=bk, w=w, kc=kc, t=t: e.matmul(bk[:], lhsT=hT[:, kc, t * 128:(t + 1) * 128], rhs=w[:, kc, :], start=(kc == 0), stop=(kc == 15)),
                             reads=[wn], writes=["bank%d" % bi], inc=(kc == 15))
                    if t % 2 == 0:
                        p.op("act", lambda e, bk=bk, v_=v_: e.activation(out=v_[:, :, 0:256], in_=bk[:].rearrange("p (a b) -> p a b", a=2), func=AF.Copy), reads=["bank%d" % bi], writes=[vn])
                    else:
                        p.op("dve", lambda e, bk=bk, v_=v_: e.tensor_copy(out=v_[:, :, 0:256], in_=bk[:].rearrange("p (a b) -> p a b", a=2)), reads=["bank%d" % bi], writes=[vn])
                    r0 = toff + t * 128
                    p.dma("act", lambda e, v_=v_, r0=r0, vj=vj: e.dma_start(out=cx.VD.ap()[r0:r0 + 128, 2 * vj:2 * vj + 2, :], in_=v_[:]), reads=[vn], writes=["vd%d" % cnt])
        p.emit()


def attn_consts(p, cx, st0, nmax):
    negc = p.sb(st0, "negc", [128, 16], F32)
    negcp = p.sb(st0, "negcp", [128, 16], F32)
    nlam = p.sb(st0, "nlam", [128, 1], F32)
    HG = p.sb(st0, "HG", [128, 256], F32)
    with ExitStack() as st:
        cf = cx.cf
        r = p.sb(st, "acr", [1, 64], F32)
        lm = p.sb(st, "lm", [1, 4, 128], F32)
        pf = p.sb(st, "pf", [128, 1], F32)
        for i, nm_ in enumerate([cx.LQ1, cx.LK1, cx.LQ2, cx.LK2]):
            p.dma("sp", lambda e, i=i, nm_=nm_: e.dma_start(out=lm[:, i, :], in_=nm_.ap()), writes=["lm"])
        p.dma("sp", lambda e: e.dma_start(out=pf[:], in_=cx.PREVFLAG.ap()), writes=["pf"])
        p.dma("sp", lambda e: e.dma_start(out=HG[:], in_=bc(cx.ATTN_HG.ap()[0, :])), writes=["HG"])
        p.op("dve", lambda e: e.tensor_scalar(out=HG[:], in0=HG[:], scalar1=0.8, scalar2=None, op0=ALU.mult), reads=["HG"], writes=["HG"])
        p.op("dve", lambda e: e.tensor_tensor(out=r[:, 0:16], in0=nmax[:, 0:16], in1=nmax[:, 16:32], op=ALU.mult), reads=["nmax"], writes=["acr"])
        p.op("act", lambda e: e.activation(out=r[:, 0:16], in_=r[:, 0:16], func=AF.Sqrt), reads=["acr"], writes=["acr"])
        p.op("dve", lambda e: e.tensor_scalar(out=r[:, 0:16], in0=r[:, 0:16], scalar1=-(128.0 ** -0.5), scalar2=None, op0=ALU.mult), reads=["acr"], writes=["acr"])
        p.op("dve", lambda e: e.tensor_tensor(out=lm[:, 0, :], in0=lm[:, 0, :], in1=lm[:, 1, :], op=ALU.mult), reads=["lm"], writes=["lm"])
        p.op("dve", lambda e: e.tensor_tensor(out=lm[:, 2, :], in0=lm[:, 2, :], in1=lm[:, 3, :], op=ALU.mult), reads=["lm"], writes=["lm"])
        p.op("dve", lambda e: e.reduce_sum(out=r[:, 32:33], in_=lm[:, 0, :], axis=AX.X), reads=["lm"], writes=["acr"])
        p.op("dve", lambda e: e.reduce_sum(out=r[:, 33:34], in_=lm[:, 2, :], axis=AX.X), reads=["lm"], writes=["acr"])
        p.op("act", lambda e: e.activation(out=r[:, 32:34], in_=r[:, 32:34], func=AF.Exp), reads=["acr"], writes=["acr"])
        p.op("dve", lambda e: e.tensor_tensor(out=r[:, 16:17], in0=r[:, 33:34], in1=r[:, 32:33], op=ALU.subtract), reads=["acr"], writes=["acr"])
        p.op("dve", lambda e: e.tensor_scalar(out=r[:, 16:17], in0=r[:, 16:17], scalar1=-0.2, scalar2=None, op0=ALU.add), reads=["acr"], writes=["acr"])
        p.op("pe", lambda e: e.matmul(cx.bank[0][:, 0:18], lhsT=cf[0:1, C_ON:C_ON + 128], rhs=r[0:1, 0:18], start=True, stop=True), reads=["acr"], writes=["bank0"])
        p.op("dve", lambda e: e.tensor_copy(out=negc[:], in_=cx.bank[0][:, 0:16]), reads=["bank0"], writes=["negc"])
        p.op("dve", lambda e: e.tensor_copy(out=nlam[:], in_=cx.bank[0][:, 16:17]), reads=["bank0"], writes=["nlam"])
        p.op("dve", lambda e: e.tensor_scalar(out=negcp[:], in0=negc[:], scalar1=pf[:, 0:1], scalar2=None, op0=ALU.add), reads=["negc", "pf"], writes=["negcp"])
        p.emit()
    return negc, negcp, nlam, HG


def attn_core(p, cx, negc, negcp, nlam, HG):
    SCALE = 128.0 ** -0.5
    with ExitStack() as st:
        QT = [[p.sb(st, "QT%d_%d" % (b, m), [128, TOK], BF16) for m in range(2)] for b in range(2)]
        KT = [[p.sb(st, "KT%d_%d" % (b, m), [128, 2 * TOK], BF16) for m in range(2)] for b in range(2)]
        V = [p.sb(st, "V%d" % b, [128, 32, 257], BF16) for b in range(2)]
        PT = [p.sb(st, "PT%d" % m, [128, 36, 512], BF16) for m in range(2)]
        o0 = p.sb(st, "o0", [128, 4, 256], F32)
        oa = [p.sb(st, "oa%d" % i, [128, 4, 256], BF16) for i in range(2)]
        rd = [p.sb(st, "rd%d" % i, [128, 8], F32) for i in range(4)]
        junk = p.sb(st, "cjunk", [128, 256], BF16)
        vdv = cx.VD.ap().rearrange("(t k) h e -> h k t e", k=128)

        def loads(h):
            b = h % 2
            for m in range(2):
                p.dma("sp", lambda e, b=b, m=m, h=h: e.dma_start(out=QT[b][m][:], in_=cx.QTD.ap()[2 * h + m]), writes=["QT%d_%d" % (b, m)])
                p.dma("sp", lambda e, b=b, m=m, h=h: e.dma_start(out=KT[b][m][:], in_=cx.KTD.ap()[2 * h + m]), writes=["KT%d_%d" % (b, m)])
            p.dma("act", lambda e, b=b, h=h: e.dma_start(out=V[b][:], in_=vdv[h]), writes=["V%d" % b])

        stb = [0]
        accb = [0]
        oac = [0]
        rdc = [0]

        def stageA(h, g, m):
            b = h % 2
            hm = 2 * h + m
            steps = []
            nk = 16 + 4 * g + 4
            for j in range(nk):
                def step(j=j):
                    qoff = max(0, j - 16 - 4 * g)
                    bi = stb[0] % 3
                    stb[0] += 1
                    bk = cx.bank[bi]
                    p.op("pe", lambda e: e.matmul(bk[:, qoff * 128:512], lhsT=KT[b][m][:, j * 128:(j + 1) * 128], rhs=QT[b][m][:, g * 512 + qoff * 128:(g + 1) * 512], start=True, stop=True),
                         reads=["KT%d_%d" % (b, m), "QT%d_%d" % (b, m)], writes=["bank%d" % bi])
                    bias = negcp[:, hm:hm + 1] if j < 16 else negc[:, hm:hm + 1]
                    p.op("act", lambda e: e.activation(out=PT[m][:, j, qoff * 128:512], in_=bk[:, qoff * 128:512], func=AF.Exp, bias=bias, scale=SCALE),
                         reads=["bank%d" % bi], writes=["PT%d_%d" % (m, j)])
                    if j >= 16 + 4 * g:
                        ii = j - 16 - 4 * g
                        p.op("pool", lambda e: e.memset(PT[m][64:128, j, ii * 128:ii * 128 + 64], 0.0), reads=[], writes=["PT%d_%d" % (m, j)])
                steps.append(step)
            return steps

        def stageB(h, g, m):
            b = h % 2
            steps = []
            for ii in range(4):
                ai = 3 + accb[0] % 5
                accb[0] += 1
                acc = cx.bank[ai]
                an = "bank%d" % ai
                nkk = 16 + 4 * g + ii + 1
                for j in range(nkk):
                    def step(j=j, ii=ii, acc=acc, an=an, nkk=nkk):
                        p.op("pe", lambda e: e.matmul(acc[:, 0:257], lhsT=PT[m][:, j, ii * 128:(ii + 1) * 128], rhs=V[b][:, j, :], start=(j == 0), stop=(j == nkk - 1)),
                             reads=["PT%d_%d" % (m, j), "V%d" % b], writes=[an], inc=(j == nkk - 1))
                    steps.append(step)

                def fin(ii=ii, acc=acc, an=an):
                    r = rd[rdc[0] % 4]
                    rn = "rd%d" % (rdc[0] % 4)
                    rdc[0] += 1
                    p.op("dve", lambda e: e.reciprocal(out=r[:, 0:1], in_=acc[:, 256:257]), reads=[an], writes=[rn])
                    if m == 0:
                        p.op("act", lambda e: e.activation(out=o0[:, ii, :], in_=acc[:, 0:256], func=AF.Copy, scale=r[:, 0:1]), reads=[an, rn], writes=["o0_%d" % ii])
                    else:
                        o = oa[oac[0] % 2]
                        on = "oa%d" % (oac[0] % 2)
                        p.op("dve", lambda e: e.tensor_tensor(out=r[:, 1:2], in0=r[:, 0:1], in1=nlam[:, 0:1], op=ALU.mult), reads=[rn], writes=[rn])
                        p.op("dve", lambda e: e.scalar_tensor_tensor(out=o0[:, ii, :], in0=acc[:, 0:256], scalar=r[:, 1:2], in1=o0[:, ii, :], op0=ALU.mult, op1=ALU.add),
                             reads=[an, rn, "o0_%d" % ii], writes=["o0_%d" % ii])
                        p.op("act", lambda e: e.activation(out=junk[:], in_=o0[:, ii, :], func=AF.Square, accum_out=r[:, 2:3]), reads=["o0_%d" % ii], writes=["cjunk", rn])
                        p.op("dve", lambda e: e.tensor_scalar(out=r[:, 3:4], in0=r[:, 2:3], scalar1=1.0 / 256, scalar2=EPS, op0=ALU.mult, op1=ALU.add), reads=[rn], writes=[rn])
                        p.op("act", lambda e: e.activation(out=r[:, 4:5], in_=r[:, 3:4], func=AF.Sqrt), reads=[rn], writes=[rn])
                        p.op("dve", lambda e: e.reciprocal(out=r[:, 5:6], in_=r[:, 4:5]), reads=[rn], writes=[rn])
                        p.op("dve", lambda e: e.scalar_tensor_tensor(out=o[:, ii, :], in0=o0[:, ii, :], scalar=r[:, 5:6], in1=HG[:], op0=ALU.mult, op1=ALU.mult),
                             reads=["o0_%d" % ii, rn], writes=[on + "_%d" % ii])
                        if ii == 3:
                            oac[0] += 1
                            dst = cx.OAD.ap()[g * 512:(g + 1) * 512, h * 256:(h + 1) * 256].rearrange("(i q) c -> q i c", q=128)
                            p.dma("sp", lambda e: e.dma_start(out=dst, in_=o[:]), reads=[on + "_%d" % i_ for i_ in range(4)], writes=["oad_%d_%d" % (h, g)])
                steps.append(fin)
            return steps

        def interleave(A, B):
            na, nb = len(A), len(B)
            ia = ib = 0
            while ia < na or ib < nb:
                if ib >= nb or (ia < na and ia * max(nb, 1) <= ib * max(na, 1)):
                    A[ia]()
                    ia += 1
                else:
                    B[ib]()
                    ib += 1

        loads(0)
        pend = []
        for h in range(8):
            for g in range(4):
                for m in range(2):
                    A = stageA(h, g, m)
                    interleave(A, pend)
                    pend = stageB(h, g, m)
                    if g == 0 and m == 0 and h + 1 < 8:
                        loads(h + 1)
        interleave([], pend)
        p.emit()


def attn_layer(p, cx, XIN, XPREV, XOUT):
    with ExitStack() as st0:
        nmax = p.sb(st0, "nmax", [1, 32], F32)
        p.op("dve", lambda e: e.memset(nmax[:], 0.0), writes=["nmax"])
        with ExitStack() as st1:
            G, SH, GT = load_mod_tiles(p, cx, st1, 0, 0, cx.NORM_MIX_G)
            COS = p.sb(st1, "COS", [32, TOK], F32)
            SINS = p.sb(st1, "SINS", [32, TOK], F32)
            hT = p.sb(st1, "hT", [128, 16, TOK], BF16)
            rope_tables(p, cx, cx.POS_PREV.ap()[0, :], COS, SINS, "p")
            norm_to_hT(p, cx, XPREV.ap(), G, SH, hT)
            attn_proj(p, cx, hT, False, COS, SINS, nmax)
            rope_tables(p, cx, cx.POS_OWN.ap()[0, :], COS, SINS, "o")
            norm_to_hT(p, cx, XIN.ap(), G, SH, hT)
            attn_proj(p, cx, hT, True, COS, SINS, nmax)
        negc, negcp, nlam, HG = attn_consts(p, cx, st0, nmax)
        attn_core(p, cx, negc, negcp, nlam, HG)
    with ExitStack() as st2:
        GT = p.sb(st2, "GT", [128, D], F32)
        p.dma("sp", lambda e: e.dma_start(out=GT[:], in_=bc(cx.MODD.ap()[0, 2 * D:3 * D])), writes=["GT"])
        proj_out_stage(p, cx, cx.OAD.ap(), cx.ATTN_W_OUT.ap(), GT, XIN.ap(), XOUT.ap())


def mlstm_proj(p, cx, hT, full=True):
    with ExitStack() as st:
        wp = [p.sb(st, "wp%d" % i, [128, 16, 512], BF16) for i in range(2)]
        wgt = p.sb(st, "wgt", [128, 16, 16], BF16)
        qk = [p.sb(st, "qk%d" % i, [128, 512], BF16) for i in range(4)]
        vs = [p.sb(st, "vs%d" % i, [128, 2, 257], BF16) for i in range(4)]
        ob = [p.sb(st, "ob%d" % i, [128, 512], BF16) for i in range(4)]
        gsb = [p.sb(st, "gsb%d" % i, [128, 16], F32) for i in range(2)]
        for i in range(4):
            p.op("pool", lambda e, i=i: e.memset(vs[i][:], 1.0), writes=["vs%d" % i])
        wv = cx.ML_W_IN.ap().rearrange("(kc k) c -> k kc c", k=128)
        p.dma("pool", lambda e: e.dma_start(out=wgt[:], in_=wv[:, :, 3 * D:3 * D + 16]), writes=["wgt"])
        cnt = 0
        for j in range(12 if full else 8):
            w = wp[j % 2]
            wn = "wp%d" % (j % 2)
            p.dma("pool", lambda e, w=w, j=j: e.dma_start(out=w[:], in_=wv[:, :, j * 512:(j + 1) * 512]), writes=[wn])
            if j < 4:
                for cc in range(4):
                    gcc = j * 4 + cc
                    for tg in range(4):
                        bi = cnt % 4
                        bk = cx.bank[bi]
                        q_ = qk[cnt % 4]
                        qn = "qk%d" % (cnt % 4)
                        cnt += 1
                        for kc in range(16):
                            p.op("pe", lambda e, bk=bk, w=w, kc=kc, cc=cc, tg=tg: e.matmul(bk[:], lhsT=w[:, kc, cc * 128:(cc + 1) * 128], rhs=hT[:, kc, tg * 512:(tg + 1) * 512], start=(kc == 0), stop=(kc == 15)),
                                 reads=[wn], writes=["bank%d" % bi], inc=(kc == 15))
                        if cnt % 2 == 0:
                            p.op("act", lambda e, bk=bk, q_=q_: e.activation(out=q_[:], in_=bk[:], func=AF.Copy), reads=["bank%d" % bi], writes=[qn])
                        else:
                            p.op("dve", lambda e, bk=bk, q_=q_: e.tensor_copy(out=q_[:], in_=bk[:]), reads=["bank%d" % bi], writes=[qn])
                        dst = cx.QKP.ap()[gcc, :, 3 + tg * 512:3 + (tg + 1) * 512]
                        p.dma("sp", lambda e, dst=dst, q_=q_: e.dma_start(out=dst, in_=q_[:]), reads=[qn], writes=["qkd%d" % cnt])
            else:
                for t in range(NT):
                    bi = cnt % 4
                    bk = cx.bank[bi]
                    cnt += 1
                    for kc in range(16):
                        p.op("pe", lambda e, bk=bk, w=w, kc=kc, t=t: e.matmul(bk[:], lhsT=hT[:, kc, t * 128:(t + 1) * 128], rhs=w[:, kc, :], start=(kc == 0), stop=(kc == 15)),
                             reads=[wn], writes=["bank%d" % bi], inc=(kc == 15))
                    if j < 8:
                        vj = j - 4
                        v_ = vs[cnt % 4]
                        vn = "vs%d" % (cnt % 4)
                        if t % 2 == 0:
                            p.op("act", lambda e, bk=bk, v_=v_: e.activation(out=v_[:, :, 0:256], in_=bk[:].rearrange("p (a b) -> p a b", a=2), func=AF.Copy), reads=["bank%d" % bi], writes=[vn])
                        else:
                            p.op("dve", lambda e, bk=bk, v_=v_: e.tensor_copy(out=v_[:, :, 0:256], in_=bk[:].rearrange("p (a b) -> p a b", a=2)), reads=["bank%d" % bi], writes=[vn])
                        p.dma("act", lambda e, v_=v_, t=t, vj=vj: e.dma_start(out=cx.VM.ap()[t * 128:(t + 1) * 128, 2 * vj:2 * vj + 2, :], in_=v_[:]), reads=[vn], writes=["vd%d" % cnt])
                    else:
                        oj = j - 8
                        o_ = ob[cnt % 4]
                        on = "ob%d" % (cnt % 4)
                        if t % 2 == 0:
                            p.op("act", lambda e, bk=bk, o_=o_: e.activation(out=o_[:], in_=bk[:], func=AF.Copy), reads=["bank%d" % bi], writes=[on])
                        else:
                            p.op("dve", lambda e, bk=bk, o_=o_: e.tensor_copy(out=o_[:], in_=bk[:]), reads=["bank%d" % bi], writes=[on])
                        p.dma("act", lambda e, o_=o_, t=t, oj=oj: e.dma_start(out=cx.OPRE.ap()[t * 128:(t + 1) * 128, oj * 512:(oj + 1) * 512], in_=o_[:]), reads=[on], writes=["od%d" % cnt])
        for t in range(NT):
            bk = cx.bank[4 + t % 2]
            g_ = gsb[t % 2]
            for kc in range(16):
                p.op("pe", lambda e, bk=bk, kc=kc, t=t: e.matmul(bk[:, 0:16], lhsT=hT[:, kc, t * 128:(t + 1) * 128], rhs=wgt[:, kc, :], start=(kc == 0), stop=(kc == 15)),
                     reads=["wgt"], writes=["bank%d" % (4 + t % 2)], inc=(kc == 15))
            p.op("dve", lambda e, bk=bk, g_=g_: e.tensor_copy(out=g_[:], in_=bk[:, 0:16]), reads=["bank%d" % (4 + t % 2)], writes=["gsb%d" % (t % 2)])
            p.dma("sp", lambda e, g_=g_, t=t: e.dma_start(out=cx.GATES.ap()[t * 128:(t + 1) * 128, :], in_=g_[:]), reads=["gsb%d" % (t % 2)], writes=["gd%d" % t])
        p.emit()


def mlstm_rec(p, cx, full=True, st_in="dram", st_out="dram"):
    RS = 128.0 ** -0.5
    with ExitStack() as st:
        cf, cb = cx.cf, cx.cb
        C = p.sb(st, "Cst", [128, 8, 257], F32)
        Cb = p.sb(st, "Cstb", [128, 8, 257], BF16)
        CW = p.sb(st, "CW", [128, 16, 4], F32)
        CB = p.sb(st, "CB", [128, 16], F32)
        GB = p.sb(st, "GB", [128, 16], F32)
        HGm = p.sb(st, "HGm", [128, D], F32)
        hl = p.sb(st, "hl", [128, 16, 3], F32)
        hlb = p.sb(st, "hlb", [128, 16, 3], BF16)
        qkp = [p.sb(st, "qkp%d" % i, [128, 16, 131], BF16) for i in range(2)]
        vt = [p.sb(st, "vt%d" % i, [128, 8, 257], BF16) for i in range(2)]
        gt_ = [p.sb(st, "gtl%d" % i, [128, 16], F32) for i in range(2)]
        op_ = [p.sb(st, "opl%d" % i, [128, D], BF16) for i in range(2)]
        sig = p.sb(st, "sig", [128, D], F32)
        cacc = [p.sb(st, "cacc%d" % i, [128, 128], F32) for i in range(4)]
        qs = p.sb(st, "qs", [128, 16, 128], BF16)
        gs = p.sb(st, "gs", [128, 64], F32)
        PTm = [p.sb(st, "PTm%d" % i, [128, 128], BF16) for i in range(2)]
        Kp = [p.sb(st, "Kp%d" % i, [128, 128], BF16) for i in range(2)]
        tmpC = [p.sb(st, "tmpC%d" % i, [128, 257], F32) for i in range(2)]
        ho = [p.sb(st, "ho%d" % i, [128, 256], F32) for i in range(2)]
        hn = [p.sb(st, "hn%d" % i, [128, D], BF16) for i in range(2)]
        rr = [p.sb(st, "rr%d" % i, [128, 8], F32) for i in range(4)]
        junk = p.sb(st, "mjunk", [128, 256], BF16)
        if st_in == "dram":
            p.dma("sp", lambda e: e.dma_start(out=C[:], in_=cx.STATE_IN.ap()), writes=["Cst"])
        elif st_in == "zero":
            p.op("dve", lambda e: e.memset(C[:], 0.0), writes=["Cst"])
        else:
            of = p.sb(st, "oddf", [128, 1], F32)
            p.dma("sp", lambda e: e.dma_start(out=of[:], in_=cx.ODDFLAG.ap()), writes=["oddf"])
            p.dma("sp", lambda e: e.dma_start(out=C[:].rearrange("p h e -> p (h e)"), in_=cx.ST_ALL.ap()[0:128, 0:8 * 257]), writes=["Cst"])
            p.op("dve", lambda e: e.tensor_scalar(out=C[:], in0=C[:], scalar1=of[:, 0:1], scalar2=None, op0=ALU.mult), reads=["Cst", "oddf"], writes=["Cst"])
        p.op("act", lambda e: e.activation(out=Cb[:], in_=C[:], func=AF.Copy), reads=["Cst"], writes=["Cstb"])
        p.dma("sp", lambda e: e.dma_start(out=CW[:], in_=cx.CONV_W.ap()), writes=["CW"])
        p.dma("sp", lambda e: e.dma_start(out=CB[:], in_=cx.CONV_B.ap()), writes=["CB"])
        p.dma("sp", lambda e: e.dma_start(out=GB[:], in_=bc(cx.GATE_B.ap()[0, :])), writes=["GB"])
        p.dma("sp", lambda e: e.dma_start(out=HGm[:], in_=bc(cx.ML_HG.ap()[0, :])), writes=["HGm"])
        if st_in == "dram":
            p.dma("sp", lambda e: e.dma_start(out=hl[:], in_=cx.HALO_IN.ap()), writes=["hl"])
        elif st_in == "zero":
            p.op("dve", lambda e: e.memset(hl[:], 0.0), writes=["hl"])
        else:
            p.dma("sp", lambda e: e.dma_start(out=hl[:].rearrange("p c t -> p (c t)"), in_=cx.ST_ALL.ap()[0:128, 8 * 257:8 * 257 + 48]), writes=["hl"])
            p.op("dve", lambda e: e.tensor_scalar(out=hl[:], in0=hl[:], scalar1=of[:, 0:1], scalar2=None, op0=ALU.mult), reads=["hl", "oddf"], writes=["hl"])
        p.op("dve", lambda e: e.tensor_copy(out=hlb[:], in_=hl[:]), reads=["hl"], writes=["hlb"])
        qkv = cx.QKP.ap().rearrange("c k t -> k c t")
        p.dma("sp", lambda e: e.dma_start(out=qkv[:, :, 0:3], in_=hlb[:], allow_slow_non_contiguous=True), reads=["hlb"], writes=["halo"])
        for c in range(NT):
            b = c % 2
            q_, v_, g_, o_ = qkp[b], vt[b], gt_[b], op_[b]
            rdh = ["halo"] if c == 0 else []
            p.dma("sp", lambda e, q_=q_, c=c: e.dma_start(out=q_[:], in_=qkv[:, :, c * 128:c * 128 + 131]), reads=rdh, writes=["qkp%d" % b])
            p.dma("act", lambda e, v_=v_, c=c: e.dma_start(out=v_[:], in_=cx.VM.ap()[c * 128:(c + 1) * 128]), writes=["vt%d" % b])
            p.dma("sp", lambda e, g_=g_, c=c: e.dma_start(out=g_[:], in_=cx.GATES.ap()[c * 128:(c + 1) * 128, :]), writes=["gtl%d" % b])
            if full:
                p.dma("act", lambda e, o_=o_, c=c: e.dma_start(out=o_[:], in_=cx.OPRE.ap()[c * 128:(c + 1) * 128, :]), writes=["opl%d" % b])
                p.op("act", lambda e, o_=o_: e.activation(out=sig[:], in_=o_[:], func=AF.Sigmoid), reads=["opl%d" % b], writes=["sig"])
            p.op("dve", lambda e, g_=g_: e.tensor_tensor(out=gs[:, 0:16], in0=g_[:], in1=GB[:], op=ALU.add), reads=["gtl%d" % b, "GB"], writes=["gs"])
            p.op("act", lambda e: e.activation(out=gs[:, 16:24], in_=gs[:, 8:16], func=AF.Exp, scale=-1.0), reads=["gs"], writes=["gs"])
            p.op("dve", lambda e: e.tensor_scalar(out=gs[:, 16:24], in0=gs[:, 16:24], scalar1=1.0, scalar2=None, op0=ALU.add), reads=["gs"], writes=["gs"])
            p.op("act", lambda e: e.activation(out=gs[:, 16:24], in_=gs[:, 16:24], func=AF.Ln), reads=["gs"], writes=["gs"])
            gbk = cx.bank[0]
            p.op("pe", lambda e: e.matmul(gbk[:, 0:8], lhsT=cf[:, C_TRI:C_TRI + 128], rhs=gs[:, 16:24], start=True, stop=True), reads=["gs"], writes=["bank0"])
            p.op("pe", lambda e: e.matmul(gbk[:, 8:16], lhsT=cf[:, C_ON:C_ON + 128], rhs=gs[:, 16:24], start=False, stop=True, skip_group_check=True), reads=["gs"], writes=["bank0"])
            p.op("dve", lambda e: e.tensor_tensor(out=gs[:, 32:40], in0=gs[:, 0:8], in1=gbk[:, 0:8], op=ALU.add), reads=["gs", "bank0"], writes=["gs"])
            p.op("act", lambda e: e.activation(out=gs[:, 32:40], in_=gs[:, 32:40], func=AF.Exp), reads=["gs"], writes=["gs"])
            p.op("dve", lambda e: e.tensor_scalar(out=gs[:, 32:40], in0=gs[:, 32:40], scalar1=RS, scalar2=None, op0=ALU.mult), reads=["gs"], writes=["gs"])
            p.op("act", lambda e: e.activation(out=gs[:, 40:56], in_=gbk[:, 0:16], func=AF.Exp, scale=-1.0), reads=["bank0"], writes=["gs"])
            for g4 in range(0, 16, 4):
                ccs = [cc for cc in range(g4, g4 + 4) if full or cc >= 8]
                for cc in ccs:
                    a_ = cacc[cc % 4]
                    an = "cacc%d" % (cc % 4)
                    p.op("act", lambda e, a_=a_, q_=q_, cc=cc: e.activation(out=a_[:], in_=q_[:, cc, 0:128], func=AF.Identity, scale=CW[:, cc, 0:1], bias=CB[:, cc:cc + 1]),
                         reads=["qkp%d" % b, "CW", "CB"], writes=[an])
                for j in range(1, 4):
                    for cc in ccs:
                        a_ = cacc[cc % 4]
                        an = "cacc%d" % (cc % 4)
                        p.op("dve", lambda e, a_=a_, q_=q_, cc=cc, j=j: e.scalar_tensor_tensor(out=a_[:], in0=q_[:, cc, j:j + 128], scalar=CW[:, cc, j:j + 1], in1=a_[:], op0=ALU.mult, op1=ALU.add),
                             reads=["qkp%d" % b, an], writes=[an])
                for cc in ccs:
                    a_ = cacc[cc % 4]
                    an = "cacc%d" % (cc % 4)
                    p.op("act", lambda e, a_=a_, cc=cc: e.activation(out=qs[:, cc, :], in_=a_[:], func=AF.Silu), reads=[an], writes=["qs%d" % cc])
            def head_ops(h, c=c, b=b, q_=q_, v_=v_):
                s4 = (h % 2) * 4
                bS, bT, bA, bU = cx.bank[s4], cx.bank[s4 + 1], cx.bank[s4 + 2], cx.bank[s4 + 3]
                nS, nT, nA, nU = ["bank%d" % (s4 + i) for i in range(4)]
                kT = qs[:, 8 + h, :]
                qT = qs[:, h, :]
                pt = PTm[h % 2]
                kp = Kp[h % 2]
                r = rr[h % 4]
                rn = "rr%d" % (h % 4)
                if full:
                    p.op("pe", lambda e, bS=bS, kT=kT, qT=qT: e.matmul(bS[:, 0:128], lhsT=kT, rhs=qT, start=True, stop=True), reads=["qs%d" % (8 + h), "qs%d" % h], writes=[nS])
                    yield
                    p.op("dve", lambda e, bS=bS, pt=pt, h=h: e.scalar_tensor_tensor(out=pt[:], in0=bS[:, 0:128], scalar=gs[:, 32 + h:33 + h], in1=cf[:, C_CM:C_CM + 128], op0=ALU.mult, op1=ALU.mult),
                         reads=[nS, "gs"], writes=["PTm%d" % (h % 2)])
                    yield
                bTv = bT[:].bitcast(BF16)
                p.op("pe", lambda e, bTv=bTv, kT=kT: e.transpose(out=bTv[:, 0:128], in_=kT, identity=cb[:, C_ID:C_ID + 128]), reads=["qs%d" % (8 + h)], writes=[nT])
                yield
                p.op("act", lambda e, bTv=bTv, kp=kp, h=h: e.activation(out=kp[:], in_=bTv[:, 0:128], func=AF.Copy, scale=gs[:, 32 + h:33 + h]), reads=[nT, "gs"], writes=["Kp%d" % (h % 2)])
                yield
                if full:
                    p.op("pe", lambda e, bA=bA, pt=pt, v_=v_, h=h: e.matmul(bA[:, 0:257], lhsT=pt[:], rhs=v_[:, h, :], start=True, stop=False), reads=["PTm%d" % (h % 2), "vt%d" % b], writes=[nA])
                    yield
                    p.op("pe", lambda e, bA=bA, qT=qT, h=h: e.matmul(bA[:, 0:257], lhsT=qT, rhs=Cb[:, h, :], start=False, stop=True), reads=["qs%d" % h, "Cstb%d" % h], writes=[nA])
                    yield
                p.op("pe", lambda e, bU=bU, kp=kp, v_=v_, h=h: e.matmul(bU[:, 0:257], lhsT=kp[:], rhs=v_[:, h, :], start=True, stop=True), reads=["Kp%d" % (h % 2), "vt%d" % b], writes=[nU])
                yield
                tc_ = tmpC[h % 2]
                p.op("dve", lambda e, bU=bU, tc_=tc_, h=h: e.tensor_tensor(out=tc_[:], in0=bU[:, 0:257], in1=C[:, h, :], op=ALU.add), reads=[nU, "Cst%d" % h, nA], writes=["tmpC%d" % (h % 2)])
                yield
                p.op("dve", lambda e, tc_=tc_, h=h: e.tensor_scalar(out=C[:, h, :], in0=tc_[:], scalar1=gs[:, 48 + h:49 + h], scalar2=None, op0=ALU.mult), reads=["tmpC%d" % (h % 2), "gs"], writes=["Cst%d" % h])
                yield
                p.op("act", lambda e, h=h: e.activation(out=Cb[:, h, :], in_=C[:, h, :], func=AF.Copy), reads=["Cst%d" % h], writes=["Cstb%d" % h])
                yield
                if full:
                    o_h = ho[h % 2]
                    on = "ho%d" % (h % 2)
                    hn_ = hn[b]
                    p.op("dve", lambda e, bA=bA, r=r, h=h: e.tensor_tensor(out=r[:, 0:1], in0=bA[:, 256:257], in1=gs[:, 40 + h:41 + h], op=ALU.mult), reads=[nA, "gs"], writes=[rn])
                    yield
                    p.op("dve", lambda e, r=r: e.tensor_scalar(out=r[:, 2:3], in0=r[:, 0:1], scalar1=-1.0, scalar2=None, op0=ALU.mult), reads=[rn], writes=[rn])
                    yield
                    p.op("dve", lambda e, r=r: e.tensor_scalar(out=r[:, 1:2], in0=r[:, 0:1], scalar1=r[:, 2:3], scalar2=1.0, op0=ALU.max, op1=ALU.max), reads=[rn], writes=[rn])
                    yield
                    p.op("dve", lambda e, r=r: e.reciprocal(out=r[:, 2:3], in_=r[:, 1:2]), reads=[rn], writes=[rn])
                    yield
                    p.op("dve", lambda e, r=r, h=h: e.tensor_tensor(out=r[:, 3:4], in0=r[:, 2:3], in1=gs[:, 40 + h:41 + h], op=ALU.mult), reads=[rn, "gs"], writes=[rn])
                    yield
                    p.op("act", lambda e, bA=bA, o_h=o_h, r=r: e.activation(out=o_h[:], in_=bA[:, 0:256], func=AF.Copy, scale=r[:, 3:4]), reads=[nA, rn], writes=[on])
                    yield
                    p.op("act", lambda e, o_h=o_h, r=r: e.activation(out=junk[:], in_=o_h[:], func=AF.Square, accum_out=r[:, 4:5]), reads=[on], writes=["mjunk", rn])
                    yield
                    p.op("dve", lambda e, r=r: e.tensor_scalar(out=r[:, 5:6], in0=r[:, 4:5], scalar1=1.0 / 256, scalar2=EPS, op0=ALU.mult, op1=ALU.add), reads=[rn], writes=[rn])
                    yield
                    p.op("act", lambda e, r=r: e.activation(out=r[:, 6:7], in_=r[:, 5:6], func=AF.Sqrt), reads=[rn], writes=[rn])
                    yield
                    p.op("dve", lambda e, r=r: e.reciprocal(out=r[:, 7:8], in_=r[:, 6:7]), reads=[rn], writes=[rn])
                    yield
                    p.op("dve", lambda e, o_h=o_h, r=r, h=h: e.scalar_tensor_tensor(out=o_h[:], in0=o_h[:], scalar=r[:, 7:8], in1=HGm[:, h * 256:(h + 1) * 256], op0=ALU.mult, op1=ALU.mult),
                         reads=[on, rn, "HGm"], writes=[on])
                    yield
                    p.op("pool", lambda e, o_h=o_h, hn_=hn_, h=h: e.tensor_tensor(out=hn_[:, h * 256:(h + 1) * 256], in0=o_h[:], in1=sig[:, h * 256:(h + 1) * 256], op=ALU.mult),
                         reads=[on, "sig"], writes=["hn%d_%d" % (b, h)])
                    yield
            for hp2 in range(4):
                g0, g1 = head_ops(2 * hp2), head_ops(2 * hp2 + 1)
                alive = [g0, g1]
                while alive:
                    for g_ in list(alive):
                        try:
                            next(g_)
                        except StopIteration:
                            alive.remove(g_)
            if full:
                p.dma("sp", lambda e, b=b, c=c: e.dma_start(out=cx.HN.ap()[c * 128:(c + 1) * 128, :], in_=hn[b][:]), reads=["hn%d_%d" % (b, h) for h in range(8)], writes=["hnd%d" % c])
        if st_out is not None:
            so = cx.STATE_OUT.ap() if st_out == "dram" else cx.ST_LOC.ap()[:, 0:8 * 257].rearrange("p (h e) -> p h e", h=8)
            ho_ = cx.HALO_OUT.ap() if st_out == "dram" else cx.ST_LOC.ap()[:, 8 * 257:8 * 257 + 48].rearrange("p (c t) -> p c t", c=16)
            p.dma("sp", lambda e: e.dma_start(out=so, in_=C[:]), reads=["Cst%d" % h for h in range(8)], writes=["so"])
            p.dma("sp", lambda e: e.dma_start(out=hlb[:], in_=qkv[:, :, TOK:TOK + 3], allow_slow_non_contiguous=True), reads=["halo"], writes=["hlb"])
            p.op("dve", lambda e: e.tensor_copy(out=hl[:], in_=hlb[:]), reads=["hlb"], writes=["hl"])
            p.dma("sp", lambda e: e.dma_start(out=ho_, in_=hl[:]), reads=["hl"], writes=["ho_"])
        p.emit()


def mlstm_layer(p, cx, XIN, XOUT, full=True, fused=False):
    with ExitStack() as st1:
        G, SH, GT = load_mod_tiles(p, cx, st1, 1, 0, cx.NORM_MIX_G)
        hT = p.sb(st1, "hT", [128, 16, TOK], BF16)
        norm_to_hT(p, cx, XIN.ap(), G, SH, hT)
        mlstm_proj(p, cx, hT, full)
    if fused:
        mlstm_rec(p, cx, False, st_in="zero", st_out="loc")
        p.coll(lambda e: e.collective_compute("AllGather", ALU.bypass, replica_groups=REPLICA,
                                              ins=[cx.ST_LOC.ap()], outs=[cx.ST_ALL.ap()]), writes=["ST_ALL"])
        p.emit()
        mlstm_rec(p, cx, True, st_in="gather", st_out=None)
    else:
        mlstm_rec(p, cx, full)
    if not full:
        return
    with ExitStack() as st2:
        GT = p.sb(st2, "GT", [128, D], F32)
        p.dma("sp", lambda e: e.dma_start(out=GT[:], in_=bc(cx.MODD.ap()[1, 2 * D:3 * D])), writes=["GT"])
        proj_out_stage(p, cx, cx.HN.ap(), cx.ML_W_OUT.ap(), GT, XIN.ap(), XOUT.ap())


def _common(nc, cx, st):
    cx.nc = nc
    cx.CONSTS = nc.dram_tensor("CONSTS", [128, C_W], F32, kind="ExternalInput")
    cx.NORM_MIX_G = nc.dram_tensor("NORM_MIX_G", [2, D], F32, kind="ExternalInput")
    cx.NORM_FFN_G = nc.dram_tensor("NORM_FFN_G", [2, D], F32, kind="ExternalInput")
    cx.WR = nc.dram_tensor("WR", [2, 128, 16, 36], F32, kind="ExternalInput")
    cx.BRT = nc.dram_tensor("BRT", [2, 36], F32, kind="ExternalInput")
    cx.XS = nc.dram_tensor("XS", [NSLOT + TOK, D], BF16, kind="Internal")
    cx.YS = nc.dram_tensor("YS", [NSLOT + TOK, D], F32, kind="Internal")
    p = Prog(nc, st)
    cx.bank = [st.enter_context(nc.psum_tensor("bank%d" % i, [128, 512], F32)) for i in range(8)]
    load_consts(p, cx, st)
    return p


def build_A():
    nc = bass.Bass("TRN2", target_bir_lowering=False)
    cx = Ctx()
    with ExitStack() as st:
        p = _common(nc, cx, st)
        cx.WL = lambda L: 0
        cx.MODD = nc.dram_tensor("MODD", [2, 6 * D], F32, kind="ExternalOutput")
        cx.CVT = nc.dram_tensor("CVT", [128, 16], F32, kind="ExternalInput")
        cx.ADA_W = nc.dram_tensor("ADA_W", [2, D, 6 * D], F32, kind="ExternalInput")
        cx.ADA_B = nc.dram_tensor("ADA_B", [2, 6 * D], F32, kind="ExternalInput")
        cx.ATTN_W_IN = nc.dram_tensor("ATTN_W_IN", [D, 3 * D], F32, kind="ExternalInput")
        cx.ATTN_W_OUT = nc.dram_tensor("ATTN_W_OUT", [D, D], F32, kind="ExternalInput")
        cx.LQ1 = nc.dram_tensor("LQ1", [1, 128], F32, kind="ExternalInput")
        cx.LK1 = nc.dram_tensor("LK1", [1, 128], F32, kind="ExternalInput")
        cx.LQ2 = nc.dram_tensor("LQ2", [1, 128], F32, kind="ExternalInput")
        cx.LK2 = nc.dram_tensor("LK2", [1, 128], F32, kind="ExternalInput")
        cx.ATTN_HG = nc.dram_tensor("ATTN_HG", [1, 256], F32, kind="ExternalInput")
        cx.PREVFLAG = nc.dram_tensor("PREVFLAG", [128, 1], F32, kind="ExternalInput")
        cx.POS_OWN = nc.dram_tensor("POS_OWN", [1, TOK], I32, kind="ExternalInput")
        cx.POS_PREV = nc.dram_tensor("POS_PREV", [1, TOK], I32, kind="ExternalInput")
        cx.W_GU = nc.dram_tensor("W_GU", [1, NE, D, 2 * HID], F32, kind="ExternalInput")
        cx.W_DN = nc.dram_tensor("W_DN", [1, NE, HID, D], F32, kind="ExternalInput")
        XIN = nc.dram_tensor("XIN", [TOK, D], F32, kind="ExternalInput")
        XPREV = nc.dram_tensor("XPREV", [TOK, D], F32, kind="ExternalInput")
        X1 = nc.dram_tensor("X1", [TOK, D], F32, kind="ExternalOutput")
        XMID = nc.dram_tensor("XMID", [TOK, D], F32, kind="Internal")
        cx.QTD = nc.dram_tensor("QTD", [16, 128, TOK], BF16, kind="Internal")
        cx.KTD = nc.dram_tensor("KTD", [16, 128, 2 * TOK], BF16, kind="Internal")
        cx.VD = nc.dram_tensor("VD", [2 * TOK, 8, 257], BF16, kind="Internal")
        cx.OAD = nc.dram_tensor("OAD", [TOK, D], BF16, kind="Internal")
        mod_stage(p, cx, 0)
        mod_stage(p, cx, 1)
        attn_layer(p, cx, XIN, XPREV, XMID)
        moe_stage(p, cx, 0, XMID, X1)
    return nc


def build_B(full=True):
    nc = bass.Bass("TRN2", target_bir_lowering=False)
    cx = Ctx()
    with ExitStack() as st:
        p = _common(nc, cx, st)
        cx.WL = lambda L: 0
        cx.MODD = nc.dram_tensor("MODD", [2, 6 * D], F32, kind="ExternalInput")
        cx.ML_W_IN = nc.dram_tensor("ML_W_IN", [D, 3 * D + 16], F32, kind="ExternalInput")
        cx.CONV_W = nc.dram_tensor("CONV_W", [128, 16, 4], F32, kind="ExternalInput")
        cx.CONV_B = nc.dram_tensor("CONV_B", [128, 16], F32, kind="ExternalInput")
        cx.GATE_B = nc.dram_tensor("GATE_B", [1, 16], F32, kind="ExternalInput")
        cx.ML_HG = nc.dram_tensor("ML_HG", [1, D], F32, kind="ExternalInput")
        if full:
            cx.ML_W_OUT = nc.dram_tensor("ML_W_OUT", [D, D], F32, kind="ExternalInput")
            cx.FINAL_G = nc.dram_tensor("FINAL_G", [D], F32, kind="ExternalInput")
            cx.W_GU = nc.dram_tensor("W_GU", [1, NE, D, 2 * HID], F32, kind="ExternalInput")
            cx.W_DN = nc.dram_tensor("W_DN", [1, NE, HID, D], F32, kind="ExternalInput")
            OUT = nc.dram_tensor("OUT", [TOK, D], F32, kind="ExternalOutput")
        cx.STATE_IN = nc.dram_tensor("STATE_IN", [128, 8, 257], F32, kind="ExternalInput")
        cx.HALO_IN = nc.dram_tensor("HALO_IN", [128, 16, 3], F32, kind="ExternalInput")
        cx.STATE_OUT = nc.dram_tensor("STATE_OUT", [128, 8, 257], F32, kind="ExternalOutput")
        cx.HALO_OUT = nc.dram_tensor("HALO_OUT", [128, 16, 3], F32, kind="ExternalOutput")
        XIN = nc.dram_tensor("XIN", [TOK, D], F32, kind="ExternalInput")
        XMID = nc.dram_tensor("XMID", [TOK, D], F32, kind="Internal")
        cx.QKP = nc.dram_tensor("QKP", [16, 128, 3 + TOK], BF16, kind="Internal")
        cx.VM = nc.dram_tensor("VM", [TOK, 8, 257], BF16, kind="Internal")
        cx.OPRE = nc.dram_tensor("OPRE", [TOK, D], BF16, kind="Internal")
        cx.GATES = nc.dram_tensor("GATES", [TOK, 16], F32, kind="Internal")
        cx.HN = nc.dram_tensor("HN", [TOK, D], BF16, kind="Internal")
        mlstm_layer(p, cx, XIN, XMID, full)
        if full:
            moe_stage(p, cx, 1, XMID, OUT, final_g=cx.FINAL_G)
    return nc


def build_fused():
    nc = bass.Bass("TRN2", target_bir_lowering=False)
    cx = Ctx()
    with ExitStack() as st:
        p = _common(nc, cx, st)
        cx.WL = lambda L: L
        ei = lambda name, shape, dt=F32: nc.dram_tensor(name, list(shape), dt, kind="ExternalInput")
        it = lambda name, shape, dt=F32: nc.dram_tensor(name, list(shape), dt, kind="Internal")
        cx.MODD = it("MODD", [2, 6 * D])
        cx.CVT = ei("CVT", [128, 16])
        cx.ADA_W = ei("ADA_W", [2, D, 6 * D])
        cx.ADA_B = ei("ADA_B", [2, 6 * D])
        cx.ATTN_W_IN = ei("ATTN_W_IN", [D, 3 * D])
        cx.ATTN_W_OUT = ei("ATTN_W_OUT", [D, D])
        cx.LQ1 = ei("LQ1", [1, 128]); cx.LK1 = ei("LK1", [1, 128]); cx.LQ2 = ei("LQ2", [1, 128]); cx.LK2 = ei("LK2", [1, 128])
        cx.ATTN_HG = ei("ATTN_HG", [1, 256])
        cx.PREVFLAG = ei("PREVFLAG", [128, 1])
        cx.ODDFLAG = ei("ODDFLAG", [128, 1])
        cx.POS_OWN = ei("POS_OWN", [1, TOK], I32)
        cx.POS_PREV = ei("POS_PREV", [1, TOK], I32)
        cx.W_GU = ei("W_GU", [2, NE, D, 2 * HID])
        cx.W_DN = ei("W_DN", [2, NE, HID, D])
        cx.ML_W_IN = ei("ML_W_IN", [D, 3 * D + 16])
        cx.ML_W_OUT = ei("ML_W_OUT", [D, D])
        cx.CONV_W = ei("CONV_W", [128, 16, 4]); cx.CONV_B = ei("CONV_B", [128, 16]); cx.GATE_B = ei("GATE_B", [1, 16])
        cx.ML_HG = ei("ML_HG", [1, D]); cx.FINAL_G = ei("FINAL_G", [D])
        XIN = ei("XIN", [TOK, D]); XPREV = ei("XPREV", [TOK, D])
        OUT = nc.dram_tensor("OUT", [TOK, D], F32, kind="ExternalOutput")
        XMID0 = it("XMID0", [TOK, D]); X1 = it("X1", [TOK, D]); XMID1 = it("XMID1", [TOK, D])
        cx.QTD = it("QTD", [16, 128, TOK], BF16); cx.KTD = it("KTD", [16, 128, 2 * TOK], BF16)
        cx.VD = it("VD", [2 * TOK, 8, 257], BF16); cx.OAD = it("OAD", [TOK, D], BF16)
        cx.QKP = it("QKP", [16, 128, 3 + TOK], BF16); cx.VM = it("VM", [TOK, 8, 257], BF16)
        cx.OPRE = it("OPRE", [TOK, D], BF16); cx.GATES = it("GATES", [TOK, 16]); cx.HN = it("HN", [TOK, D], BF16)
        cx.ST_LOC = it("ST_LOC", [128, 8 * 257 + 48]); cx.ST_ALL = it("ST_ALL", [256, 8 * 257 + 48])
        S = STAGES or ("mod", "attn", "moe0", "ml", "moe1")
        if "mod" in S:
            mod_stage(p, cx, 0)
            mod_stage(p, cx, 1)
        if "attn" in S:
            attn_layer(p, cx, XIN, XPREV, XMID0)
        if "moe0" in S:
            moe_stage(p, cx, 0, XMID0, X1)
        if "ml" in S:
            mlstm_layer(p, cx, X1, XMID1, True, fused=True)
        if "moe1" in S:
            moe_stage(p, cx, 1, XMID1, OUT, final_g=cx.FINAL_G)
    return nc


def kernel(**inp):
    f32 = np.float32
    x = np.asarray(inp["x"], f32)
    c = np.asarray(inp["c"], f32)
    pos = np.asarray(inp["positions"], np.int32)
    wr = np.concatenate([inp["moe_w_group"], inp["moe_w_expert"]], axis=-1).reshape(2, 16, 128, 36).transpose(0, 2, 1, 3)
    wr = np.ascontiguousarray(wr, f32)
    brt = np.ascontiguousarray(np.concatenate([inp["moe_b_group"], inp["moe_b_expert"]], axis=-1), f32)
    cw = np.ascontiguousarray(np.asarray(inp["mlstm_conv_w"][0], f32).reshape(4, 16, 128).transpose(2, 1, 0))
    cbias = np.ascontiguousarray(np.asarray(inp["mlstm_conv_b"][0], f32).reshape(16, 128).T)
    n = 8
    common = {"CONSTS": make_consts(), "NORM_MIX_G": np.ascontiguousarray(inp["norm_mix_g"], f32), "NORM_FFN_G": np.ascontiguousarray(inp["norm_ffn_g"], f32),
              "WR": wr, "BRT": brt, "ADA_W": inp["ada_w"], "ADA_B": inp["ada_b"],
              "ATTN_W_IN": inp["attn_w_in"][0], "ATTN_W_OUT": inp["attn_w_out"][0],
              "LQ1": inp["attn_lambda_q1"], "LK1": inp["attn_lambda_k1"], "LQ2": inp["attn_lambda_q2"], "LK2": inp["attn_lambda_k2"],
              "ATTN_HG": inp["attn_head_norm_g"], "W_GU": inp["moe_w_gu"], "W_DN": inp["moe_w_down"],
              "ML_W_IN": inp["mlstm_w_in"][0], "ML_W_OUT": inp["mlstm_w_out"][0], "CONV_W": cw, "CONV_B": cbias,
              "GATE_B": inp["mlstm_gate_b"], "ML_HG": inp["mlstm_head_norm_g"], "FINAL_G": inp["final_norm_g"]}
    zx = np.zeros((TOK, D), f32)
    zp = np.zeros((1, TOK), np.int32)
    maps = []
    for core in range(n):
        b, hf = core // 2, core % 2
        sl = slice(hf * TOK, (hf + 1) * TOK)
        m = dict(common)
        m.update({
            "CVT": np.ascontiguousarray(c[b].reshape(16, 128).T),
            "PREVFLAG": np.full((128, 1), 0.0 if hf == 1 else -30000.0, f32),
            "ODDFLAG": np.full((128, 1), float(hf), f32),
            "POS_OWN": np.ascontiguousarray(pos[b, sl].reshape(1, TOK)),
            "POS_PREV": np.ascontiguousarray(pos[b, :TOK].reshape(1, TOK)) if hf == 1 else zp,
            "XIN": np.ascontiguousarray(x[b, sl]),
            "XPREV": np.ascontiguousarray(x[b, :TOK]) if hf == 1 else zx,
        })
        maps.append(m)
    nc = build_fused()
    if NCORES_DEBUG:
        res = run_bass_kernel_spmd(nc, maps[:NCORES_DEBUG], core_ids=list(range(NCORES_DEBUG))).results
        return res
    res = run_bass_kernel_spmd(nc, maps, core_ids=list(range(n))).results
    out = np.empty((4, 2 * TOK, D), f32)
    for core in range(n):
        b, hf = core // 2, core % 2
        out[b, hf * TOK:(hf + 1) * TOK] = res[core]["OUT"]
    return out
```
